# Optimizing a Trainium2 kernel written in Bass

```python
import math
import jax
import jax.numpy as jnp
from jax import lax
import numpy as np

D_MODEL = 1024
BATCH = 8
SEQ = 4096
DEPTH = 2

CTX_LEN = 256
GRID_W = 64
EPS = 1e-6

MLA_HEADS = D_MODEL // 128
MLA_NOPE = 64
MLA_ROPE = 32
MLA_V = 64
MLA_Q_RANK = 3 * D_MODEL // 8
MLA_KV_RANK = D_MODEL // 4
MLA_WIDTH = MLA_HEADS * MLA_V
ROPE_THETA = 10000.0
Q_BLOCK = 128

ML_HEADS = 4
ML_HEAD_DIM = D_MODEL // (4 * ML_HEADS)
ML_WIDTH = ML_HEADS * ML_HEAD_DIM
ML_CHUNK = 64
ML_CONV = 3

HY_WIDTH = D_MODEL // 4
HY_CONV = 3
HY_BANDS = 16
HY_EMB = 1 + 2 * HY_BANDS
HY_HIDDEN = 64
HY_DECAY_TARGET = 1e-2
HY_FAST_PCT = 0.3
HY_SLOW_PCT = 1.5
HY_SHIFT = 0.05

MIX_WIDTH = MLA_WIDTH + ML_WIDTH + HY_WIDTH
N_MLA_IN = MLA_Q_RANK + MLA_KV_RANK + MLA_ROPE
N_ML_IN = 3 * ML_WIDTH + 4 * ML_HEADS
N_HY_IN = 3 * HY_WIDTH
N_IN = N_MLA_IN + N_ML_IN + N_HY_IN

D_FF = ((8 * D_MODEL // 3 + 255) // 256) * 256
FFN_CONV = 3

kernel_name = 'hybrid_mla_mlstm_hyena_dit_block'


def rmsnorm(x, g):
    xf = x.astype(jnp.float32)
    y = xf * lax.rsqrt(jnp.mean(xf * xf, axis=-1, keepdims=True) + EPS)
    return (y * g.astype(jnp.float32)).astype(x.dtype)


def dwconv(x, w, b):
    width = w.shape[0]
    pad = (width - 1) // 2
    seq = x.shape[1]
    xp = jnp.pad(x, ((0, 0), (pad, pad), (0, 0)))
    y = b
    for j in range(width):
        y = y + xp[:, j:j + seq] * w[j]
    return y


def grid_angles(rows):
    n_freq = MLA_ROPE // 4
    inv = ROPE_THETA ** (-jnp.arange(n_freq, dtype=jnp.float32) / n_freq)
    row = jnp.repeat(jnp.arange(rows, dtype=jnp.float32), GRID_W)
    col = jnp.tile(jnp.arange(GRID_W, dtype=jnp.float32), rows)
    return jnp.concatenate([row[:, None] * inv, col[:, None] * inv], axis=-1)


def axial_rope(x, ang):
    half = x.shape[-1] // 2
    shape = (ang.shape[0],) + (1,) * (x.ndim - 3) + (half,)
    cos = jnp.cos(ang).reshape(shape).astype(x.dtype)
    sin = jnp.sin(ang).reshape(shape).astype(x.dtype)
    x1, x2 = x[..., :half], x[..., half:]
    return jnp.concatenate([x1 * cos - x2 * sin, x1 * sin + x2 * cos], axis=-1)


def mla_kv(p_kv, kv_norm_g, w_ukv):
    b, seq = p_kv.shape[:2]
    kv = (rmsnorm(p_kv[..., :MLA_KV_RANK], kv_norm_g) @ w_ukv).reshape(b, seq, MLA_HEADS, MLA_NOPE + MLA_V)
    return kv[..., :MLA_NOPE], p_kv[..., MLA_KV_RANK:], kv[..., MLA_NOPE:]


def mla_q(p_q, q_norm_g, w_uq):
    b, seq = p_q.shape[:2]
    q = (rmsnorm(p_q, q_norm_g) @ w_uq).reshape(b, seq, MLA_HEADS, MLA_NOPE + MLA_ROPE)
    return q[..., :MLA_NOPE], q[..., MLA_NOPE:]


def block_attention(q_nope, q_rope, k_nope, k_rope, v):
    b, n_q, h, _ = q_nope.shape
    n_blk = n_q // Q_BLOCK
    scale = (MLA_NOPE + MLA_ROPE) ** -0.5

    def blocks(a):
        return jnp.moveaxis(a.reshape((b, n_blk, Q_BLOCK) + a.shape[2:]), 1, 0)

    def one_block(args):
        qn, qr = args
        s = jnp.einsum('bqhd,bkhd->bhqk', qn, k_nope) + jnp.einsum('bqhr,bkr->bhqk', qr, k_rope)
        p = jax.nn.softmax(s.astype(jnp.float32) * scale, axis=-1).astype(v.dtype)
        return jnp.einsum('bhqk,bkhd->bqhd', p, v)

    o = lax.map(one_block, (blocks(q_nope), blocks(q_rope)))
    return jnp.moveaxis(o, 0, 1).reshape(b, n_q, h * v.shape[-1])


def mlstm_features(p, conv_w, conv_b, wq, wk, gate_b):
    b, seq = p.shape[:2]
    w = ML_WIDTH
    u = jax.nn.silu(dwconv(p[..., :w], conv_w, conv_b)).reshape(b, seq, ML_HEADS, ML_HEAD_DIM)
    q = jnp.einsum('blhd,hde->blhe', u, wq)
    k = jnp.einsum('blhd,hde->blhe', u, wk) * (ML_HEAD_DIM ** -0.5)
    v = p[..., w:2 * w].reshape(b, seq, ML_HEADS, ML_HEAD_DIM)
    o = p[..., 2 * w:3 * w].reshape(b, seq, ML_HEADS, ML_HEAD_DIM)
    gates = p[..., 3 * w:].reshape(b, seq, 4, ML_HEADS).astype(jnp.float32) + gate_b.astype(jnp.float32)
    return q, k, v, o, gates


def mlstm_zero_state(b):
    f32 = jnp.float32
    return (jnp.zeros((b, ML_HEADS, ML_HEAD_DIM, ML_HEAD_DIM), f32),
            jnp.zeros((b, ML_HEADS, ML_HEAD_DIM), f32),
            jnp.zeros((b, ML_HEADS), f32))


def mlstm_chunk_scan(q, k, v, logi, logf, state):
    b, seq, h, dh = q.shape
    n_chunk = seq // ML_CHUNK
    f32 = jnp.float32
    causal = jnp.tril(jnp.ones((ML_CHUNK, ML_CHUNK), bool))

    def chunks(a):
        return jnp.moveaxis(a.astype(f32).reshape((b, n_chunk, ML_CHUNK) + a.shape[2:]), 1, 0)

    def step(carry, xs):
        c_mat, n_vec, m = carry
        qc, kc, vc, li, lf = xs
        cum = jnp.cumsum(lf, axis=1).transpose(0, 2, 1)
        li = li.transpose(0, 2, 1)
        dlog = jnp.where(causal, cum[..., :, None] - cum[..., None, :] + li[..., None, :], -jnp.inf)
        inter = cum + m[..., None]
        m_out = jnp.maximum(inter, jnp.max(dlog, axis=-1))
        s = jnp.einsum('bthd,bshd->bhts', qc, kc) * jnp.exp(dlog - m_out[..., None])
        a = jnp.exp(inter - m_out)
        num = jnp.einsum('bhts,bshd->bthd', s, vc) + jnp.einsum('bht,bthd,bhde->bthe', a, qc, c_mat)
        den = jnp.sum(s, axis=-1) + a * jnp.einsum('bthd,bhd->bht', qc, n_vec)
        out = num / jnp.maximum(jnp.abs(den), jnp.exp(-m_out)).transpose(0, 2, 1)[..., None]
        cum_last = cum[..., -1]
        g = cum_last[..., None] - cum + li
        m_new = jnp.maximum(cum_last + m, jnp.max(g, axis=-1))
        wts = jnp.exp(g - m_new[..., None])
        decay = jnp.exp(cum_last + m - m_new)
        c_mat = decay[..., None, None] * c_mat + jnp.einsum('bhs,bshd,bshe->bhde', wts, kc, vc)
        n_vec = decay[..., None] * n_vec + jnp.einsum('bhs,bshd->bhd', wts, kc)
        return (c_mat, n_vec, m_new), out

    state, out = lax.scan(step, state, tuple(chunks(a) for a in (q, k, v, logi, logf)))
    out = jnp.moveaxis(out, 0, 1).reshape(b, seq, h, dh)
    return out.astype(v.dtype), state


def mlstm_direction(feat, direction, state):
    q, k, v, _, gates = feat
    logi = gates[:, :, 2 * direction]
    logf = jax.nn.log_sigmoid(gates[:, :, 2 * direction + 1])
    seqs = (q, k, v, logi, logf)
    if direction == 1:
        seqs = tuple(jnp.flip(a, axis=1) for a in seqs)
    out, state = mlstm_chunk_scan(*seqs, state)
    if direction == 1:
        out = jnp.flip(out, axis=1)
    return out, state


def mlstm_output(h, o, norm_g):
    b, seq = h.shape[:2]
    h = rmsnorm(h * jax.nn.sigmoid(o), norm_g.reshape(ML_HEADS, ML_HEAD_DIM))
    return h.reshape(b, seq, ML_WIDTH)


def mlstm_mixer(px, pc, conv_w, conv_b, wq, wk, gate_b, norm_g, need_ctx):
    fx = mlstm_features(px, conv_w, conv_b, wq, wk, gate_b)
    fc = mlstm_features(pc, conv_w, conv_b, wq, wk, gate_b)
    zero = mlstm_zero_state(px.shape[0])
    hx, hc = [], []
    for direction in (0, 1):
        h_c, ctx_state = mlstm_direction(fc, direction, zero)
        h_x, _ = mlstm_direction(fx, direction, ctx_state)
        hx.append(h_x)
        hc.append(h_c)
    out_x = mlstm_output(hx[0] + hx[1], fx[3], norm_g)
    out_c = mlstm_output(hc[0] + hc[1], fc[3], norm_g) if need_ctx else None
    return out_x, out_c


def hyena_filter(seq, w1, b1, w2, b2, w3, sin_freq):
    f32 = jnp.float32
    t = jnp.linspace(0.0, 1.0, seq, dtype=f32)[:, None]
    omega = 2.0 * math.pi * jnp.arange(seq, dtype=f32) / seq
    bands = jnp.linspace(1e-4, HY_BANDS - 1, HY_BANDS, dtype=f32)
    ang = omega[:, None] * bands[None, :]
    z = jnp.concatenate([t, jnp.cos(ang), -jnp.sin(ang)], axis=-1)
    freq = sin_freq.astype(f32)
    hdn = jnp.sin(freq * (z @ w1.astype(f32) + b1.astype(f32)))
    hdn = jnp.sin(freq * (hdn @ w2.astype(f32) + b2.astype(f32)))
    filt = hdn @ w3.astype(f32)
    deltas = jnp.abs(jnp.linspace(math.log(HY_DECAY_TARGET) / HY_SLOW_PCT,
                                  math.log(HY_DECAY_TARGET) / HY_FAST_PCT, HY_WIDTH, dtype=f32))
    window = jnp.exp(-t * deltas) + HY_SHIFT
    h_fwd = filt[:, :HY_WIDTH] * window
    h_bwd = filt[:, HY_WIDTH:] * window
    l1 = jnp.sum(jnp.abs(h_fwd), axis=0) + jnp.sum(jnp.abs(h_bwd[1:]), axis=0)
    taps = jnp.concatenate([h_fwd, jnp.zeros((1, HY_WIDTH), f32), h_bwd[:0:-1]], axis=0)
    return taps / l1


def hyena_mixer(p, conv_w, conv_b, w1, b1, w2, b2, w3, sin_freq, bias_d):
    seq = p.shape[1]
    u = dwconv(p, conv_w, conv_b)
    x0, x1, v = u[..., :HY_WIDTH], u[..., HY_WIDTH:2 * HY_WIDTH], u[..., 2 * HY_WIDTH:]
    z = (x1 * v).astype(jnp.float32)
    taps = hyena_filter(seq, w1, b1, w2, b2, w3, sin_freq)
    n_fft = 2 * seq
    y = jnp.fft.irfft(jnp.fft.rfft(z, n=n_fft, axis=1) * jnp.fft.rfft(taps, n=n_fft, axis=0)[None],
                      n=n_fft, axis=1)[:, :seq]
    y = y + z * bias_d.astype(jnp.float32)
    return x0 * y.astype(p.dtype)


def conv_ffn(h, w_up, conv_w, conv_b, w_down):
    u = dwconv(h @ w_up, conv_w, conv_b)
    return (jax.nn.silu(u[..., :D_FF]) * u[..., D_FF:]) @ w_down


def setup_inputs(seed: int = 0) -> dict:
    key = jax.random.key(seed)
    ks = iter(jax.random.split(key, 48))

    def nrm(shape, scale):
        return jax.random.normal(next(ks), shape, jnp.float32) * scale

    def gain(shape):
        return 1.0 + nrm(shape, 0.05)

    d = D_MODEL
    lin = jnp.linspace(3.0, 6.0, ML_HEADS, dtype=jnp.float32)
    zer = jnp.zeros((ML_HEADS,), jnp.float32)
    gate_base = jnp.stack([zer, lin, zer, lin])
    return {
        'x': nrm((BATCH, SEQ, d), 1.0),
        'c': nrm((BATCH, d), 1.0),
        'ctx': nrm((BATCH, CTX_LEN, d), 1.0),
        'c_ctx': nrm((d,), 1.0),
        'ada_w': nrm((DEPTH, d, 6 * d), 0.5 * d ** -0.5),
        'ada_b': nrm((DEPTH, 6 * d), 0.02),
        'norm1_g': gain((DEPTH, d)),
        'norm2_g': gain((DEPTH, d)),
        'w_in': nrm((DEPTH, d, N_IN), d ** -0.5),
        'mla_q_norm_g': gain((DEPTH, MLA_Q_RANK)),
        'mla_kv_norm_g': gain((DEPTH, MLA_KV_RANK)),
        'mla_w_uq': nrm((DEPTH, MLA_Q_RANK, MLA_HEADS * (MLA_NOPE + MLA_ROPE)), MLA_Q_RANK ** -0.5),
        'mla_w_ukv': nrm((DEPTH, MLA_KV_RANK, MLA_HEADS * (MLA_NOPE + MLA_V)), MLA_KV_RANK ** -0.5),
        'ml_conv_w': nrm((DEPTH, ML_CONV, ML_WIDTH), ML_CONV ** -0.5),
        'ml_conv_b': nrm((DEPTH, ML_WIDTH), 0.02),
        'ml_wq': nrm((DEPTH, ML_HEADS, ML_HEAD_DIM, ML_HEAD_DIM), ML_HEAD_DIM ** -0.5),
        'ml_wk': nrm((DEPTH, ML_HEADS, ML_HEAD_DIM, ML_HEAD_DIM), ML_HEAD_DIM ** -0.5),
        'ml_gate_b': gate_base[None] + nrm((DEPTH, 4, ML_HEADS), 0.1),
        'ml_norm_g': gain((DEPTH, ML_WIDTH)),
        'hy_conv_w': nrm((DEPTH, HY_CONV, N_HY_IN), HY_CONV ** -0.5),
        'hy_conv_b': nrm((DEPTH, N_HY_IN), 0.02),
        'hy_w1': nrm((DEPTH, HY_EMB, HY_HIDDEN), HY_EMB ** -0.5),
        'hy_b1': nrm((DEPTH, HY_HIDDEN), 0.02),
        'hy_w2': nrm((DEPTH, HY_HIDDEN, HY_HIDDEN), HY_HIDDEN ** -0.5),
        'hy_b2': nrm((DEPTH, HY_HIDDEN), 0.02),
        'hy_w3': nrm((DEPTH, HY_HIDDEN, 2 * HY_WIDTH), HY_HIDDEN ** -0.5),
        'hy_sin_freq': gain((DEPTH, HY_HIDDEN)),
        'hy_bias_d': nrm((DEPTH, HY_WIDTH), 0.1),
        'w_out': nrm((DEPTH, MIX_WIDTH, d), MIX_WIDTH ** -0.5),
        'ffn_w_up': nrm((DEPTH, d, 2 * D_FF), d ** -0.5),
        'ffn_conv_w': nrm((DEPTH, FFN_CONV, 2 * D_FF), FFN_CONV ** -0.5),
        'ffn_conv_b': nrm((DEPTH, 2 * D_FF), 0.02),
        'ffn_w_down': nrm((DEPTH, D_FF, d), D_FF ** -0.5),
        'final_norm_g': gain((d,)),
    }


def reference(x, c, ctx, c_ctx, ada_w, ada_b, norm1_g, norm2_g, w_in, mla_q_norm_g, mla_kv_norm_g,
              mla_w_uq, mla_w_ukv, ml_conv_w, ml_conv_b, ml_wq, ml_wk, ml_gate_b, ml_norm_g,
              hy_conv_w, hy_conv_b, hy_w1, hy_b1, hy_w2, hy_b2, hy_w3, hy_sin_freq, hy_bias_d,
              w_out, ffn_w_up, ffn_conv_w, ffn_conv_b, ffn_w_down, final_norm_g):
    n_rows = x.shape[1] // GRID_W
    ang = grid_angles(n_rows)
    ml_lo = N_MLA_IN
    hy_lo = N_MLA_IN + N_ML_IN
    for i in range(DEPTH):
        last = i == DEPTH - 1
        mod_x = jax.nn.silu(c) @ ada_w[i] + ada_b[i]
        mod_c = jax.nn.silu(c_ctx) @ ada_w[i] + ada_b[i]
        sh1, sc1, g1, sh2, sc2, g2 = jnp.split(mod_x[:, None, :], 6, axis=-1)
        csh1, csc1, cg1, csh2, csc2, cg2 = jnp.split(mod_c, 6, axis=-1)

        hx = rmsnorm(x, norm1_g[i]) * (1.0 + sc1) + sh1
        hc = rmsnorm(ctx, norm1_g[i]) * (1.0 + csc1) + csh1
        px = hx @ w_in[i]
        pc = hc @ (w_in[i][:, :hy_lo] if last else w_in[i])

        kn_c, kr_c, v_c = mla_kv(pc[..., MLA_Q_RANK:N_MLA_IN], mla_kv_norm_g[i], mla_w_ukv[i])
        kn_x, kr_x, v_x = mla_kv(px[..., MLA_Q_RANK:N_MLA_IN], mla_kv_norm_g[i], mla_w_ukv[i])
        qn_x, qr_x = mla_q(px[..., :MLA_Q_RANK], mla_q_norm_g[i], mla_w_uq[i])
        att_x = block_attention(qn_x, axial_rope(qr_x, ang),
                                jnp.concatenate([kn_c, kn_x], axis=1),
                                jnp.concatenate([kr_c, axial_rope(kr_x, ang)], axis=1),
                                jnp.concatenate([v_c, v_x], axis=1))

        ml_x, ml_c = mlstm_mixer(px[..., ml_lo:hy_lo], pc[..., ml_lo:hy_lo], ml_conv_w[i], ml_conv_b[i],
                                 ml_wq[i], ml_wk[i], ml_gate_b[i], ml_norm_g[i], not last)

        hy_x = hyena_mixer(px[..., hy_lo:], hy_conv_w[i], hy_conv_b[i], hy_w1[i], hy_b1[i], hy_w2[i],
                           hy_b2[i], hy_w3[i], hy_sin_freq[i], hy_bias_d[i])

        x = x + g1 * (jnp.concatenate([att_x, ml_x, hy_x], axis=-1) @ w_out[i])
        x = x + g2 * conv_ffn(rmsnorm(x, norm2_g[i]) * (1.0 + sc2) + sh2,
                              ffn_w_up[i], ffn_conv_w[i], ffn_conv_b[i], ffn_w_down[i])

        if not last:
            qn_c, qr_c = mla_q(pc[..., :MLA_Q_RANK], mla_q_norm_g[i], mla_w_uq[i])
            att_c = block_attention(qn_c, qr_c, kn_c, kr_c, v_c)
            hy_c = hyena_mixer(pc[..., hy_lo:], hy_conv_w[i], hy_conv_b[i], hy_w1[i], hy_b1[i], hy_w2[i],
                               hy_b2[i], hy_w3[i], hy_sin_freq[i], hy_bias_d[i])
            ctx = ctx + cg1 * (jnp.concatenate([att_c, ml_c, hy_c], axis=-1) @ w_out[i])
            ctx = ctx + cg2 * conv_ffn(rmsnorm(ctx, norm2_g[i]) * (1.0 + csc2) + csh2,
                                       ffn_w_up[i], ffn_conv_w[i], ffn_conv_b[i], ffn_w_down[i])
    return rmsnorm(x, final_norm_g)
```

```python
import contextlib
import numpy as np
import ml_dtypes
import concourse.bass as bass
import concourse.mybir as mybir
from concourse.bass_utils import run_bass_kernel_spmd

F32 = mybir.dt.float32
BF16 = mybir.dt.bfloat16
I32 = mybir.dt.int32
AF = mybir.ActivationFunctionType
ALU = mybir.AluOpType
AX = mybir.AxisListType

D = 1024
SEQ = 4096
CTX = 256
TT = SEQ + CTX
DEPTH = 2
EPS = 1e-6
NH = 8
QR, KVR, ROPE = 384, 256, 32
N_MLA_IN = QR + KVR + ROPE
MLW = 256
N_ML_IN = 3 * MLW + 16
HYW = 256
N_IN = 2224
ML_LO = N_MLA_IN
HY_LO = N_MLA_IN + N_ML_IN
DFF = 2816
NFC = DFF // 128
TWO_PI = float(2 * np.pi)


class Tok:
    __slots__ = ("w", "r", "wf")

    def __init__(self):
        self.w = []
        self.wf = []
        self.r = []


class PTok(Tok):
    __slots__ = ()


class Queue:
    def __init__(self, prog, name, eng, n_dma_sems=0):
        self.p = prog
        self.name = name
        self.eng = eng
        nc = prog.nc
        self.sem = nc.alloc_semaphore(name=f"s_{name}")
        self.count = 0
        self.dma_sems = [nc.alloc_semaphore(name=f"d_{name}{i}") for i in range(n_dma_sems)]
        self.dma_counts = [0] * n_dma_sems
        self.dma_rr = 0
        self.waited = {}

    def _wait(self, tick):
        sem, val = tick
        key = id(sem)
        if self.waited.get(key, 0) >= val:
            return
        self.eng.wait_ge(sem, val)
        self.waited[key] = val
        self.p.n_waits += 1


class Prog:
    def __init__(self, nc, dma_sems=8):
        self.nc = nc
        self.n_waits = 0
        self.n_ins = 0
        self.q = {
            "pe": Queue(self, "pe", nc.tensor),
            "dve": Queue(self, "dve", nc.vector),
            "act": Queue(self, "act", nc.scalar, dma_sems),
            "pool": Queue(self, "pool", nc.gpsimd, dma_sems),
            "sp": Queue(self, "sp", nc.sync, dma_sems),
        }

    def clear_sems(self):
        for q in self.q.values():
            q.eng.sem_clear(q.sem)
            for s in q.dma_sems:
                q.eng.sem_clear(s)
        self.nc.all_engine_barrier()

    def _deps(self, q, reads, writes, skip_self=False, partial=False):
        for t in reads:
            for tk in t.w:
                if not (skip_self and tk[0] is q.sem):
                    q._wait(tk)
            if isinstance(t, PTok):
                for tk in t.r:
                    if not (skip_self and tk[0] is q.sem):
                        q._wait(tk)
        for t in writes:
            if partial and not isinstance(t, PTok):
                for tk in t.wf:
                    if not (skip_self and tk[0] is q.sem):
                        q._wait(tk)
            if not partial or isinstance(t, PTok):
                for tk in t.w:
                    if not (skip_self and tk[0] is q.sem):
                        q._wait(tk)
            for tk in t.r:
                if not (skip_self and tk[0] is q.sem):
                    q._wait(tk)

    @staticmethod
    def _compact(lst):
        best = {}
        for s, v in lst:
            k = id(s)
            if k not in best or best[k][1] < v:
                best[k] = (s, v)
        return list(best.values())

    def _record(self, tick, reads, writes, partial=False):
        for t in reads:
            if isinstance(t, PTok):
                t.w = [tick]
                t.wf = [tick]
                t.r = []
                continue
            t.r.append(tick)
            if len(t.r) > 48:
                t.r = self._compact(t.r)
        for t in writes:
            if partial and not isinstance(t, PTok):
                t.w.append(tick)
                if len(t.w) > 48:
                    t.w = self._compact(t.w)
            else:
                t.w = [tick]
                t.wf = [tick]
                t.r = []

    def op(self, qname, fn, reads=(), writes=(), skip_self=False, partial=False):
        q = self.q[qname]
        self._deps(q, reads, writes, skip_self=skip_self, partial=partial)
        ins = fn(q.eng)
        q.count += 1
        ins.then_inc(q.sem, 1)
        self._record((q.sem, q.count), reads, writes, partial=partial)
        self.n_ins += 1
        return ins

    def dma(self, qname, out, in_, reads=(), writes=(), partial=False, **kw):
        q = self.q[qname]
        j = q.dma_rr
        q.dma_rr = (j + 1) % len(q.dma_sems)
        sem = q.dma_sems[j]
        if q.dma_counts[j] > 0:
            q._wait((sem, q.dma_counts[j]))
        self._deps(q, reads, writes, partial=partial)
        ins = q.eng.dma_start(out=out, in_=in_, **kw)
        q.dma_counts[j] += 16
        ins.then_inc(sem, 16)
        self._record((sem, q.dma_counts[j]), reads, writes, partial=partial)
        self.n_ins += 1
        return ins

    def barrier(self):
        ticks = []
        for q in self.q.values():
            if q.count:
                ticks.append((q.sem, q.count))
            for j, s in enumerate(q.dma_sems):
                if q.dma_counts[j]:
                    ticks.append((s, q.dma_counts[j]))
        for q in self.q.values():
            for tk in ticks:
                q._wait(tk)

    def finish(self):
        ticks = []
        for q in self.q.values():
            if q.count:
                ticks.append((q.sem, q.count))
            for j, s in enumerate(q.dma_sems):
                if q.dma_counts[j]:
                    ticks.append((s, q.dma_counts[j]))
        for tk in ticks:
            self.q["sp"]._wait(tk)


def _consts():
    c = {}
    c["ident"] = np.eye(128, dtype=np.float32).astype(ml_dtypes.bfloat16)
    n_freq = ROPE // 4
    inv = (10000.0 ** (-np.arange(n_freq, dtype=np.float32) / n_freq)).astype(np.float32)
    row = np.repeat(np.arange(SEQ // 64, dtype=np.float32), 64)
    col = np.tile(np.arange(64, dtype=np.float32), SEQ // 64)
    ang = np.concatenate([row[:, None] * inv, col[:, None] * inv], axis=-1).astype(np.float32)
    cos = np.cos(ang).astype(np.float32).T
    sin = np.sin(ang).astype(np.float32).T
    c["rope_cos"] = np.ascontiguousarray(np.concatenate([cos, cos], 0))
    c["rope_sin"] = np.ascontiguousarray(np.concatenate([sin, sin], 0))
    ss_, tt_ = np.meshgrid(np.arange(128), np.arange(128), indexing="ij")
    c["ml_mask"] = np.stack([(ss_ <= tt_), (ss_ >= tt_)], 1).astype(np.float32).astype(ml_dtypes.bfloat16)
    selA = np.zeros((96, 4, 128), np.float32)
    selW = np.zeros((96, 8), np.float32)
    for d in range(2):
        base = 32 + 4 if d == 0 else 64 + 12
        for h in range(4):
            selA[base + h, (h // 2) * 2 + d, (h % 2) * 64:(h % 2) * 64 + 64] = 1.0
            selW[8 * d + h, d * 4 + h] = 1.0
            selW[base + h, d * 4 + h] = -1.0
    c["ml_selA"] = selA
    c["ml_selW"] = selW
    for L in (256, 4096):
        f32 = np.float32
        t = np.linspace(0.0, 1.0, L, dtype=f32)[:, None]
        omega = (f32(2.0 * np.pi) * np.arange(L, dtype=f32) / f32(L)).astype(f32)
        bands = np.linspace(1e-4, 15, 16, dtype=f32)
        ang = (omega[:, None] * bands[None, :]).astype(f32)
        z = np.concatenate([t, np.cos(ang).astype(f32), -np.sin(ang).astype(f32)], axis=-1).astype(f32)
        c[f"hy_z{L}"] = np.ascontiguousarray(z.T)
        deltas = np.abs(np.linspace(np.log(1e-2) / 1.5, np.log(1e-2) / 0.3, 256, dtype=f32)).astype(f32)
        window = (np.exp(-t * deltas).astype(f32) + f32(0.05)).astype(f32)
        wb = window.copy()
        wb[0] = 0.0
        c[f"hy_win{L}"] = np.ascontiguousarray(np.stack([window, wb], axis=1))
        NFp = L + 128
        n = 2 * L
        idx = np.arange(L + 1, dtype=np.int64)
        prod = (idx[:, None] * idx[None, :]) % n
        angm = prod.astype(np.float64) * (2.0 * np.pi / n)
        Cm = np.zeros((NFp, NFp), np.float32)
        Sm = np.zeros((NFp, NFp), np.float32)
        Cm[:L + 1, :L + 1] = np.cos(angm)
        Sm[:L + 1, :L + 1] = np.sin(angm)
        c[f"hy_C{L}"] = Cm.astype(ml_dtypes.bfloat16)
        c[f"hy_S{L}"] = Sm.astype(ml_dtypes.bfloat16)
        wfv = np.zeros(NFp, np.float32)
        wfv[:L + 1] = 2.0 / n
        wfv[0] = 1.0 / n
        wfv[L] = 1.0 / n
        c[f"hy_wf{L}"] = np.ascontiguousarray(wfv.reshape(-1, 128).T)
    return c


class Builder:
    def __init__(self, debug=None):
        self.debug = debug or set()
        self.nc = nc = bass.Bass("TRN2", target_bir_lowering=False)
        self.P = Prog(nc)
        self.inp = {}
        self.stack = contextlib.ExitStack()

    def din(self, name, shape, dt=F32):
        ap = self.nc.dram_tensor(name, list(shape), dt, kind="ExternalInput").ap()
        self.inp[name] = ap
        return ap

    def dscr(self, name, shape, dt=F32):
        kind = "ExternalOutput" if name in self.debug else "Internal"
        return self.nc.dram_tensor(name, list(shape), dt, kind=kind).ap()

    def sb(self, st, name, shape, dt=F32):
        self.uid = getattr(self, "uid", 0) + 1
        return st.enter_context(self.nc.sbuf_tensor(f"{name}_{self.uid}", list(shape), dt)).ap()

    def declare(self):
        din = self.din
        self.x = din("x", [SEQ, D])
        self.c = din("c", [D])
        self.ctx = din("ctx", [CTX, D])
        self.c_ctx = din("c_ctx", [D])
        self.ada_w = din("ada_w", [DEPTH, D, 6 * D])
        self.ada_b = din("ada_b", [DEPTH, 6 * D])
        self.norm1_g = din("norm1_g", [DEPTH, D])
        self.norm2_g = din("norm2_g", [DEPTH, D])
        self.w_in = din("w_in", [DEPTH, D, N_IN])
        self.mla_q_norm_g = din("mla_q_norm_g", [DEPTH, QR])
        self.mla_kv_norm_g = din("mla_kv_norm_g", [DEPTH, KVR])
        self.mla_w_uq = din("mla_w_uq", [DEPTH, QR, NH * 96])
        self.mla_w_ukv = din("mla_w_ukv", [DEPTH, KVR, NH * 128])
        self.ml_conv_w = din("ml_conv_w", [DEPTH, 3, MLW])
        self.ml_conv_b = din("ml_conv_b", [DEPTH, MLW])
        self.ml_wq = din("ml_wq", [DEPTH, 4, 64, 64])
        self.ml_wk = din("ml_wk", [DEPTH, 4, 64, 64])
        self.ml_gate_b = din("ml_gate_b", [DEPTH, 16])
        self.ml_norm_g = din("ml_norm_g", [DEPTH, MLW])
        self.hy_conv_w = din("hy_conv_w", [DEPTH, 3, 3 * HYW])
        self.hy_conv_b = din("hy_conv_b", [DEPTH, 3 * HYW])
        self.hy_w1 = din("hy_w1", [DEPTH, 33, 64])
        self.hy_b1 = din("hy_b1", [DEPTH, 64])
        self.hy_w2 = din("hy_w2", [DEPTH, 64, 64])
        self.hy_b2 = din("hy_b2", [DEPTH, 64])
        self.hy_w3 = din("hy_w3", [DEPTH, 64, 2 * HYW])
        self.hy_sin_freq = din("hy_sin_freq", [DEPTH, 64])
        self.hy_bias_d = din("hy_bias_d", [DEPTH, HYW])
        self.w_out = din("w_out", [DEPTH, D, D])
        self.ffn_w_up = din("ffn_w_up", [DEPTH, D, 2 * DFF])
        self.ffn_conv_w = din("ffn_conv_w", [DEPTH, 3, 2 * DFF])
        self.ffn_conv_b = din("ffn_conv_b", [DEPTH, 2 * DFF])
        self.ffn_w_down = din("ffn_w_down", [DEPTH, DFF, D])
        self.final_norm_g = din("final_norm_g", [D])
        self.c_ident = din("ident", [128, 128], BF16)
        self.c_rope_cos = din("rope_cos", [32, SEQ])
        self.c_rope_sin = din("rope_sin", [32, SEQ])
        self.c_ml_mask = din("ml_mask", [128, 2, 128], BF16)
        self.c_ml_selA = din("ml_selA", [96, 4, 128])
        self.c_ml_selW = din("ml_selW", [96, 8])
        for L in (256, 4096):
            NFp = L + 128
            setattr(self, f"c_hy_z{L}", din(f"hy_z{L}", [33, L]))
            setattr(self, f"c_hy_win{L}", din(f"hy_win{L}", [L, 2, 256]))
            setattr(self, f"c_hy_C{L}", din(f"hy_C{L}", [NFp, NFp], BF16))
            setattr(self, f"c_hy_S{L}", din(f"hy_S{L}", [NFp, NFp], BF16))
            setattr(self, f"c_hy_wf{L}", din(f"hy_wf{L}", [128, L // 128 + 1]))
        self.out = self.nc.dram_tensor("out", [SEQ, D], F32, kind="ExternalOutput").ap()
        self.XS = self.dscr("XS", [TT, D])
        self.MOD = self.dscr("MOD", [2, 6 * D])
        self.PT = self.dscr("PT", [N_IN + 32, TT])
        self.PVO = self.dscr("PVO", [TT, 512])
        self.MIXT = self.dscr("MIXT", [D, TT], BF16)
        self.psall = self.nc.alloc_psum_tensor("psall", [128, 8 * 512], F32).ap()
        self.ps = [self.psall[:, i * 512:(i + 1) * 512] for i in range(8)]
        self.pst = [PTok() for _ in range(8)]

    def vec_pc(self, st, name, src, n, q="sp"):
        t = self.sb(st, name, [128, n])
        tok = Tok()
        self.P.dma(q, t, src.rearrange("(c p) -> p c", p=128), writes=[tok], allow_slow_non_contiguous=True)
        return t, tok

    def phase_mod(self, li):
        nc, P = self.nc, self.P
        lst = self.lst
        self.modT = self.sb(lst, "modT", [128, 48, 2])
        self.t_mod = Tok()
        self.s1 = self.sb(lst, "s1", [128, 8, 2])
        self.s2 = self.sb(lst, "s2", [128, 8, 2])
        self.t_s12 = Tok()
        with contextlib.ExitStack() as st:
            cc = self.sb(st, "cc", [128, 8, 2])
            t_cc = Tok()
            P.dma("sp", cc[:, :, 0], self.c.rearrange("(c p) -> p c", p=128), writes=[t_cc], partial=True, allow_slow_non_contiguous=True)
            P.dma("sp", cc[:, :, 1], self.c_ctx.rearrange("(c p) -> p c", p=128), writes=[t_cc], partial=True, allow_slow_non_contiguous=True)
            sc = self.sb(st, "sc", [128, 8, 2])
            t_sc = Tok()
            P.op("act", lambda e: e.activation(out=sc, in_=cc, func=AF.Silu), reads=[t_cc], writes=[t_sc])
            ab = self.sb(st, "ab", [128, 48])
            t_ab = Tok()
            P.dma("sp", ab, self.ada_b[li].rearrange("(c p) -> p c", p=128), writes=[t_ab], allow_slow_non_contiguous=True)
            g12 = self.sb(st, "g12", [128, 8, 2])
            t_g12 = Tok()
            P.dma("sp", g12[:, :, 0], self.norm1_g[li].rearrange("(c p) -> p c", p=128), writes=[t_g12], partial=True, allow_slow_non_contiguous=True)
            P.dma("sp", g12[:, :, 1], self.norm2_g[li].rearrange("(c p) -> p c", p=128), writes=[t_g12], partial=True, allow_slow_non_contiguous=True)
            wt = [self.sb(st, f"adaw{i}", [128, 8, 512]) for i in range(2)]
            t_wt = [Tok(), Tok()]
            acc = self.ps[0]
            t_acc = self.pst[0]
            for nb in range(12):
                s = nb % 2
                P.dma("sp" if nb % 2 == 0 else "act", wt[s],
                      self.ada_w[li, :, nb * 512:(nb + 1) * 512].rearrange("(c p) n -> p c n", p=128),
                      writes=[t_wt[s]])
                for j in range(4):
                    n = nb * 4 + j
                    for k in range(8):
                        P.op("pe", lambda e, s=s, j=j, k=k, n=n: e.matmul(
                            acc[:, 2 * n:2 * n + 2], lhsT=wt[s][:, k, j * 128:(j + 1) * 128], rhs=sc[:, k, :],
                            start=(k == 0), stop=(k == 7)),
                            reads=[t_wt[s], t_sc], writes=[t_acc], skip_self=True)
            mod = self.modT
            P.op("dve", lambda e: e.tensor_tensor(out=mod, in0=acc[:, 0:96].rearrange("p (n v) -> p n v", v=2),
                                                  in1=ab.unsqueeze(2).to_broadcast([128, 48, 2]), op=ALU.add),
                 reads=[t_acc, t_ab], writes=[self.t_mod])
            for (dst, gi, c0) in ((self.s1, 0, 8), (self.s2, 1, 32)):
                P.op("dve", lambda e, dst=dst, gi=gi, c0=c0: e.scalar_tensor_tensor(
                    out=dst, in0=mod[:, c0:c0 + 8, :], scalar=1.0, in1=g12[:, :, gi:gi + 1].to_broadcast([128, 8, 2]),
                    op0=ALU.add, op1=ALU.mult), reads=[self.t_mod, t_g12], writes=[self.t_s12], partial=True)
            for v in range(2):
                P.dma("sp", self.MOD[v].rearrange("(c p) -> p c", p=128), mod[:, :, v], reads=[self.t_mod], writes=[self.t_MOD],
                      partial=True, allow_slow_non_contiguous=True)
            P.barrier()

    def norm_transpose(self, st_bufs, rows_ap, t_rows, svec, bvec, v, dst, t_dst, col0, ps_i):
        P = self.P
        xt, t_xt, sq, t_sq, ss, t_ss, xn, t_xn = st_bufs
        P.dma("sp", xt, rows_ap, reads=[t_rows], writes=[t_xt])
        P.op("act", lambda e: e.activation(out=sq, in_=xt, func=AF.Square, accum_out=ss[:, 0:1]), reads=[t_xt], writes=[t_sq, t_ss])
        P.op("dve", lambda e: e.tensor_scalar(out=ss[:, 1:2], in0=ss[:, 0:1], scalar1=1.0 / D, scalar2=EPS, op0=ALU.mult, op1=ALU.add),
             reads=[t_ss], writes=[t_ss])
        P.op("act", lambda e: e.activation(out=ss[:, 2:3], in_=ss[:, 1:2], func=AF.Sqrt), reads=[t_ss], writes=[t_ss])
        P.op("dve", lambda e: e.reciprocal(out=ss[:, 3:4], in_=ss[:, 2:3]), reads=[t_ss], writes=[t_ss])
        P.op("dve", lambda e: e.tensor_scalar(out=xn, in0=xt, scalar1=ss[:, 3:4], scalar2=None, op0=ALU.mult), reads=[t_xt, t_ss], writes=[t_xn])
        pb = self.ps[ps_i].bitcast(BF16)
        t_pb = self.pst[ps_i]
        for c in range(8):
            P.op("pe", lambda e, c=c: e.transpose(pb[:, c * 128:(c + 1) * 128], xn[:, c * 128:(c + 1) * 128], self.identb),
                 reads=[t_xn, self.t_ident], writes=[t_pb], skip_self=True, partial=(c > 0))
        for c in range(8):
            if c % 2 == 0:
                P.op("act", lambda e, c=c: e.activation(out=dst[:, c, col0:col0 + 128], in_=pb[:, c * 128:(c + 1) * 128], func=AF.Identity,
                                                         scale=svec[:, c, v:v + 1], bias=bvec[:, c, v:v + 1]),
                     reads=[t_pb, self.t_s12, self.t_mod], writes=[t_dst], partial=True)
            else:
                P.op("dve", lambda e, c=c: e.tensor_scalar(out=dst[:, c, col0:col0 + 128], in0=pb[:, c * 128:(c + 1) * 128],
                                                           scalar1=svec[:, c, v:v + 1], scalar2=bvec[:, c, v:v + 1], op0=ALU.mult, op1=ALU.add),
                     reads=[t_pb, self.t_s12, self.t_mod], writes=[t_dst], partial=True)

    def load_cast_weight(self, st, dst, t_dst, src_ap, ncols, blk=512, k_chunks=8, engs=None):
        P = self.P
        engs = engs or ("pool", "dve", "act")
        stg = [self.sb(st, f"wstg{id(dst) % 9973}_{i}", [128, k_chunks, blk]) for i in range(2)]
        t_stg = [Tok(), Tok()]
        i = 0
        for c0 in range(0, ncols, blk):
            w = min(blk, ncols - c0)
            s = i % 2
            P.dma("sp" if i % 2 == 0 else "act", stg[s][:, :, 0:w], src_ap[:, c0:c0 + w].rearrange("(c p) n -> p c n", p=128), writes=[t_stg[s]])
            eng = engs[i % len(engs)]
            if eng == "act":
                P.op("act", lambda e, s=s, c0=c0, w=w: e.copy(out=dst[:, :, c0:c0 + w], in_=stg[s][:, :, 0:w]), reads=[t_stg[s]], writes=[t_dst], partial=True)
            else:
                P.op(eng, lambda e, s=s, c0=c0, w=w: e.tensor_copy(out=dst[:, :, c0:c0 + w], in_=stg[s][:, :, 0:w]), reads=[t_stg[s]], writes=[t_dst], partial=True)
            i += 1

    def phase_inproj(self, li):
        nc, P = self.nc, self.P
        NW = N_IN + 32
        with contextlib.ExitStack() as st:
            wb = self.sb(st, "winb", [128, 8, NW], BF16)
            t_wb = Tok()
            with contextlib.ExitStack() as st2:
                self.load_cast_weight(st2, wb, t_wb, self.w_in[li], N_IN)
                P.op("dve", lambda e: e.tensor_scalar(out=wb[:, :, N_IN:N_IN + 16], in0=wb[:, :, 656:672], scalar1=-1.0, scalar2=None, op0=ALU.mult),
                     reads=[t_wb], writes=[t_wb], partial=True)
                P.op("dve", lambda e: e.tensor_copy(out=wb[:, :, N_IN + 16:N_IN + 32], in_=wb[:, :, 640:656]), reads=[t_wb], writes=[t_wb], partial=True)
                P.barrier()
            NB = 2
            bufs = []
            for i in range(NB):
                bufs.append((self.sb(st, f"xt{i}", [128, D]), Tok(), self.sb(st, f"sq{i}", [128, D], BF16), Tok(),
                             self.sb(st, f"ss{i}", [128, 4]), Tok(), self.sb(st, f"xn{i}", [128, D], BF16), Tok()))
            hT = [self.sb(st, f"hT{i}", [128, 8, 256], BF16) for i in range(2)]
            t_hT = [Tok(), Tok()]
            stage = [self.sb(st, f"stg{i}", [128, 16, 256]) for i in range(2)]
            t_stage = [Tok(), Tok()]
            svo = [self.sb(st, f"svo{i}", [128, 512]) for i in range(2)]
            t_svo = [Tok(), Tok()]
            chunks = [(0, 128), (128, 128), (256, 128), (384, 128), (512, 128), (640, 32), (672, 128), (800, 128), (1440, 16)]
            chunks += [(HY_LO + 128 * i, 128) for i in range(6)] + [(N_IN, 32)]
            groups = [(0, 3), (3, 2), (5, 1), (6, 2), (8, 1), (9, 6), (15, 1)]
            nsub = 0
            for ti in range(TT // 256):
                t0 = ti * 256
                v = 1 if ti == 0 else 0
                hs = ti % 2
                for sub in range(2):
                    self.norm_transpose(bufs[nsub % NB], self.XS[t0 + sub * 128:t0 + (sub + 1) * 128, :], self.t_XS, self.s1,
                                        self.modT[:, 0:8, :], v, hT[hs], t_hT[hs], sub * 128, ps_i=nsub % 2)
                    nsub += 1
                sg = stage[hs]
                for ci, (c0, M) in enumerate(chunks):
                    pi = 2 + ci % 4
                    for k in range(8):
                        P.op("pe", lambda e, pi=pi, k=k, c0=c0, M=M, hs=hs: e.matmul(self.ps[pi][0:M, 0:256], lhsT=wb[:, k, c0:c0 + M], rhs=hT[hs][:, k, :],
                                                                               start=(k == 0), stop=(k == 7)),
                             reads=[t_wb, t_hT[hs]], writes=[self.pst[pi]], skip_self=True)
                    if ci % 2 == 0:
                        P.op("act", lambda e, pi=pi, M=M, ci=ci: e.copy(out=sg[0:M, ci, :], in_=self.ps[pi][0:M, 0:256]), reads=[self.pst[pi]], writes=[t_stage[hs]], partial=True)
                    else:
                        P.op("dve", lambda e, pi=pi, M=M, ci=ci: e.tensor_copy(out=sg[0:M, ci, :], in_=self.ps[pi][0:M, 0:256]), reads=[self.pst[pi]], writes=[t_stage[hs]], partial=True)
                for (g0, gn) in groups:
                    c0, M = chunks[g0]
                    dst = self.PT[c0:c0 + M * gn, t0:t0 + 256]
                    if gn > 1:
                        dst = dst.rearrange("(c p) t -> p c t", p=128)
                        P.dma("pool", dst, sg[:, g0:g0 + gn, :], reads=[t_stage[hs]], writes=[self.t_PT], partial=True)
                    else:
                        P.dma("pool", dst, sg[0:M, g0, :], reads=[t_stage[hs]], writes=[self.t_PT], partial=True)
                for sub in range(2):
                    pi = 6 + sub
                    for k in range(8):
                        P.op("pe", lambda e, pi=pi, k=k, sub=sub, hs=hs: e.matmul(self.ps[pi], lhsT=hT[hs][:, k, sub * 128:(sub + 1) * 128], rhs=wb[:, k, 928:1440],
                                                                               start=(k == 0), stop=(k == 7)),
                             reads=[t_wb, t_hT[hs]], writes=[self.pst[pi]], skip_self=True)
                    P.op("act" if sub == 0 else "dve", (lambda e, pi=pi, sub=sub: e.copy(out=svo[sub], in_=self.ps[pi])) if sub == 0 else
                         (lambda e, pi=pi, sub=sub: e.tensor_copy(out=svo[sub], in_=self.ps[pi])), reads=[self.pst[pi]], writes=[t_svo[sub]])
                    P.dma("pool", self.PVO[t0 + sub * 128:t0 + (sub + 1) * 128, :], svo[sub], reads=[t_svo[sub]], writes=[self.t_PVO], partial=True)
            P.barrier()

    def rms_bcast(self, src, nk, n, ones, t_ones, nfeat, ps_i, R, t_R, sq, t_sq, t_src):
        P = self.P
        P.op("act", lambda e: e.activation(out=sq[:, 0:nk, 0:n], in_=src[:, 0:nk, 0:n], func=AF.Square), reads=[t_src], writes=[t_sq])
        ps, t_ps = self.ps[ps_i], self.pst[ps_i]
        for k in range(nk):
            P.op("pe", lambda e, k=k: e.matmul(ps[:, 0:n], lhsT=ones, rhs=sq[:, k, 0:n], start=(k == 0), stop=(k == nk - 1)),
                 reads=[t_ones, t_sq], writes=[t_ps], skip_self=True)
        P.op("dve", lambda e: e.tensor_scalar(out=R[:, 0:n], in0=ps[:, 0:n], scalar1=1.0 / nfeat, scalar2=EPS, op0=ALU.mult, op1=ALU.add),
             reads=[t_ps], writes=[t_R])
        P.op("act", lambda e: e.activation(out=R[:, 0:n], in_=R[:, 0:n], func=AF.Sqrt), reads=[t_R], writes=[t_R])
        P.op("dve", lambda e: e.reciprocal(out=R[:, 0:n], in_=R[:, 0:n]), reads=[t_R], writes=[t_R])

    def phase_mla(self, li):
        nc, P = self.nc, self.P
        last = li == DEPTH - 1
        scale = float(96 ** -0.5)
        with contextlib.ExitStack() as st:
            sb = lambda name, shape, dt=F32: self.sb(st, name, shape, dt)
            ones = sb("onesb", [128, 128], BF16); t_ones = Tok()
            P.op("pool", lambda e: e.memset(ones, 1.0), writes=[t_ones])
            KT = sb("KT", [128, NH, TT], BF16); t_KT = Tok()
            VP = sb("VP", [128, TT // 128, NH, 65], BF16); t_VP = Tok()
            P.op("pool", lambda e: e.memset(VP, 1.0), writes=[t_VP])
            sel65 = sb("sel65", [128, 64]); t_sel = Tok()
            P.op("pool", lambda e: e.memset(sel65, 0.0), writes=[t_sel])
            P.op("pool", lambda e: e.memset(sel65[64:65, :], 1.0), reads=[t_sel], writes=[t_sel])
            wqb = sb("wqb", [128, 3, NH, 192], BF16); t_wq = Tok()
            wkb = sb("wkb", [128, 2, NH, 64], BF16); t_wk = Tok()
            wvb = sb("wvb", [128, 2, NH, 64], BF16); t_wv = Tok()
            mk = sb("mk", [128, NH]); t_mk = Tok()
            P.op("pool", lambda e: e.memset(mk, 0.0), writes=[t_mk])
            with contextlib.ExitStack() as st2:
                wq = self.sb(st2, "wq32", [128, 3, NH * 96]); t_wq32 = Tok()
                wkv = self.sb(st2, "wkv32", [128, 2, NH * 128]); t_wkv32 = Tok()
                gq, t_gq = self.vec_pc(st2, "gq", self.mla_q_norm_g[li], 3)
                gkv, t_gkv = self.vec_pc(st2, "gkv", self.mla_kv_norm_g[li], 2)
                P.dma("sp", wq, self.mla_w_uq[li].rearrange("(c p) n -> p c n", p=128), writes=[t_wq32])
                P.dma("act", wkv, self.mla_w_ukv[li].rearrange("(c p) n -> p c n", p=128), writes=[t_wkv32])
                for k in range(3):
                    P.op("dve", lambda e, k=k: e.tensor_scalar(out=wq[:, k, :], in0=wq[:, k, :], scalar1=gq[:, k:k + 1], scalar2=None, op0=ALU.mult),
                         reads=[t_gq], writes=[t_wq32])
                for k in range(2):
                    P.op("dve", lambda e, k=k: e.tensor_scalar(out=wkv[:, k, :], in0=wkv[:, k, :], scalar1=gkv[:, k:k + 1], scalar2=None, op0=ALU.mult),
                         reads=[t_gkv], writes=[t_wkv32])
                wq4 = wq.rearrange("p k (h d) -> p k h d", d=96)
                wkv4 = wkv.rearrange("p k (h d) -> p k h d", d=128)
                P.op("pool", lambda e: e.memset(wqb, 0.0), writes=[t_wq])
                for k in range(3):
                    P.op("dve", lambda e, k=k: e.tensor_copy(out=wqb[:, k, :, 0:96], in_=wq4[:, k, :, 0:96]), reads=[t_wq32], writes=[t_wq], partial=True)
                    P.op("dve", lambda e, k=k: e.tensor_scalar(out=wqb[:, k, :, 160:176], in0=wq4[:, k, :, 80:96], scalar1=-1.0, scalar2=None, op0=ALU.mult),
                         reads=[t_wq32], writes=[t_wq], partial=True)
                    P.op("dve", lambda e, k=k: e.tensor_copy(out=wqb[:, k, :, 176:192], in_=wq4[:, k, :, 64:80]), reads=[t_wq32], writes=[t_wq], partial=True)
                for k in range(2):
                    P.op("dve", lambda e, k=k: e.tensor_copy(out=wkb[:, k, :, :], in_=wkv4[:, k, :, 0:64]), reads=[t_wkv32], writes=[t_wk], partial=True)
                    P.op("dve", lambda e, k=k: e.tensor_copy(out=wvb[:, k, :, :], in_=wkv4[:, k, :, 64:128]), reads=[t_wkv32], writes=[t_wv], partial=True)
                P.barrier()
            NBUF = 2
            pin = [sb(f"pin{i}", [128, 3, 512]) for i in range(NBUF)]; t_pin = [Tok() for _ in range(NBUF)]
            cs = [sb(f"cs{i}", [128, 2, 512]) for i in range(NBUF)]; t_cs = [Tok() for _ in range(NBUF)]
            sq = sb("sqm", [128, 3, 512], BF16); t_sq = Tok()
            R = sb("Rm", [128, 512]); t_R = Tok()
            pn = sb("pn", [128, 3, 512], BF16); t_pn = Tok()
            tmpa = sb("tmpa", [128, 512]); t_tmpa = Tok()
            tmpb = sb("tmpb", [128, 512]); t_tmpb = Tok()
            sqk = sb("sqk", [128, NH, 512], BF16); t_sqk = Tok()
            stA = contextlib.ExitStack()
            krin = [self.sb(stA, f"krin{i}", [128, 2, 512]) for i in range(NBUF)]; t_krin = [Tok() for _ in range(NBUF)]
            krb = self.sb(stA, "krb", [128, 512], BF16); t_krb = Tok()
            mtmp = self.sb(stA, "mtmp", [128, NH]); t_mtmp = Tok()
            tchunks = [(0, CTX)] + [(CTX + 512 * j, 512) for j in range(SEQ // 512)]

            def load_rope_tables(bi, t0, n):
                p0 = t0 - CTX
                P.dma("act", cs[bi][64:96, 0, 0:n], self.c_rope_cos[:, p0:p0 + n], writes=[t_cs[bi]], partial=True)
                P.dma("act", cs[bi][64:96, 1, 0:n], self.c_rope_sin[:, p0:p0 + n], writes=[t_cs[bi]], partial=True)

            for ci, (t0, n) in enumerate(tchunks):
                bi = ci % NBUF
                is_ctx = ci == 0
                P.dma("sp", pin[bi][:, 0:2, 0:n], self.PT[QR:QR + KVR, t0:t0 + n].rearrange("(c p) t -> p c t", p=128), reads=[self.t_PT], writes=[t_pin[bi]])
                P.dma("sp", krin[bi][64:96, 0, 0:n], self.PT[640:672, t0:t0 + n], reads=[self.t_PT], writes=[t_krin[bi]], partial=True)
                if not is_ctx:
                    P.dma("sp", krin[bi][64:96, 1, 0:n], self.PT[N_IN:N_IN + 32, t0:t0 + n], reads=[self.t_PT], writes=[t_krin[bi]], partial=True)
                    load_rope_tables(bi, t0, n)
                self.rms_bcast(pin[bi], 2, n, ones, t_ones, KVR, 6, R, t_R, sq, t_sq, t_pin[bi])
                P.op("dve", lambda e, bi=bi, n=n: e.tensor_tensor(out=pn[:, 0:2, 0:n], in0=pin[bi][:, 0:2, 0:n], in1=R[:, 0:n].unsqueeze(1).to_broadcast([128, 2, n]), op=ALU.mult),
                     reads=[t_pin[bi], t_R], writes=[t_pn])
                if is_ctx:
                    P.op("dve", lambda e, bi=bi, n=n: e.tensor_copy(out=krb[64:96, 0:n], in_=krin[bi][64:96, 0, 0:n]), reads=[t_krin[bi]], writes=[t_krb])
                else:
                    P.op("dve", lambda e, bi=bi, n=n: e.tensor_tensor(out=tmpa[64:96, 0:n], in0=krin[bi][64:96, 0, 0:n], in1=cs[bi][64:96, 0, 0:n], op=ALU.mult), reads=[t_krin[bi], t_cs[bi]], writes=[t_tmpa])
                    P.op("dve", lambda e, bi=bi, n=n: e.tensor_tensor(out=tmpb[64:96, 0:n], in0=krin[bi][64:96, 1, 0:n], in1=cs[bi][64:96, 1, 0:n], op=ALU.mult), reads=[t_krin[bi], t_cs[bi]], writes=[t_tmpb])
                    P.op("dve", lambda e, n=n: e.tensor_tensor(out=krb[64:96, 0:n], in0=tmpa[64:96, 0:n], in1=tmpb[64:96, 0:n], op=ALU.add), reads=[t_tmpa, t_tmpb], writes=[t_krb])
                P.op("pool", lambda e, t0=t0, n=n: e.tensor_copy(out=KT[64:96, :, t0:t0 + n], in_=krb[64:96, 0:n].unsqueeze(1).to_broadcast([32, NH, n])), reads=[t_krb], writes=[t_KT], partial=True)
                for h in range(NH):
                    pi = 4 + h % 2
                    for k in range(2):
                        P.op("pe", lambda e, h=h, k=k, pi=pi, n=n: e.matmul(self.ps[pi][0:64, 0:n], lhsT=wkb[:, k, h, :], rhs=pn[:, k, 0:n], start=(k == 0), stop=(k == 1)),
                             reads=[t_wk, t_pn], writes=[self.pst[pi]], skip_self=True)
                    if h % 2 == 0:
                        P.op("act", lambda e, h=h, pi=pi, t0=t0, n=n: e.copy(out=KT[0:64, h, t0:t0 + n], in_=self.ps[pi][0:64, 0:n]), reads=[self.pst[pi]], writes=[t_KT], partial=True)
                    else:
                        P.op("dve", lambda e, h=h, pi=pi, t0=t0, n=n: e.tensor_copy(out=KT[0:64, h, t0:t0 + n], in_=self.ps[pi][0:64, 0:n]), reads=[self.pst[pi]], writes=[t_KT], partial=True)
                for sub in range(n // 128):
                    j = (t0 + sub * 128) // 128
                    pi = 2 + sub % 2
                    for k in range(2):
                        P.op("pe", lambda e, k=k, pi=pi, sub=sub: e.matmul(self.ps[pi], lhsT=pn[:, k, sub * 128:(sub + 1) * 128], rhs=wvb[:, k, :, :].rearrange("p h d -> p (h d)"), start=(k == 0), stop=(k == 1)),
                             reads=[t_wv, t_pn], writes=[self.pst[pi]], skip_self=True)
                    src = self.ps[pi].rearrange("p (h d) -> p h d", d=64)
                    if sub % 2 == 0:
                        P.op("act", lambda e, j=j, src=src: e.copy(out=VP[:, j, :, 0:64], in_=src), reads=[self.pst[pi]], writes=[t_VP], partial=True)
                    else:
                        P.op("dve", lambda e, j=j, src=src: e.tensor_copy(out=VP[:, j, :, 0:64], in_=src), reads=[self.pst[pi]], writes=[t_VP], partial=True)
                P.op("act", lambda e, t0=t0, n=n: e.activation(out=sqk[0:96, :, 0:n], in_=KT[0:96, :, t0:t0 + n], func=AF.Square), reads=[t_KT], writes=[t_sqk])
                for h in range(NH):
                    pi = 7
                    P.op("pe", lambda e, h=h, n=n: e.matmul(self.ps[7][:, 0:n], lhsT=ones[0:96, :], rhs=sqk[0:96, h, 0:n], start=True, stop=True),
                         reads=[t_ones, t_sqk], writes=[self.pst[7]], skip_self=True)
                    P.op("dve", lambda e, h=h, n=n: e.tensor_reduce(out=mtmp[:, h:h + 1], in_=self.ps[7][:, 0:n], axis=AX.X, op=ALU.max), reads=[self.pst[7]], writes=[t_mtmp], partial=True)
                P.op("dve", lambda e: e.tensor_tensor(out=mk, in0=mk, in1=mtmp, op=ALU.max), reads=[t_mtmp], writes=[t_mk])
            P.barrier()
            stA.close()
            QT = [sb(f"QT{i}", [128, NH, 512], BF16) for i in range(2)]; t_QT = [Tok(), Tok()]
            pts = [sb(f"pts{i}", [128, 2, 512], BF16) for i in range(3)]; t_pts = [Tok() for _ in range(3)]
            den = sb("den", [128, 512]); t_den = Tok()
            rden = sb("rden", [128, 512]); t_rden = Tok()
            ot = [sb(f"ot{i}", [128, 512], BF16) for i in range(2)]; t_ot = [Tok(), Tok()]
            mq = [sb(f"mq{i}", [128, NH]) for i in range(2)]; t_mq = [Tok(), Tok()]
            negm = [sb(f"negm{i}", [128, NH]) for i in range(2)]; t_negm = [Tok(), Tok()]
            qchunks = ([] if last else [(0, CTX)]) + tchunks[1:]
            nq = len(qchunks)

            def prologue_parts(qi):
                t0, n = qchunks[qi]
                bi = qi % NBUF
                is_ctx = t0 == 0
                qt, t_qt = QT[qi % 2], t_QT[qi % 2]
                parts = []

                def part_load():
                    P.dma("sp", pin[bi][:, 0:3, 0:n], self.PT[0:QR, t0:t0 + n].rearrange("(c p) t -> p c t", p=128), reads=[self.t_PT], writes=[t_pin[bi]])
                    if not is_ctx:
                        load_rope_tables(bi, t0, n)
                    self.rms_bcast(pin[bi], 3, n, ones, t_ones, QR, 5, R, t_R, sq, t_sq, t_pin[bi])
                    P.op("dve", lambda e: e.tensor_tensor(out=pn[:, 0:3, 0:n], in0=pin[bi][:, 0:3, 0:n], in1=R[:, 0:n].unsqueeze(1).to_broadcast([128, 3, n]), op=ALU.mult),
                         reads=[t_pin[bi], t_R], writes=[t_pn])
                parts.append(part_load)

                def part_head(h):
                    for k in range(3):
                        P.op("pe", lambda e, k=k: e.matmul(self.ps[7][0:96, 0:n], lhsT=wqb[:, k, h, 0:96], rhs=pn[:, k, 0:n], start=(k == 0), stop=(k == 2)),
                             reads=[t_wq, t_pn], writes=[self.pst[7]], skip_self=True)
                    if is_ctx:
                        P.op("dve", lambda e: e.tensor_copy(out=qt[0:96, h, 0:n], in_=self.ps[7][0:96, 0:n]), reads=[self.pst[7]], writes=[t_qt], partial=True)
                        return
                    for k in range(3):
                        P.op("pe", lambda e, k=k: e.matmul(self.ps[6][0:96, 0:n], lhsT=wqb[:, k, h, 96:192], rhs=pn[:, k, 0:n], start=(k == 0), stop=(k == 2)),
                             reads=[t_wq, t_pn], writes=[self.pst[6]], skip_self=True)
                    P.op("dve", lambda e: e.tensor_tensor(out=tmpa[64:96, 0:n], in0=self.ps[7][64:96, 0:n], in1=cs[bi][64:96, 0, 0:n], op=ALU.mult), reads=[self.pst[7], t_cs[bi]], writes=[t_tmpa])
                    P.op("dve", lambda e: e.tensor_copy(out=qt[0:64, h, 0:n], in_=self.ps[7][0:64, 0:n]), reads=[self.pst[7]], writes=[t_qt], partial=True)
                    P.op("dve", lambda e: e.tensor_tensor(out=tmpb[64:96, 0:n], in0=self.ps[6][64:96, 0:n], in1=cs[bi][64:96, 1, 0:n], op=ALU.mult), reads=[self.pst[6], t_cs[bi]], writes=[t_tmpb])
                    P.op("dve", lambda e: e.tensor_tensor(out=qt[64:96, h, 0:n], in0=tmpa[64:96, 0:n], in1=tmpb[64:96, 0:n], op=ALU.add), reads=[t_tmpa, t_tmpb], writes=[t_qt], partial=True)
                for h in range(NH):
                    parts.append(lambda h=h: part_head(h))

                def part_norms():
                    P.op("pool", lambda e: e.tensor_tensor(out=sqk[0:96, :, 0:n], in0=qt[0:96, :, 0:n], in1=qt[0:96, :, 0:n], op=ALU.mult), reads=[t_qt], writes=[t_sqk])
                    for h in range(NH):
                        P.op("pe", lambda e, h=h: e.matmul(self.ps[7][:, 0:n], lhsT=ones[0:96, :], rhs=sqk[0:96, h, 0:n], start=True, stop=True),
                             reads=[t_ones, t_sqk], writes=[self.pst[7]], skip_self=True)
                        P.op("dve", lambda e, h=h: e.tensor_reduce(out=mq[qi % 2][:, h:h + 1], in_=self.ps[7][:, 0:n], axis=AX.X, op=ALU.max), reads=[self.pst[7]], writes=[t_mq[qi % 2]], partial=True)
                    nm, t_nm = negm[qi % 2], t_negm[qi % 2]
                    P.op("dve", lambda e: e.tensor_tensor(out=nm, in0=mq[qi % 2], in1=mk, op=ALU.mult), reads=[t_mq[qi % 2], t_mk], writes=[t_nm])
                    P.op("act", lambda e: e.activation(out=nm, in_=nm, func=AF.Sqrt), reads=[t_nm], writes=[t_nm])
                    P.op("dve", lambda e: e.tensor_scalar(out=nm, in0=nm, scalar1=-scale, scalar2=None, op0=ALU.mult), reads=[t_nm], writes=[t_nm])
                parts.append(part_norms)
                return parts

            def groups_of(qi):
                t0, n = qchunks[qi]
                ktiles = list(range(CTX // 128)) if t0 == 0 else list(range(TT // 128))
                return [ktiles[i:i + 2] for i in range(0, len(ktiles), 2)]

            def s_mm(qi, h, g):
                t0, n = qchunks[qi]
                qt, t_qt = QT[qi % 2], t_QT[qi % 2]
                b0 = 2 * (g % 2)
                for i, j in enumerate(groups_of(qi)[g]):
                    P.op("pe", lambda e, j=j, i=i: e.matmul(self.ps[b0 + i][:, 0:n], lhsT=KT[0:96, h, j * 128:(j + 1) * 128], rhs=qt[0:96, h, 0:n], start=True, stop=True),
                         reads=[t_KT, t_qt], writes=[self.pst[b0 + i]], skip_self=True)

            for part in prologue_parts(0):
                part()
            jobs = [(qi, h) for qi in range(nq) for h in range(NH)]
            assigned = {0: [0], 1: [1, 2], 2: [3, 4], 3: [5, 6], 4: [7, 8], 5: [9], 6: [], 7: []}
            pcount = 0
            pre_issued = set()
            nxt_parts = None
            for ji, (qi, h) in enumerate(jobs):
                t0, n = qchunks[qi]
                groups = groups_of(qi)
                ng = len(groups)
                po = 4
                nm, t_nm = negm[qi % 2], t_negm[qi % 2]
                if h == 0:
                    nxt_parts = prologue_parts(qi + 1) if qi + 1 < nq else None
                if (qi, h) not in pre_issued:
                    s_mm(qi, h, 0)
                    if ng > 1:
                        s_mm(qi, h, 1)
                for g in range(ng):
                    b0 = 2 * (g % 2)
                    nj = len(groups[g])
                    pb = pcount % 3
                    pcount += 1
                    src = self.psall[:, b0 * 512:(b0 + nj) * 512].rearrange("p (a c) -> p a c", c=512)[:, :, 0:n]
                    P.op("act", lambda e, pb=pb, nj=nj, src=src: e.activation(out=pts[pb][:, 0:nj, 0:n], in_=src, func=AF.Exp, bias=nm[:, h:h + 1], scale=scale),
                         reads=[self.pst[b0 + i] for i in range(nj)] + [t_nm], writes=[t_pts[pb]])
                    for i, j in enumerate(groups[g]):
                        first = (g == 0 and i == 0)
                        lastk = (g == ng - 1 and i == nj - 1)
                        P.op("pe", lambda e, pb=pb, j=j, i=i, first=first, lastk=lastk: e.matmul(self.ps[po][0:65, 0:n], lhsT=VP[:, j, h, :], rhs=pts[pb][:, i, 0:n], start=first, stop=lastk),
                             reads=[t_VP, t_pts[pb]], writes=[self.pst[po]], skip_self=True)
                    if g + 2 < ng:
                        s_mm(qi, h, g + 2)
                if ji + 1 < len(jobs):
                    qn, hn = jobs[ji + 1]
                    if qn == qi:
                        s_mm(qn, hn, 0)
                        if len(groups_of(qn)) > 1:
                            s_mm(qn, hn, 1)
                        pre_issued.add((qn, hn))
                o = ot[h % 2]
                t_o = t_ot[h % 2]
                P.op("act", lambda e: e.copy(out=den[0:65, 0:n], in_=self.ps[po][0:65, 0:n]), reads=[self.pst[po]], writes=[t_den])
                P.op("pe", lambda e: e.matmul(self.ps[5][0:64, 0:n], lhsT=sel65[0:65, :], rhs=den[0:65, 0:n], start=True, stop=True),
                     reads=[t_sel, t_den], writes=[self.pst[5]], skip_self=True)
                P.op("dve", lambda e: e.reciprocal(out=rden[0:64, 0:n], in_=self.ps[5][0:64, 0:n]), reads=[self.pst[5]], writes=[t_rden])
                P.op("dve", lambda e, o=o: e.tensor_tensor(out=o[0:64, 0:n], in0=den[0:64, 0:n], in1=rden[0:64, 0:n], op=ALU.mult),
                     reads=[t_den, t_rden], writes=[t_o])
                P.dma("pool", self.MIXT[h * 64:(h + 1) * 64, t0:t0 + n], o[0:64, 0:n], reads=[t_o], writes=[self.t_MIXT], partial=True)
                if nxt_parts is not None:
                    for k in assigned[h]:
                        nxt_parts[k]()
            P.barrier()

    def phase_mlstm(self, li):
        nc, P = self.nc, self.P
        NT = TT // 128
        with contextlib.ExitStack() as st:
            sb = lambda name, shape, dt=F32: self.sb(st, name, shape, dt)
            ub = [sb(f"ub{p}", [128, TT], BF16) for p in range(2)]; t_ub = [Tok(), Tok()]
            kT = [sb(f"kT{p}", [128, TT], BF16) for p in range(2)]; t_kT = [Tok(), Tok()]
            qd = [[sb(f"qd{p}{d}", [128, TT], BF16) for d in range(2)] for p in range(2)]
            t_qd = [[Tok(), Tok()], [Tok(), Tok()]]
            ktok = sb("ktok", [128, NT, 256], BF16); t_ktok = Tok()
            wtok = sb("wtok", [128, NT, 8]); t_wtok = Tok()
            ecol = sb("ecol", [128, 4, NT]); t_ecol = Tok()
            hF = sb("hF", [128, NT, 256]); t_hF = Tok()
            CbS = [[sb(f"CbS{p}{d}", [128, NT + 1, 65], BF16) for d in range(2)] for p in range(2)]
            t_CbS = [[Tok(), Tok()], [Tok(), Tok()]]
            Cst = [[sb(f"Cst{p}{d}", [128, 65]) for d in range(2)] for p in range(2)]
            t_Cst = [[Tok(), Tok()], [Tok(), Tok()]]
            masks = sb("mlmask", [128, 2, 128], BF16); t_masks = Tok()
            P.dma("sp", masks, self.c_ml_mask, writes=[t_masks])
            wqb = sb("mlwq", [128, 2, 128], BF16); wkb = sb("mlwk", [128, 2, 128], BF16); t_w = Tok()
            gml, t_gml = self.vec_pc(st, "gml", self.ml_norm_g[li], 2)
            cw = sb("mlcw", [128, 2, 3]); cb = sb("mlcb", [128, 2]); t_cw = Tok()
            for jj in range(3):
                P.dma("sp", cw[:, :, jj], self.ml_conv_w[li, jj].rearrange("(c p) -> p c", p=128), writes=[t_cw], partial=True, allow_slow_non_contiguous=True)
            P.dma("sp", cb, self.ml_conv_b[li].rearrange("(c p) -> p c", p=128), writes=[t_cw], partial=True, allow_slow_non_contiguous=True)
            for p in range(2):
                for d in range(2):
                    P.op("pool", lambda e, p=p, d=d: e.memset(Cst[p][d], 0.0), writes=[t_Cst[p][d]])
                    P.op("pool", lambda e, p=p, d=d: e.memset(CbS[p][d][:, 0, :], 0.0), writes=[t_CbS[p][d]])
            with contextlib.ExitStack() as st2:
                sb2 = lambda name, shape, dt=F32: self.sb(st2, name, shape, dt)
                w32 = sb2("mlw32", [128, 4, 128]); t_w32 = Tok()
                P.op("pool", lambda e: e.memset(w32, 0.0), writes=[t_w32])
                for p in range(2):
                    for hh in range(2):
                        P.dma("sp", w32[hh * 64:(hh + 1) * 64, p, hh * 64:(hh + 1) * 64], self.ml_wq[li, 2 * p + hh], reads=[], writes=[t_w32], partial=True)
                        P.dma("sp", w32[hh * 64:(hh + 1) * 64, 2 + p, hh * 64:(hh + 1) * 64], self.ml_wk[li, 2 * p + hh], reads=[], writes=[t_w32], partial=True)
                P.op("dve", lambda e: e.tensor_copy(out=wqb, in_=w32[:, 0:2, :]), reads=[t_w32], writes=[t_w], partial=True)
                P.op("dve", lambda e: e.tensor_scalar(out=wkb, in0=w32[:, 2:4, :], scalar1=0.125, scalar2=None, op0=ALU.mult), reads=[t_w32], writes=[t_w], partial=True)
                selA = sb2("selA", [128, 4, 128]); selW = sb2("selW", [128, 8]); t_sel = Tok()
                P.dma("act", selA[0:96], self.c_ml_selA, writes=[t_sel], partial=True)
                P.dma("act", selW[0:96], self.c_ml_selW, writes=[t_sel], partial=True)
                gb = sb2("mlgb", [16, 1]); t_gb = Tok()
                P.dma("sp", gb, self.ml_gate_b[li].rearrange("(p o) -> p o", o=1), writes=[t_gb], allow_slow_non_contiguous=True)
                ones16 = sb2("ones16", [16, 128]); t_o16 = Tok()
                P.op("pool", lambda e: e.memset(ones16, 1.0), writes=[t_o16])
                X = sb2("mlX", [128, TT]); t_X = Tok()
                P.op("pool", lambda e: e.memset(X, 0.0), writes=[t_X])
                P.dma("sp", X[0:16, :], self.PT[1440:1456, :], reads=[self.t_PT], writes=[t_X], partial=True)
                P.op("dve", lambda e: e.tensor_scalar(out=X[0:16, :], in0=X[0:16, :], scalar1=gb[:, 0:1], scalar2=None, op0=ALU.add), reads=[t_gb, t_X], writes=[t_X])
                with contextlib.ExitStack() as st3:
                    LF = self.sb(st3, "mlLF", [16, TT]); t_LF = Tok()
                    CF = self.sb(st3, "mlCF", [16, TT]); t_CF = Tok()
                    P.op("act", lambda e: e.activation(out=LF, in_=X[0:16, :], func=AF.Exp, scale=-1.0), reads=[t_X], writes=[t_LF])
                    P.op("act", lambda e: e.activation(out=LF, in_=LF, func=AF.Ln, bias=1.0), reads=[t_LF], writes=[t_LF])
                    P.op("dve", lambda e: e.tensor_scalar(out=LF, in0=LF, scalar1=-1.0, scalar2=None, op0=ALU.mult), reads=[t_LF], writes=[t_LF])
                    for j in range(NT):
                        P.op("dve", lambda e, j=j: e.tensor_tensor_scan(out=CF[:, j * 128:(j + 1) * 128], data0=ones16, data1=LF[:, j * 128:(j + 1) * 128],
                                                                        initial=0.0, op0=ALU.mult, op1=ALU.add), reads=[t_LF, t_o16], writes=[t_CF], partial=True)
                    P.op("dve", lambda e: e.tensor_tensor(out=LF, in0=LF, in1=CF, op=ALU.subtract), reads=[t_CF], writes=[t_LF])
                    LF3 = LF.rearrange("p (j t) -> p j t", t=128)
                    CF3 = CF.rearrange("p (j t) -> p j t", t=128)
                    P.op("dve", lambda e: e.tensor_tensor(out=LF3, in0=LF3, in1=CF3[:, :, 127:128].to_broadcast([16, NT, 128]), op=ALU.add), reads=[t_CF], writes=[t_LF])
                    P.op("act", lambda e: e.copy(out=X[32:48, :], in_=CF), reads=[t_CF], writes=[t_X], partial=True)
                    P.op("act", lambda e: e.copy(out=X[64:80, :], in_=LF), reads=[t_LF], writes=[t_X], partial=True)
                    P.barrier()
                for j in range(NT):
                    P.op("pe", lambda e, j=j: e.matmul(self.ps[7][:, j * 8:(j + 1) * 8], lhsT=X[0:96, j * 128:(j + 1) * 128], rhs=selW[0:96, :], start=True, stop=True),
                         reads=[t_X, t_sel], writes=[self.pst[7]], skip_self=True)
                P.op("act", lambda e: e.activation(out=wtok.rearrange("p j g -> p (j g)"), in_=self.ps[7][:, 0:NT * 8], func=AF.Exp), reads=[self.pst[7]], writes=[t_wtok])
                pc32 = sb2("mlpc", [128, TT]); t_pc = Tok()
                u32 = sb2("mlu", [128, TT]); t_u = Tok()
                abc = [sb2(f"abc{i}", [128, 512]) for i in range(2)]; t_abc = [Tok(), Tok()]
                blocks = [(0, 256)] + [(256 + 512 * i, 512) for i in range(8)]
                segs = [(0, CTX), (CTX, TT)]
                nabc = 0
                for p in range(2):
                    P.dma("sp", pc32, self.PT[ML_LO + p * 128:ML_LO + (p + 1) * 128, :], reads=[self.t_PT], writes=[t_pc])
                    P.op("dve", lambda e, p=p: e.tensor_scalar(out=u32, in0=pc32, scalar1=cw[:, p, 1:2], scalar2=cb[:, p:p + 1], op0=ALU.mult, op1=ALU.add), reads=[t_pc, t_cw], writes=[t_u])
                    for (a, b) in segs:
                        P.op("dve", lambda e, p=p, a=a, b=b: e.scalar_tensor_tensor(out=u32[:, a + 1:b], in0=pc32[:, a:b - 1], scalar=cw[:, p, 0:1], in1=u32[:, a + 1:b], op0=ALU.mult, op1=ALU.add),
                             reads=[t_pc, t_cw], writes=[t_u])
                        P.op("dve", lambda e, p=p, a=a, b=b: e.scalar_tensor_tensor(out=u32[:, a:b - 1], in0=pc32[:, a + 1:b], scalar=cw[:, p, 2:3], in1=u32[:, a:b - 1], op0=ALU.mult, op1=ALU.add),
                             reads=[t_pc, t_cw], writes=[t_u])
                    P.op("act", lambda e, p=p: e.activation(out=ub[p], in_=u32, func=AF.Silu), reads=[t_u], writes=[t_ub[p]])
                    for (b0, n) in blocks:
                        P.op("pe", lambda e, p=p, b0=b0, n=n: e.matmul(self.ps[0][:, 0:n], lhsT=wqb[:, p, :], rhs=ub[p][:, b0:b0 + n], start=True, stop=True),
                             reads=[t_w, t_ub[p]], writes=[self.pst[0]], skip_self=True)
                        for d in range(2):
                            ai = nabc % 2
                            nabc += 1
                            P.op("pe", lambda e, p=p, d=d, b0=b0, n=n: e.matmul(self.ps[6][:, 0:n], lhsT=selA[0:96, p * 2 + d, :], rhs=X[0:96, b0:b0 + n], start=True, stop=True),
                                 reads=[t_sel, t_X], writes=[self.pst[6]], skip_self=True)
                            P.op("act", lambda e, ai=ai, n=n: e.activation(out=abc[ai][:, 0:n], in_=self.ps[6][:, 0:n], func=AF.Exp), reads=[self.pst[6]], writes=[t_abc[ai]])
                            P.op("dve", lambda e, p=p, d=d, ai=ai, b0=b0, n=n: e.tensor_tensor(out=qd[p][d][:, b0:b0 + n], in0=self.ps[0][:, 0:n], in1=abc[ai][:, 0:n], op=ALU.mult),
                                 reads=[self.pst[0], t_abc[ai]], writes=[t_qd[p][d]], partial=True)
                            c0 = 127 if d == 0 else 0
                            P.op("pool", lambda e, p=p, d=d, ai=ai, b0=b0, n=n, c0=c0: e.tensor_copy(out=ecol[:, p * 2 + d, b0 // 128:(b0 + n) // 128], in_=abc[ai][:, c0:n:128]),
                                 reads=[t_abc[ai]], writes=[t_ecol], partial=True)
                        P.op("pe", lambda e, p=p, b0=b0, n=n: e.matmul(self.ps[1][:, 0:n], lhsT=wkb[:, p, :], rhs=ub[p][:, b0:b0 + n], start=True, stop=True),
                             reads=[t_w, t_ub[p]], writes=[self.pst[1]], skip_self=True)
                        P.op("act", lambda e, p=p, b0=b0, n=n: e.copy(out=kT[p][:, b0:b0 + n], in_=self.ps[1][:, 0:n]), reads=[self.pst[1]], writes=[t_kT[p]], partial=True)
                    for j in range(NT):
                        pi = 2 + j % 2
                        P.op("pe", lambda e, p=p, j=j, pi=pi: e.matmul(self.ps[pi][:, 0:128], lhsT=ub[p][:, j * 128:(j + 1) * 128], rhs=wkb[:, p, :], start=True, stop=True),
                             reads=[t_w, t_ub[p]], writes=[self.pst[pi]], skip_self=True)
                        if j % 2 == 0:
                            P.op("act", lambda e, p=p, j=j, pi=pi: e.copy(out=ktok[:, j, p * 128:(p + 1) * 128], in_=self.ps[pi][:, 0:128]), reads=[self.pst[pi]], writes=[t_ktok], partial=True)
                        else:
                            P.op("dve", lambda e, p=p, j=j, pi=pi: e.tensor_copy(out=ktok[:, j, p * 128:(p + 1) * 128], in_=self.ps[pi][:, 0:128]), reads=[self.pst[pi]], writes=[t_ktok], partial=True)
                P.barrier()
            NB = 4
            hB = sb("hB", [128, NT, 256]); t_hB = Tok()
            vo = [sb(f"mlvo{i}", [128, 512]) for i in range(NB)]; t_vo = [Tok() for _ in range(NB)]
            vw = [sb(f"mlvw{i}", [128, 4, 65], BF16) for i in range(NB)]; t_vw = [Tok() for _ in range(NB)]
            Ssb = [sb(f"mlS{i}", [128, 4, 128], BF16) for i in range(NB)]; t_S = [Tok() for _ in range(NB)]
            tmpC = [sb(f"mltmpC{d}", [128, 65]) for d in range(2)]; t_tmpC = [Tok(), Tok()]
            dd = [sb(f"mldd{d}", [128, 4]) for d in range(2)]; t_dd = [Tok(), Tok()]
            orders = [list(range(NT)), [1, 0] + list(range(NT - 1, 1, -1))]
            step = 0
            for si in range(NT):
                for d in range(2):
                    j = orders[d][si]
                    bi = step % NB
                    step += 1
                    c0, c1 = j * 128, (j + 1) * 128
                    P.dma("sp", vo[bi][:, 0:256], self.PVO[c0:c1, 0:256], reads=[self.t_PVO], writes=[t_vo[bi]])
                    P.op("dve", lambda e, bi=bi, j=j, d=d: e.tensor_tensor(out=vw[bi][:, :, 0:64], in0=vo[bi][:, 0:256].rearrange("p (h c) -> p h c", c=64),
                                                                   in1=wtok[:, j, d * 4:(d + 1) * 4].unsqueeze(2).to_broadcast([128, 4, 64]), op=ALU.mult),
                         reads=[t_vo[bi], t_wtok], writes=[t_vw[bi]])
                    P.op("act", lambda e, bi=bi, j=j, d=d: e.copy(out=vw[bi][:, :, 64], in_=wtok[:, j, d * 4:(d + 1) * 4]), reads=[t_wtok], writes=[t_vw[bi]], partial=True)
                    for h in range(4):
                        p, r0 = h // 2, (h % 2) * 64
                        pS = 2 + h % 2
                        P.op("pe", lambda e, h=h, p=p, r0=r0, d=d, c0=c0, c1=c1, pS=pS: e.matmul(self.ps[pS][:, (h // 2) * 128:(h // 2 + 1) * 128], lhsT=kT[p][r0:r0 + 64, c0:c1], rhs=qd[p][d][r0:r0 + 64, c0:c1], start=True, stop=True),
                             reads=[t_kT[p], t_qd[p][d]], writes=[self.pst[pS]], skip_self=True)
                    for eo in range(2):
                        P.op("dve", lambda e, bi=bi, eo=eo, d=d: e.tensor_tensor(out=Ssb[bi][:, eo::2, :], in0=self.ps[2 + eo][:, 0:256].rearrange("p (h t) -> p h t", t=128),
                                                                         in1=masks[:, d, :].unsqueeze(1).to_broadcast([128, 2, 128]), op=ALU.mult),
                             reads=[self.pst[2 + eo], t_masks], writes=[t_S[bi]], partial=(eo > 0))
                    for h in range(4):
                        p, r0 = h // 2, (h % 2) * 64
                        pN = 4 + h % 2
                        cN = (h // 2) * 65
                        P.op("pe", lambda e, h=h, bi=bi, pN=pN, cN=cN: e.matmul(self.ps[pN][:, cN:cN + 65], lhsT=Ssb[bi][:, h, :], rhs=vw[bi][:, h, :], start=True, stop=False),
                             reads=[t_S[bi], t_vw[bi]], writes=[self.pst[pN]], skip_self=True)
                        P.op("pe", lambda e, h=h, p=p, r0=r0, d=d, si=si, c0=c0, c1=c1, pN=pN, cN=cN: e.matmul(self.ps[pN][:, cN:cN + 65], lhsT=qd[p][d][r0:r0 + 64, c0:c1], rhs=CbS[p][d][r0:r0 + 64, si, :], start=False, stop=True),
                             reads=[t_qd[p][d], t_CbS[p][d]], writes=[self.pst[pN]], skip_self=True)
                    for p in range(2):
                        pU = p + 6 * d
                        P.op("pe", lambda e, p=p, bi=bi, j=j, pU=pU: e.matmul(self.ps[pU][:, 0:130], lhsT=ktok[:, j, p * 128:(p + 1) * 128], rhs=vw[bi][:, 2 * p:2 * p + 2, :].rearrange("p a c -> p (a c)"), start=True, stop=True),
                             reads=[t_ktok, t_vw[bi]], writes=[self.pst[pU]], skip_self=True)
                        P.op("dve", lambda e, p=p, d=d, pU=pU: e.tensor_tensor(out=tmpC[d][0:64, :], in0=self.ps[pU][0:64, 0:65], in1=Cst[p][d][0:64, :], op=ALU.add), reads=[self.pst[pU], t_Cst[p][d]], writes=[t_tmpC[d]], partial=True)
                        P.op("dve", lambda e, p=p, d=d, pU=pU: e.tensor_tensor(out=tmpC[d][64:128, :], in0=self.ps[pU][64:128, 65:130], in1=Cst[p][d][64:128, :], op=ALU.add), reads=[self.pst[pU], t_Cst[p][d]], writes=[t_tmpC[d]], partial=True)
                        P.op("dve", lambda e, p=p, d=d, j=j: e.tensor_scalar(out=Cst[p][d], in0=tmpC[d], scalar1=ecol[:, p * 2 + d, j:j + 1], scalar2=None, op0=ALU.mult), reads=[t_tmpC[d], t_ecol], writes=[t_Cst[p][d]])
                        P.op("act", lambda e, p=p, d=d, si=si: e.copy(out=CbS[p][d][:, si + 1, :], in_=Cst[p][d]), reads=[t_Cst[p][d]], writes=[t_CbS[p][d]], partial=True)
                    for eo in range(2):
                        num = self.ps[4 + eo][:, 0:130].rearrange("p (h c) -> p h c", c=65)
                        dde = dd[d][:, 2 * eo:2 * eo + 2]
                        P.op("dve", lambda e, num=num, dde=dde: e.tensor_scalar(out=dde, in0=num[:, :, 64], scalar1=-1.0, scalar2=None, op0=ALU.mult), reads=[self.pst[4 + eo]], writes=[t_dd[d]], partial=(eo > 0))
                        P.op("dve", lambda e, num=num, dde=dde: e.scalar_tensor_tensor(out=dde, in0=num[:, :, 64], scalar=1.0, in1=dde, op0=ALU.max, op1=ALU.max), reads=[self.pst[4 + eo]], writes=[t_dd[d]], partial=True)
                        P.op("dve", lambda e, dde=dde: e.reciprocal(out=dde, in_=dde), reads=[t_dd[d]], writes=[t_dd[d]], partial=True)
                        dst = (hF if d == 0 else hB)[:, j, :].rearrange("p (h c) -> p h c", c=64)[:, eo::2, :]
                        P.op("dve", lambda e, num=num, dde=dde, dst=dst: e.tensor_tensor(out=dst, in0=num[:, :, 0:64], in1=dde.unsqueeze(2).to_broadcast([128, 2, 64]), op=ALU.mult),
                             reads=[self.pst[4 + eo], t_dd[d]], writes=[t_hF if d == 0 else t_hB], partial=True)
            hbs = [sb(f"mlhb{i}", [128, 256]) for i in range(2)]; t_hbs = [Tok(), Tok()]
            sgs = [sb(f"mlsg{i}", [128, 256]) for i in range(2)]; t_sgs = [Tok(), Tok()]
            hsq = sb("mlhsq", [128, 256]); t_hsq = Tok()
            ssn = sb("mlssn", [128, 8]); t_ssn = Tok()
            hn = [sb(f"mlhn{i}", [128, 256], BF16) for i in range(2)]; t_hn = [Tok(), Tok()]
            oT = [sb(f"mloT{i}", [128, 2, 128], BF16) for i in range(2)]; t_oT = [Tok(), Tok()]
            for j in range(NT):
                bi = j % 2
                c0, c1 = j * 128, (j + 1) * 128
                hb, t_hb, sg, t_sg = hbs[bi], t_hbs[bi], sgs[bi], t_sgs[bi]
                P.dma("sp", vo[bi][:, 256:512], self.PVO[c0:c1, 256:512], reads=[self.t_PVO], writes=[t_vo[bi]])
                P.op("pool", lambda e, j=j, hb=hb: e.tensor_tensor(out=hb, in0=hF[:, j, :], in1=hB[:, j, :], op=ALU.add), reads=[t_hF, t_hB], writes=[t_hb])
                P.op("act", lambda e, bi=bi, sg=sg: e.activation(out=sg, in_=vo[bi][:, 256:512], func=AF.Sigmoid), reads=[t_vo[bi]], writes=[t_sg])
                P.op("pool", lambda e, hb=hb, sg=sg: e.tensor_tensor(out=hb, in0=hb, in1=sg, op=ALU.mult), reads=[t_sg], writes=[t_hb])
                P.op("pool", lambda e, hb=hb: e.tensor_tensor(out=hsq, in0=hb, in1=hb, op=ALU.mult), reads=[t_hb], writes=[t_hsq])
                P.op("dve", lambda e: e.tensor_reduce(out=ssn[:, 0:4], in_=hsq.rearrange("p (h c) -> p h c", c=64), axis=AX.X, op=ALU.add), reads=[t_hsq], writes=[t_ssn])
                P.op("dve", lambda e: e.tensor_scalar(out=ssn[:, 0:4], in0=ssn[:, 0:4], scalar1=1.0 / 64, scalar2=EPS, op0=ALU.mult, op1=ALU.add), reads=[t_ssn], writes=[t_ssn])
                P.op("act", lambda e: e.activation(out=ssn[:, 0:4], in_=ssn[:, 0:4], func=AF.Sqrt), reads=[t_ssn], writes=[t_ssn])
                P.op("dve", lambda e: e.reciprocal(out=ssn[:, 4:8], in_=ssn[:, 0:4]), reads=[t_ssn], writes=[t_ssn])
                P.op("dve", lambda e, bi=bi, hb=hb: e.tensor_tensor(out=hn[bi].rearrange("p (h c) -> p h c", c=64), in0=hb.rearrange("p (h c) -> p h c", c=64), in1=ssn[:, 4:8].unsqueeze(2).to_broadcast([128, 4, 64]), op=ALU.mult),
                     reads=[t_hb, t_ssn], writes=[t_hn[bi]])
                pi = 6 + bi
                pT = self.ps[pi].bitcast(BF16)
                for p in range(2):
                    P.op("pe", lambda e, p=p, bi=bi, pT=pT: e.transpose(pT[:, p * 128:(p + 1) * 128], hn[bi][:, p * 128:(p + 1) * 128], self.identb), reads=[t_hn[bi], self.t_ident], writes=[self.pst[pi]], skip_self=True)
                for p in range(2):
                    P.op("act", lambda e, p=p, bi=bi, pT=pT: e.activation(out=oT[bi][:, p, :], in_=pT[:, p * 128:(p + 1) * 128], func=AF.Copy, scale=gml[:, p:p + 1]), reads=[self.pst[pi], t_gml], writes=[t_oT[bi]], partial=(p > 0))
                P.dma("pool", self.MIXT[512:768, c0:c1].rearrange("(c p) t -> p c t", p=128), oT[bi], reads=[t_oT[bi]], writes=[self.t_MIXT], partial=True)
            P.barrier()

    def phase_hyena(self, li):
        if li < DEPTH - 1:
            self.hyena_seq(li, 0, CTX, self.c_hy_z256, self.c_hy_win256, self.c_hy_C256, self.c_hy_S256, self.c_hy_wf256)
        self.hyena_seq(li, CTX, SEQ, self.c_hy_z4096, self.c_hy_win4096, self.c_hy_C4096, self.c_hy_S4096, self.c_hy_wf4096)

    def hyena_seq(self, li, tok0, L, c_z, c_win, c_C, c_S, c_wf):
        nc, P = self.nc, self.P
        NTL = L // 128
        NF = NTL + 1
        NB = (L + 511) // 512
        BW = min(L, 512)
        with contextlib.ExitStack() as st:
            sb = lambda name, shape, dt=F32: self.sb(st, name, shape, dt)
            x0T = sb("hyx0T", [128, 2, L], BF16); t_x0 = Tok()
            zT = sb("hyzT", [128, 2, L], BF16); t_zT = Tok()
            Z = sb("hyZ", [128, NTL, 256], BF16); t_Z = Tok()
            HS = sb("hyHS", [128, NTL, 256], BF16); t_HS = Tok()
            HD = sb("hyHD", [128, NTL, 256], BF16); t_HD = Tok()
            Asp = sb("hyA", [128, NF, 256], BF16); t_A = Tok()
            Bsp = sb("hyB", [128, NF, 256], BF16); t_B = Tok()
            rl1 = sb("hyrl1", [128, 256]); t_rl1 = Tok()
            wf = sb("hywf", [128, NF]); t_wf = Tok()
            P.dma("sp", wf, c_wf, writes=[t_wf])
            bd, t_bd = self.vec_pc(st, "hybd", self.hy_bias_d[li], 2)
            cw = sb("hycw", [128, 6, 3]); cb = sb("hycb", [128, 6]); t_cw = Tok()
            for jj in range(3):
                P.dma("sp", cw[:, :, jj], self.hy_conv_w[li, jj].rearrange("(c p) -> p c", p=128), writes=[t_cw], partial=True, allow_slow_non_contiguous=True)
            P.dma("sp", cb, self.hy_conv_b[li].rearrange("(c p) -> p c", p=128), writes=[t_cw], partial=True, allow_slow_non_contiguous=True)
            with contextlib.ExitStack() as st2:
                sb2 = lambda name, shape, dt=F32: self.sb(st2, name, shape, dt)
                zemb = sb2("hyzemb", [33, L]); t_zemb = Tok()
                P.dma("sp", zemb, c_z, writes=[t_zemb])
                w1 = sb2("hyw1", [33, 64]); w2 = sb2("hyw2", [64, 64]); w3 = sb2("hyw3", [64, 512]); t_wm = Tok()
                P.dma("act", w1, self.hy_w1[li], writes=[t_wm], partial=True)
                P.dma("act", w2, self.hy_w2[li], writes=[t_wm], partial=True)
                P.dma("act", w3, self.hy_w3[li], writes=[t_wm], partial=True)
                fr = sb2("hyfr", [64, 4]); t_fr = Tok()
                P.dma("sp", fr[:, 0:1], self.hy_sin_freq[li].rearrange("(p o) -> p o", o=1), writes=[t_fr], partial=True, allow_slow_non_contiguous=True)
                P.dma("sp", fr[:, 1:2], self.hy_b1[li].rearrange("(p o) -> p o", o=1), writes=[t_fr], partial=True, allow_slow_non_contiguous=True)
                P.dma("sp", fr[:, 2:3], self.hy_b2[li].rearrange("(p o) -> p o", o=1), writes=[t_fr], partial=True, allow_slow_non_contiguous=True)
                P.op("dve", lambda e: e.tensor_scalar(out=fr[:, 1:3], in0=fr[:, 1:3], scalar1=fr[:, 0:1], scalar2=None, op0=ALU.mult), reads=[t_fr], writes=[t_fr])
                h1 = sb2("hyh1", [64, L]); t_h1 = Tok()
                h2 = sb2("hyh2", [64, L]); t_h2 = Tok()
                tt = sb2("hytt", [64, 512]); t_tt = Tok()
                ti = sb2("hyti", [64, 512], I32); t_ti = Tok()
                tf = sb2("hytf", [64, 512]); t_tf = Tok()
                ones32 = sb2("hyones", [128, 128]); t_ones = Tok()
                P.op("pool", lambda e: e.memset(ones32, 1.0), writes=[t_ones])

                def sin_layer(wm, kdim, src, t_src, bcol, dst, t_dst):
                    for b in range(NB):
                        c0 = b * 512
                        P.op("pe", lambda e, c0=c0: e.matmul(self.ps[0][0:64, 0:BW], lhsT=wm[0:kdim, :], rhs=src[0:kdim, c0:c0 + BW], start=True, stop=True),
                             reads=[t_wm, t_src], writes=[self.pst[0]], skip_self=True)
                        P.op("dve", lambda e: e.tensor_scalar(out=tt[:, 0:BW], in0=self.ps[0][0:64, 0:BW], scalar1=fr[:, 0:1], scalar2=fr[:, bcol:bcol + 1], op0=ALU.mult, op1=ALU.add),
                             reads=[self.pst[0], t_fr], writes=[t_tt])
                        P.op("dve", lambda e: e.tensor_scalar(out=ti[:, 0:BW], in0=tt[:, 0:BW], scalar1=1.0 / TWO_PI, scalar2=None, op0=ALU.mult), reads=[t_tt], writes=[t_ti])
                        P.op("dve", lambda e: e.tensor_copy(out=tf[:, 0:BW], in_=ti[:, 0:BW]), reads=[t_ti], writes=[t_tf])
                        P.op("dve", lambda e: e.scalar_tensor_tensor(out=tt[:, 0:BW], in0=tf[:, 0:BW], scalar=-TWO_PI, in1=tt[:, 0:BW], op0=ALU.mult, op1=ALU.add), reads=[t_tf], writes=[t_tt])
                        P.op("act", lambda e, c0=c0: e.activation(out=dst[:, c0:c0 + BW], in_=tt[:, 0:BW], func=AF.Sin), reads=[t_tt], writes=[t_dst], partial=True)
                sin_layer(w1, 33, zemb, t_zemb, 1, h1, t_h1)
                sin_layer(w2, 64, h1, t_h1, 2, h2, t_h2)
                win = [sb2(f"hywin{i}", [128, 2, 256]) for i in range(2)]; t_win = [Tok(), Tok()]
                hfb = [sb2(f"hyhfb{i}", [128, 2, 256]) for i in range(2)]; t_hfb = [Tok(), Tok()]
                hab = [sb2(f"hyhab{i}", [128, 2, 256]) for i in range(2)]; t_hab = [Tok(), Tok()]
                for j in range(NTL):
                    bi = j % 2
                    P.dma("sp", win[bi], c_win[j * 128:(j + 1) * 128], writes=[t_win[bi]])
                    P.op("pe", lambda e, j=j: e.matmul(self.ps[1], lhsT=h2[:, j * 128:(j + 1) * 128], rhs=w3, start=True, stop=True),
                         reads=[t_h2, t_wm], writes=[self.pst[1]], skip_self=True)
                    P.op("dve", lambda e, bi=bi: e.tensor_tensor(out=hfb[bi], in0=self.ps[1].rearrange("p (a c) -> p a c", a=2), in1=win[bi], op=ALU.mult),
                         reads=[self.pst[1], t_win[bi]], writes=[t_hfb[bi]])
                    P.op("pool", lambda e, bi=bi, j=j: e.tensor_tensor(out=HS[:, j, :], in0=hfb[bi][:, 0, :], in1=hfb[bi][:, 1, :], op=ALU.add), reads=[t_hfb[bi]], writes=[t_HS], partial=True)
                    P.op("pool", lambda e, bi=bi, j=j: e.tensor_tensor(out=HD[:, j, :], in0=hfb[bi][:, 0, :], in1=hfb[bi][:, 1, :], op=ALU.subtract), reads=[t_hfb[bi]], writes=[t_HD], partial=True)
                    P.op("act", lambda e, bi=bi: e.activation(out=hab[bi], in_=hfb[bi], func=AF.Abs), reads=[t_hfb[bi]], writes=[t_hab[bi]])
                    for a in range(2):
                        P.op("pe", lambda e, bi=bi, a=a, j=j: e.matmul(self.ps[2][:, 0:256], lhsT=ones32, rhs=hab[bi][:, a, :], start=(j == 0 and a == 0), stop=(j == NTL - 1 and a == 1)),
                             reads=[t_ones, t_hab[bi]], writes=[self.pst[2]], skip_self=True)
                P.op("dve", lambda e: e.reciprocal(out=rl1, in_=self.ps[2][:, 0:256]), reads=[self.pst[2]], writes=[t_rl1])
                P.barrier()
            with contextlib.ExitStack() as st2:
                sb2 = lambda name, shape, dt=F32: self.sb(st2, name, shape, dt)
                pin = [sb2(f"hypin{i}", [128, L]) for i in range(2)]; t_pin = [Tok(), Tok()]
                uu = [sb2(f"hyu{i}", [128, L]) for i in range(2)]; t_uu = [Tok(), Tok()]

                def conv(ch, bi):
                    P.dma("sp" if bi == 0 else "act", pin[bi], self.PT[HY_LO + ch * 128:HY_LO + (ch + 1) * 128, tok0:tok0 + L], reads=[self.t_PT], writes=[t_pin[bi]])
                    eng = "dve"
                    P.op(eng, lambda e: e.tensor_scalar(out=uu[bi], in0=pin[bi], scalar1=cw[:, ch, 1:2], scalar2=cb[:, ch:ch + 1], op0=ALU.mult, op1=ALU.add), reads=[t_pin[bi], t_cw], writes=[t_uu[bi]])
                    P.op(eng, lambda e: e.scalar_tensor_tensor(out=uu[bi][:, 1:L], in0=pin[bi][:, 0:L - 1], scalar=cw[:, ch, 0:1], in1=uu[bi][:, 1:L], op0=ALU.mult, op1=ALU.add), reads=[t_pin[bi], t_cw], writes=[t_uu[bi]])
                    P.op(eng, lambda e: e.scalar_tensor_tensor(out=uu[bi][:, 0:L - 1], in0=pin[bi][:, 1:L], scalar=cw[:, ch, 2:3], in1=uu[bi][:, 0:L - 1], op0=ALU.mult, op1=ALU.add), reads=[t_pin[bi], t_cw], writes=[t_uu[bi]])
                for cc in range(2):
                    conv(cc, 0)
                    P.op("act", lambda e, cc=cc: e.copy(out=x0T[:, cc, :], in_=uu[0]), reads=[t_uu[0]], writes=[t_x0], partial=True)
                    conv(2 + cc, 0)
                    conv(4 + cc, 1)
                    P.op("pool", lambda e, cc=cc: e.tensor_tensor(out=zT[:, cc, :], in0=uu[0], in1=uu[1], op=ALU.mult), reads=[t_uu[0], t_uu[1]], writes=[t_zT], partial=True)
                g = 0
                for j0 in range(0, NTL, 2):
                    pi = 3 + g % 2
                    g += 1
                    pT = self.ps[pi].bitcast(BF16)
                    nj = min(2, NTL - j0)
                    for jj in range(nj):
                        for cc in range(2):
                            P.op("pe", lambda e, jj=jj, cc=cc, j0=j0, pT=pT: e.transpose(pT[:, (jj * 2 + cc) * 128:(jj * 2 + cc + 1) * 128], zT[:, cc, (j0 + jj) * 128:(j0 + jj + 1) * 128], self.identb),
                                 reads=[t_zT, self.t_ident], writes=[self.pst[pi]], skip_self=True)
                    if g % 2 == 0:
                        P.op("act", lambda e, j0=j0, nj=nj, pT=pT: e.copy(out=Z[:, j0:j0 + nj, :].rearrange("p j c -> p (j c)"), in_=pT[:, 0:nj * 256]), reads=[self.pst[pi]], writes=[t_Z], partial=True)
                    else:
                        P.op("dve", lambda e, j0=j0, nj=nj, pT=pT: e.tensor_copy(out=Z[:, j0:j0 + nj, :].rearrange("p j c -> p (j c)"), in_=pT[:, 0:nj * 256]), reads=[self.pst[pi]], writes=[t_Z], partial=True)
                P.barrier()
            with contextlib.ExitStack() as st2:
                sb2 = lambda name, shape, dt=F32: self.sb(st2, name, shape, dt)
                CT = [sb2(f"hyCT{i}", [128, NTL, 128], BF16) for i in range(2)]; t_CT = [Tok(), Tok()]
                ST = [sb2(f"hyST{i}", [128, NTL, 128], BF16) for i in range(2)]; t_ST = [Tok(), Tok()]
                hcs = [sb2(f"hyhcs{i}", [128, 2, 256]) for i in range(2)]; t_hcs = [Tok(), Tok()]
                t1 = sb2("hyt1", [128, 256]); t_t1 = Tok()
                t2 = sb2("hyt2", [128, 256]); t_t2 = Tok()
                t3 = sb2("hyt3", [128, 256]); t_t3 = Tok()
                t4 = sb2("hyt4", [128, 256]); t_t4 = Tok()
                for fc in range(NF):
                    bi = fc % 2
                    P.dma("sp", CT[bi], c_C[0:L, fc * 128:(fc + 1) * 128].rearrange("(j p) f -> p j f", p=128), writes=[t_CT[bi]])
                    P.dma("act", ST[bi], c_S[0:L, fc * 128:(fc + 1) * 128].rearrange("(j p) f -> p j f", p=128), writes=[t_ST[bi]])
                    pc, psn = 2 * bi, 2 * bi + 1
                    for (mat, t_mat, pi, rhs2, t_rhs2) in ((CT[bi], t_CT[bi], pc, HS, t_HS), (ST[bi], t_ST[bi], psn, HD, t_HD)):
                        for half, (rt, t_rt) in enumerate(((Z, t_Z), (rhs2, t_rhs2))):
                            for j in range(NTL):
                                P.op("pe", lambda e, mat=mat, pi=pi, rt=rt, j=j, half=half: e.matmul(self.ps[pi][:, half * 256:(half + 1) * 256], lhsT=mat[:, j, :], rhs=rt[:, j, :], start=(j == 0), stop=(j == NTL - 1)),
                                     reads=[t_mat, t_rt], writes=[self.pst[pi]], skip_self=True)
                    P.op("act", lambda e, bi=bi, pc=pc: e.copy(out=hcs[bi][:, 0, :], in_=self.ps[pc][:, 256:512]), reads=[self.pst[pc]], writes=[t_hcs[bi]], partial=True)
                    P.op("act", lambda e, bi=bi, psn=psn: e.copy(out=hcs[bi][:, 1, :], in_=self.ps[psn][:, 256:512]), reads=[self.pst[psn]], writes=[t_hcs[bi]], partial=True)
                    P.op("dve", lambda e, bi=bi, pc=pc: e.tensor_tensor(out=t1, in0=self.ps[pc][:, 0:256], in1=hcs[bi][:, 0, :], op=ALU.mult), reads=[self.pst[pc], t_hcs[bi]], writes=[t_t1])
                    P.op("dve", lambda e, bi=bi, pc=pc: e.tensor_tensor(out=t3, in0=self.ps[pc][:, 0:256], in1=hcs[bi][:, 1, :], op=ALU.mult), reads=[self.pst[pc], t_hcs[bi]], writes=[t_t3])
                    P.op("dve", lambda e, bi=bi, psn=psn: e.tensor_tensor(out=t2, in0=self.ps[psn][:, 0:256], in1=hcs[bi][:, 1, :], op=ALU.mult), reads=[self.pst[psn], t_hcs[bi]], writes=[t_t2])
                    P.op("dve", lambda e, bi=bi, psn=psn: e.tensor_tensor(out=t4, in0=self.ps[psn][:, 0:256], in1=hcs[bi][:, 0, :], op=ALU.mult), reads=[self.pst[psn], t_hcs[bi]], writes=[t_t4])
                    P.op("pool", lambda e: e.tensor_tensor(out=t1, in0=t1, in1=t2, op=ALU.subtract), reads=[t_t2], writes=[t_t1])
                    P.op("pool", lambda e: e.tensor_tensor(out=t3, in0=t3, in1=t4, op=ALU.add), reads=[t_t4], writes=[t_t3])
                    P.op("dve", lambda e, fc=fc: e.scalar_tensor_tensor(out=Asp[:, fc, :], in0=t1, scalar=wf[:, fc:fc + 1], in1=rl1, op0=ALU.mult, op1=ALU.mult), reads=[t_t1, t_wf, t_rl1], writes=[t_A], partial=True)
                    P.op("dve", lambda e, fc=fc: e.scalar_tensor_tensor(out=Bsp[:, fc, :], in0=t3, scalar=wf[:, fc:fc + 1], in1=rl1, op0=ALU.mult, op1=ALU.mult), reads=[t_t3, t_wf, t_rl1], writes=[t_B], partial=True)
                P.barrier()
            with contextlib.ExitStack() as st2:
                sb2 = lambda name, shape, dt=F32: self.sb(st2, name, shape, dt)
                TW = min(L, 1024)
                GC = [sb2(f"hyGC{i}", [128, TW], BF16) for i in range(3)]; t_GC = [Tok() for _ in range(3)]
                GS = [sb2(f"hyGS{i}", [128, TW], BF16) for i in range(3)]; t_GS = [Tok() for _ in range(3)]
                yt = sb2("hyyt", [128, 512]); t_yt = Tok()
                yo = [sb2(f"hyyo{i}", [128, 512], BF16) for i in range(2)]; t_yo = [Tok(), Tok()]
                nld = 0
                for tb0 in range(0, L, TW):
                    nsub = TW // BW
                    for fc in range(NF):
                        gi = nld % 3
                        nld += 1
                        P.dma("sp", GC[gi], c_C[fc * 128:(fc + 1) * 128, tb0:tb0 + TW], writes=[t_GC[gi]])
                        P.dma("act", GS[gi], c_S[fc * 128:(fc + 1) * 128, tb0:tb0 + TW], writes=[t_GS[gi]])
                        for sub in range(nsub):
                            for cc in range(2):
                                pi = sub * 2 + cc
                                P.op("pe", lambda e, gi=gi, fc=fc, sub=sub, cc=cc, pi=pi: e.matmul(self.ps[pi][:, 0:BW], lhsT=Asp[:, fc, cc * 128:(cc + 1) * 128], rhs=GC[gi][:, sub * BW:(sub + 1) * BW], start=(fc == 0), stop=False),
                                     reads=[t_A, t_GC[gi]], writes=[self.pst[pi]], skip_self=True)
                                P.op("pe", lambda e, gi=gi, fc=fc, sub=sub, cc=cc, pi=pi: e.matmul(self.ps[pi][:, 0:BW], lhsT=Bsp[:, fc, cc * 128:(cc + 1) * 128], rhs=GS[gi][:, sub * BW:(sub + 1) * BW], start=False, stop=(fc == NF - 1)),
                                     reads=[t_B, t_GS[gi]], writes=[self.pst[pi]], skip_self=True)
                    for sub in range(nsub):
                        for cc in range(2):
                            pi = sub * 2 + cc
                            c0 = tb0 + sub * BW
                            oi = pi % 2
                            P.op("dve", lambda e, cc=cc, c0=c0, pi=pi: e.scalar_tensor_tensor(out=yt[:, 0:BW], in0=zT[:, cc, c0:c0 + BW], scalar=bd[:, cc:cc + 1], in1=self.ps[pi][:, 0:BW], op0=ALU.mult, op1=ALU.add),
                                 reads=[t_zT, t_bd, self.pst[pi]], writes=[t_yt])
                            P.op("pool", lambda e, cc=cc, c0=c0, oi=oi: e.tensor_tensor(out=yo[oi][:, 0:BW], in0=yt[:, 0:BW], in1=x0T[:, cc, c0:c0 + BW], op=ALU.mult), reads=[t_yt, t_x0], writes=[t_yo[oi]])
                            P.dma("pool", self.MIXT[768 + cc * 128:768 + (cc + 1) * 128, tok0 + c0:tok0 + c0 + BW], yo[oi][:, 0:BW], reads=[t_yo[oi]], writes=[self.t_MIXT], partial=True)
                P.barrier()

    def phase_wout(self, li):
        nc, P = self.nc, self.P
        last = li == DEPTH - 1
        with contextlib.ExitStack() as st:
            sb = lambda name, shape, dt=F32: self.sb(st, name, shape, dt)
            wo = sb("woutb", [128, 8, D], BF16); t_wo = Tok()
            with contextlib.ExitStack() as st2:
                self.load_cast_weight(st2, wo, t_wo, self.w_out[li], D)
                P.barrier()
            G1 = sb("G1rep", [128, 2, D]); t_G1 = Tok()
            for v in range(2):
                P.dma("sp", G1[:, v, :], self.MOD[v, 2 * D:3 * D].partition_broadcast(128), reads=[self.t_MOD], writes=[t_G1], partial=True)
            mx = [sb(f"mixT{i}", [128, 8, 128], BF16) for i in range(2)]; t_mx = [Tok(), Tok()]
            xt = [sb(f"wxt{i}", [128, D]) for i in range(2)]; t_xt = [Tok(), Tok()]
            tm = [sb(f"wtm{i}", [128, 512]) for i in range(2)]; t_tm = [Tok(), Tok()]
            tiles = list(range(2 if last else 0, TT // 128))
            for n, j in enumerate(tiles):
                bi = n % 2
                v = 1 if j < 2 else 0
                c0, c1 = j * 128, (j + 1) * 128
                P.dma("sp", mx[bi], self.MIXT[:, c0:c1].rearrange("(c p) t -> p c t", p=128), reads=[self.t_MIXT], writes=[t_mx[bi]])
                P.dma("act", xt[bi], self.XS[c0:c1, :], reads=[self.t_XS], writes=[t_xt[bi]])
                for half in range(2):
                    pi = (n * 2 + half) % 4
                    for k in range(8):
                        P.op("pe", lambda e, bi=bi, k=k, half=half, pi=pi: e.matmul(self.ps[pi], lhsT=mx[bi][:, k, :], rhs=wo[:, k, half * 512:(half + 1) * 512], start=(k == 0), stop=(k == 7)),
                             reads=[t_mx[bi], t_wo], writes=[self.pst[pi]], skip_self=True)
                    P.op("dve", lambda e, half=half, pi=pi, v=v: e.tensor_tensor(out=tm[half], in0=self.ps[pi], in1=G1[:, v, half * 512:(half + 1) * 512], op=ALU.mult),
                         reads=[self.pst[pi], t_G1], writes=[t_tm[half]])
                    P.op("pool", lambda e, bi=bi, half=half: e.tensor_tensor(out=xt[bi][:, half * 512:(half + 1) * 512], in0=xt[bi][:, half * 512:(half + 1) * 512], in1=tm[half], op=ALU.add),
                         reads=[t_tm[half]], writes=[t_xt[bi]])
                P.dma("pool", self.XS[c0:c1, :], xt[bi], reads=[t_xt[bi]], writes=[self.t_XS], partial=True)
            P.barrier()

    def phase_ffn(self, li):
        nc, P = self.nc, self.P
        last = li == DEPTH - 1
        with contextlib.ExitStack() as st:
            sb = lambda name, shape, dt=F32: self.sb(st, name, shape, dt)
            wup = sb("wupb", [128, 8, 2 * DFF], BF16); t_wup = Tok()
            wdn = sb("wdnb", [128, NFC, D], BF16); t_wdn = Tok()
            with contextlib.ExitStack() as st2:
                self.load_cast_weight(st2, wup, t_wup, self.ffn_w_up[li], 2 * DFF)
                P.barrier()
            with contextlib.ExitStack() as st2:
                self.load_cast_weight(st2, wdn, t_wdn, self.ffn_w_down[li], D, blk=256, k_chunks=NFC)
                P.barrier()
            G2 = sb("G2rep", [128, 2, D]); t_G2 = Tok()
            for v in range(2):
                P.dma("sp", G2[:, v, :], self.MOD[v, 5 * D:6 * D].partition_broadcast(128), reads=[self.t_MOD], writes=[t_G2], partial=True)
            if last:
                FG = sb("FGrep", [128, D]); t_FG = Tok()
                P.dma("sp", FG, self.final_norm_g.partition_broadcast(128), writes=[t_FG])
            cw = sb("ffcw", [128, 2 * NFC, 3]); cbv = sb("ffcb", [128, 2 * NFC]); t_cw = Tok()
            for jj in range(3):
                P.dma("sp", cw[:, :, jj], self.ffn_conv_w[li, jj].rearrange("(c p) -> p c", p=128), writes=[t_cw], partial=True, allow_slow_non_contiguous=True)
            P.dma("sp", cbv, self.ffn_conv_b[li].rearrange("(c p) -> p c", p=128), writes=[t_cw], partial=True, allow_slow_non_contiguous=True)
            hx = [sb(f"hTx{i}", [128, 8, 258], BF16) for i in range(3)]; t_hx = [Tok() for _ in range(3)]
            gT = sb("ffgT", [128, NFC, 256], BF16); t_gT = Tok()
            xts = [sb(f"fxt{i}", [128, D]) for i in range(4)]; t_xts = [Tok() for _ in range(4)]
            sqj = sb("fsq", [128, D], BF16); t_sqj = Tok()
            sss = [sb(f"fss{i}", [128, 4]) for i in range(4)]; t_sss = [Tok() for _ in range(4)]
            xns = [sb(f"fxn{i}", [128, D], BF16) for i in range(2)]; t_xns = [Tok(), Tok()]
            ga = [sb(f"ffga{i}", [128, 256]) for i in range(2)]; t_ga = [Tok(), Tok()]
            gb = [sb(f"ffgb{i}", [128, 256]) for i in range(2)]; t_gb = [Tok(), Tok()]
            tmo = [sb(f"fftm{i}", [128, 512]) for i in range(2)]; t_tmo = [Tok(), Tok()]
            fss = sb("ffss", [128, 4]); t_fss = Tok()
            tiles = list(range(1 if last else 0, TT // 256))
            seq_first = {0, 1}
            seq_last = {0, TT // 256 - 1}
            nsub = 0
            sets_of = {}

            def stage_a(ti):
                nonlocal nsub
                t0 = ti * 256
                v = 1 if ti == 0 else 0
                hb = ti % 3
                sets_of[ti] = []
                for sub in range(2):
                    si = nsub % 4
                    nsub += 1
                    sets_of[ti].append(si)
                    bufs = (xts[si], t_xts[si], sqj, t_sqj, sss[si], t_sss[si], xns[si % 2], t_xns[si % 2])
                    self.norm_transpose(bufs, self.XS[t0 + sub * 128:t0 + (sub + 1) * 128, :], self.t_XS, self.s2, self.modT[:, 24:32, :], v,
                                        hx[hb], t_hx[hb], 1 + sub * 128, ps_i=6 + sub)
                if ti in seq_first:
                    P.op("pool", lambda e, hb=hb: e.memset(hx[hb][:, :, 0:1], 0.0), writes=[t_hx[hb]], partial=True)
                if ti in seq_last:
                    P.op("pool", lambda e, hb=hb: e.memset(hx[hb][:, :, 257:258], 0.0), writes=[t_hx[hb]], partial=True)

            def halo(ta, tb):
                a, b = ta % 3, tb % 3
                P.op("pool", lambda e: e.tensor_copy(out=hx[a][:, :, 257:258], in_=hx[b][:, :, 1:2]), reads=[t_hx[b]], writes=[t_hx[a]], partial=True)
                P.op("pool", lambda e: e.tensor_copy(out=hx[b][:, :, 0:1], in_=hx[a][:, :, 256:257]), reads=[t_hx[a]], writes=[t_hx[b]], partial=True)

            def stage_b(ti):
                t0 = ti * 256
                v = 1 if ti == 0 else 0
                hb = ti % 3
                for jc in range(NFC):
                    gi = jc % 2
                    for (which, col0, pi, acc, t_acc) in ((0, jc * 128, 0 + 2 * gi, ga[gi], t_ga[gi]), (1, DFF + jc * 128, 1 + 2 * gi, gb[gi], t_gb[gi])):
                        ch = (col0 // 128)
                        for k in range(8):
                            P.op("pe", lambda e, k=k, col0=col0, pi=pi: e.matmul(self.ps[pi][:, 0:258], lhsT=wup[:, k, col0:col0 + 128], rhs=hx[hb][:, k, :], start=(k == 0), stop=(k == 7)),
                                 reads=[t_wup, t_hx[hb]], writes=[self.pst[pi]], skip_self=True)
                        P.op("act", lambda e, pi=pi, acc=acc, ch=ch: e.activation(out=acc, in_=self.ps[pi][:, 1:257], func=AF.Identity, scale=cw[:, ch, 1:2], bias=cbv[:, ch:ch + 1]),
                             reads=[self.pst[pi], t_cw], writes=[t_acc])
                        P.op("dve", lambda e, pi=pi, acc=acc, ch=ch: e.scalar_tensor_tensor(out=acc, in0=self.ps[pi][:, 0:256], scalar=cw[:, ch, 0:1], in1=acc, op0=ALU.mult, op1=ALU.add),
                             reads=[self.pst[pi], t_cw], writes=[t_acc])
                        P.op("dve", lambda e, pi=pi, acc=acc, ch=ch: e.scalar_tensor_tensor(out=acc, in0=self.ps[pi][:, 2:258], scalar=cw[:, ch, 2:3], in1=acc, op0=ALU.mult, op1=ALU.add),
                             reads=[self.pst[pi], t_cw], writes=[t_acc])
                    P.op("act", lambda e, gi=gi: e.activation(out=ga[gi], in_=ga[gi], func=AF.Silu), reads=[t_ga[gi]], writes=[t_ga[gi]])
                    P.op("pool", lambda e, gi=gi, jc=jc: e.tensor_tensor(out=gT[:, jc, :], in0=ga[gi], in1=gb[gi], op=ALU.mult), reads=[t_ga[gi], t_gb[gi]], writes=[t_gT], partial=True)
                for sub in range(2):
                    si = sets_of[ti][sub]
                    xt, t_xt = xts[si], t_xts[si]
                    for half in range(2):
                        pi = 4 + half
                        for jc in range(NFC):
                            P.op("pe", lambda e, jc=jc, sub=sub, half=half, pi=pi: e.matmul(self.ps[pi], lhsT=gT[:, jc, sub * 128:(sub + 1) * 128], rhs=wdn[:, jc, half * 512:(half + 1) * 512], start=(jc == 0), stop=(jc == NFC - 1)),
                                 reads=[t_gT, t_wdn], writes=[self.pst[pi]], skip_self=True)
                        P.op("dve", lambda e, half=half, pi=pi: e.tensor_tensor(out=tmo[half], in0=self.ps[pi], in1=G2[:, v, half * 512:(half + 1) * 512], op=ALU.mult),
                             reads=[self.pst[pi], t_G2], writes=[t_tmo[half]])
                        P.op("pool", lambda e, half=half, xt=xt: e.tensor_tensor(out=xt[:, half * 512:(half + 1) * 512], in0=xt[:, half * 512:(half + 1) * 512], in1=tmo[half], op=ALU.add),
                             reads=[t_tmo[half]], writes=[t_xt])
                    r0 = t0 + sub * 128
                    if not last:
                        P.dma("pool", self.XS[r0:r0 + 128, :], xt, reads=[t_xt], writes=[self.t_XS], partial=True)
                    else:
                        P.op("act", lambda e, xt=xt: e.activation(out=sqj, in_=xt, func=AF.Square, accum_out=fss[:, 0:1]), reads=[t_xt], writes=[t_sqj, t_fss])
                        P.op("dve", lambda e: e.tensor_scalar(out=fss[:, 1:2], in0=fss[:, 0:1], scalar1=1.0 / D, scalar2=EPS, op0=ALU.mult, op1=ALU.add), reads=[t_fss], writes=[t_fss])
                        P.op("act", lambda e: e.activation(out=fss[:, 2:3], in_=fss[:, 1:2], func=AF.Sqrt), reads=[t_fss], writes=[t_fss])
                        P.op("dve", lambda e: e.reciprocal(out=fss[:, 3:4], in_=fss[:, 2:3]), reads=[t_fss], writes=[t_fss])
                        P.op("dve", lambda e, xt=xt: e.scalar_tensor_tensor(out=xt, in0=xt, scalar=fss[:, 3:4], in1=FG, op0=ALU.mult, op1=ALU.mult), reads=[t_fss, t_FG], writes=[t_xt])
                        P.dma("pool", self.out[r0 - CTX:r0 - CTX + 128, :], xt, reads=[t_xt], writes=[self.t_out], partial=True)

            prev = None
            for ti in tiles:
                stage_a(ti)
                if prev is not None:
                    if prev not in seq_last:
                        halo(prev, ti)
                    stage_b(prev)
                prev = ti
            stage_b(prev)
            P.barrier()

    def build(self, stop_after=None):
        nc, P = self.nc, self.P
        self.declare()
        P.clear_sems()
        gst = self.stack
        self.identb = self.sb(gst, "identb", [128, 128], BF16)
        self.t_ident = Tok()
        P.dma("sp", self.identb, self.c_ident, writes=[self.t_ident])
        self.t_XS = Tok()
        self.t_MOD, self.t_PT, self.t_PVO, self.t_MIXT, self.t_out = Tok(), Tok(), Tok(), Tok(), Tok()
        P.dma("sp", self.XS[0:CTX, :], self.ctx, writes=[self.t_XS], partial=True)
        for i in range(16):
            P.dma("act" if i % 2 else "sp", self.XS[CTX + i * 256:CTX + (i + 1) * 256, :], self.x[i * 256:(i + 1) * 256, :], writes=[self.t_XS], partial=True)
        done = False
        for li in range(DEPTH):
            with contextlib.ExitStack() as lst:
                self.lst = lst
                for name, fn in (("mod", self.phase_mod), ("inproj", self.phase_inproj), ("mla", self.phase_mla), ("mlstm", self.phase_mlstm), ("hyena", self.phase_hyena), ("wout", self.phase_wout), ("ffn", self.phase_ffn)):
                    fn(li)
                    if stop_after == (name, li):
                        done = True
                        break
                P.barrier()
            if done:
                break
        P.finish()
        return nc


def _prep_inputs(inputs, b):
    m = {}
    for k, v in inputs.items():
        v = np.asarray(v)
        if k in ("x", "ctx", "c"):
            m[k] = np.ascontiguousarray(v[b])
        elif k == "ml_gate_b":
            m[k] = np.ascontiguousarray(v.reshape(DEPTH, 16))
        else:
            m[k] = np.ascontiguousarray(v)
    return m


def kernel(**inputs):
    bld = Builder()
    nc = bld.build()
    consts = _consts()
    in_maps = []
    for b in range(8):
        m = _prep_inputs(inputs, b)
        m.update(consts)
        in_maps.append({k: m[k] for k in bld.inp})
    res = run_bass_kernel_spmd(nc, in_maps, core_ids=list(range(8)))
    return np.stack([np.asarray(r["out"]) for r in res.results], axis=0).astype(np.float32)
```

```python
import contextlib
import numpy as np
import ml_dtypes
import concourse.bass as bass
import concourse.mybir as mybir
from concourse.bass_utils import run_bass_kernel_spmd

F32 = mybir.dt.float32
BF16 = mybir.dt.bfloat16
I32 = mybir.dt.int32
AF = mybir.ActivationFunctionType
ALU = mybir.AluOpType
AX = mybir.AxisListType

D = 1024
SEQ = 4096
CTX = 256
TT = SEQ + CTX
DEPTH = 2
EPS = 1e-6
NH = 8
QR, KVR, ROPE = 384, 256, 32
N_MLA_IN = QR + KVR + ROPE
MLW = 256
N_ML_IN = 3 * MLW + 16
HYW = 256
N_IN = 2224
ML_LO = N_MLA_IN
HY_LO = N_MLA_IN + N_ML_IN
DFF = 2816
NFC = DFF // 128
TWO_PI = float(2 * np.pi)


class Tok:
    __slots__ = ("w", "r", "wf")

    def __init__(self):
        self.w = []
        self.wf = []
        self.r = []


class PTok(Tok):
    __slots__ = ()


class Queue:
    def __init__(self, prog, name, eng, n_dma_sems=0):
        self.p = prog
        self.name = name
        self.eng = eng
        nc = prog.nc
        self.sem = nc.alloc_semaphore(name=f"s_{name}")
        self.count = 0
        self.dma_sems = [nc.alloc_semaphore(name=f"d_{name}{i}") for i in range(n_dma_sems)]
        self.dma_counts = [0] * n_dma_sems
        self.dma_rr = 0
        self.waited = {}

    def _wait(self, tick):
        sem, val = tick
        key = id(sem)
        if self.waited.get(key, 0) >= val:
            return
        self.eng.wait_ge(sem, val)
        self.waited[key] = val
        self.p.n_waits += 1


class Prog:
    def __init__(self, nc, dma_sems=8):
        self.nc = nc
        self.n_waits = 0
        self.n_ins = 0
        self.q = {
            "pe": Queue(self, "pe", nc.tensor),
            "dve": Queue(self, "dve", nc.vector),
            "act": Queue(self, "act", nc.scalar, dma_sems),
            "pool": Queue(self, "pool", nc.gpsimd, dma_sems),
            "sp": Queue(self, "sp", nc.sync, dma_sems),
        }

    def clear_sems(self):
        for q in self.q.values():
            q.eng.sem_clear(q.sem)
            for s in q.dma_sems:
                q.eng.sem_clear(s)
        self.nc.all_engine_barrier()

    def _deps(self, q, reads, writes, skip_self=False, partial=False):
        for t in reads:
            for tk in t.w:
                if not (skip_self and tk[0] is q.sem):
                    q._wait(tk)
            if isinstance(t, PTok):
                for tk in t.r:
                    if not (skip_self and tk[0] is q.sem):
                        q._wait(tk)
        for t in writes:
            if partial and not isinstance(t, PTok):
                for tk in t.wf:
                    if not (skip_self and tk[0] is q.sem):
                        q._wait(tk)
            if not partial or isinstance(t, PTok):
                for tk in t.w:
                    if not (skip_self and tk[0] is q.sem):
                        q._wait(tk)
            for tk in t.r:
                if not (skip_self and tk[0] is q.sem):
                    q._wait(tk)

    @staticmethod
    def _compact(lst):
        best = {}
        for s, v in lst:
            k = id(s)
            if k not in best or best[k][1] < v:
                best[k] = (s, v)
        return list(best.values())

    def _record(self, tick, reads, writes, partial=False):
        for t in reads:
            if isinstance(t, PTok):
                t.w = [tick]
                t.wf = [tick]
                t.r = []
                continue
            t.r.append(tick)
            if len(t.r) > 48:
                t.r = self._compact(t.r)
        for t in writes:
            if partial and not isinstance(t, PTok):
                t.w.append(tick)
                if len(t.w) > 48:
                    t.w = self._compact(t.w)
            else:
                t.w = [tick]
                t.wf = [tick]
                t.r = []

    def op(self, qname, fn, reads=(), writes=(), skip_self=False, partial=False):
        q = self.q[qname]
        self._deps(q, reads, writes, skip_self=skip_self, partial=partial)
        ins = fn(q.eng)
        q.count += 1
        ins.then_inc(q.sem, 1)
        self._record((q.sem, q.count), reads, writes, partial=partial)
        self.n_ins += 1
        return ins

    def dma(self, qname, out, in_, reads=(), writes=(), partial=False, **kw):
        q = self.q[qname]
        j = q.dma_rr
        q.dma_rr = (j + 1) % len(q.dma_sems)
        sem = q.dma_sems[j]
        if q.dma_counts[j] > 0:
            q._wait((sem, q.dma_counts[j]))
        self._deps(q, reads, writes, partial=partial)
        ins = q.eng.dma_start(out=out, in_=in_, **kw)
        q.dma_counts[j] += 16
        ins.then_inc(sem, 16)
        self._record((sem, q.dma_counts[j]), reads, writes, partial=partial)
        self.n_ins += 1
        return ins

    def barrier(self):
        ticks = []
        for q in self.q.values():
            if q.count:
                ticks.append((q.sem, q.count))
            for j, s in enumerate(q.dma_sems):
                if q.dma_counts[j]:
                    ticks.append((s, q.dma_counts[j]))
        for q in self.q.values():
            for tk in ticks:
                q._wait(tk)

    def finish(self):
        ticks = []
        for q in self.q.values():
            if q.count:
                ticks.append((q.sem, q.count))
            for j, s in enumerate(q.dma_sems):
                if q.dma_counts[j]:
                    ticks.append((s, q.dma_counts[j]))
        for tk in ticks:
            self.q["sp"]._wait(tk)


def _consts():
    c = {}
    c["ident"] = np.eye(128, dtype=np.float32).astype(ml_dtypes.bfloat16)
    n_freq = ROPE // 4
    inv = (10000.0 ** (-np.arange(n_freq, dtype=np.float32) / n_freq)).astype(np.float32)
    row = np.repeat(np.arange(SEQ // 64, dtype=np.float32), 64)
    col = np.tile(np.arange(64, dtype=np.float32), SEQ // 64)
    ang = np.concatenate([row[:, None] * inv, col[:, None] * inv], axis=-1).astype(np.float32)
    cos = np.cos(ang).astype(np.float32).T
    sin = np.sin(ang).astype(np.float32).T
    c["rope_cos"] = np.ascontiguousarray(np.concatenate([cos, cos], 0))
    c["rope_sin"] = np.ascontiguousarray(np.concatenate([sin, sin], 0))
    ss_, tt_ = np.meshgrid(np.arange(128), np.arange(128), indexing="ij")
    c["ml_mask"] = np.stack([(ss_ <= tt_), (ss_ >= tt_)], 1).astype(np.float32).astype(ml_dtypes.bfloat16)
    selA = np.zeros((96, 4, 128), np.float32)
    selW = np.zeros((96, 8), np.float32)
    for d in range(2):
        base = 32 + 4 if d == 0 else 64 + 12
        for h in range(4):
            selA[base + h, (h // 2) * 2 + d, (h % 2) * 64:(h % 2) * 64 + 64] = 1.0
            selW[8 * d + h, d * 4 + h] = 1.0
            selW[base + h, d * 4 + h] = -1.0
    c["ml_selA"] = selA
    c["ml_selW"] = selW
    for L in (256, 4096):
        f32 = np.float32
        t = np.linspace(0.0, 1.0, L, dtype=f32)[:, None]
        omega = (f32(2.0 * np.pi) * np.arange(L, dtype=f32) / f32(L)).astype(f32)
        bands = np.linspace(1e-4, 15, 16, dtype=f32)
        ang = (omega[:, None] * bands[None, :]).astype(f32)
        z = np.concatenate([t, np.cos(ang).astype(f32), -np.sin(ang).astype(f32)], axis=-1).astype(f32)
        c[f"hy_z{L}"] = np.ascontiguousarray(z.T)
        deltas = np.abs(np.linspace(np.log(1e-2) / 1.5, np.log(1e-2) / 0.3, 256, dtype=f32)).astype(f32)
        window = (np.exp(-t * deltas).astype(f32) + f32(0.05)).astype(f32)
        wb = window.copy()
        wb[0] = 0.0
        c[f"hy_win{L}"] = np.ascontiguousarray(np.stack([window, wb], axis=1))
        NFp = L + 128
        n = 2 * L
        idx = np.arange(L + 1, dtype=np.int64)
        prod = (idx[:, None] * idx[None, :]) % n
        angm = prod.astype(np.float64) * (2.0 * np.pi / n)
        Cm = np.zeros((NFp, NFp), np.float32)
        Sm = np.zeros((NFp, NFp), np.float32)
        Cm[:L + 1, :L + 1] = np.cos(angm)
        Sm[:L + 1, :L + 1] = np.sin(angm)
        c[f"hy_C{L}"] = Cm.astype(ml_dtypes.bfloat16)
        c[f"hy_S{L}"] = Sm.astype(ml_dtypes.bfloat16)
        wfv = np.zeros(NFp, np.float32)
        wfv[:L + 1] = 2.0 / n
        wfv[0] = 1.0 / n
        wfv[L] = 1.0 / n
        c[f"hy_wf{L}"] = np.ascontiguousarray(wfv.reshape(-1, 128).T)
    return c


class Builder:
    def __init__(self, debug=None):
        self.debug = debug or set()
        self.nc = nc = bass.Bass("TRN2", target_bir_lowering=False)
        self.P = Prog(nc)
        self.inp = {}
        self.stack = contextlib.ExitStack()

    def din(self, name, shape, dt=F32):
        ap = self.nc.dram_tensor(name, list(shape), dt, kind="ExternalInput").ap()
        self.inp[name] = ap
        return ap

    def dscr(self, name, shape, dt=F32):
        kind = "ExternalOutput" if name in self.debug else "Internal"
        return self.nc.dram_tensor(name, list(shape), dt, kind=kind).ap()

    def sb(self, st, name, shape, dt=F32):
        self.uid = getattr(self, "uid", 0) + 1
        return st.enter_context(self.nc.sbuf_tensor(f"{name}_{self.uid}", list(shape), dt)).ap()

    def declare(self):
        din = self.din
        self.x = din("x", [SEQ, D])
        self.c = din("c", [D])
        self.ctx = din("ctx", [CTX, D])
        self.c_ctx = din("c_ctx", [D])
        self.ada_w = din("ada_w", [DEPTH, D, 6 * D])
        self.ada_b = din("ada_b", [DEPTH, 6 * D])
        self.norm1_g = din("norm1_g", [DEPTH, D])
        self.norm2_g = din("norm2_g", [DEPTH, D])
        self.w_in = din("w_in", [DEPTH, D, N_IN])
        self.mla_q_norm_g = din("mla_q_norm_g", [DEPTH, QR])
        self.mla_kv_norm_g = din("mla_kv_norm_g", [DEPTH, KVR])
        self.mla_w_uq = din("mla_w_uq", [DEPTH, QR, NH * 96])
        self.mla_w_ukv = din("mla_w_ukv", [DEPTH, KVR, NH * 128])
        self.ml_conv_w = din("ml_conv_w", [DEPTH, 3, MLW])
        self.ml_conv_b = din("ml_conv_b", [DEPTH, MLW])
        self.ml_wq = din("ml_wq", [DEPTH, 4, 64, 64])
        self.ml_wk = din("ml_wk", [DEPTH, 4, 64, 64])
        self.ml_gate_b = din("ml_gate_b", [DEPTH, 16])
        self.ml_norm_g = din("ml_norm_g", [DEPTH, MLW])
        self.hy_conv_w = din("hy_conv_w", [DEPTH, 3, 3 * HYW])
        self.hy_conv_b = din("hy_conv_b", [DEPTH, 3 * HYW])
        self.hy_w1 = din("hy_w1", [DEPTH, 33, 64])
        self.hy_b1 = din("hy_b1", [DEPTH, 64])
        self.hy_w2 = din("hy_w2", [DEPTH, 64, 64])
        self.hy_b2 = din("hy_b2", [DEPTH, 64])
        self.hy_w3 = din("hy_w3", [DEPTH, 64, 2 * HYW])
        self.hy_sin_freq = din("hy_sin_freq", [DEPTH, 64])
        self.hy_bias_d = din("hy_bias_d", [DEPTH, HYW])
        self.w_out = din("w_out", [DEPTH, D, D])
        self.ffn_w_up = din("ffn_w_up", [DEPTH, D, 2 * DFF])
        self.ffn_conv_w = din("ffn_conv_w", [DEPTH, 3, 2 * DFF])
        self.ffn_conv_b = din("ffn_conv_b", [DEPTH, 2 * DFF])
        self.ffn_w_down = din("ffn_w_down", [DEPTH, DFF, D])
        self.final_norm_g = din("final_norm_g", [D])
        self.c_ident = din("ident", [128, 128], BF16)
        self.c_rope_cos = din("rope_cos", [32, SEQ])
        self.c_rope_sin = din("rope_sin", [32, SEQ])
        self.c_ml_mask = din("ml_mask", [128, 2, 128], BF16)
        self.c_ml_selA = din("ml_selA", [96, 4, 128])
        self.c_ml_selW = din("ml_selW", [96, 8])
        for L in (256, 4096):
            NFp = L + 128
            setattr(self, f"c_hy_z{L}", din(f"hy_z{L}", [33, L]))
            setattr(self, f"c_hy_win{L}", din(f"hy_win{L}", [L, 2, 256]))
            setattr(self, f"c_hy_C{L}", din(f"hy_C{L}", [NFp, NFp], BF16))
            setattr(self, f"c_hy_S{L}", din(f"hy_S{L}", [NFp, NFp], BF16))
            setattr(self, f"c_hy_wf{L}", din(f"hy_wf{L}", [128, L // 128 + 1]))
        self.out = self.nc.dram_tensor("out", [SEQ, D], F32, kind="ExternalOutput").ap()
        self.XS = self.dscr("XS", [TT, D])
        self.MOD = self.dscr("MOD", [2, 6 * D])
        self.PT = self.dscr("PT", [N_IN + 32, TT])
        self.PVO = self.dscr("PVO", [TT, 512])
        self.MIXT = self.dscr("MIXT", [D, TT], BF16)
        self.psall = self.nc.alloc_psum_tensor("psall", [128, 8 * 512], F32).ap()
        self.ps = [self.psall[:, i * 512:(i + 1) * 512] for i in range(8)]
        self.pst = [PTok() for _ in range(8)]

    def vec_pc(self, st, name, src, n, q="sp"):
        t = self.sb(st, name, [128, n])
        tok = Tok()
        self.P.dma(q, t, src.rearrange("(c p) -> p c", p=128), writes=[tok], allow_slow_non_contiguous=True)
        return t, tok

    def phase_mod(self, li):
        nc, P = self.nc, self.P
        lst = self.lst
        self.modT = self.sb(lst, "modT", [128, 48, 2])
        self.t_mod = Tok()
        self.s1 = self.sb(lst, "s1", [128, 8, 2])
        self.s2 = self.sb(lst, "s2", [128, 8, 2])
        self.t_s12 = Tok()
        with contextlib.ExitStack() as st:
            cc = self.sb(st, "cc", [128, 8, 2])
            t_cc = Tok()
            P.dma("sp", cc[:, :, 0], self.c.rearrange("(c p) -> p c", p=128), writes=[t_cc], partial=True, allow_slow_non_contiguous=True)
            P.dma("sp", cc[:, :, 1], self.c_ctx.rearrange("(c p) -> p c", p=128), writes=[t_cc], partial=True, allow_slow_non_contiguous=True)
            sc = self.sb(st, "sc", [128, 8, 2])
            t_sc = Tok()
            P.op("act", lambda e: e.activation(out=sc, in_=cc, func=AF.Silu), reads=[t_cc], writes=[t_sc])
            ab = self.sb(st, "ab", [128, 48])
            t_ab = Tok()
            P.dma("sp", ab, self.ada_b[li].rearrange("(c p) -> p c", p=128), writes=[t_ab], allow_slow_non_contiguous=True)
            g12 = self.sb(st, "g12", [128, 8, 2])
            t_g12 = Tok()
            P.dma("sp", g12[:, :, 0], self.norm1_g[li].rearrange("(c p) -> p c", p=128), writes=[t_g12], partial=True, allow_slow_non_contiguous=True)
            P.dma("sp", g12[:, :, 1], self.norm2_g[li].rearrange("(c p) -> p c", p=128), writes=[t_g12], partial=True, allow_slow_non_contiguous=True)
            wt = [self.sb(st, f"adaw{i}", [128, 8, 512]) for i in range(2)]
            t_wt = [Tok(), Tok()]
            acc = self.ps[0]
            t_acc = self.pst[0]
            for nb in range(12):
                s = nb % 2
                P.dma("sp" if nb % 2 == 0 else "act", wt[s],
                      self.ada_w[li, :, nb * 512:(nb + 1) * 512].rearrange("(c p) n -> p c n", p=128),
                      writes=[t_wt[s]])
                for j in range(4):
                    n = nb * 4 + j
                    for k in range(8):
                        P.op("pe", lambda e, s=s, j=j, k=k, n=n: e.matmul(
                            acc[:, 2 * n:2 * n + 2], lhsT=wt[s][:, k, j * 128:(j + 1) * 128], rhs=sc[:, k, :],
                            start=(k == 0), stop=(k == 7)),
                            reads=[t_wt[s], t_sc], writes=[t_acc], skip_self=True)
            mod = self.modT
            P.op("dve", lambda e: e.tensor_tensor(out=mod, in0=acc[:, 0:96].rearrange("p (n v) -> p n v", v=2),
                                                  in1=ab.unsqueeze(2).to_broadcast([128, 48, 2]), op=ALU.add),
                 reads=[t_acc, t_ab], writes=[self.t_mod])
            for (dst, gi, c0) in ((self.s1, 0, 8), (self.s2, 1, 32)):
                P.op("dve", lambda e, dst=dst, gi=gi, c0=c0: e.scalar_tensor_tensor(
                    out=dst, in0=mod[:, c0:c0 + 8, :], scalar=1.0, in1=g12[:, :, gi:gi + 1].to_broadcast([128, 8, 2]),
                    op0=ALU.add, op1=ALU.mult), reads=[self.t_mod, t_g12], writes=[self.t_s12], partial=True)
            for v in range(2):
                P.dma("sp", self.MOD[v].rearrange("(c p) -> p c", p=128), mod[:, :, v], reads=[self.t_mod], writes=[self.t_MOD],
                      partial=True, allow_slow_non_contiguous=True)
            P.barrier()

    def norm_transpose(self, st_bufs, rows_ap, t_rows, svec, bvec, v, dst, t_dst, col0, ps_i):
        P = self.P
        xt, t_xt, sq, t_sq, ss, t_ss, xn, t_xn = st_bufs
        P.dma("sp", xt, rows_ap, reads=[t_rows], writes=[t_xt])
        P.op("act", lambda e: e.activation(out=sq, in_=xt, func=AF.Square, accum_out=ss[:, 0:1]), reads=[t_xt], writes=[t_sq, t_ss])
        P.op("dve", lambda e: e.tensor_scalar(out=ss[:, 1:2], in0=ss[:, 0:1], scalar1=1.0 / D, scalar2=EPS, op0=ALU.mult, op1=ALU.add),
             reads=[t_ss], writes=[t_ss])
        P.op("act", lambda e: e.activation(out=ss[:, 2:3], in_=ss[:, 1:2], func=AF.Sqrt), reads=[t_ss], writes=[t_ss])
        P.op("dve", lambda e: e.reciprocal(out=ss[:, 3:4], in_=ss[:, 2:3]), reads=[t_ss], writes=[t_ss])
        P.op("dve", lambda e: e.tensor_scalar(out=xn, in0=xt, scalar1=ss[:, 3:4], scalar2=None, op0=ALU.mult), reads=[t_xt, t_ss], writes=[t_xn])
        pb = self.ps[ps_i].bitcast(BF16)
        t_pb = self.pst[ps_i]
        for c in range(8):
            P.op("pe", lambda e, c=c: e.transpose(pb[:, c * 128:(c + 1) * 128], xn[:, c * 128:(c + 1) * 128], self.identb),
                 reads=[t_xn, self.t_ident], writes=[t_pb], skip_self=True, partial=(c > 0))
        for c in range(8):
            if c % 2 == 0:
                P.op("act", lambda e, c=c: e.activation(out=dst[:, c, col0:col0 + 128], in_=pb[:, c * 128:(c + 1) * 128], func=AF.Identity,
                                                         scale=svec[:, c, v:v + 1], bias=bvec[:, c, v:v + 1]),
                     reads=[t_pb, self.t_s12, self.t_mod], writes=[t_dst], partial=True)
            else:
                P.op("dve", lambda e, c=c: e.tensor_scalar(out=dst[:, c, col0:col0 + 128], in0=pb[:, c * 128:(c + 1) * 128],
                                                           scalar1=svec[:, c, v:v + 1], scalar2=bvec[:, c, v:v + 1], op0=ALU.mult, op1=ALU.add),
                     reads=[t_pb, self.t_s12, self.t_mod], writes=[t_dst], partial=True)

    def load_cast_weight(self, st, dst, t_dst, src_ap, ncols, blk=512, k_chunks=8, engs=None):
        P = self.P
        engs = engs or ("pool", "dve", "act")
        stg = [self.sb(st, f"wstg{id(dst) % 9973}_{i}", [128, k_chunks, blk]) for i in range(2)]
        t_stg = [Tok(), Tok()]
        i = 0
        for c0 in range(0, ncols, blk):
            w = min(blk, ncols - c0)
            s = i % 2
            P.dma("sp" if i % 2 == 0 else "act", stg[s][:, :, 0:w], src_ap[:, c0:c0 + w].rearrange("(c p) n -> p c n", p=128), writes=[t_stg[s]])
            eng = engs[i % len(engs)]
            if eng == "act":
                P.op("act", lambda e, s=s, c0=c0, w=w: e.copy(out=dst[:, :, c0:c0 + w], in_=stg[s][:, :, 0:w]), reads=[t_stg[s]], writes=[t_dst], partial=True)
            else:
                P.op(eng, lambda e, s=s, c0=c0, w=w: e.tensor_copy(out=dst[:, :, c0:c0 + w], in_=stg[s][:, :, 0:w]), reads=[t_stg[s]], writes=[t_dst], partial=True)
            i += 1

    def phase_inproj(self, li):
        nc, P = self.nc, self.P
        NW = N_IN + 32
        with contextlib.ExitStack() as st:
            wb = self.sb(st, "winb", [128, 8, NW], BF16)
            t_wb = Tok()
            with contextlib.ExitStack() as st2:
                self.load_cast_weight(st2, wb, t_wb, self.w_in[li], N_IN)
                P.op("dve", lambda e: e.tensor_scalar(out=wb[:, :, N_IN:N_IN + 16], in0=wb[:, :, 656:672], scalar1=-1.0, scalar2=None, op0=ALU.mult),
                     reads=[t_wb], writes=[t_wb], partial=True)
                P.op("dve", lambda e: e.tensor_copy(out=wb[:, :, N_IN + 16:N_IN + 32], in_=wb[:, :, 640:656]), reads=[t_wb], writes=[t_wb], partial=True)
                P.barrier()
            NB = 2
            bufs = []
            for i in range(NB):
                bufs.append((self.sb(st, f"xt{i}", [128, D]), Tok(), self.sb(st, f"sq{i}", [128, D], BF16), Tok(),
                             self.sb(st, f"ss{i}", [128, 4]), Tok(), self.sb(st, f"xn{i}", [128, D], BF16), Tok()))
            hT = [self.sb(st, f"hT{i}", [128, 8, 256], BF16) for i in range(2)]
            t_hT = [Tok(), Tok()]
            stage = [self.sb(st, f"stg{i}", [128, 16, 256]) for i in range(2)]
            t_stage = [Tok(), Tok()]
            svo = [self.sb(st, f"svo{i}", [128, 512]) for i in range(2)]
            t_svo = [Tok(), Tok()]
            chunks = [(0, 128), (128, 128), (256, 128), (384, 128), (512, 128), (640, 32), (672, 128), (800, 128), (1440, 16)]
            chunks += [(HY_LO + 128 * i, 128) for i in range(6)] + [(N_IN, 32)]
            groups = [(0, 3), (3, 2), (5, 1), (6, 2), (8, 1), (9, 6), (15, 1)]
            nsub = 0
            for ti in range(TT // 256):
                t0 = ti * 256
                v = 1 if ti == 0 else 0
                hs = ti % 2
                for sub in range(2):
                    self.norm_transpose(bufs[nsub % NB], self.XS[t0 + sub * 128:t0 + (sub + 1) * 128, :], self.t_XS, self.s1,
                                        self.modT[:, 0:8, :], v, hT[hs], t_hT[hs], sub * 128, ps_i=nsub % 2)
                    nsub += 1
                sg = stage[hs]
                for ci, (c0, M) in enumerate(chunks):
                    pi = 2 + ci % 4
                    for k in range(8):
                        P.op("pe", lambda e, pi=pi, k=k, c0=c0, M=M, hs=hs: e.matmul(self.ps[pi][0:M, 0:256], lhsT=wb[:, k, c0:c0 + M], rhs=hT[hs][:, k, :],
                                                                               start=(k == 0), stop=(k == 7)),
                             reads=[t_wb, t_hT[hs]], writes=[self.pst[pi]], skip_self=True)
                    if ci % 2 == 0:
                        P.op("act", lambda e, pi=pi, M=M, ci=ci: e.copy(out=sg[0:M, ci, :], in_=self.ps[pi][0:M, 0:256]), reads=[self.pst[pi]], writes=[t_stage[hs]], partial=True)
                    else:
                        P.op("dve", lambda e, pi=pi, M=M, ci=ci: e.tensor_copy(out=sg[0:M, ci, :], in_=self.ps[pi][0:M, 0:256]), reads=[self.pst[pi]], writes=[t_stage[hs]], partial=True)
                for (g0, gn) in groups:
                    c0, M = chunks[g0]
                    dst = self.PT[c0:c0 + M * gn, t0:t0 + 256]
                    if gn > 1:
                        dst = dst.rearrange("(c p) t -> p c t", p=128)
                        P.dma("pool", dst, sg[:, g0:g0 + gn, :], reads=[t_stage[hs]], writes=[self.t_PT], partial=True)
                    else:
                        P.dma("pool", dst, sg[0:M, g0, :], reads=[t_stage[hs]], writes=[self.t_PT], partial=True)
                for sub in range(2):
                    pi = 6 + sub
                    for k in range(8):
                        P.op("pe", lambda e, pi=pi, k=k, sub=sub, hs=hs: e.matmul(self.ps[pi], lhsT=hT[hs][:, k, sub * 128:(sub + 1) * 128], rhs=wb[:, k, 928:1440],
                                                                               start=(k == 0), stop=(k == 7)),
                             reads=[t_wb, t_hT[hs]], writes=[self.pst[pi]], skip_self=True)
                    P.op("act" if sub == 0 else "dve", (lambda e, pi=pi, sub=sub: e.copy(out=svo[sub], in_=self.ps[pi])) if sub == 0 else
                         (lambda e, pi=pi, sub=sub: e.tensor_copy(out=svo[sub], in_=self.ps[pi])), reads=[self.pst[pi]], writes=[t_svo[sub]])
                    P.dma("pool", self.PVO[t0 + sub * 128:t0 + (sub + 1) * 128, :], svo[sub], reads=[t_svo[sub]], writes=[self.t_PVO], partial=True)
            P.barrier()

    def rms_bcast(self, src, nk, n, ones, t_ones, nfeat, ps_i, R, t_R, sq, t_sq, t_src):
        P = self.P
        P.op("act", lambda e: e.activation(out=sq[:, 0:nk, 0:n], in_=src[:, 0:nk, 0:n], func=AF.Square), reads=[t_src], writes=[t_sq])
        ps, t_ps = self.ps[ps_i], self.pst[ps_i]
        for k in range(nk):
            P.op("pe", lambda e, k=k: e.matmul(ps[:, 0:n], lhsT=ones, rhs=sq[:, k, 0:n], start=(k == 0), stop=(k == nk - 1)),
                 reads=[t_ones, t_sq], writes=[t_ps], skip_self=True)
        P.op("dve", lambda e: e.tensor_scalar(out=R[:, 0:n], in0=ps[:, 0:n], scalar1=1.0 / nfeat, scalar2=EPS, op0=ALU.mult, op1=ALU.add),
             reads=[t_ps], writes=[t_R])
        P.op("act", lambda e: e.activation(out=R[:, 0:n], in_=R[:, 0:n], func=AF.Sqrt), reads=[t_R], writes=[t_R])
        P.op("dve", lambda e: e.reciprocal(out=R[:, 0:n], in_=R[:, 0:n]), reads=[t_R], writes=[t_R])

    def phase_mla(self, li):
        nc, P = self.nc, self.P
        last = li == DEPTH - 1
        scale = float(96 ** -0.5)
        with contextlib.ExitStack() as st:
            sb = lambda name, shape, dt=F32: self.sb(st, name, shape, dt)
            ones = sb("onesb", [128, 128], BF16); t_ones = Tok()
            P.op("pool", lambda e: e.memset(ones, 1.0), writes=[t_ones])
            KT = sb("KT", [128, NH, TT], BF16); t_KT = Tok()
            VP = sb("VP", [128, TT // 128, NH, 65], BF16); t_VP = Tok()
            P.op("pool", lambda e: e.memset(VP, 1.0), writes=[t_VP])
            sel65 = sb("sel65", [128, 64]); t_sel = Tok()
            P.op("pool", lambda e: e.memset(sel65, 0.0), writes=[t_sel])
            P.op("pool", lambda e: e.memset(sel65[64:65, :], 1.0), reads=[t_sel], writes=[t_sel])
            wqb = sb("wqb", [128, 3, NH, 192], BF16); t_wq = Tok()
            wkb = sb("wkb", [128, 2, NH, 64], BF16); t_wk = Tok()
            wvb = sb("wvb", [128, 2, NH, 64], BF16); t_wv = Tok()
            mk = sb("mk", [128, NH]); t_mk = Tok()
            P.op("pool", lambda e: e.memset(mk, 0.0), writes=[t_mk])
            with contextlib.ExitStack() as st2:
                wq = self.sb(st2, "wq32", [128, 3, NH * 96]); t_wq32 = Tok()
                wkv = self.sb(st2, "wkv32", [128, 2, NH * 128]); t_wkv32 = Tok()
                gq, t_gq = self.vec_pc(st2, "gq", self.mla_q_norm_g[li], 3)
                gkv, t_gkv = self.vec_pc(st2, "gkv", self.mla_kv_norm_g[li], 2)
                P.dma("sp", wq, self.mla_w_uq[li].rearrange("(c p) n -> p c n", p=128), writes=[t_wq32])
                P.dma("act", wkv, self.mla_w_ukv[li].rearrange("(c p) n -> p c n", p=128), writes=[t_wkv32])
                for k in range(3):
                    P.op("dve", lambda e, k=k: e.tensor_scalar(out=wq[:, k, :], in0=wq[:, k, :], scalar1=gq[:, k:k + 1], scalar2=None, op0=ALU.mult),
                         reads=[t_gq], writes=[t_wq32])
                for k in range(2):
                    P.op("dve", lambda e, k=k: e.tensor_scalar(out=wkv[:, k, :], in0=wkv[:, k, :], scalar1=gkv[:, k:k + 1], scalar2=None, op0=ALU.mult),
                         reads=[t_gkv], writes=[t_wkv32])
                wq4 = wq.rearrange("p k (h d) -> p k h d", d=96)
                wkv4 = wkv.rearrange("p k (h d) -> p k h d", d=128)
                P.op("pool", lambda e: e.memset(wqb, 0.0), writes=[t_wq])
                for k in range(3):
                    P.op("dve", lambda e, k=k: e.tensor_copy(out=wqb[:, k, :, 0:96], in_=wq4[:, k, :, 0:96]), reads=[t_wq32], writes=[t_wq], partial=True)
                    P.op("dve", lambda e, k=k: e.tensor_scalar(out=wqb[:, k, :, 160:176], in0=wq4[:, k, :, 80:96], scalar1=-1.0, scalar2=None, op0=ALU.mult),
                         reads=[t_wq32], writes=[t_wq], partial=True)
                    P.op("dve", lambda e, k=k: e.tensor_copy(out=wqb[:, k, :, 176:192], in_=wq4[:, k, :, 64:80]), reads=[t_wq32], writes=[t_wq], partial=True)
                for k in range(2):
                    P.op("dve", lambda e, k=k: e.tensor_copy(out=wkb[:, k, :, :], in_=wkv4[:, k, :, 0:64]), reads=[t_wkv32], writes=[t_wk], partial=True)
                    P.op("dve", lambda e, k=k: e.tensor_copy(out=wvb[:, k, :, :], in_=wkv4[:, k, :, 64:128]), reads=[t_wkv32], writes=[t_wv], partial=True)
                P.barrier()
            NBUF = 2
            pin = [sb(f"pin{i}", [128, 3, 512]) for i in range(NBUF)]; t_pin = [Tok() for _ in range(NBUF)]
            cs = [sb(f"cs{i}", [128, 2, 512]) for i in range(NBUF)]; t_cs = [Tok() for _ in range(NBUF)]
            sq = sb("sqm", [128, 3, 512], BF16); t_sq = Tok()
            R = sb("Rm", [128, 512]); t_R = Tok()
            pn = sb("pn", [128, 3, 512], BF16); t_pn = Tok()
            tmpa = sb("tmpa", [128, 512]); t_tmpa = Tok()
            tmpb = sb("tmpb", [128, 512]); t_tmpb = Tok()
            sqk = sb("sqk", [128, NH, 512], BF16); t_sqk = Tok()
            stA = contextlib.ExitStack()
            krin = [self.sb(stA, f"krin{i}", [128, 2, 512]) for i in range(NBUF)]; t_krin = [Tok() for _ in range(NBUF)]
            krb = self.sb(stA, "krb", [128, 512], BF16); t_krb = Tok()
            mtmp = self.sb(stA, "mtmp", [128, NH]); t_mtmp = Tok()
            tchunks = [(0, CTX)] + [(CTX + 512 * j, 512) for j in range(SEQ // 512)]

            def load_rope_tables(bi, t0, n):
                p0 = t0 - CTX
                P.dma("act", cs[bi][64:96, 0, 0:n], self.c_rope_cos[:, p0:p0 + n], writes=[t_cs[bi]], partial=True)
                P.dma("act", cs[bi][64:96, 1, 0:n], self.c_rope_sin[:, p0:p0 + n], writes=[t_cs[bi]], partial=True)

            for ci, (t0, n) in enumerate(tchunks):
                bi = ci % NBUF
                is_ctx = ci == 0
                P.dma("sp", pin[bi][:, 0:2, 0:n], self.PT[QR:QR + KVR, t0:t0 + n].rearrange("(c p) t -> p c t", p=128), reads=[self.t_PT], writes=[t_pin[bi]])
                P.dma("sp", krin[bi][64:96, 0, 0:n], self.PT[640:672, t0:t0 + n], reads=[self.t_PT], writes=[t_krin[bi]], partial=True)
                if not is_ctx:
                    P.dma("sp", krin[bi][64:96, 1, 0:n], self.PT[N_IN:N_IN + 32, t0:t0 + n], reads=[self.t_PT], writes=[t_krin[bi]], partial=True)
                    load_rope_tables(bi, t0, n)
                self.rms_bcast(pin[bi], 2, n, ones, t_ones, KVR, 6, R, t_R, sq, t_sq, t_pin[bi])
                P.op("dve", lambda e, bi=bi, n=n: e.tensor_tensor(out=pn[:, 0:2, 0:n], in0=pin[bi][:, 0:2, 0:n], in1=R[:, 0:n].unsqueeze(1).to_broadcast([128, 2, n]), op=ALU.mult),
                     reads=[t_pin[bi], t_R], writes=[t_pn])
                if is_ctx:
                    P.op("dve", lambda e, bi=bi, n=n: e.tensor_copy(out=krb[64:96, 0:n], in_=krin[bi][64:96, 0, 0:n]), reads=[t_krin[bi]], writes=[t_krb])
                else:
                    P.op("dve", lambda e, bi=bi, n=n: e.tensor_tensor(out=tmpa[64:96, 0:n], in0=krin[bi][64:96, 0, 0:n], in1=cs[bi][64:96, 0, 0:n], op=ALU.mult), reads=[t_krin[bi], t_cs[bi]], writes=[t_tmpa])
                    P.op("dve", lambda e, bi=bi, n=n: e.tensor_tensor(out=tmpb[64:96, 0:n], in0=krin[bi][64:96, 1, 0:n], in1=cs[bi][64:96, 1, 0:n], op=ALU.mult), reads=[t_krin[bi], t_cs[bi]], writes=[t_tmpb])
                    P.op("dve", lambda e, n=n: e.tensor_tensor(out=krb[64:96, 0:n], in0=tmpa[64:96, 0:n], in1=tmpb[64:96, 0:n], op=ALU.add), reads=[t_tmpa, t_tmpb], writes=[t_krb])
                P.op("pool", lambda e, t0=t0, n=n: e.tensor_copy(out=KT[64:96, :, t0:t0 + n], in_=krb[64:96, 0:n].unsqueeze(1).to_broadcast([32, NH, n])), reads=[t_krb], writes=[t_KT], partial=True)
                for h in range(NH):
                    pi = 4 + h % 2
                    for k in range(2):
                        P.op("pe", lambda e, h=h, k=k, pi=pi, n=n: e.matmul(self.ps[pi][0:64, 0:n], lhsT=wkb[:, k, h, :], rhs=pn[:, k, 0:n], start=(k == 0), stop=(k == 1)),
                             reads=[t_wk, t_pn], writes=[self.pst[pi]], skip_self=True)
                    if h % 2 == 0:
                        P.op("act", lambda e, h=h, pi=pi, t0=t0, n=n: e.copy(out=KT[0:64, h, t0:t0 + n], in_=self.ps[pi][0:64, 0:n]), reads=[self.pst[pi]], writes=[t_KT], partial=True)
                    else:
                        P.op("dve", lambda e, h=h, pi=pi, t0=t0, n=n: e.tensor_copy(out=KT[0:64, h, t0:t0 + n], in_=self.ps[pi][0:64, 0:n]), reads=[self.pst[pi]], writes=[t_KT], partial=True)
                for sub in range(n // 128):
                    j = (t0 + sub * 128) // 128
                    pi = 2 + sub % 2
                    for k in range(2):
                        P.op("pe", lambda e, k=k, pi=pi, sub=sub: e.matmul(self.ps[pi], lhsT=pn[:, k, sub * 128:(sub + 1) * 128], rhs=wvb[:, k, :, :].rearrange("p h d -> p (h d)"), start=(k == 0), stop=(k == 1)),
                             reads=[t_wv, t_pn], writes=[self.pst[pi]], skip_self=True)
                    src = self.ps[pi].rearrange("p (h d) -> p h d", d=64)
                    if sub % 2 == 0:
                        P.op("act", lambda e, j=j, src=src: e.copy(out=VP[:, j, :, 0:64], in_=src), reads=[self.pst[pi]], writes=[t_VP], partial=True)
                    else:
                        P.op("dve", lambda e, j=j, src=src: e.tensor_copy(out=VP[:, j, :, 0:64], in_=src), reads=[self.pst[pi]], writes=[t_VP], partial=True)
                P.op("act", lambda e, t0=t0, n=n: e.activation(out=sqk[0:96, :, 0:n], in_=KT[0:96, :, t0:t0 + n], func=AF.Square), reads=[t_KT], writes=[t_sqk])
                for h in range(NH):
                    pi = 7
                    P.op("pe", lambda e, h=h, n=n: e.matmul(self.ps[7][:, 0:n], lhsT=ones[0:96, :], rhs=sqk[0:96, h, 0:n], start=True, stop=True),
                         reads=[t_ones, t_sqk], writes=[self.pst[7]], skip_self=True)
                    P.op("dve", lambda e, h=h, n=n: e.tensor_reduce(out=mtmp[:, h:h + 1], in_=self.ps[7][:, 0:n], axis=AX.X, op=ALU.max), reads=[self.pst[7]], writes=[t_mtmp], partial=True)
                P.op("dve", lambda e: e.tensor_tensor(out=mk, in0=mk, in1=mtmp, op=ALU.max), reads=[t_mtmp], writes=[t_mk])
            P.barrier()
            stA.close()
            QT = [sb(f"QT{i}", [128, NH, 512], BF16) for i in range(2)]; t_QT = [Tok(), Tok()]
            pts = [sb(f"pts{i}", [128, 2, 512], BF16) for i in range(3)]; t_pts = [Tok() for _ in range(3)]
            den = sb("den", [128, 512]); t_den = Tok()
            rden = sb("rden", [128, 512]); t_rden = Tok()
            ot = [sb(f"ot{i}", [128, 512], BF16) for i in range(2)]; t_ot = [Tok(), Tok()]
            mq = [sb(f"mq{i}", [128, NH]) for i in range(2)]; t_mq = [Tok(), Tok()]
            negm = [sb(f"negm{i}", [128, NH]) for i in range(2)]; t_negm = [Tok(), Tok()]
            qchunks = ([] if last else [(0, CTX)]) + tchunks[1:]
            nq = len(qchunks)

            def prologue_parts(qi):
                t0, n = qchunks[qi]
                bi = qi % NBUF
                is_ctx = t0 == 0
                qt, t_qt = QT[qi % 2], t_QT[qi % 2]
                parts = {}

                def part_load():
                    P.dma("sp", pin[bi][:, 0:3, 0:n], self.PT[0:QR, t0:t0 + n].rearrange("(c p) t -> p c t", p=128), reads=[self.t_PT], writes=[t_pin[bi]])
                    if not is_ctx:
                        load_rope_tables(bi, t0, n)
                    self.rms_bcast(pin[bi], 3, n, ones, t_ones, QR, 5, R, t_R, sq, t_sq, t_pin[bi])
                    P.op("dve", lambda e: e.tensor_tensor(out=pn[:, 0:3, 0:n], in0=pin[bi][:, 0:3, 0:n], in1=R[:, 0:n].unsqueeze(1).to_broadcast([128, 3, n]), op=ALU.mult),
                         reads=[t_pin[bi], t_R], writes=[t_pn])
                parts["load"] = part_load

                def part_A(h):
                    for k in range(3):
                        P.op("pe", lambda e, k=k: e.matmul(self.ps[7][0:96, 0:n], lhsT=wqb[:, k, h, 0:96], rhs=pn[:, k, 0:n], start=(k == 0), stop=(k == 2)),
                             reads=[t_wq, t_pn], writes=[self.pst[7]], skip_self=True)
                    if is_ctx:
                        P.op("dve", lambda e: e.tensor_copy(out=qt[0:96, h, 0:n], in_=self.ps[7][0:96, 0:n]), reads=[self.pst[7]], writes=[t_qt], partial=True)
                        return
                    P.op("dve", lambda e: e.tensor_tensor(out=tmpa[64:96, 0:n], in0=self.ps[7][64:96, 0:n], in1=cs[bi][64:96, 0, 0:n], op=ALU.mult), reads=[self.pst[7], t_cs[bi]], writes=[t_tmpa])
                    P.op("dve", lambda e: e.tensor_copy(out=qt[0:64, h, 0:n], in_=self.ps[7][0:64, 0:n]), reads=[self.pst[7]], writes=[t_qt], partial=True)

                def part_B(h):
                    if is_ctx:
                        return
                    for k in range(3):
                        P.op("pe", lambda e, k=k: e.matmul(self.ps[6][0:96, 0:n], lhsT=wqb[:, k, h, 96:192], rhs=pn[:, k, 0:n], start=(k == 0), stop=(k == 2)),
                             reads=[t_wq, t_pn], writes=[self.pst[6]], skip_self=True)
                    P.op("dve", lambda e: e.tensor_tensor(out=tmpb[64:96, 0:n], in0=self.ps[6][64:96, 0:n], in1=cs[bi][64:96, 1, 0:n], op=ALU.mult), reads=[self.pst[6], t_cs[bi]], writes=[t_tmpb])
                    P.op("dve", lambda e: e.tensor_tensor(out=qt[64:96, h, 0:n], in0=tmpa[64:96, 0:n], in1=tmpb[64:96, 0:n], op=ALU.add), reads=[t_tmpa, t_tmpb], writes=[t_qt], partial=True)

                def part_N(h):
                    P.op("dve", lambda e: e.tensor_tensor(out=sqk[0:96, h, 0:n], in0=qt[0:96, h, 0:n], in1=qt[0:96, h, 0:n], op=ALU.mult), reads=[t_qt], writes=[t_sqk], partial=True)
                    P.op("pe", lambda e: e.matmul(self.ps[7][:, 0:n], lhsT=ones[0:96, :], rhs=sqk[0:96, h, 0:n], start=True, stop=True),
                         reads=[t_ones, t_sqk], writes=[self.pst[7]], skip_self=True)
                    P.op("dve", lambda e: e.tensor_reduce(out=mq[qi % 2][:, h:h + 1], in_=self.ps[7][:, 0:n], axis=AX.X, op=ALU.max), reads=[self.pst[7]], writes=[t_mq[qi % 2]], partial=True)

                def part_fin():
                    nm, t_nm = negm[qi % 2], t_negm[qi % 2]
                    P.op("dve", lambda e: e.tensor_tensor(out=nm, in0=mq[qi % 2], in1=mk, op=ALU.mult), reads=[t_mq[qi % 2], t_mk], writes=[t_nm])
                    P.op("act", lambda e: e.activation(out=nm, in_=nm, func=AF.Sqrt), reads=[t_nm], writes=[t_nm])
                    P.op("dve", lambda e: e.tensor_scalar(out=nm, in0=nm, scalar1=-scale, scalar2=None, op0=ALU.mult), reads=[t_nm], writes=[t_nm])
                for h in range(NH):
                    parts[("A", h)] = (lambda h=h: part_A(h))
                    parts[("B", h)] = (lambda h=h: part_B(h))
                    parts[("N", h)] = (lambda h=h: part_N(h))
                parts["fin"] = part_fin
                return parts

            def emit_all(parts):
                parts["load"]()
                for h in range(NH):
                    parts[("A", h)](); parts[("B", h)](); parts[("N", h)]()
                parts["fin"]()

            def groups_of(qi):
                t0, n = qchunks[qi]
                ktiles = list(range(CTX // 128)) if t0 == 0 else list(range(TT // 128))
                return [ktiles[i:i + 2] for i in range(0, len(ktiles), 2)]

            def s_mm(qi, h, g):
                t0, n = qchunks[qi]
                qt, t_qt = QT[qi % 2], t_QT[qi % 2]
                b0 = 2 * (g % 2)
                for i, j in enumerate(groups_of(qi)[g]):
                    P.op("pe", lambda e, j=j, i=i: e.matmul(self.ps[b0 + i][:, 0:n], lhsT=KT[0:96, h, j * 128:(j + 1) * 128], rhs=qt[0:96, h, 0:n], start=True, stop=True),
                         reads=[t_KT, t_qt], writes=[self.pst[b0 + i]], skip_self=True)

            emit_all(prologue_parts(0))
            jobs = [(qi, h) for qi in range(nq) for h in range(NH)]
            pcount = 0
            pre_issued = set()
            nxt_parts = None
            for ji, (qi, h) in enumerate(jobs):
                t0, n = qchunks[qi]
                groups = groups_of(qi)
                ng = len(groups)
                po = 4
                nm, t_nm = negm[qi % 2], t_negm[qi % 2]
                if h == 0:
                    nxt_parts = prologue_parts(qi + 1) if qi + 1 < nq else None
                sched = {}
                if nxt_parts is not None and ng >= 16:
                    if h == 0:
                        sched[2] = ["load"]
                    else:
                        sched[3] = [("A", h - 1)]
                        sched[7] = [("B", h - 1)]
                        sched[11] = [("N", h - 1)]
                    if h == NH - 1:
                        sched[12] = [("A", h)]
                        sched[14] = [("B", h)]
                        sched[16] = [("N", h), "fin"]
                if (qi, h) not in pre_issued:
                    s_mm(qi, h, 0)
                    if ng > 1:
                        s_mm(qi, h, 1)
                for g in range(ng):
                    b0 = 2 * (g % 2)
                    nj = len(groups[g])
                    pb = pcount % 3
                    pcount += 1
                    src = self.psall[:, b0 * 512:(b0 + nj) * 512].rearrange("p (a c) -> p a c", c=512)[:, :, 0:n]
                    P.op("act", lambda e, pb=pb, nj=nj, src=src: e.activation(out=pts[pb][:, 0:nj, 0:n], in_=src, func=AF.Exp, bias=nm[:, h:h + 1], scale=scale),
                         reads=[self.pst[b0 + i] for i in range(nj)] + [t_nm], writes=[t_pts[pb]])
                    for i, j in enumerate(groups[g]):
                        first = (g == 0 and i == 0)
                        lastk = (g == ng - 1 and i == nj - 1)
                        P.op("pe", lambda e, pb=pb, j=j, i=i, first=first, lastk=lastk: e.matmul(self.ps[po][0:65, 0:n], lhsT=VP[:, j, h, :], rhs=pts[pb][:, i, 0:n], start=first, stop=lastk),
                             reads=[t_VP, t_pts[pb]], writes=[self.pst[po]], skip_self=True)
                    if g + 2 < ng:
                        s_mm(qi, h, g + 2)
                    for key in sched.get(g, []):
                        nxt_parts[key]()
                if ji + 1 < len(jobs):
                    qn, hn = jobs[ji + 1]
                    if qn == qi:
                        s_mm(qn, hn, 0)
                        if len(groups_of(qn)) > 1:
                            s_mm(qn, hn, 1)
                        pre_issued.add((qn, hn))
                o = ot[h % 2]
                t_o = t_ot[h % 2]
                P.op("act", lambda e: e.copy(out=den[0:65, 0:n], in_=self.ps[po][0:65, 0:n]), reads=[self.pst[po]], writes=[t_den])
                P.op("pe", lambda e: e.matmul(self.ps[5][0:64, 0:n], lhsT=sel65[0:65, :], rhs=den[0:65, 0:n], start=True, stop=True),
                     reads=[t_sel, t_den], writes=[self.pst[5]], skip_self=True)
                P.op("dve", lambda e: e.reciprocal(out=rden[0:64, 0:n], in_=self.ps[5][0:64, 0:n]), reads=[self.pst[5]], writes=[t_rden])
                P.op("dve", lambda e, o=o: e.tensor_tensor(out=o[0:64, 0:n], in0=den[0:64, 0:n], in1=rden[0:64, 0:n], op=ALU.mult),
                     reads=[t_den, t_rden], writes=[t_o])
                P.dma("pool", self.MIXT[h * 64:(h + 1) * 64, t0:t0 + n], o[0:64, 0:n], reads=[t_o], writes=[self.t_MIXT], partial=True)
                if nxt_parts is not None and ng < 16 and h == NH - 1:
                    emit_all(nxt_parts)
            P.barrier()

    def phase_mlstm(self, li):
        nc, P = self.nc, self.P
        NT = TT // 128
        with contextlib.ExitStack() as st:
            sb = lambda name, shape, dt=F32: self.sb(st, name, shape, dt)
            ub = [sb(f"ub{p}", [128, TT], BF16) for p in range(2)]; t_ub = [Tok(), Tok()]
            kT = [sb(f"kT{p}", [128, TT], BF16) for p in range(2)]; t_kT = [Tok(), Tok()]
            qd = [[sb(f"qd{p}{d}", [128, TT], BF16) for d in range(2)] for p in range(2)]
            t_qd = [[Tok(), Tok()], [Tok(), Tok()]]
            ktok = sb("ktok", [128, NT, 256], BF16); t_ktok = Tok()
            wtok = sb("wtok", [128, NT, 8]); t_wtok = Tok()
            ecol = sb("ecol", [128, 4, NT]); t_ecol = Tok()
            hF = sb("hF", [128, NT, 256]); t_hF = Tok()
            CbS = [[sb(f"CbS{p}{d}", [128, NT + 1, 65], BF16) for d in range(2)] for p in range(2)]
            t_CbS = [[Tok(), Tok()], [Tok(), Tok()]]
            Cst = [[sb(f"Cst{p}{d}", [128, 65]) for d in range(2)] for p in range(2)]
            t_Cst = [[Tok(), Tok()], [Tok(), Tok()]]
            masks = sb("mlmask", [128, 2, 128], BF16); t_masks = Tok()
            P.dma("sp", masks, self.c_ml_mask, writes=[t_masks])
            wqb = sb("mlwq", [128, 2, 128], BF16); wkb = sb("mlwk", [128, 2, 128], BF16); t_w = Tok()
            gml, t_gml = self.vec_pc(st, "gml", self.ml_norm_g[li], 2)
            cw = sb("mlcw", [128, 2, 3]); cb = sb("mlcb", [128, 2]); t_cw = Tok()
            for jj in range(3):
                P.dma("sp", cw[:, :, jj], self.ml_conv_w[li, jj].rearrange("(c p) -> p c", p=128), writes=[t_cw], partial=True, allow_slow_non_contiguous=True)
            P.dma("sp", cb, self.ml_conv_b[li].rearrange("(c p) -> p c", p=128), writes=[t_cw], partial=True, allow_slow_non_contiguous=True)
            for p in range(2):
                for d in range(2):
                    P.op("pool", lambda e, p=p, d=d: e.memset(Cst[p][d], 0.0), writes=[t_Cst[p][d]])
                    P.op("pool", lambda e, p=p, d=d: e.memset(CbS[p][d][:, 0, :], 0.0), writes=[t_CbS[p][d]])
            with contextlib.ExitStack() as st2:
                sb2 = lambda name, shape, dt=F32: self.sb(st2, name, shape, dt)
                w32 = sb2("mlw32", [128, 4, 128]); t_w32 = Tok()
                P.op("pool", lambda e: e.memset(w32, 0.0), writes=[t_w32])
                for p in range(2):
                    for hh in range(2):
                        P.dma("sp", w32[hh * 64:(hh + 1) * 64, p, hh * 64:(hh + 1) * 64], self.ml_wq[li, 2 * p + hh], reads=[], writes=[t_w32], partial=True)
                        P.dma("sp", w32[hh * 64:(hh + 1) * 64, 2 + p, hh * 64:(hh + 1) * 64], self.ml_wk[li, 2 * p + hh], reads=[], writes=[t_w32], partial=True)
                P.op("dve", lambda e: e.tensor_copy(out=wqb, in_=w32[:, 0:2, :]), reads=[t_w32], writes=[t_w], partial=True)
                P.op("dve", lambda e: e.tensor_scalar(out=wkb, in0=w32[:, 2:4, :], scalar1=0.125, scalar2=None, op0=ALU.mult), reads=[t_w32], writes=[t_w], partial=True)
                selA = sb2("selA", [128, 4, 128]); selW = sb2("selW", [128, 8]); t_sel = Tok()
                P.dma("act", selA[0:96], self.c_ml_selA, writes=[t_sel], partial=True)
                P.dma("act", selW[0:96], self.c_ml_selW, writes=[t_sel], partial=True)
                gb = sb2("mlgb", [16, 1]); t_gb = Tok()
                P.dma("sp", gb, self.ml_gate_b[li].rearrange("(p o) -> p o", o=1), writes=[t_gb], allow_slow_non_contiguous=True)
                ones16 = sb2("ones16", [16, 128]); t_o16 = Tok()
                P.op("pool", lambda e: e.memset(ones16, 1.0), writes=[t_o16])
                X = sb2("mlX", [128, TT]); t_X = Tok()
                P.op("pool", lambda e: e.memset(X, 0.0), writes=[t_X])
                P.dma("sp", X[0:16, :], self.PT[1440:1456, :], reads=[self.t_PT], writes=[t_X], partial=True)
                P.op("dve", lambda e: e.tensor_scalar(out=X[0:16, :], in0=X[0:16, :], scalar1=gb[:, 0:1], scalar2=None, op0=ALU.add), reads=[t_gb, t_X], writes=[t_X])
                with contextlib.ExitStack() as st3:
                    LF = self.sb(st3, "mlLF", [16, TT]); t_LF = Tok()
                    CF = self.sb(st3, "mlCF", [16, TT]); t_CF = Tok()
                    P.op("act", lambda e: e.activation(out=LF, in_=X[0:16, :], func=AF.Exp, scale=-1.0), reads=[t_X], writes=[t_LF])
                    P.op("act", lambda e: e.activation(out=LF, in_=LF, func=AF.Ln, bias=1.0), reads=[t_LF], writes=[t_LF])
                    P.op("dve", lambda e: e.tensor_scalar(out=LF, in0=LF, scalar1=-1.0, scalar2=None, op0=ALU.mult), reads=[t_LF], writes=[t_LF])
                    for j in range(NT):
                        P.op("dve", lambda e, j=j: e.tensor_tensor_scan(out=CF[:, j * 128:(j + 1) * 128], data0=ones16, data1=LF[:, j * 128:(j + 1) * 128],
                                                                        initial=0.0, op0=ALU.mult, op1=ALU.add), reads=[t_LF, t_o16], writes=[t_CF], partial=True)
                    P.op("dve", lambda e: e.tensor_tensor(out=LF, in0=LF, in1=CF, op=ALU.subtract), reads=[t_CF], writes=[t_LF])
                    LF3 = LF.rearrange("p (j t) -> p j t", t=128)
                    CF3 = CF.rearrange("p (j t) -> p j t", t=128)
                    P.op("dve", lambda e: e.tensor_tensor(out=LF3, in0=LF3, in1=CF3[:, :, 127:128].to_broadcast([16, NT, 128]), op=ALU.add), reads=[t_CF], writes=[t_LF])
                    P.op("act", lambda e: e.copy(out=X[32:48, :], in_=CF), reads=[t_CF], writes=[t_X], partial=True)
                    P.op("act", lambda e: e.copy(out=X[64:80, :], in_=LF), reads=[t_LF], writes=[t_X], partial=True)
                    P.barrier()
                for j in range(NT):
                    P.op("pe", lambda e, j=j: e.matmul(self.ps[7][:, j * 8:(j + 1) * 8], lhsT=X[0:96, j * 128:(j + 1) * 128], rhs=selW[0:96, :], start=True, stop=True),
                         reads=[t_X, t_sel], writes=[self.pst[7]], skip_self=True)
                P.op("act", lambda e: e.activation(out=wtok.rearrange("p j g -> p (j g)"), in_=self.ps[7][:, 0:NT * 8], func=AF.Exp), reads=[self.pst[7]], writes=[t_wtok])
                pc32 = sb2("mlpc", [128, TT]); t_pc = Tok()
                u32 = sb2("mlu", [128, TT]); t_u = Tok()
                abc = [sb2(f"abc{i}", [128, 512]) for i in range(2)]; t_abc = [Tok(), Tok()]
                blocks = [(0, 256)] + [(256 + 512 * i, 512) for i in range(8)]
                segs = [(0, CTX), (CTX, TT)]
                nabc = 0
                for p in range(2):
                    P.dma("sp", pc32, self.PT[ML_LO + p * 128:ML_LO + (p + 1) * 128, :], reads=[self.t_PT], writes=[t_pc])
                    P.op("dve", lambda e, p=p: e.tensor_scalar(out=u32, in0=pc32, scalar1=cw[:, p, 1:2], scalar2=cb[:, p:p + 1], op0=ALU.mult, op1=ALU.add), reads=[t_pc, t_cw], writes=[t_u])
                    for (a, b) in segs:
                        P.op("dve", lambda e, p=p, a=a, b=b: e.scalar_tensor_tensor(out=u32[:, a + 1:b], in0=pc32[:, a:b - 1], scalar=cw[:, p, 0:1], in1=u32[:, a + 1:b], op0=ALU.mult, op1=ALU.add),
                             reads=[t_pc, t_cw], writes=[t_u])
                        P.op("dve", lambda e, p=p, a=a, b=b: e.scalar_tensor_tensor(out=u32[:, a:b - 1], in0=pc32[:, a + 1:b], scalar=cw[:, p, 2:3], in1=u32[:, a:b - 1], op0=ALU.mult, op1=ALU.add),
                             reads=[t_pc, t_cw], writes=[t_u])
                    P.op("act", lambda e, p=p: e.activation(out=ub[p], in_=u32, func=AF.Silu), reads=[t_u], writes=[t_ub[p]])
                    for (b0, n) in blocks:
                        P.op("pe", lambda e, p=p, b0=b0, n=n: e.matmul(self.ps[0][:, 0:n], lhsT=wqb[:, p, :], rhs=ub[p][:, b0:b0 + n], start=True, stop=True),
                             reads=[t_w, t_ub[p]], writes=[self.pst[0]], skip_self=True)
                        for d in range(2):
                            ai = nabc % 2
                            nabc += 1
                            P.op("pe", lambda e, p=p, d=d, b0=b0, n=n: e.matmul(self.ps[6][:, 0:n], lhsT=selA[0:96, p * 2 + d, :], rhs=X[0:96, b0:b0 + n], start=True, stop=True),
                                 reads=[t_sel, t_X], writes=[self.pst[6]], skip_self=True)
                            P.op("act", lambda e, ai=ai, n=n: e.activation(out=abc[ai][:, 0:n], in_=self.ps[6][:, 0:n], func=AF.Exp), reads=[self.pst[6]], writes=[t_abc[ai]])
                            P.op("dve", lambda e, p=p, d=d, ai=ai, b0=b0, n=n: e.tensor_tensor(out=qd[p][d][:, b0:b0 + n], in0=self.ps[0][:, 0:n], in1=abc[ai][:, 0:n], op=ALU.mult),
                                 reads=[self.pst[0], t_abc[ai]], writes=[t_qd[p][d]], partial=True)
                            c0 = 127 if d == 0 else 0
                            P.op("pool", lambda e, p=p, d=d, ai=ai, b0=b0, n=n, c0=c0: e.tensor_copy(out=ecol[:, p * 2 + d, b0 // 128:(b0 + n) // 128], in_=abc[ai][:, c0:n:128]),
                                 reads=[t_abc[ai]], writes=[t_ecol], partial=True)
                        P.op("pe", lambda e, p=p, b0=b0, n=n: e.matmul(self.ps[1][:, 0:n], lhsT=wkb[:, p, :], rhs=ub[p][:, b0:b0 + n], start=True, stop=True),
                             reads=[t_w, t_ub[p]], writes=[self.pst[1]], skip_self=True)
                        P.op("act", lambda e, p=p, b0=b0, n=n: e.copy(out=kT[p][:, b0:b0 + n], in_=self.ps[1][:, 0:n]), reads=[self.pst[1]], writes=[t_kT[p]], partial=True)
                    for j in range(NT):
                        pi = 2 + j % 2
                        P.op("pe", lambda e, p=p, j=j, pi=pi: e.matmul(self.ps[pi][:, 0:128], lhsT=ub[p][:, j * 128:(j + 1) * 128], rhs=wkb[:, p, :], start=True, stop=True),
                             reads=[t_w, t_ub[p]], writes=[self.pst[pi]], skip_self=True)
                        if j % 2 == 0:
                            P.op("act", lambda e, p=p, j=j, pi=pi: e.copy(out=ktok[:, j, p * 128:(p + 1) * 128], in_=self.ps[pi][:, 0:128]), reads=[self.pst[pi]], writes=[t_ktok], partial=True)
                        else:
                            P.op("dve", lambda e, p=p, j=j, pi=pi: e.tensor_copy(out=ktok[:, j, p * 128:(p + 1) * 128], in_=self.ps[pi][:, 0:128]), reads=[self.pst[pi]], writes=[t_ktok], partial=True)
                P.barrier()
            NB = 4
            hB = sb("hB", [128, NT, 256]); t_hB = Tok()
            vo = [sb(f"mlvo{i}", [128, 512]) for i in range(NB)]; t_vo = [Tok() for _ in range(NB)]
            vw = [sb(f"mlvw{i}", [128, 4, 65], BF16) for i in range(NB)]; t_vw = [Tok() for _ in range(NB)]
            Ssb = [sb(f"mlS{i}", [128, 4, 128], BF16) for i in range(NB)]; t_S = [Tok() for _ in range(NB)]
            tmpC = [sb(f"mltmpC{d}", [128, 65]) for d in range(2)]; t_tmpC = [Tok(), Tok()]
            dd = [sb(f"mldd{d}", [128, 4]) for d in range(2)]; t_dd = [Tok(), Tok()]
            orders = [list(range(NT)), [1, 0] + list(range(NT - 1, 1, -1))]
            step = 0
            for si in range(NT):
                for d in range(2):
                    j = orders[d][si]
                    bi = step % NB
                    step += 1
                    c0, c1 = j * 128, (j + 1) * 128
                    P.dma("sp", vo[bi][:, 0:256], self.PVO[c0:c1, 0:256], reads=[self.t_PVO], writes=[t_vo[bi]])
                    P.op("dve", lambda e, bi=bi, j=j, d=d: e.tensor_tensor(out=vw[bi][:, :, 0:64], in0=vo[bi][:, 0:256].rearrange("p (h c) -> p h c", c=64),
                                                                   in1=wtok[:, j, d * 4:(d + 1) * 4].unsqueeze(2).to_broadcast([128, 4, 64]), op=ALU.mult),
                         reads=[t_vo[bi], t_wtok], writes=[t_vw[bi]])
                    P.op("act", lambda e, bi=bi, j=j, d=d: e.copy(out=vw[bi][:, :, 64], in_=wtok[:, j, d * 4:(d + 1) * 4]), reads=[t_wtok], writes=[t_vw[bi]], partial=True)
                    for h in range(4):
                        p, r0 = h // 2, (h % 2) * 64
                        pS = 2 + h % 2
                        P.op("pe", lambda e, h=h, p=p, r0=r0, d=d, c0=c0, c1=c1, pS=pS: e.matmul(self.ps[pS][:, (h // 2) * 128:(h // 2 + 1) * 128], lhsT=kT[p][r0:r0 + 64, c0:c1], rhs=qd[p][d][r0:r0 + 64, c0:c1], start=True, stop=True),
                             reads=[t_kT[p], t_qd[p][d]], writes=[self.pst[pS]], skip_self=True)
                    for eo in range(2):
                        P.op("dve", lambda e, bi=bi, eo=eo, d=d: e.tensor_tensor(out=Ssb[bi][:, eo::2, :], in0=self.ps[2 + eo][:, 0:256].rearrange("p (h t) -> p h t", t=128),
                                                                         in1=masks[:, d, :].unsqueeze(1).to_broadcast([128, 2, 128]), op=ALU.mult),
                             reads=[self.pst[2 + eo], t_masks], writes=[t_S[bi]], partial=(eo > 0))
                    for h in range(4):
                        p, r0 = h // 2, (h % 2) * 64
                        pN = 4 + h % 2
                        cN = (h // 2) * 65
                        P.op("pe", lambda e, h=h, bi=bi, pN=pN, cN=cN: e.matmul(self.ps[pN][:, cN:cN + 65], lhsT=Ssb[bi][:, h, :], rhs=vw[bi][:, h, :], start=True, stop=False),
                             reads=[t_S[bi], t_vw[bi]], writes=[self.pst[pN]], skip_self=True)
                        P.op("pe", lambda e, h=h, p=p, r0=r0, d=d, si=si, c0=c0, c1=c1, pN=pN, cN=cN: e.matmul(self.ps[pN][:, cN:cN + 65], lhsT=qd[p][d][r0:r0 + 64, c0:c1], rhs=CbS[p][d][r0:r0 + 64, si, :], start=False, stop=True),
                             reads=[t_qd[p][d], t_CbS[p][d]], writes=[self.pst[pN]], skip_self=True)
                    for p in range(2):
                        pU = p + 6 * d
                        P.op("pe", lambda e, p=p, bi=bi, j=j, pU=pU: e.matmul(self.ps[pU][:, 0:130], lhsT=ktok[:, j, p * 128:(p + 1) * 128], rhs=vw[bi][:, 2 * p:2 * p + 2, :].rearrange("p a c -> p (a c)"), start=True, stop=True),
                             reads=[t_ktok, t_vw[bi]], writes=[self.pst[pU]], skip_self=True)
                        P.op("dve", lambda e, p=p, d=d, pU=pU: e.tensor_tensor(out=tmpC[d][0:64, :], in0=self.ps[pU][0:64, 0:65], in1=Cst[p][d][0:64, :], op=ALU.add), reads=[self.pst[pU], t_Cst[p][d]], writes=[t_tmpC[d]], partial=True)
                        P.op("dve", lambda e, p=p, d=d, pU=pU: e.tensor_tensor(out=tmpC[d][64:128, :], in0=self.ps[pU][64:128, 65:130], in1=Cst[p][d][64:128, :], op=ALU.add), reads=[self.pst[pU], t_Cst[p][d]], writes=[t_tmpC[d]], partial=True)
                        P.op("dve", lambda e, p=p, d=d, j=j: e.tensor_scalar(out=Cst[p][d], in0=tmpC[d], scalar1=ecol[:, p * 2 + d, j:j + 1], scalar2=None, op0=ALU.mult), reads=[t_tmpC[d], t_ecol], writes=[t_Cst[p][d]])
                        P.op("act", lambda e, p=p, d=d, si=si: e.copy(out=CbS[p][d][:, si + 1, :], in_=Cst[p][d]), reads=[t_Cst[p][d]], writes=[t_CbS[p][d]], partial=True)
                    for eo in range(2):
                        num = self.ps[4 + eo][:, 0:130].rearrange("p (h c) -> p h c", c=65)
                        dde = dd[d][:, 2 * eo:2 * eo + 2]
                        P.op("dve", lambda e, num=num, dde=dde: e.tensor_scalar(out=dde, in0=num[:, :, 64], scalar1=-1.0, scalar2=None, op0=ALU.mult), reads=[self.pst[4 + eo]], writes=[t_dd[d]], partial=(eo > 0))
                        P.op("dve", lambda e, num=num, dde=dde: e.scalar_tensor_tensor(out=dde, in0=num[:, :, 64], scalar=1.0, in1=dde, op0=ALU.max, op1=ALU.max), reads=[self.pst[4 + eo]], writes=[t_dd[d]], partial=True)
                        P.op("dve", lambda e, dde=dde: e.reciprocal(out=dde, in_=dde), reads=[t_dd[d]], writes=[t_dd[d]], partial=True)
                        dst = (hF if d == 0 else hB)[:, j, :].rearrange("p (h c) -> p h c", c=64)[:, eo::2, :]
                        P.op("dve", lambda e, num=num, dde=dde, dst=dst: e.tensor_tensor(out=dst, in0=num[:, :, 0:64], in1=dde.unsqueeze(2).to_broadcast([128, 2, 64]), op=ALU.mult),
                             reads=[self.pst[4 + eo], t_dd[d]], writes=[t_hF if d == 0 else t_hB], partial=True)
            hbs = [sb(f"mlhb{i}", [128, 256]) for i in range(2)]; t_hbs = [Tok(), Tok()]
            sgs = [sb(f"mlsg{i}", [128, 256]) for i in range(2)]; t_sgs = [Tok(), Tok()]
            hsq = sb("mlhsq", [128, 256]); t_hsq = Tok()
            ssn = sb("mlssn", [128, 8]); t_ssn = Tok()
            hn = [sb(f"mlhn{i}", [128, 256], BF16) for i in range(2)]; t_hn = [Tok(), Tok()]
            oT = [sb(f"mloT{i}", [128, 2, 128], BF16) for i in range(2)]; t_oT = [Tok(), Tok()]
            for j in range(NT):
                bi = j % 2
                c0, c1 = j * 128, (j + 1) * 128
                hb, t_hb, sg, t_sg = hbs[bi], t_hbs[bi], sgs[bi], t_sgs[bi]
                P.dma("sp", vo[bi][:, 256:512], self.PVO[c0:c1, 256:512], reads=[self.t_PVO], writes=[t_vo[bi]])
                P.op("pool", lambda e, j=j, hb=hb: e.tensor_tensor(out=hb, in0=hF[:, j, :], in1=hB[:, j, :], op=ALU.add), reads=[t_hF, t_hB], writes=[t_hb])
                P.op("act", lambda e, bi=bi, sg=sg: e.activation(out=sg, in_=vo[bi][:, 256:512], func=AF.Sigmoid), reads=[t_vo[bi]], writes=[t_sg])
                P.op("pool", lambda e, hb=hb, sg=sg: e.tensor_tensor(out=hb, in0=hb, in1=sg, op=ALU.mult), reads=[t_sg], writes=[t_hb])
                P.op("pool", lambda e, hb=hb: e.tensor_tensor(out=hsq, in0=hb, in1=hb, op=ALU.mult), reads=[t_hb], writes=[t_hsq])
                P.op("dve", lambda e: e.tensor_reduce(out=ssn[:, 0:4], in_=hsq.rearrange("p (h c) -> p h c", c=64), axis=AX.X, op=ALU.add), reads=[t_hsq], writes=[t_ssn])
                P.op("dve", lambda e: e.tensor_scalar(out=ssn[:, 0:4], in0=ssn[:, 0:4], scalar1=1.0 / 64, scalar2=EPS, op0=ALU.mult, op1=ALU.add), reads=[t_ssn], writes=[t_ssn])
                P.op("act", lambda e: e.activation(out=ssn[:, 0:4], in_=ssn[:, 0:4], func=AF.Sqrt), reads=[t_ssn], writes=[t_ssn])
                P.op("dve", lambda e: e.reciprocal(out=ssn[:, 4:8], in_=ssn[:, 0:4]), reads=[t_ssn], writes=[t_ssn])
                P.op("dve", lambda e, bi=bi, hb=hb: e.tensor_tensor(out=hn[bi].rearrange("p (h c) -> p h c", c=64), in0=hb.rearrange("p (h c) -> p h c", c=64), in1=ssn[:, 4:8].unsqueeze(2).to_broadcast([128, 4, 64]), op=ALU.mult),
                     reads=[t_hb, t_ssn], writes=[t_hn[bi]])
                pi = 6 + bi
                pT = self.ps[pi].bitcast(BF16)
                for p in range(2):
                    P.op("pe", lambda e, p=p, bi=bi, pT=pT: e.transpose(pT[:, p * 128:(p + 1) * 128], hn[bi][:, p * 128:(p + 1) * 128], self.identb), reads=[t_hn[bi], self.t_ident], writes=[self.pst[pi]], skip_self=True)
                for p in range(2):
                    P.op("act", lambda e, p=p, bi=bi, pT=pT: e.activation(out=oT[bi][:, p, :], in_=pT[:, p * 128:(p + 1) * 128], func=AF.Copy, scale=gml[:, p:p + 1]), reads=[self.pst[pi], t_gml], writes=[t_oT[bi]], partial=(p > 0))
                P.dma("pool", self.MIXT[512:768, c0:c1].rearrange("(c p) t -> p c t", p=128), oT[bi], reads=[t_oT[bi]], writes=[self.t_MIXT], partial=True)
            P.barrier()

    def phase_hyena(self, li):
        if li < DEPTH - 1:
            self.hyena_seq(li, 0, CTX, self.c_hy_z256, self.c_hy_win256, self.c_hy_C256, self.c_hy_S256, self.c_hy_wf256)
        self.hyena_seq(li, CTX, SEQ, self.c_hy_z4096, self.c_hy_win4096, self.c_hy_C4096, self.c_hy_S4096, self.c_hy_wf4096)

    def hyena_seq(self, li, tok0, L, c_z, c_win, c_C, c_S, c_wf):
        nc, P = self.nc, self.P
        NTL = L // 128
        NF = NTL + 1
        NB = (L + 511) // 512
        BW = min(L, 512)
        with contextlib.ExitStack() as st:
            sb = lambda name, shape, dt=F32: self.sb(st, name, shape, dt)
            x0T = sb("hyx0T", [128, 2, L], BF16); t_x0 = Tok()
            zT = sb("hyzT", [128, 2, L], BF16); t_zT = Tok()
            Z = sb("hyZ", [128, NTL, 256], BF16); t_Z = Tok()
            HS = sb("hyHS", [128, NTL, 256], BF16); t_HS = Tok()
            HD = sb("hyHD", [128, NTL, 256], BF16); t_HD = Tok()
            Asp = sb("hyA", [128, NF, 256], BF16); t_A = Tok()
            Bsp = sb("hyB", [128, NF, 256], BF16); t_B = Tok()
            rl1 = sb("hyrl1", [128, 256]); t_rl1 = Tok()
            wf = sb("hywf", [128, NF]); t_wf = Tok()
            P.dma("sp", wf, c_wf, writes=[t_wf])
            bd, t_bd = self.vec_pc(st, "hybd", self.hy_bias_d[li], 2)
            cw = sb("hycw", [128, 6, 3]); cb = sb("hycb", [128, 6]); t_cw = Tok()
            for jj in range(3):
                P.dma("sp", cw[:, :, jj], self.hy_conv_w[li, jj].rearrange("(c p) -> p c", p=128), writes=[t_cw], partial=True, allow_slow_non_contiguous=True)
            P.dma("sp", cb, self.hy_conv_b[li].rearrange("(c p) -> p c", p=128), writes=[t_cw], partial=True, allow_slow_non_contiguous=True)
            with contextlib.ExitStack() as st2:
                sb2 = lambda name, shape, dt=F32: self.sb(st2, name, shape, dt)
                zemb = sb2("hyzemb", [33, L]); t_zemb = Tok()
                P.dma("sp", zemb, c_z, writes=[t_zemb])
                w1 = sb2("hyw1", [33, 64]); w2 = sb2("hyw2", [64, 64]); w3 = sb2("hyw3", [64, 512]); t_wm = Tok()
                P.dma("act", w1, self.hy_w1[li], writes=[t_wm], partial=True)
                P.dma("act", w2, self.hy_w2[li], writes=[t_wm], partial=True)
                P.dma("act", w3, self.hy_w3[li], writes=[t_wm], partial=True)
                fr = sb2("hyfr", [64, 4]); t_fr = Tok()
                P.dma("sp", fr[:, 0:1], self.hy_sin_freq[li].rearrange("(p o) -> p o", o=1), writes=[t_fr], partial=True, allow_slow_non_contiguous=True)
                P.dma("sp", fr[:, 1:2], self.hy_b1[li].rearrange("(p o) -> p o", o=1), writes=[t_fr], partial=True, allow_slow_non_contiguous=True)
                P.dma("sp", fr[:, 2:3], self.hy_b2[li].rearrange("(p o) -> p o", o=1), writes=[t_fr], partial=True, allow_slow_non_contiguous=True)
                P.op("dve", lambda e: e.tensor_scalar(out=fr[:, 1:3], in0=fr[:, 1:3], scalar1=fr[:, 0:1], scalar2=None, op0=ALU.mult), reads=[t_fr], writes=[t_fr])
                h1 = sb2("hyh1", [64, L]); t_h1 = Tok()
                h2 = sb2("hyh2", [64, L]); t_h2 = Tok()
                tt = sb2("hytt", [64, 512]); t_tt = Tok()
                ti = sb2("hyti", [64, 512], I32); t_ti = Tok()
                tf = sb2("hytf", [64, 512]); t_tf = Tok()
                ones32 = sb2("hyones", [128, 128]); t_ones = Tok()
                P.op("pool", lambda e: e.memset(ones32, 1.0), writes=[t_ones])

                def sin_layer(wm, kdim, src, t_src, bcol, dst, t_dst):
                    for b in range(NB):
                        c0 = b * 512
                        P.op("pe", lambda e, c0=c0: e.matmul(self.ps[0][0:64, 0:BW], lhsT=wm[0:kdim, :], rhs=src[0:kdim, c0:c0 + BW], start=True, stop=True),
                             reads=[t_wm, t_src], writes=[self.pst[0]], skip_self=True)
                        P.op("dve", lambda e: e.tensor_scalar(out=tt[:, 0:BW], in0=self.ps[0][0:64, 0:BW], scalar1=fr[:, 0:1], scalar2=fr[:, bcol:bcol + 1], op0=ALU.mult, op1=ALU.add),
                             reads=[self.pst[0], t_fr], writes=[t_tt])
                        P.op("dve", lambda e: e.tensor_scalar(out=ti[:, 0:BW], in0=tt[:, 0:BW], scalar1=1.0 / TWO_PI, scalar2=None, op0=ALU.mult), reads=[t_tt], writes=[t_ti])
                        P.op("dve", lambda e: e.tensor_copy(out=tf[:, 0:BW], in_=ti[:, 0:BW]), reads=[t_ti], writes=[t_tf])
                        P.op("dve", lambda e: e.scalar_tensor_tensor(out=tt[:, 0:BW], in0=tf[:, 0:BW], scalar=-TWO_PI, in1=tt[:, 0:BW], op0=ALU.mult, op1=ALU.add), reads=[t_tf], writes=[t_tt])
                        P.op("act", lambda e, c0=c0: e.activation(out=dst[:, c0:c0 + BW], in_=tt[:, 0:BW], func=AF.Sin), reads=[t_tt], writes=[t_dst], partial=True)
                sin_layer(w1, 33, zemb, t_zemb, 1, h1, t_h1)
                sin_layer(w2, 64, h1, t_h1, 2, h2, t_h2)
                win = [sb2(f"hywin{i}", [128, 2, 256]) for i in range(2)]; t_win = [Tok(), Tok()]
                hfb = [sb2(f"hyhfb{i}", [128, 2, 256]) for i in range(2)]; t_hfb = [Tok(), Tok()]
                hab = [sb2(f"hyhab{i}", [128, 2, 256]) for i in range(2)]; t_hab = [Tok(), Tok()]
                for j in range(NTL):
                    bi = j % 2
                    P.dma("sp", win[bi], c_win[j * 128:(j + 1) * 128], writes=[t_win[bi]])
                    P.op("pe", lambda e, j=j: e.matmul(self.ps[1], lhsT=h2[:, j * 128:(j + 1) * 128], rhs=w3, start=True, stop=True),
                         reads=[t_h2, t_wm], writes=[self.pst[1]], skip_self=True)
                    P.op("dve", lambda e, bi=bi: e.tensor_tensor(out=hfb[bi], in0=self.ps[1].rearrange("p (a c) -> p a c", a=2), in1=win[bi], op=ALU.mult),
                         reads=[self.pst[1], t_win[bi]], writes=[t_hfb[bi]])
                    P.op("pool", lambda e, bi=bi, j=j: e.tensor_tensor(out=HS[:, j, :], in0=hfb[bi][:, 0, :], in1=hfb[bi][:, 1, :], op=ALU.add), reads=[t_hfb[bi]], writes=[t_HS], partial=True)
                    P.op("pool", lambda e, bi=bi, j=j: e.tensor_tensor(out=HD[:, j, :], in0=hfb[bi][:, 0, :], in1=hfb[bi][:, 1, :], op=ALU.subtract), reads=[t_hfb[bi]], writes=[t_HD], partial=True)
                    P.op("act", lambda e, bi=bi: e.activation(out=hab[bi], in_=hfb[bi], func=AF.Abs), reads=[t_hfb[bi]], writes=[t_hab[bi]])
                    for a in range(2):
                        P.op("pe", lambda e, bi=bi, a=a, j=j: e.matmul(self.ps[2][:, 0:256], lhsT=ones32, rhs=hab[bi][:, a, :], start=(j == 0 and a == 0), stop=(j == NTL - 1 and a == 1)),
                             reads=[t_ones, t_hab[bi]], writes=[self.pst[2]], skip_self=True)
                P.op("dve", lambda e: e.reciprocal(out=rl1, in_=self.ps[2][:, 0:256]), reads=[self.pst[2]], writes=[t_rl1])
                P.barrier()
            with contextlib.ExitStack() as st2:
                sb2 = lambda name, shape, dt=F32: self.sb(st2, name, shape, dt)
                pin = [sb2(f"hypin{i}", [128, L]) for i in range(2)]; t_pin = [Tok(), Tok()]
                uu = [sb2(f"hyu{i}", [128, L]) for i in range(2)]; t_uu = [Tok(), Tok()]

                def conv(ch, bi):
                    P.dma("sp" if bi == 0 else "act", pin[bi], self.PT[HY_LO + ch * 128:HY_LO + (ch + 1) * 128, tok0:tok0 + L], reads=[self.t_PT], writes=[t_pin[bi]])
                    eng = "dve"
                    P.op(eng, lambda e: e.tensor_scalar(out=uu[bi], in0=pin[bi], scalar1=cw[:, ch, 1:2], scalar2=cb[:, ch:ch + 1], op0=ALU.mult, op1=ALU.add), reads=[t_pin[bi], t_cw], writes=[t_uu[bi]])
                    P.op(eng, lambda e: e.scalar_tensor_tensor(out=uu[bi][:, 1:L], in0=pin[bi][:, 0:L - 1], scalar=cw[:, ch, 0:1], in1=uu[bi][:, 1:L], op0=ALU.mult, op1=ALU.add), reads=[t_pin[bi], t_cw], writes=[t_uu[bi]])
                    P.op(eng, lambda e: e.scalar_tensor_tensor(out=uu[bi][:, 0:L - 1], in0=pin[bi][:, 1:L], scalar=cw[:, ch, 2:3], in1=uu[bi][:, 0:L - 1], op0=ALU.mult, op1=ALU.add), reads=[t_pin[bi], t_cw], writes=[t_uu[bi]])
                for cc in range(2):
                    conv(cc, 0)
                    P.op("act", lambda e, cc=cc: e.copy(out=x0T[:, cc, :], in_=uu[0]), reads=[t_uu[0]], writes=[t_x0], partial=True)
                    conv(2 + cc, 0)
                    conv(4 + cc, 1)
                    P.op("pool", lambda e, cc=cc: e.tensor_tensor(out=zT[:, cc, :], in0=uu[0], in1=uu[1], op=ALU.mult), reads=[t_uu[0], t_uu[1]], writes=[t_zT], partial=True)
                g = 0
                for j0 in range(0, NTL, 2):
                    pi = 3 + g % 2
                    g += 1
                    pT = self.ps[pi].bitcast(BF16)
                    nj = min(2, NTL - j0)
                    for jj in range(nj):
                        for cc in range(2):
                            P.op("pe", lambda e, jj=jj, cc=cc, j0=j0, pT=pT: e.transpose(pT[:, (jj * 2 + cc) * 128:(jj * 2 + cc + 1) * 128], zT[:, cc, (j0 + jj) * 128:(j0 + jj + 1) * 128], self.identb),
                                 reads=[t_zT, self.t_ident], writes=[self.pst[pi]], skip_self=True)
                    if g % 2 == 0:
                        P.op("act", lambda e, j0=j0, nj=nj, pT=pT: e.copy(out=Z[:, j0:j0 + nj, :].rearrange("p j c -> p (j c)"), in_=pT[:, 0:nj * 256]), reads=[self.pst[pi]], writes=[t_Z], partial=True)
                    else:
                        P.op("dve", lambda e, j0=j0, nj=nj, pT=pT: e.tensor_copy(out=Z[:, j0:j0 + nj, :].rearrange("p j c -> p (j c)"), in_=pT[:, 0:nj * 256]), reads=[self.pst[pi]], writes=[t_Z], partial=True)
                P.barrier()
            with contextlib.ExitStack() as st2:
                sb2 = lambda name, shape, dt=F32: self.sb(st2, name, shape, dt)
                CT = [sb2(f"hyCT{i}", [128, NTL, 128], BF16) for i in range(2)]; t_CT = [Tok(), Tok()]
                ST = [sb2(f"hyST{i}", [128, NTL, 128], BF16) for i in range(2)]; t_ST = [Tok(), Tok()]
                hcs = [sb2(f"hyhcs{i}", [128, 2, 256]) for i in range(2)]; t_hcs = [Tok(), Tok()]
                t1 = sb2("hyt1", [128, 256]); t_t1 = Tok()
                t2 = sb2("hyt2", [128, 256]); t_t2 = Tok()
                t3 = sb2("hyt3", [128, 256]); t_t3 = Tok()
                t4 = sb2("hyt4", [128, 256]); t_t4 = Tok()
                for fc in range(NF):
                    bi = fc % 2
                    P.dma("sp", CT[bi], c_C[0:L, fc * 128:(fc + 1) * 128].rearrange("(j p) f -> p j f", p=128), writes=[t_CT[bi]])
                    P.dma("act", ST[bi], c_S[0:L, fc * 128:(fc + 1) * 128].rearrange("(j p) f -> p j f", p=128), writes=[t_ST[bi]])
                    pc, psn = 2 * bi, 2 * bi + 1
                    for (mat, t_mat, pi, rhs2, t_rhs2) in ((CT[bi], t_CT[bi], pc, HS, t_HS), (ST[bi], t_ST[bi], psn, HD, t_HD)):
                        for half, (rt, t_rt) in enumerate(((Z, t_Z), (rhs2, t_rhs2))):
                            for j in range(NTL):
                                P.op("pe", lambda e, mat=mat, pi=pi, rt=rt, j=j, half=half: e.matmul(self.ps[pi][:, half * 256:(half + 1) * 256], lhsT=mat[:, j, :], rhs=rt[:, j, :], start=(j == 0), stop=(j == NTL - 1)),
                                     reads=[t_mat, t_rt], writes=[self.pst[pi]], skip_self=True)
                    P.op("act", lambda e, bi=bi, pc=pc: e.copy(out=hcs[bi][:, 0, :], in_=self.ps[pc][:, 256:512]), reads=[self.pst[pc]], writes=[t_hcs[bi]], partial=True)
                    P.op("act", lambda e, bi=bi, psn=psn: e.copy(out=hcs[bi][:, 1, :], in_=self.ps[psn][:, 256:512]), reads=[self.pst[psn]], writes=[t_hcs[bi]], partial=True)
                    P.op("dve", lambda e, bi=bi, pc=pc: e.tensor_tensor(out=t1, in0=self.ps[pc][:, 0:256], in1=hcs[bi][:, 0, :], op=ALU.mult), reads=[self.pst[pc], t_hcs[bi]], writes=[t_t1])
                    P.op("dve", lambda e, bi=bi, pc=pc: e.tensor_tensor(out=t3, in0=self.ps[pc][:, 0:256], in1=hcs[bi][:, 1, :], op=ALU.mult), reads=[self.pst[pc], t_hcs[bi]], writes=[t_t3])
                    P.op("dve", lambda e, bi=bi, psn=psn: e.tensor_tensor(out=t2, in0=self.ps[psn][:, 0:256], in1=hcs[bi][:, 1, :], op=ALU.mult), reads=[self.pst[psn], t_hcs[bi]], writes=[t_t2])
                    P.op("dve", lambda e, bi=bi, psn=psn: e.tensor_tensor(out=t4, in0=self.ps[psn][:, 0:256], in1=hcs[bi][:, 0, :], op=ALU.mult), reads=[self.pst[psn], t_hcs[bi]], writes=[t_t4])
                    P.op("pool", lambda e: e.tensor_tensor(out=t1, in0=t1, in1=t2, op=ALU.subtract), reads=[t_t2], writes=[t_t1])
                    P.op("pool", lambda e: e.tensor_tensor(out=t3, in0=t3, in1=t4, op=ALU.add), reads=[t_t4], writes=[t_t3])
                    P.op("dve", lambda e, fc=fc: e.scalar_tensor_tensor(out=Asp[:, fc, :], in0=t1, scalar=wf[:, fc:fc + 1], in1=rl1, op0=ALU.mult, op1=ALU.mult), reads=[t_t1, t_wf, t_rl1], writes=[t_A], partial=True)
                    P.op("dve", lambda e, fc=fc: e.scalar_tensor_tensor(out=Bsp[:, fc, :], in0=t3, scalar=wf[:, fc:fc + 1], in1=rl1, op0=ALU.mult, op1=ALU.mult), reads=[t_t3, t_wf, t_rl1], writes=[t_B], partial=True)
                P.barrier()
            with contextlib.ExitStack() as st2:
                sb2 = lambda name, shape, dt=F32: self.sb(st2, name, shape, dt)
                TW = min(L, 1024)
                GC = [sb2(f"hyGC{i}", [128, TW], BF16) for i in range(3)]; t_GC = [Tok() for _ in range(3)]
                GS = [sb2(f"hyGS{i}", [128, TW], BF16) for i in range(3)]; t_GS = [Tok() for _ in range(3)]
                yt = sb2("hyyt", [128, 512]); t_yt = Tok()
                yo = [sb2(f"hyyo{i}", [128, 512], BF16) for i in range(2)]; t_yo = [Tok(), Tok()]
                nld = 0
                for tb0 in range(0, L, TW):
                    nsub = TW // BW
                    for fc in range(NF):
                        gi = nld % 3
                        nld += 1
                        P.dma("sp", GC[gi], c_C[fc * 128:(fc + 1) * 128, tb0:tb0 + TW], writes=[t_GC[gi]])
                        P.dma("act", GS[gi], c_S[fc * 128:(fc + 1) * 128, tb0:tb0 + TW], writes=[t_GS[gi]])
                        for sub in range(nsub):
                            for cc in range(2):
                                pi = sub * 2 + cc
                                P.op("pe", lambda e, gi=gi, fc=fc, sub=sub, cc=cc, pi=pi: e.matmul(self.ps[pi][:, 0:BW], lhsT=Asp[:, fc, cc * 128:(cc + 1) * 128], rhs=GC[gi][:, sub * BW:(sub + 1) * BW], start=(fc == 0), stop=False),
                                     reads=[t_A, t_GC[gi]], writes=[self.pst[pi]], skip_self=True)
                                P.op("pe", lambda e, gi=gi, fc=fc, sub=sub, cc=cc, pi=pi: e.matmul(self.ps[pi][:, 0:BW], lhsT=Bsp[:, fc, cc * 128:(cc + 1) * 128], rhs=GS[gi][:, sub * BW:(sub + 1) * BW], start=False, stop=(fc == NF - 1)),
                                     reads=[t_B, t_GS[gi]], writes=[self.pst[pi]], skip_self=True)
                    for sub in range(nsub):
                        for cc in range(2):
                            pi = sub * 2 + cc
                            c0 = tb0 + sub * BW
                            oi = pi % 2
                            P.op("dve", lambda e, cc=cc, c0=c0, pi=pi: e.scalar_tensor_tensor(out=yt[:, 0:BW], in0=zT[:, cc, c0:c0 + BW], scalar=bd[:, cc:cc + 1], in1=self.ps[pi][:, 0:BW], op0=ALU.mult, op1=ALU.add),
                                 reads=[t_zT, t_bd, self.pst[pi]], writes=[t_yt])
                            P.op("pool", lambda e, cc=cc, c0=c0, oi=oi: e.tensor_tensor(out=yo[oi][:, 0:BW], in0=yt[:, 0:BW], in1=x0T[:, cc, c0:c0 + BW], op=ALU.mult), reads=[t_yt, t_x0], writes=[t_yo[oi]])
                            P.dma("pool", self.MIXT[768 + cc * 128:768 + (cc + 1) * 128, tok0 + c0:tok0 + c0 + BW], yo[oi][:, 0:BW], reads=[t_yo[oi]], writes=[self.t_MIXT], partial=True)
                P.barrier()

    def phase_wout(self, li):
        nc, P = self.nc, self.P
        last = li == DEPTH - 1
        with contextlib.ExitStack() as st:
            sb = lambda name, shape, dt=F32: self.sb(st, name, shape, dt)
            wo = sb("woutb", [128, 8, D], BF16); t_wo = Tok()
            with contextlib.ExitStack() as st2:
                self.load_cast_weight(st2, wo, t_wo, self.w_out[li], D)
                P.barrier()
            G1 = sb("G1rep", [128, 2, D]); t_G1 = Tok()
            for v in range(2):
                P.dma("sp", G1[:, v, :], self.MOD[v, 2 * D:3 * D].partition_broadcast(128), reads=[self.t_MOD], writes=[t_G1], partial=True)
            mx = [sb(f"mixT{i}", [128, 8, 128], BF16) for i in range(2)]; t_mx = [Tok(), Tok()]
            xt = [sb(f"wxt{i}", [128, D]) for i in range(2)]; t_xt = [Tok(), Tok()]
            tm = [sb(f"wtm{i}", [128, 512]) for i in range(2)]; t_tm = [Tok(), Tok()]
            tiles = list(range(2 if last else 0, TT // 128))
            for n, j in enumerate(tiles):
                bi = n % 2
                v = 1 if j < 2 else 0
                c0, c1 = j * 128, (j + 1) * 128
                P.dma("sp", mx[bi], self.MIXT[:, c0:c1].rearrange("(c p) t -> p c t", p=128), reads=[self.t_MIXT], writes=[t_mx[bi]])
                P.dma("act", xt[bi], self.XS[c0:c1, :], reads=[self.t_XS], writes=[t_xt[bi]])
                for half in range(2):
                    pi = (n * 2 + half) % 4
                    for k in range(8):
                        P.op("pe", lambda e, bi=bi, k=k, half=half, pi=pi: e.matmul(self.ps[pi], lhsT=mx[bi][:, k, :], rhs=wo[:, k, half * 512:(half + 1) * 512], start=(k == 0), stop=(k == 7)),
                             reads=[t_mx[bi], t_wo], writes=[self.pst[pi]], skip_self=True)
                    P.op("dve", lambda e, half=half, pi=pi, v=v: e.tensor_tensor(out=tm[half], in0=self.ps[pi], in1=G1[:, v, half * 512:(half + 1) * 512], op=ALU.mult),
                         reads=[self.pst[pi], t_G1], writes=[t_tm[half]])
                    P.op("pool", lambda e, bi=bi, half=half: e.tensor_tensor(out=xt[bi][:, half * 512:(half + 1) * 512], in0=xt[bi][:, half * 512:(half + 1) * 512], in1=tm[half], op=ALU.add),
                         reads=[t_tm[half]], writes=[t_xt[bi]])
                P.dma("pool", self.XS[c0:c1, :], xt[bi], reads=[t_xt[bi]], writes=[self.t_XS], partial=True)
            P.barrier()

    def phase_ffn(self, li):
        nc, P = self.nc, self.P
        last = li == DEPTH - 1
        with contextlib.ExitStack() as st:
            sb = lambda name, shape, dt=F32: self.sb(st, name, shape, dt)
            wup = sb("wupb", [128, 8, 2 * DFF], BF16); t_wup = Tok()
            wdn = sb("wdnb", [128, NFC, D], BF16); t_wdn = Tok()
            with contextlib.ExitStack() as st2:
                self.load_cast_weight(st2, wup, t_wup, self.ffn_w_up[li], 2 * DFF)
                P.barrier()
            with contextlib.ExitStack() as st2:
                self.load_cast_weight(st2, wdn, t_wdn, self.ffn_w_down[li], D, blk=256, k_chunks=NFC)
                P.barrier()
            G2 = sb("G2rep", [128, 2, D]); t_G2 = Tok()
            for v in range(2):
                P.dma("sp", G2[:, v, :], self.MOD[v, 5 * D:6 * D].partition_broadcast(128), reads=[self.t_MOD], writes=[t_G2], partial=True)
            if last:
                FG = sb("FGrep", [128, D]); t_FG = Tok()
                P.dma("sp", FG, self.final_norm_g.partition_broadcast(128), writes=[t_FG])
            cw = sb("ffcw", [128, 2 * NFC, 3]); cbv = sb("ffcb", [128, 2 * NFC]); t_cw = Tok()
            for jj in range(3):
                P.dma("sp", cw[:, :, jj], self.ffn_conv_w[li, jj].rearrange("(c p) -> p c", p=128), writes=[t_cw], partial=True, allow_slow_non_contiguous=True)
            P.dma("sp", cbv, self.ffn_conv_b[li].rearrange("(c p) -> p c", p=128), writes=[t_cw], partial=True, allow_slow_non_contiguous=True)
            hx = [sb(f"hTx{i}", [128, 8, 258], BF16) for i in range(3)]; t_hx = [Tok() for _ in range(3)]
            gT = sb("ffgT", [128, NFC, 256], BF16); t_gT = Tok()
            xts = [sb(f"fxt{i}", [128, D]) for i in range(4)]; t_xts = [Tok() for _ in range(4)]
            sqj = sb("fsq", [128, D], BF16); t_sqj = Tok()
            sss = [sb(f"fss{i}", [128, 4]) for i in range(4)]; t_sss = [Tok() for _ in range(4)]
            xns = [sb(f"fxn{i}", [128, D], BF16) for i in range(2)]; t_xns = [Tok(), Tok()]
            ga = [sb(f"ffga{i}", [128, 256]) for i in range(2)]; t_ga = [Tok(), Tok()]
            gb = [sb(f"ffgb{i}", [128, 256]) for i in range(2)]; t_gb = [Tok(), Tok()]
            tmo = [sb(f"fftm{i}", [128, 512]) for i in range(2)]; t_tmo = [Tok(), Tok()]
            fss = sb("ffss", [128, 4]); t_fss = Tok()
            tiles = list(range(1 if last else 0, TT // 256))
            seq_first = {0, 1}
            seq_last = {0, TT // 256 - 1}
            nsub = 0
            sets_of = {}

            def stage_a(ti):
                nonlocal nsub
                t0 = ti * 256
                v = 1 if ti == 0 else 0
                hb = ti % 3
                sets_of[ti] = []
                for sub in range(2):
                    si = nsub % 4
                    nsub += 1
                    sets_of[ti].append(si)
                    bufs = (xts[si], t_xts[si], sqj, t_sqj, sss[si], t_sss[si], xns[si % 2], t_xns[si % 2])
                    self.norm_transpose(bufs, self.XS[t0 + sub * 128:t0 + (sub + 1) * 128, :], self.t_XS, self.s2, self.modT[:, 24:32, :], v,
                                        hx[hb], t_hx[hb], 1 + sub * 128, ps_i=6 + sub)
                if ti in seq_first:
                    P.op("pool", lambda e, hb=hb: e.memset(hx[hb][:, :, 0:1], 0.0), writes=[t_hx[hb]], partial=True)
                if ti in seq_last:
                    P.op("pool", lambda e, hb=hb: e.memset(hx[hb][:, :, 257:258], 0.0), writes=[t_hx[hb]], partial=True)

            def halo(ta, tb):
                a, b = ta % 3, tb % 3
                P.op("pool", lambda e: e.tensor_copy(out=hx[a][:, :, 257:258], in_=hx[b][:, :, 1:2]), reads=[t_hx[b]], writes=[t_hx[a]], partial=True)
                P.op("pool", lambda e: e.tensor_copy(out=hx[b][:, :, 0:1], in_=hx[a][:, :, 256:257]), reads=[t_hx[a]], writes=[t_hx[b]], partial=True)

            def stage_b(ti):
                t0 = ti * 256
                v = 1 if ti == 0 else 0
                hb = ti % 3
                for jc in range(NFC):
                    gi = jc % 2
                    for (which, col0, pi, acc, t_acc) in ((0, jc * 128, 0 + 2 * gi, ga[gi], t_ga[gi]), (1, DFF + jc * 128, 1 + 2 * gi, gb[gi], t_gb[gi])):
                        ch = (col0 // 128)
                        for k in range(8):
                            P.op("pe", lambda e, k=k, col0=col0, pi=pi: e.matmul(self.ps[pi][:, 0:258], lhsT=wup[:, k, col0:col0 + 128], rhs=hx[hb][:, k, :], start=(k == 0), stop=(k == 7)),
                                 reads=[t_wup, t_hx[hb]], writes=[self.pst[pi]], skip_self=True)
                        P.op("act", lambda e, pi=pi, acc=acc, ch=ch: e.activation(out=acc, in_=self.ps[pi][:, 1:257], func=AF.Identity, scale=cw[:, ch, 1:2], bias=cbv[:, ch:ch + 1]),
                             reads=[self.pst[pi], t_cw], writes=[t_acc])
                        P.op("dve", lambda e, pi=pi, acc=acc, ch=ch: e.scalar_tensor_tensor(out=acc, in0=self.ps[pi][:, 0:256], scalar=cw[:, ch, 0:1], in1=acc, op0=ALU.mult, op1=ALU.add),
                             reads=[self.pst[pi], t_cw], writes=[t_acc])
                        P.op("dve", lambda e, pi=pi, acc=acc, ch=ch: e.scalar_tensor_tensor(out=acc, in0=self.ps[pi][:, 2:258], scalar=cw[:, ch, 2:3], in1=acc, op0=ALU.mult, op1=ALU.add),
                             reads=[self.pst[pi], t_cw], writes=[t_acc])
                    P.op("act", lambda e, gi=gi: e.activation(out=ga[gi], in_=ga[gi], func=AF.Silu), reads=[t_ga[gi]], writes=[t_ga[gi]])
                    P.op("pool", lambda e, gi=gi, jc=jc: e.tensor_tensor(out=gT[:, jc, :], in0=ga[gi], in1=gb[gi], op=ALU.mult), reads=[t_ga[gi], t_gb[gi]], writes=[t_gT], partial=True)
                for sub in range(2):
                    si = sets_of[ti][sub]
                    xt, t_xt = xts[si], t_xts[si]
                    for half in range(2):
                        pi = 4 + half
                        for jc in range(NFC):
                            P.op("pe", lambda e, jc=jc, sub=sub, half=half, pi=pi: e.matmul(self.ps[pi], lhsT=gT[:, jc, sub * 128:(sub + 1) * 128], rhs=wdn[:, jc, half * 512:(half + 1) * 512], start=(jc == 0), stop=(jc == NFC - 1)),
                                 reads=[t_gT, t_wdn], writes=[self.pst[pi]], skip_self=True)
                        P.op("dve", lambda e, half=half, pi=pi: e.tensor_tensor(out=tmo[half], in0=self.ps[pi], in1=G2[:, v, half * 512:(half + 1) * 512], op=ALU.mult),
                             reads=[self.pst[pi], t_G2], writes=[t_tmo[half]])
                        P.op("pool", lambda e, half=half, xt=xt: e.tensor_tensor(out=xt[:, half * 512:(half + 1) * 512], in0=xt[:, half * 512:(half + 1) * 512], in1=tmo[half], op=ALU.add),
                             reads=[t_tmo[half]], writes=[t_xt])
                    r0 = t0 + sub * 128
                    if not last:
                        P.dma("pool", self.XS[r0:r0 + 128, :], xt, reads=[t_xt], writes=[self.t_XS], partial=True)
                    else:
                        P.op("act", lambda e, xt=xt: e.activation(out=sqj, in_=xt, func=AF.Square, accum_out=fss[:, 0:1]), reads=[t_xt], writes=[t_sqj, t_fss])
                        P.op("dve", lambda e: e.tensor_scalar(out=fss[:, 1:2], in0=fss[:, 0:1], scalar1=1.0 / D, scalar2=EPS, op0=ALU.mult, op1=ALU.add), reads=[t_fss], writes=[t_fss])
                        P.op("act", lambda e: e.activation(out=fss[:, 2:3], in_=fss[:, 1:2], func=AF.Sqrt), reads=[t_fss], writes=[t_fss])
                        P.op("dve", lambda e: e.reciprocal(out=fss[:, 3:4], in_=fss[:, 2:3]), reads=[t_fss], writes=[t_fss])
                        P.op("dve", lambda e, xt=xt: e.scalar_tensor_tensor(out=xt, in0=xt, scalar=fss[:, 3:4], in1=FG, op0=ALU.mult, op1=ALU.mult), reads=[t_fss, t_FG], writes=[t_xt])
                        P.dma("pool", self.out[r0 - CTX:r0 - CTX + 128, :], xt, reads=[t_xt], writes=[self.t_out], partial=True)

            prev = None
            for ti in tiles:
                stage_a(ti)
                if prev is not None:
                    if prev not in seq_last:
                        halo(prev, ti)
                    stage_b(prev)
                prev = ti
            stage_b(prev)
            P.barrier()

    def build(self, stop_after=None):
        nc, P = self.nc, self.P
        self.declare()
        P.clear_sems()
        gst = self.stack
        self.identb = self.sb(gst, "identb", [128, 128], BF16)
        self.t_ident = Tok()
        P.dma("sp", self.identb, self.c_ident, writes=[self.t_ident])
        self.t_XS = Tok()
        self.t_MOD, self.t_PT, self.t_PVO, self.t_MIXT, self.t_out = Tok(), Tok(), Tok(), Tok(), Tok()
        P.dma("sp", self.XS[0:CTX, :], self.ctx, writes=[self.t_XS], partial=True)
        for i in range(16):
            P.dma("act" if i % 2 else "sp", self.XS[CTX + i * 256:CTX + (i + 1) * 256, :], self.x[i * 256:(i + 1) * 256, :], writes=[self.t_XS], partial=True)
        done = False
        for li in range(DEPTH):
            with contextlib.ExitStack() as lst:
                self.lst = lst
                for name, fn in (("mod", self.phase_mod), ("inproj", self.phase_inproj), ("mla", self.phase_mla), ("mlstm", self.phase_mlstm), ("hyena", self.phase_hyena), ("wout", self.phase_wout), ("ffn", self.phase_ffn)):
                    fn(li)
                    if stop_after == (name, li):
                        done = True
                        break
                P.barrier()
            if done:
                break
        P.finish()
        return nc


def _prep_inputs(inputs, b):
    m = {}
    for k, v in inputs.items():
        v = np.asarray(v)
        if k in ("x", "ctx", "c"):
            m[k] = np.ascontiguousarray(v[b])
        elif k == "ml_gate_b":
            m[k] = np.ascontiguousarray(v.reshape(DEPTH, 16))
        else:
            m[k] = np.ascontiguousarray(v)
    return m


def kernel(**inputs):
    bld = Builder()
    nc = bld.build()
    consts = _consts()
    in_maps = []
    for b in range(8):
        m = _prep_inputs(inputs, b)
        m.update(consts)
        in_maps.append({k: m[k] for k in bld.inp})
    res = run_bass_kernel_spmd(nc, in_maps, core_ids=list(range(8)))
    return np.stack([np.asarray(r["out"]) for r in res.results], axis=0).astype(np.float32)
```

```python
import contextlib
import numpy as np
import ml_dtypes
import concourse.bass as bass
import concourse.mybir as mybir
from concourse.bass_utils import run_bass_kernel_spmd

F32 = mybir.dt.float32
BF16 = mybir.dt.bfloat16
I32 = mybir.dt.int32
AF = mybir.ActivationFunctionType
ALU = mybir.AluOpType
AX = mybir.AxisListType

D = 1024
SEQ = 4096
CTX = 256
TT = SEQ + CTX
DEPTH = 2
EPS = 1e-6
NH = 8
QR, KVR, ROPE = 384, 256, 32
N_MLA_IN = QR + KVR + ROPE
MLW = 256
N_ML_IN = 3 * MLW + 16
HYW = 256
N_IN = 2224
ML_LO = N_MLA_IN
HY_LO = N_MLA_IN + N_ML_IN
DFF = 2816
NFC = DFF // 128
TWO_PI = float(2 * np.pi)


class Tok:
    __slots__ = ("w", "r", "wf")

    def __init__(self):
        self.w = []
        self.wf = []
        self.r = []


class PTok(Tok):
    __slots__ = ()


class Queue:
    def __init__(self, prog, name, eng, n_dma_sems=0):
        self.p = prog
        self.name = name
        self.eng = eng
        nc = prog.nc
        self.sem = nc.alloc_semaphore(name=f"s_{name}")
        self.count = 0
        self.dma_sems = [nc.alloc_semaphore(name=f"d_{name}{i}") for i in range(n_dma_sems)]
        self.dma_counts = [0] * n_dma_sems
        self.dma_rr = 0
        self.waited = {}

    def _wait(self, tick):
        sem, val = tick
        key = id(sem)
        if self.waited.get(key, 0) >= val:
            return
        self.eng.wait_ge(sem, val)
        self.waited[key] = val
        self.p.n_waits += 1


class Prog:
    def __init__(self, nc, dma_sems=8):
        self.nc = nc
        self.n_waits = 0
        self.n_ins = 0
        self.q = {
            "pe": Queue(self, "pe", nc.tensor),
            "dve": Queue(self, "dve", nc.vector),
            "act": Queue(self, "act", nc.scalar, dma_sems),
            "pool": Queue(self, "pool", nc.gpsimd, dma_sems),
            "sp": Queue(self, "sp", nc.sync, dma_sems),
        }

    def clear_sems(self):
        for q in self.q.values():
            q.eng.sem_clear(q.sem)
            for s in q.dma_sems:
                q.eng.sem_clear(s)
        self.nc.all_engine_barrier()

    def _deps(self, q, reads, writes, skip_self=False, partial=False):
        for t in reads:
            for tk in t.w:
                if not (skip_self and tk[0] is q.sem):
                    q._wait(tk)
            if isinstance(t, PTok):
                for tk in t.r:
                    if not (skip_self and tk[0] is q.sem):
                        q._wait(tk)
        for t in writes:
            if partial and not isinstance(t, PTok):
                for tk in t.wf:
                    if not (skip_self and tk[0] is q.sem):
                        q._wait(tk)
            if not partial or isinstance(t, PTok):
                for tk in t.w:
                    if not (skip_self and tk[0] is q.sem):
                        q._wait(tk)
            for tk in t.r:
                if not (skip_self and tk[0] is q.sem):
                    q._wait(tk)

    @staticmethod
    def _compact(lst):
        best = {}
        for s, v in lst:
            k = id(s)
            if k not in best or best[k][1] < v:
                best[k] = (s, v)
        return list(best.values())

    def _record(self, tick, reads, writes, partial=False):
        for t in reads:
            if isinstance(t, PTok):
                t.w = [tick]
                t.wf = [tick]
                t.r = []
                continue
            t.r.append(tick)
            if len(t.r) > 48:
                t.r = self._compact(t.r)
        for t in writes:
            if partial and not isinstance(t, PTok):
                t.w.append(tick)
                if len(t.w) > 48:
                    t.w = self._compact(t.w)
            else:
                t.w = [tick]
                t.wf = [tick]
                t.r = []

    def op(self, qname, fn, reads=(), writes=(), skip_self=False, partial=False):
        q = self.q[qname]
        self._deps(q, reads, writes, skip_self=skip_self, partial=partial)
        ins = fn(q.eng)
        q.count += 1
        ins.then_inc(q.sem, 1)
        self._record((q.sem, q.count), reads, writes, partial=partial)
        self.n_ins += 1
        return ins

    def dma(self, qname, out, in_, reads=(), writes=(), partial=False, **kw):
        q = self.q[qname]
        j = q.dma_rr
        q.dma_rr = (j + 1) % len(q.dma_sems)
        sem = q.dma_sems[j]
        if q.dma_counts[j] > 0:
            q._wait((sem, q.dma_counts[j]))
        self._deps(q, reads, writes, partial=partial)
        ins = q.eng.dma_start(out=out, in_=in_, **kw)
        q.dma_counts[j] += 16
        ins.then_inc(sem, 16)
        self._record((sem, q.dma_counts[j]), reads, writes, partial=partial)
        self.n_ins += 1
        return ins

    def barrier(self):
        ticks = []
        for q in self.q.values():
            if q.count:
                ticks.append((q.sem, q.count))
            for j, s in enumerate(q.dma_sems):
                if q.dma_counts[j]:
                    ticks.append((s, q.dma_counts[j]))
        for q in self.q.values():
            for tk in ticks:
                q._wait(tk)

    def finish(self):
        ticks = []
        for q in self.q.values():
            if q.count:
                ticks.append((q.sem, q.count))
            for j, s in enumerate(q.dma_sems):
                if q.dma_counts[j]:
                    ticks.append((s, q.dma_counts[j]))
        for tk in ticks:
            self.q["sp"]._wait(tk)


def _consts():
    c = {}
    c["ident"] = np.eye(128, dtype=np.float32).astype(ml_dtypes.bfloat16)
    n_freq = ROPE // 4
    inv = (10000.0 ** (-np.arange(n_freq, dtype=np.float32) / n_freq)).astype(np.float32)
    row = np.repeat(np.arange(SEQ // 64, dtype=np.float32), 64)
    col = np.tile(np.arange(64, dtype=np.float32), SEQ // 64)
    ang = np.concatenate([row[:, None] * inv, col[:, None] * inv], axis=-1).astype(np.float32)
    cos = np.cos(ang).astype(np.float32).T
    sin = np.sin(ang).astype(np.float32).T
    c["rope_cos"] = np.ascontiguousarray(np.concatenate([cos, cos], 0))
    c["rope_sin"] = np.ascontiguousarray(np.concatenate([sin, sin], 0))
    ss_, tt_ = np.meshgrid(np.arange(128), np.arange(128), indexing="ij")
    c["ml_mask"] = np.stack([(ss_ <= tt_), (ss_ >= tt_)], 1).astype(np.float32).astype(ml_dtypes.bfloat16)
    selA = np.zeros((96, 4, 128), np.float32)
    selW = np.zeros((96, 8), np.float32)
    for d in range(2):
        base = 32 + 4 if d == 0 else 64 + 12
        for h in range(4):
            selA[base + h, (h // 2) * 2 + d, (h % 2) * 64:(h % 2) * 64 + 64] = 1.0
            selW[8 * d + h, d * 4 + h] = 1.0
            selW[base + h, d * 4 + h] = -1.0
    c["ml_selA"] = selA
    c["ml_selW"] = selW
    for L in (256, 4096):
        f32 = np.float32
        t = np.linspace(0.0, 1.0, L, dtype=f32)[:, None]
        omega = (f32(2.0 * np.pi) * np.arange(L, dtype=f32) / f32(L)).astype(f32)
        bands = np.linspace(1e-4, 15, 16, dtype=f32)
        ang = (omega[:, None] * bands[None, :]).astype(f32)
        z = np.concatenate([t, np.cos(ang).astype(f32), -np.sin(ang).astype(f32)], axis=-1).astype(f32)
        c[f"hy_z{L}"] = np.ascontiguousarray(z.T)
        deltas = np.abs(np.linspace(np.log(1e-2) / 1.5, np.log(1e-2) / 0.3, 256, dtype=f32)).astype(f32)
        window = (np.exp(-t * deltas).astype(f32) + f32(0.05)).astype(f32)
        wb = window.copy()
        wb[0] = 0.0
        c[f"hy_win{L}"] = np.ascontiguousarray(np.stack([window, wb], axis=1))
        NFp = L + 128
        n = 2 * L
        idx = np.arange(L + 1, dtype=np.int64)
        prod = (idx[:, None] * idx[None, :]) % n
        angm = prod.astype(np.float64) * (2.0 * np.pi / n)
        Cm = np.zeros((NFp, NFp), np.float32)
        Sm = np.zeros((NFp, NFp), np.float32)
        Cm[:L + 1, :L + 1] = np.cos(angm)
        Sm[:L + 1, :L + 1] = np.sin(angm)
        c[f"hy_C{L}"] = Cm.astype(ml_dtypes.bfloat16)
        c[f"hy_S{L}"] = Sm.astype(ml_dtypes.bfloat16)
        wfv = np.zeros(NFp, np.float32)
        wfv[:L + 1] = 2.0 / n
        wfv[0] = 1.0 / n
        wfv[L] = 1.0 / n
        c[f"hy_wf{L}"] = np.ascontiguousarray(wfv.reshape(-1, 128).T)
    return c


class Builder:
    def __init__(self, debug=None):
        self.debug = debug or set()
        self.nc = nc = bass.Bass("TRN2", target_bir_lowering=False)
        self.P = Prog(nc)
        self.inp = {}
        self.stack = contextlib.ExitStack()

    def din(self, name, shape, dt=F32):
        ap = self.nc.dram_tensor(name, list(shape), dt, kind="ExternalInput").ap()
        self.inp[name] = ap
        return ap

    def dscr(self, name, shape, dt=F32):
        kind = "ExternalOutput" if name in self.debug else "Internal"
        return self.nc.dram_tensor(name, list(shape), dt, kind=kind).ap()

    def sb(self, st, name, shape, dt=F32):
        self.uid = getattr(self, "uid", 0) + 1
        return st.enter_context(self.nc.sbuf_tensor(f"{name}_{self.uid}", list(shape), dt)).ap()

    def declare(self):
        din = self.din
        self.x = din("x", [SEQ, D])
        self.c = din("c", [D])
        self.ctx = din("ctx", [CTX, D])
        self.c_ctx = din("c_ctx", [D])
        self.ada_w = din("ada_w", [DEPTH, D, 6 * D])
        self.ada_b = din("ada_b", [DEPTH, 6 * D])
        self.norm1_g = din("norm1_g", [DEPTH, D])
        self.norm2_g = din("norm2_g", [DEPTH, D])
        self.w_in = din("w_in", [DEPTH, D, N_IN])
        self.mla_q_norm_g = din("mla_q_norm_g", [DEPTH, QR])
        self.mla_kv_norm_g = din("mla_kv_norm_g", [DEPTH, KVR])
        self.mla_w_uq = din("mla_w_uq", [DEPTH, QR, NH * 96])
        self.mla_w_ukv = din("mla_w_ukv", [DEPTH, KVR, NH * 128])
        self.ml_conv_w = din("ml_conv_w", [DEPTH, 3, MLW])
        self.ml_conv_b = din("ml_conv_b", [DEPTH, MLW])
        self.ml_wq = din("ml_wq", [DEPTH, 4, 64, 64])
        self.ml_wk = din("ml_wk", [DEPTH, 4, 64, 64])
        self.ml_gate_b = din("ml_gate_b", [DEPTH, 16])
        self.ml_norm_g = din("ml_norm_g", [DEPTH, MLW])
        self.hy_conv_w = din("hy_conv_w", [DEPTH, 3, 3 * HYW])
        self.hy_conv_b = din("hy_conv_b", [DEPTH, 3 * HYW])
        self.hy_w1 = din("hy_w1", [DEPTH, 33, 64])
        self.hy_b1 = din("hy_b1", [DEPTH, 64])
        self.hy_w2 = din("hy_w2", [DEPTH, 64, 64])
        self.hy_b2 = din("hy_b2", [DEPTH, 64])
        self.hy_w3 = din("hy_w3", [DEPTH, 64, 2 * HYW])
        self.hy_sin_freq = din("hy_sin_freq", [DEPTH, 64])
        self.hy_bias_d = din("hy_bias_d", [DEPTH, HYW])
        self.w_out = din("w_out", [DEPTH, D, D])
        self.ffn_w_up = din("ffn_w_up", [DEPTH, D, 2 * DFF])
        self.ffn_conv_w = din("ffn_conv_w", [DEPTH, 3, 2 * DFF])
        self.ffn_conv_b = din("ffn_conv_b", [DEPTH, 2 * DFF])
        self.ffn_w_down = din("ffn_w_down", [DEPTH, DFF, D])
        self.final_norm_g = din("final_norm_g", [D])
        self.c_ident = din("ident", [128, 128], BF16)
        self.c_rope_cos = din("rope_cos", [32, SEQ])
        self.c_rope_sin = din("rope_sin", [32, SEQ])
        self.c_ml_mask = din("ml_mask", [128, 2, 128], BF16)
        self.c_ml_selA = din("ml_selA", [96, 4, 128])
        self.c_ml_selW = din("ml_selW", [96, 8])
        for L in (256, 4096):
            NFp = L + 128
            setattr(self, f"c_hy_z{L}", din(f"hy_z{L}", [33, L]))
            setattr(self, f"c_hy_win{L}", din(f"hy_win{L}", [L, 2, 256]))
            setattr(self, f"c_hy_C{L}", din(f"hy_C{L}", [NFp, NFp], BF16))
            setattr(self, f"c_hy_S{L}", din(f"hy_S{L}", [NFp, NFp], BF16))
            setattr(self, f"c_hy_wf{L}", din(f"hy_wf{L}", [128, L // 128 + 1]))
        self.out = self.nc.dram_tensor("out", [SEQ, D], F32, kind="ExternalOutput").ap()
        self.XS = self.dscr("XS", [TT, D])
        self.MOD = self.dscr("MOD", [2, 6 * D])
        self.PT = self.dscr("PT", [N_IN + 32, TT])
        self.PVO = self.dscr("PVO", [TT, 512])
        self.MIXT = self.dscr("MIXT", [D, TT], BF16)
        self.psall = self.nc.alloc_psum_tensor("psall", [128, 8 * 512], F32).ap()
        self.ps = [self.psall[:, i * 512:(i + 1) * 512] for i in range(8)]
        self.pst = [PTok() for _ in range(8)]

    def vec_pc(self, st, name, src, n, q="sp"):
        t = self.sb(st, name, [128, n])
        tok = Tok()
        self.P.dma(q, t, src.rearrange("(c p) -> p c", p=128), writes=[tok], allow_slow_non_contiguous=True)
        return t, tok

    def phase_mod(self, li):
        nc, P = self.nc, self.P
        lst = self.lst
        self.modT = self.sb(lst, "modT", [128, 48, 2])
        self.t_mod = Tok()
        self.s1 = self.sb(lst, "s1", [128, 8, 2])
        self.s2 = self.sb(lst, "s2", [128, 8, 2])
        self.t_s12 = Tok()
        with contextlib.ExitStack() as st:
            cc = self.sb(st, "cc", [128, 8, 2])
            t_cc = Tok()
            P.dma("sp", cc[:, :, 0], self.c.rearrange("(c p) -> p c", p=128), writes=[t_cc], partial=True, allow_slow_non_contiguous=True)
            P.dma("sp", cc[:, :, 1], self.c_ctx.rearrange("(c p) -> p c", p=128), writes=[t_cc], partial=True, allow_slow_non_contiguous=True)
            sc = self.sb(st, "sc", [128, 8, 2])
            t_sc = Tok()
            P.op("act", lambda e: e.activation(out=sc, in_=cc, func=AF.Silu), reads=[t_cc], writes=[t_sc])
            ab = self.sb(st, "ab", [128, 48])
            t_ab = Tok()
            P.dma("sp", ab, self.ada_b[li].rearrange("(c p) -> p c", p=128), writes=[t_ab], allow_slow_non_contiguous=True)
            g12 = self.sb(st, "g12", [128, 8, 2])
            t_g12 = Tok()
            P.dma("sp", g12[:, :, 0], self.norm1_g[li].rearrange("(c p) -> p c", p=128), writes=[t_g12], partial=True, allow_slow_non_contiguous=True)
            P.dma("sp", g12[:, :, 1], self.norm2_g[li].rearrange("(c p) -> p c", p=128), writes=[t_g12], partial=True, allow_slow_non_contiguous=True)
            wt = [self.sb(st, f"adaw{i}", [128, 8, 512]) for i in range(2)]
            t_wt = [Tok(), Tok()]
            acc = self.ps[0]
            t_acc = self.pst[0]
            for nb in range(12):
                s = nb % 2
                P.dma("sp" if nb % 2 == 0 else "act", wt[s],
                      self.ada_w[li, :, nb * 512:(nb + 1) * 512].rearrange("(c p) n -> p c n", p=128),
                      writes=[t_wt[s]])
                for j in range(4):
                    n = nb * 4 + j
                    for k in range(8):
                        P.op("pe", lambda e, s=s, j=j, k=k, n=n: e.matmul(
                            acc[:, 2 * n:2 * n + 2], lhsT=wt[s][:, k, j * 128:(j + 1) * 128], rhs=sc[:, k, :],
                            start=(k == 0), stop=(k == 7)),
                            reads=[t_wt[s], t_sc], writes=[t_acc], skip_self=True)
            mod = self.modT
            P.op("dve", lambda e: e.tensor_tensor(out=mod, in0=acc[:, 0:96].rearrange("p (n v) -> p n v", v=2),
                                                  in1=ab.unsqueeze(2).to_broadcast([128, 48, 2]), op=ALU.add),
                 reads=[t_acc, t_ab], writes=[self.t_mod])
            for (dst, gi, c0) in ((self.s1, 0, 8), (self.s2, 1, 32)):
                P.op("dve", lambda e, dst=dst, gi=gi, c0=c0: e.scalar_tensor_tensor(
                    out=dst, in0=mod[:, c0:c0 + 8, :], scalar=1.0, in1=g12[:, :, gi:gi + 1].to_broadcast([128, 8, 2]),
                    op0=ALU.add, op1=ALU.mult), reads=[self.t_mod, t_g12], writes=[self.t_s12], partial=True)
            for v in range(2):
                P.dma("sp", self.MOD[v].rearrange("(c p) -> p c", p=128), mod[:, :, v], reads=[self.t_mod], writes=[self.t_MOD],
                      partial=True, allow_slow_non_contiguous=True)
            P.barrier()

    def norm_transpose(self, st_bufs, rows_ap, t_rows, svec, bvec, v, dst, t_dst, col0, ps_i):
        P = self.P
        xt, t_xt, sq, t_sq, ss, t_ss, xn, t_xn = st_bufs
        P.dma("sp", xt, rows_ap, reads=[t_rows], writes=[t_xt])
        P.op("act", lambda e: e.activation(out=sq, in_=xt, func=AF.Square, accum_out=ss[:, 0:1]), reads=[t_xt], writes=[t_sq, t_ss])
        P.op("dve", lambda e: e.tensor_scalar(out=ss[:, 1:2], in0=ss[:, 0:1], scalar1=1.0 / D, scalar2=EPS, op0=ALU.mult, op1=ALU.add),
             reads=[t_ss], writes=[t_ss])
        P.op("act", lambda e: e.activation(out=ss[:, 2:3], in_=ss[:, 1:2], func=AF.Sqrt), reads=[t_ss], writes=[t_ss])
        P.op("dve", lambda e: e.reciprocal(out=ss[:, 3:4], in_=ss[:, 2:3]), reads=[t_ss], writes=[t_ss])
        P.op("dve", lambda e: e.tensor_scalar(out=xn, in0=xt, scalar1=ss[:, 3:4], scalar2=None, op0=ALU.mult), reads=[t_xt, t_ss], writes=[t_xn])
        pb = self.ps[ps_i].bitcast(BF16)
        t_pb = self.pst[ps_i]
        for c in range(8):
            P.op("pe", lambda e, c=c: e.transpose(pb[:, c * 128:(c + 1) * 128], xn[:, c * 128:(c + 1) * 128], self.identb),
                 reads=[t_xn, self.t_ident], writes=[t_pb], skip_self=True, partial=(c > 0))
        for c in range(8):
            if c % 2 == 0:
                P.op("act", lambda e, c=c: e.activation(out=dst[:, c, col0:col0 + 128], in_=pb[:, c * 128:(c + 1) * 128], func=AF.Identity,
                                                         scale=svec[:, c, v:v + 1], bias=bvec[:, c, v:v + 1]),
                     reads=[t_pb, self.t_s12, self.t_mod], writes=[t_dst], partial=True)
            else:
                P.op("dve", lambda e, c=c: e.tensor_scalar(out=dst[:, c, col0:col0 + 128], in0=pb[:, c * 128:(c + 1) * 128],
                                                           scalar1=svec[:, c, v:v + 1], scalar2=bvec[:, c, v:v + 1], op0=ALU.mult, op1=ALU.add),
                     reads=[t_pb, self.t_s12, self.t_mod], writes=[t_dst], partial=True)

    def load_cast_weight(self, st, dst, t_dst, src_ap, ncols, blk=512, k_chunks=8, engs=None):
        P = self.P
        engs = engs or ("pool", "dve", "act")
        stg = [self.sb(st, f"wstg{id(dst) % 9973}_{i}", [128, k_chunks, blk]) for i in range(2)]
        t_stg = [Tok(), Tok()]
        i = 0
        for c0 in range(0, ncols, blk):
            w = min(blk, ncols - c0)
            s = i % 2
            P.dma("sp" if i % 2 == 0 else "act", stg[s][:, :, 0:w], src_ap[:, c0:c0 + w].rearrange("(c p) n -> p c n", p=128), writes=[t_stg[s]])
            eng = engs[i % len(engs)]
            if eng == "act":
                P.op("act", lambda e, s=s, c0=c0, w=w: e.copy(out=dst[:, :, c0:c0 + w], in_=stg[s][:, :, 0:w]), reads=[t_stg[s]], writes=[t_dst], partial=True)
            else:
                P.op(eng, lambda e, s=s, c0=c0, w=w: e.tensor_copy(out=dst[:, :, c0:c0 + w], in_=stg[s][:, :, 0:w]), reads=[t_stg[s]], writes=[t_dst], partial=True)
            i += 1

    def phase_inproj(self, li):
        nc, P = self.nc, self.P
        NW = N_IN + 32
        with contextlib.ExitStack() as st:
            wb = self.sb(st, "winb", [128, 8, NW], BF16)
            t_wb = Tok()
            with contextlib.ExitStack() as st2:
                self.load_cast_weight(st2, wb, t_wb, self.w_in[li], N_IN)
                P.op("dve", lambda e: e.tensor_scalar(out=wb[:, :, N_IN:N_IN + 16], in0=wb[:, :, 656:672], scalar1=-1.0, scalar2=None, op0=ALU.mult),
                     reads=[t_wb], writes=[t_wb], partial=True)
                P.op("dve", lambda e: e.tensor_copy(out=wb[:, :, N_IN + 16:N_IN + 32], in_=wb[:, :, 640:656]), reads=[t_wb], writes=[t_wb], partial=True)
                P.barrier()
            NB = 2
            bufs = []
            for i in range(NB):
                bufs.append((self.sb(st, f"xt{i}", [128, D]), Tok(), self.sb(st, f"sq{i}", [128, D], BF16), Tok(),
                             self.sb(st, f"ss{i}", [128, 4]), Tok(), self.sb(st, f"xn{i}", [128, D], BF16), Tok()))
            hT = [self.sb(st, f"hT{i}", [128, 8, 256], BF16) for i in range(2)]
            t_hT = [Tok(), Tok()]
            stage = [self.sb(st, f"stg{i}", [128, 16, 256]) for i in range(2)]
            t_stage = [Tok(), Tok()]
            svo = [self.sb(st, f"svo{i}", [128, 512]) for i in range(2)]
            t_svo = [Tok(), Tok()]
            chunks = [(0, 128), (128, 128), (256, 128), (384, 128), (512, 128), (640, 32), (672, 128), (800, 128), (1440, 16)]
            chunks += [(HY_LO + 128 * i, 128) for i in range(6)] + [(N_IN, 32)]
            groups = [(0, 3), (3, 2), (5, 1), (6, 2), (8, 1), (9, 6), (15, 1)]
            nsub = 0
            for ti in range(TT // 256):
                t0 = ti * 256
                v = 1 if ti == 0 else 0
                hs = ti % 2
                for sub in range(2):
                    self.norm_transpose(bufs[nsub % NB], self.XS[t0 + sub * 128:t0 + (sub + 1) * 128, :], self.t_XS, self.s1,
                                        self.modT[:, 0:8, :], v, hT[hs], t_hT[hs], sub * 128, ps_i=nsub % 2)
                    nsub += 1
                sg = stage[hs]
                for ci, (c0, M) in enumerate(chunks):
                    pi = 2 + ci % 4
                    for k in range(8):
                        P.op("pe", lambda e, pi=pi, k=k, c0=c0, M=M, hs=hs: e.matmul(self.ps[pi][0:M, 0:256], lhsT=wb[:, k, c0:c0 + M], rhs=hT[hs][:, k, :],
                                                                               start=(k == 0), stop=(k == 7)),
                             reads=[t_wb, t_hT[hs]], writes=[self.pst[pi]], skip_self=True)
                    if ci % 2 == 0:
                        P.op("act", lambda e, pi=pi, M=M, ci=ci: e.copy(out=sg[0:M, ci, :], in_=self.ps[pi][0:M, 0:256]), reads=[self.pst[pi]], writes=[t_stage[hs]], partial=True)
                    else:
                        P.op("dve", lambda e, pi=pi, M=M, ci=ci: e.tensor_copy(out=sg[0:M, ci, :], in_=self.ps[pi][0:M, 0:256]), reads=[self.pst[pi]], writes=[t_stage[hs]], partial=True)
                for (g0, gn) in groups:
                    c0, M = chunks[g0]
                    dst = self.PT[c0:c0 + M * gn, t0:t0 + 256]
                    if gn > 1:
                        dst = dst.rearrange("(c p) t -> p c t", p=128)
                        P.dma("pool", dst, sg[:, g0:g0 + gn, :], reads=[t_stage[hs]], writes=[self.t_PT], partial=True)
                    else:
                        P.dma("pool", dst, sg[0:M, g0, :], reads=[t_stage[hs]], writes=[self.t_PT], partial=True)
                for sub in range(2):
                    pi = 6 + sub
                    for k in range(8):
                        P.op("pe", lambda e, pi=pi, k=k, sub=sub, hs=hs: e.matmul(self.ps[pi], lhsT=hT[hs][:, k, sub * 128:(sub + 1) * 128], rhs=wb[:, k, 928:1440],
                                                                               start=(k == 0), stop=(k == 7)),
                             reads=[t_wb, t_hT[hs]], writes=[self.pst[pi]], skip_self=True)
                    P.op("act" if sub == 0 else "dve", (lambda e, pi=pi, sub=sub: e.copy(out=svo[sub], in_=self.ps[pi])) if sub == 0 else
                         (lambda e, pi=pi, sub=sub: e.tensor_copy(out=svo[sub], in_=self.ps[pi])), reads=[self.pst[pi]], writes=[t_svo[sub]])
                    P.dma("pool", self.PVO[t0 + sub * 128:t0 + (sub + 1) * 128, :], svo[sub], reads=[t_svo[sub]], writes=[self.t_PVO], partial=True)
            P.barrier()

    def rms_bcast(self, src, nk, n, ones, t_ones, nfeat, ps_i, R, t_R, sq, t_sq, t_src):
        P = self.P
        P.op("act", lambda e: e.activation(out=sq[:, 0:nk, 0:n], in_=src[:, 0:nk, 0:n], func=AF.Square), reads=[t_src], writes=[t_sq])
        ps, t_ps = self.ps[ps_i], self.pst[ps_i]
        for k in range(nk):
            P.op("pe", lambda e, k=k: e.matmul(ps[:, 0:n], lhsT=ones, rhs=sq[:, k, 0:n], start=(k == 0), stop=(k == nk - 1)),
                 reads=[t_ones, t_sq], writes=[t_ps], skip_self=True)
        P.op("dve", lambda e: e.tensor_scalar(out=R[:, 0:n], in0=ps[:, 0:n], scalar1=1.0 / nfeat, scalar2=EPS, op0=ALU.mult, op1=ALU.add),
             reads=[t_ps], writes=[t_R])
        P.op("act", lambda e: e.activation(out=R[:, 0:n], in_=R[:, 0:n], func=AF.Sqrt), reads=[t_R], writes=[t_R])
        P.op("dve", lambda e: e.reciprocal(out=R[:, 0:n], in_=R[:, 0:n]), reads=[t_R], writes=[t_R])

    def phase_mla(self, li):
        nc, P = self.nc, self.P
        last = li == DEPTH - 1
        scale = float(96 ** -0.5)
        with contextlib.ExitStack() as st:
            sb = lambda name, shape, dt=F32: self.sb(st, name, shape, dt)
            ones = sb("onesb", [128, 128], BF16); t_ones = Tok()
            P.op("pool", lambda e: e.memset(ones, 1.0), writes=[t_ones])
            KT = sb("KT", [128, NH, TT], BF16); t_KT = Tok()
            VP = sb("VP", [128, TT // 128, NH, 65], BF16); t_VP = Tok()
            P.op("pool", lambda e: e.memset(VP, 1.0), writes=[t_VP])
            sel65 = sb("sel65", [128, 64]); t_sel = Tok()
            P.op("pool", lambda e: e.memset(sel65, 0.0), writes=[t_sel])
            P.op("pool", lambda e: e.memset(sel65[64:65, :], 1.0), reads=[t_sel], writes=[t_sel])
            wqb = sb("wqb", [128, 3, NH, 192], BF16); t_wq = Tok()
            wkb = sb("wkb", [128, 2, NH, 64], BF16); t_wk = Tok()
            wvb = sb("wvb", [128, 2, NH, 64], BF16); t_wv = Tok()
            mk = sb("mk", [128, NH]); t_mk = Tok()
            P.op("pool", lambda e: e.memset(mk, 0.0), writes=[t_mk])
            with contextlib.ExitStack() as st2:
                wq = self.sb(st2, "wq32", [128, 3, NH * 96]); t_wq32 = Tok()
                wkv = self.sb(st2, "wkv32", [128, 2, NH * 128]); t_wkv32 = Tok()
                gq, t_gq = self.vec_pc(st2, "gq", self.mla_q_norm_g[li], 3)
                gkv, t_gkv = self.vec_pc(st2, "gkv", self.mla_kv_norm_g[li], 2)
                P.dma("sp", wq, self.mla_w_uq[li].rearrange("(c p) n -> p c n", p=128), writes=[t_wq32])
                P.dma("act", wkv, self.mla_w_ukv[li].rearrange("(c p) n -> p c n", p=128), writes=[t_wkv32])
                for k in range(3):
                    P.op("dve", lambda e, k=k: e.tensor_scalar(out=wq[:, k, :], in0=wq[:, k, :], scalar1=gq[:, k:k + 1], scalar2=None, op0=ALU.mult),
                         reads=[t_gq], writes=[t_wq32])
                for k in range(2):
                    P.op("dve", lambda e, k=k: e.tensor_scalar(out=wkv[:, k, :], in0=wkv[:, k, :], scalar1=gkv[:, k:k + 1], scalar2=None, op0=ALU.mult),
                         reads=[t_gkv], writes=[t_wkv32])
                wq4 = wq.rearrange("p k (h d) -> p k h d", d=96)
                wkv4 = wkv.rearrange("p k (h d) -> p k h d", d=128)
                P.op("pool", lambda e: e.memset(wqb, 0.0), writes=[t_wq])
                for k in range(3):
                    P.op("dve", lambda e, k=k: e.tensor_copy(out=wqb[:, k, :, 0:96], in_=wq4[:, k, :, 0:96]), reads=[t_wq32], writes=[t_wq], partial=True)
                    P.op("dve", lambda e, k=k: e.tensor_scalar(out=wqb[:, k, :, 160:176], in0=wq4[:, k, :, 80:96], scalar1=-1.0, scalar2=None, op0=ALU.mult),
                         reads=[t_wq32], writes=[t_wq], partial=True)
                    P.op("dve", lambda e, k=k: e.tensor_copy(out=wqb[:, k, :, 176:192], in_=wq4[:, k, :, 64:80]), reads=[t_wq32], writes=[t_wq], partial=True)
                for k in range(2):
                    P.op("dve", lambda e, k=k: e.tensor_copy(out=wkb[:, k, :, :], in_=wkv4[:, k, :, 0:64]), reads=[t_wkv32], writes=[t_wk], partial=True)
                    P.op("dve", lambda e, k=k: e.tensor_copy(out=wvb[:, k, :, :], in_=wkv4[:, k, :, 64:128]), reads=[t_wkv32], writes=[t_wv], partial=True)
                P.barrier()
            NBUF = 2
            pin = [sb(f"pin{i}", [128, 3, 512]) for i in range(NBUF)]; t_pin = [Tok() for _ in range(NBUF)]
            cs = [sb(f"cs{i}", [128, 2, 512]) for i in range(NBUF)]; t_cs = [Tok() for _ in range(NBUF)]
            sq = sb("sqm", [128, 3, 512], BF16); t_sq = Tok()
            R = sb("Rm", [128, 512]); t_R = Tok()
            pn = sb("pn", [128, 3, 512], BF16); t_pn = Tok()
            tmpa = sb("tmpa", [128, 512]); t_tmpa = Tok()
            tmpb = sb("tmpb", [128, 512]); t_tmpb = Tok()
            sqk = sb("sqk", [128, NH, 512], BF16); t_sqk = Tok()
            B_singles = (sq, t_sq, R, t_R, pn, t_pn, tmpa, t_tmpa, tmpb, t_tmpb, sqk, t_sqk)
            stA = contextlib.ExitStack()
            krin = [self.sb(stA, f"krin{i}", [128, 2, 512]) for i in range(NBUF)]; t_krin = [Tok() for _ in range(NBUF)]
            tchunks = [(0, CTX)] + [(CTX + 512 * j, 512) for j in range(SEQ // 512)]

            def load_rope_tables(bi, t0, n):
                p0 = t0 - CTX
                P.dma("act", cs[bi][64:96, 0, 0:n], self.c_rope_cos[:, p0:p0 + n], writes=[t_cs[bi]], partial=True)
                P.dma("act", cs[bi][64:96, 1, 0:n], self.c_rope_sin[:, p0:p0 + n], writes=[t_cs[bi]], partial=True)

            A_sq = [self.sb(stA, f"Asq{i}", [128, 3, 512], BF16) for i in range(2)]; tA_sq = [Tok(), Tok()]
            A_R = [self.sb(stA, f"AR{i}", [128, 512]) for i in range(2)]; tA_R = [Tok(), Tok()]
            A_pn = [self.sb(stA, f"Apn{i}", [128, 2, 512], BF16) for i in range(2)]; tA_pn = [Tok(), Tok()]
            A_ta = [self.sb(stA, f"Ata{i}", [128, 512]) for i in range(2)]; tA_ta = [Tok(), Tok()]
            A_tb = [self.sb(stA, f"Atb{i}", [128, 512]) for i in range(2)]; tA_tb = [Tok(), Tok()]
            A_krb = [self.sb(stA, f"Akrb{i}", [128, 512], BF16) for i in range(2)]; tA_krb = [Tok(), Tok()]
            A_sqk = [self.sb(stA, f"Asqk{i}", [128, NH, 512], BF16) for i in range(2)]; tA_sqk = [Tok(), Tok()]
            A_mt = [self.sb(stA, f"Amt{i}", [128, NH]) for i in range(2)]; tA_mt = [Tok(), Tok()]
            for ci, (t0, n) in enumerate(tchunks):
                bi = ci % NBUF
                is_ctx = ci == 0
                sq, t_sq, R, t_R, pn, t_pn = A_sq[bi], tA_sq[bi], A_R[bi], tA_R[bi], A_pn[bi], tA_pn[bi]
                tmpa, t_tmpa, tmpb, t_tmpb, krb, t_krb = A_ta[bi], tA_ta[bi], A_tb[bi], tA_tb[bi], A_krb[bi], tA_krb[bi]
                sqk, t_sqk, mtmp, t_mtmp = A_sqk[bi], tA_sqk[bi], A_mt[bi], tA_mt[bi]
                P.dma("sp", pin[bi][:, 0:2, 0:n], self.PT[QR:QR + KVR, t0:t0 + n].rearrange("(c p) t -> p c t", p=128), reads=[self.t_PT], writes=[t_pin[bi]])
                P.dma("sp", krin[bi][64:96, 0, 0:n], self.PT[640:672, t0:t0 + n], reads=[self.t_PT], writes=[t_krin[bi]], partial=True)
                if not is_ctx:
                    P.dma("sp", krin[bi][64:96, 1, 0:n], self.PT[N_IN:N_IN + 32, t0:t0 + n], reads=[self.t_PT], writes=[t_krin[bi]], partial=True)
                    load_rope_tables(bi, t0, n)
                self.rms_bcast(pin[bi], 2, n, ones, t_ones, KVR, 6, R, t_R, sq, t_sq, t_pin[bi])
                P.op("dve", lambda e, bi=bi, n=n: e.tensor_tensor(out=pn[:, 0:2, 0:n], in0=pin[bi][:, 0:2, 0:n], in1=R[:, 0:n].unsqueeze(1).to_broadcast([128, 2, n]), op=ALU.mult),
                     reads=[t_pin[bi], t_R], writes=[t_pn])
                if is_ctx:
                    P.op("dve", lambda e, bi=bi, n=n: e.tensor_copy(out=krb[64:96, 0:n], in_=krin[bi][64:96, 0, 0:n]), reads=[t_krin[bi]], writes=[t_krb])
                else:
                    P.op("dve", lambda e, bi=bi, n=n: e.tensor_tensor(out=tmpa[64:96, 0:n], in0=krin[bi][64:96, 0, 0:n], in1=cs[bi][64:96, 0, 0:n], op=ALU.mult), reads=[t_krin[bi], t_cs[bi]], writes=[t_tmpa])
                    P.op("dve", lambda e, bi=bi, n=n: e.tensor_tensor(out=tmpb[64:96, 0:n], in0=krin[bi][64:96, 1, 0:n], in1=cs[bi][64:96, 1, 0:n], op=ALU.mult), reads=[t_krin[bi], t_cs[bi]], writes=[t_tmpb])
                    P.op("dve", lambda e, n=n: e.tensor_tensor(out=krb[64:96, 0:n], in0=tmpa[64:96, 0:n], in1=tmpb[64:96, 0:n], op=ALU.add), reads=[t_tmpa, t_tmpb], writes=[t_krb])
                P.op("pool", lambda e, t0=t0, n=n: e.tensor_copy(out=KT[64:96, :, t0:t0 + n], in_=krb[64:96, 0:n].unsqueeze(1).to_broadcast([32, NH, n])), reads=[t_krb], writes=[t_KT], partial=True)
                for h in range(NH):
                    pi = 4 + h % 2
                    for k in range(2):
                        P.op("pe", lambda e, h=h, k=k, pi=pi, n=n: e.matmul(self.ps[pi][0:64, 0:n], lhsT=wkb[:, k, h, :], rhs=pn[:, k, 0:n], start=(k == 0), stop=(k == 1)),
                             reads=[t_wk, t_pn], writes=[self.pst[pi]], skip_self=True)
                    if h % 2 == 0:
                        P.op("act", lambda e, h=h, pi=pi, t0=t0, n=n: e.copy(out=KT[0:64, h, t0:t0 + n], in_=self.ps[pi][0:64, 0:n]), reads=[self.pst[pi]], writes=[t_KT], partial=True)
                    else:
                        P.op("dve", lambda e, h=h, pi=pi, t0=t0, n=n: e.tensor_copy(out=KT[0:64, h, t0:t0 + n], in_=self.ps[pi][0:64, 0:n]), reads=[self.pst[pi]], writes=[t_KT], partial=True)
                for sub in range(n // 128):
                    j = (t0 + sub * 128) // 128
                    pi = 2 + sub % 2
                    for k in range(2):
                        P.op("pe", lambda e, k=k, pi=pi, sub=sub: e.matmul(self.ps[pi], lhsT=pn[:, k, sub * 128:(sub + 1) * 128], rhs=wvb[:, k, :, :].rearrange("p h d -> p (h d)"), start=(k == 0), stop=(k == 1)),
                             reads=[t_wv, t_pn], writes=[self.pst[pi]], skip_self=True)
                    src = self.ps[pi].rearrange("p (h d) -> p h d", d=64)
                    if sub % 2 == 0:
                        P.op("act", lambda e, j=j, src=src: e.copy(out=VP[:, j, :, 0:64], in_=src), reads=[self.pst[pi]], writes=[t_VP], partial=True)
                    else:
                        P.op("dve", lambda e, j=j, src=src: e.tensor_copy(out=VP[:, j, :, 0:64], in_=src), reads=[self.pst[pi]], writes=[t_VP], partial=True)
                P.op("act", lambda e, t0=t0, n=n: e.activation(out=sqk[0:96, :, 0:n], in_=KT[0:96, :, t0:t0 + n], func=AF.Square), reads=[t_KT], writes=[t_sqk])
                for h in range(NH):
                    pi = 7
                    P.op("pe", lambda e, h=h, n=n: e.matmul(self.ps[7][:, 0:n], lhsT=ones[0:96, :], rhs=sqk[0:96, h, 0:n], start=True, stop=True),
                         reads=[t_ones, t_sqk], writes=[self.pst[7]], skip_self=True)
                    P.op("dve", lambda e, h=h, n=n: e.tensor_reduce(out=mtmp[:, h:h + 1], in_=self.ps[7][:, 0:n], axis=AX.X, op=ALU.max), reads=[self.pst[7]], writes=[t_mtmp], partial=True)
                P.op("dve", lambda e: e.tensor_tensor(out=mk, in0=mk, in1=mtmp, op=ALU.max), reads=[t_mtmp], writes=[t_mk])
            P.barrier()
            stA.close()
            sq, t_sq, R, t_R, pn, t_pn, tmpa, t_tmpa, tmpb, t_tmpb, sqk, t_sqk = B_singles
            QT = [sb(f"QT{i}", [128, NH, 512], BF16) for i in range(2)]; t_QT = [Tok(), Tok()]
            pts = [sb(f"pts{i}", [128, 2, 512], BF16) for i in range(3)]; t_pts = [Tok() for _ in range(3)]
            den = sb("den", [128, 512]); t_den = Tok()
            rden = sb("rden", [128, 512]); t_rden = Tok()
            ot = [sb(f"ot{i}", [128, 512], BF16) for i in range(2)]; t_ot = [Tok(), Tok()]
            mq = [sb(f"mq{i}", [128, NH]) for i in range(2)]; t_mq = [Tok(), Tok()]
            negm = [sb(f"negm{i}", [128, NH]) for i in range(2)]; t_negm = [Tok(), Tok()]
            qchunks = ([] if last else [(0, CTX)]) + tchunks[1:]
            nq = len(qchunks)

            def prologue_parts(qi):
                t0, n = qchunks[qi]
                bi = qi % NBUF
                is_ctx = t0 == 0
                qt, t_qt = QT[qi % 2], t_QT[qi % 2]
                parts = {}

                def part_load():
                    P.dma("sp", pin[bi][:, 0:3, 0:n], self.PT[0:QR, t0:t0 + n].rearrange("(c p) t -> p c t", p=128), reads=[self.t_PT], writes=[t_pin[bi]])
                    if not is_ctx:
                        load_rope_tables(bi, t0, n)
                    self.rms_bcast(pin[bi], 3, n, ones, t_ones, QR, 5, R, t_R, sq, t_sq, t_pin[bi])
                    P.op("dve", lambda e: e.tensor_tensor(out=pn[:, 0:3, 0:n], in0=pin[bi][:, 0:3, 0:n], in1=R[:, 0:n].unsqueeze(1).to_broadcast([128, 3, n]), op=ALU.mult),
                         reads=[t_pin[bi], t_R], writes=[t_pn])
                parts["load"] = part_load

                def part_A(h):
                    for k in range(3):
                        P.op("pe", lambda e, k=k: e.matmul(self.ps[7][0:96, 0:n], lhsT=wqb[:, k, h, 0:96], rhs=pn[:, k, 0:n], start=(k == 0), stop=(k == 2)),
                             reads=[t_wq, t_pn], writes=[self.pst[7]], skip_self=True)
                    if is_ctx:
                        P.op("dve", lambda e: e.tensor_copy(out=qt[0:96, h, 0:n], in_=self.ps[7][0:96, 0:n]), reads=[self.pst[7]], writes=[t_qt], partial=True)
                        return
                    P.op("dve", lambda e: e.tensor_tensor(out=tmpa[64:96, 0:n], in0=self.ps[7][64:96, 0:n], in1=cs[bi][64:96, 0, 0:n], op=ALU.mult), reads=[self.pst[7], t_cs[bi]], writes=[t_tmpa])
                    P.op("dve", lambda e: e.tensor_copy(out=qt[0:64, h, 0:n], in_=self.ps[7][0:64, 0:n]), reads=[self.pst[7]], writes=[t_qt], partial=True)

                def part_B(h):
                    if is_ctx:
                        return
                    for k in range(3):
                        P.op("pe", lambda e, k=k: e.matmul(self.ps[6][0:96, 0:n], lhsT=wqb[:, k, h, 96:192], rhs=pn[:, k, 0:n], start=(k == 0), stop=(k == 2)),
                             reads=[t_wq, t_pn], writes=[self.pst[6]], skip_self=True)
                    P.op("dve", lambda e: e.tensor_tensor(out=tmpb[64:96, 0:n], in0=self.ps[6][64:96, 0:n], in1=cs[bi][64:96, 1, 0:n], op=ALU.mult), reads=[self.pst[6], t_cs[bi]], writes=[t_tmpb])
                    P.op("dve", lambda e: e.tensor_tensor(out=qt[64:96, h, 0:n], in0=tmpa[64:96, 0:n], in1=tmpb[64:96, 0:n], op=ALU.add), reads=[t_tmpa, t_tmpb], writes=[t_qt], partial=True)

                def part_N(h):
                    P.op("dve", lambda e: e.tensor_tensor(out=sqk[0:96, h, 0:n], in0=qt[0:96, h, 0:n], in1=qt[0:96, h, 0:n], op=ALU.mult), reads=[t_qt], writes=[t_sqk], partial=True)
                    P.op("pe", lambda e: e.matmul(self.ps[7][:, 0:n], lhsT=ones[0:96, :], rhs=sqk[0:96, h, 0:n], start=True, stop=True),
                         reads=[t_ones, t_sqk], writes=[self.pst[7]], skip_self=True)
                    P.op("dve", lambda e: e.tensor_reduce(out=mq[qi % 2][:, h:h + 1], in_=self.ps[7][:, 0:n], axis=AX.X, op=ALU.max), reads=[self.pst[7]], writes=[t_mq[qi % 2]], partial=True)

                def part_fin():
                    nm, t_nm = negm[qi % 2], t_negm[qi % 2]
                    P.op("dve", lambda e: e.tensor_tensor(out=nm, in0=mq[qi % 2], in1=mk, op=ALU.mult), reads=[t_mq[qi % 2], t_mk], writes=[t_nm])
                    P.op("act", lambda e: e.activation(out=nm, in_=nm, func=AF.Sqrt), reads=[t_nm], writes=[t_nm])
                    P.op("dve", lambda e: e.tensor_scalar(out=nm, in0=nm, scalar1=-scale, scalar2=None, op0=ALU.mult), reads=[t_nm], writes=[t_nm])
                for h in range(NH):
                    parts[("A", h)] = (lambda h=h: part_A(h))
                    parts[("B", h)] = (lambda h=h: part_B(h))
                    parts[("N", h)] = (lambda h=h: part_N(h))
                parts["fin"] = part_fin
                return parts

            def emit_all(parts):
                parts["load"]()
                for h in range(NH):
                    parts[("A", h)](); parts[("B", h)](); parts[("N", h)]()
                parts["fin"]()

            def groups_of(qi):
                t0, n = qchunks[qi]
                ktiles = list(range(CTX // 128)) if t0 == 0 else list(range(TT // 128))
                return [ktiles[i:i + 2] for i in range(0, len(ktiles), 2)]

            def s_mm(qi, h, g):
                t0, n = qchunks[qi]
                qt, t_qt = QT[qi % 2], t_QT[qi % 2]
                b0 = 2 * (g % 2)
                for i, j in enumerate(groups_of(qi)[g]):
                    P.op("pe", lambda e, j=j, i=i: e.matmul(self.ps[b0 + i][:, 0:n], lhsT=KT[0:96, h, j * 128:(j + 1) * 128], rhs=qt[0:96, h, 0:n], start=True, stop=True),
                         reads=[t_KT, t_qt], writes=[self.pst[b0 + i]], skip_self=True)

            emit_all(prologue_parts(0))
            jobs = [(qi, h) for qi in range(nq) for h in range(NH)]
            pcount = 0
            pre_issued = set()
            nxt_parts = None
            for ji, (qi, h) in enumerate(jobs):
                t0, n = qchunks[qi]
                groups = groups_of(qi)
                ng = len(groups)
                po = 4
                nm, t_nm = negm[qi % 2], t_negm[qi % 2]
                if h == 0:
                    nxt_parts = prologue_parts(qi + 1) if qi + 1 < nq else None
                sched = {}
                if nxt_parts is not None and ng >= 16:
                    if h == 0:
                        sched[2] = ["load"]
                    else:
                        sched[3] = [("A", h - 1)]
                        sched[7] = [("B", h - 1)]
                        sched[11] = [("N", h - 1)]
                    if h == NH - 1:
                        sched[12] = [("A", h)]
                        sched[14] = [("B", h)]
                        sched[16] = [("N", h), "fin"]
                if (qi, h) not in pre_issued:
                    s_mm(qi, h, 0)
                    if ng > 1:
                        s_mm(qi, h, 1)
                for g in range(ng):
                    b0 = 2 * (g % 2)
                    nj = len(groups[g])
                    pb = pcount % 3
                    pcount += 1
                    src = self.psall[:, b0 * 512:(b0 + nj) * 512].rearrange("p (a c) -> p a c", c=512)[:, :, 0:n]
                    P.op("act", lambda e, pb=pb, nj=nj, src=src: e.activation(out=pts[pb][:, 0:nj, 0:n], in_=src, func=AF.Exp, bias=nm[:, h:h + 1], scale=scale),
                         reads=[self.pst[b0 + i] for i in range(nj)] + [t_nm], writes=[t_pts[pb]])
                    for i, j in enumerate(groups[g]):
                        first = (g == 0 and i == 0)
                        lastk = (g == ng - 1 and i == nj - 1)
                        P.op("pe", lambda e, pb=pb, j=j, i=i, first=first, lastk=lastk: e.matmul(self.ps[po][0:65, 0:n], lhsT=VP[:, j, h, :], rhs=pts[pb][:, i, 0:n], start=first, stop=lastk),
                             reads=[t_VP, t_pts[pb]], writes=[self.pst[po]], skip_self=True)
                    if g + 2 < ng:
                        s_mm(qi, h, g + 2)
                    for key in sched.get(g, []):
                        nxt_parts[key]()
                if ji + 1 < len(jobs):
                    qn, hn = jobs[ji + 1]
                    if qn == qi:
                        s_mm(qn, hn, 0)
                        if len(groups_of(qn)) > 1:
                            s_mm(qn, hn, 1)
                        pre_issued.add((qn, hn))
                o = ot[h % 2]
                t_o = t_ot[h % 2]
                P.op("act", lambda e: e.copy(out=den[0:65, 0:n], in_=self.ps[po][0:65, 0:n]), reads=[self.pst[po]], writes=[t_den])
                P.op("pe", lambda e: e.matmul(self.ps[5][0:64, 0:n], lhsT=sel65[0:65, :], rhs=den[0:65, 0:n], start=True, stop=True),
                     reads=[t_sel, t_den], writes=[self.pst[5]], skip_self=True)
                P.op("dve", lambda e: e.reciprocal(out=rden[0:64, 0:n], in_=self.ps[5][0:64, 0:n]), reads=[self.pst[5]], writes=[t_rden])
                P.op("dve", lambda e, o=o: e.tensor_tensor(out=o[0:64, 0:n], in0=den[0:64, 0:n], in1=rden[0:64, 0:n], op=ALU.mult),
                     reads=[t_den, t_rden], writes=[t_o])
                P.dma("pool", self.MIXT[h * 64:(h + 1) * 64, t0:t0 + n], o[0:64, 0:n], reads=[t_o], writes=[self.t_MIXT], partial=True)
                if nxt_parts is not None and ng < 16 and h == NH - 1:
                    emit_all(nxt_parts)
            P.barrier()

    def phase_mlstm(self, li):
        nc, P = self.nc, self.P
        NT = TT // 128
        with contextlib.ExitStack() as st:
            sb = lambda name, shape, dt=F32: self.sb(st, name, shape, dt)
            ub = [sb(f"ub{p}", [128, TT], BF16) for p in range(2)]; t_ub = [Tok(), Tok()]
            kT = [sb(f"kT{p}", [128, TT], BF16) for p in range(2)]; t_kT = [Tok(), Tok()]
            qd = [[sb(f"qd{p}{d}", [128, TT], BF16) for d in range(2)] for p in range(2)]
            t_qd = [[Tok(), Tok()], [Tok(), Tok()]]
            ktok = sb("ktok", [128, NT, 256], BF16); t_ktok = Tok()
            wtok = sb("wtok", [128, NT, 8]); t_wtok = Tok()
            ecol = sb("ecol", [128, 4, NT]); t_ecol = Tok()
            hF = sb("hF", [128, NT, 256]); t_hF = Tok()
            CbS = [[sb(f"CbS{p}{d}", [128, NT + 1, 65], BF16) for d in range(2)] for p in range(2)]
            t_CbS = [[Tok(), Tok()], [Tok(), Tok()]]
            Cst = [[sb(f"Cst{p}{d}", [128, 65]) for d in range(2)] for p in range(2)]
            t_Cst = [[Tok(), Tok()], [Tok(), Tok()]]
            masks = sb("mlmask", [128, 2, 128], BF16); t_masks = Tok()
            P.dma("sp", masks, self.c_ml_mask, writes=[t_masks])
            wqb = sb("mlwq", [128, 2, 128], BF16); wkb = sb("mlwk", [128, 2, 128], BF16); t_w = Tok()
            gml, t_gml = self.vec_pc(st, "gml", self.ml_norm_g[li], 2)
            cw = sb("mlcw", [128, 2, 3]); cb = sb("mlcb", [128, 2]); t_cw = Tok()
            for jj in range(3):
                P.dma("sp", cw[:, :, jj], self.ml_conv_w[li, jj].rearrange("(c p) -> p c", p=128), writes=[t_cw], partial=True, allow_slow_non_contiguous=True)
            P.dma("sp", cb, self.ml_conv_b[li].rearrange("(c p) -> p c", p=128), writes=[t_cw], partial=True, allow_slow_non_contiguous=True)
            for p in range(2):
                for d in range(2):
                    P.op("pool", lambda e, p=p, d=d: e.memset(Cst[p][d], 0.0), writes=[t_Cst[p][d]])
                    P.op("pool", lambda e, p=p, d=d: e.memset(CbS[p][d][:, 0, :], 0.0), writes=[t_CbS[p][d]])
            with contextlib.ExitStack() as st2:
                sb2 = lambda name, shape, dt=F32: self.sb(st2, name, shape, dt)
                w32 = sb2("mlw32", [128, 4, 128]); t_w32 = Tok()
                P.op("pool", lambda e: e.memset(w32, 0.0), writes=[t_w32])
                for p in range(2):
                    for hh in range(2):
                        P.dma("sp", w32[hh * 64:(hh + 1) * 64, p, hh * 64:(hh + 1) * 64], self.ml_wq[li, 2 * p + hh], reads=[], writes=[t_w32], partial=True)
                        P.dma("sp", w32[hh * 64:(hh + 1) * 64, 2 + p, hh * 64:(hh + 1) * 64], self.ml_wk[li, 2 * p + hh], reads=[], writes=[t_w32], partial=True)
                P.op("dve", lambda e: e.tensor_copy(out=wqb, in_=w32[:, 0:2, :]), reads=[t_w32], writes=[t_w], partial=True)
                P.op("dve", lambda e: e.tensor_scalar(out=wkb, in0=w32[:, 2:4, :], scalar1=0.125, scalar2=None, op0=ALU.mult), reads=[t_w32], writes=[t_w], partial=True)
                selA = sb2("selA", [128, 4, 128]); selW = sb2("selW", [128, 8]); t_sel = Tok()
                P.dma("act", selA[0:96], self.c_ml_selA, writes=[t_sel], partial=True)
                P.dma("act", selW[0:96], self.c_ml_selW, writes=[t_sel], partial=True)
                gb = sb2("mlgb", [16, 1]); t_gb = Tok()
                P.dma("sp", gb, self.ml_gate_b[li].rearrange("(p o) -> p o", o=1), writes=[t_gb], allow_slow_non_contiguous=True)
                ones16 = sb2("ones16", [16, 128]); t_o16 = Tok()
                P.op("pool", lambda e: e.memset(ones16, 1.0), writes=[t_o16])
                X = sb2("mlX", [128, TT]); t_X = Tok()
                P.op("pool", lambda e: e.memset(X, 0.0), writes=[t_X])
                P.dma("sp", X[0:16, :], self.PT[1440:1456, :], reads=[self.t_PT], writes=[t_X], partial=True)
                P.op("dve", lambda e: e.tensor_scalar(out=X[0:16, :], in0=X[0:16, :], scalar1=gb[:, 0:1], scalar2=None, op0=ALU.add), reads=[t_gb, t_X], writes=[t_X])
                with contextlib.ExitStack() as st3:
                    LF = self.sb(st3, "mlLF", [16, TT]); t_LF = Tok()
                    CF = self.sb(st3, "mlCF", [16, TT]); t_CF = Tok()
                    P.op("act", lambda e: e.activation(out=LF, in_=X[0:16, :], func=AF.Exp, scale=-1.0), reads=[t_X], writes=[t_LF])
                    P.op("act", lambda e: e.activation(out=LF, in_=LF, func=AF.Ln, bias=1.0), reads=[t_LF], writes=[t_LF])
                    P.op("dve", lambda e: e.tensor_scalar(out=LF, in0=LF, scalar1=-1.0, scalar2=None, op0=ALU.mult), reads=[t_LF], writes=[t_LF])
                    for j in range(NT):
                        P.op("dve", lambda e, j=j: e.tensor_tensor_scan(out=CF[:, j * 128:(j + 1) * 128], data0=ones16, data1=LF[:, j * 128:(j + 1) * 128],
                                                                        initial=0.0, op0=ALU.mult, op1=ALU.add), reads=[t_LF, t_o16], writes=[t_CF], partial=True)
                    P.op("dve", lambda e: e.tensor_tensor(out=LF, in0=LF, in1=CF, op=ALU.subtract), reads=[t_CF], writes=[t_LF])
                    LF3 = LF.rearrange("p (j t) -> p j t", t=128)
                    CF3 = CF.rearrange("p (j t) -> p j t", t=128)
                    P.op("dve", lambda e: e.tensor_tensor(out=LF3, in0=LF3, in1=CF3[:, :, 127:128].to_broadcast([16, NT, 128]), op=ALU.add), reads=[t_CF], writes=[t_LF])
                    P.op("act", lambda e: e.copy(out=X[32:48, :], in_=CF), reads=[t_CF], writes=[t_X], partial=True)
                    P.op("act", lambda e: e.copy(out=X[64:80, :], in_=LF), reads=[t_LF], writes=[t_X], partial=True)
                    P.barrier()
                for j in range(NT):
                    P.op("pe", lambda e, j=j: e.matmul(self.ps[7][:, j * 8:(j + 1) * 8], lhsT=X[0:96, j * 128:(j + 1) * 128], rhs=selW[0:96, :], start=True, stop=True),
                         reads=[t_X, t_sel], writes=[self.pst[7]], skip_self=True)
                P.op("act", lambda e: e.activation(out=wtok.rearrange("p j g -> p (j g)"), in_=self.ps[7][:, 0:NT * 8], func=AF.Exp), reads=[self.pst[7]], writes=[t_wtok])
                pc32 = sb2("mlpc", [128, TT]); t_pc = Tok()
                u32 = sb2("mlu", [128, TT]); t_u = Tok()
                abc = [sb2(f"abc{i}", [128, 512]) for i in range(2)]; t_abc = [Tok(), Tok()]
                blocks = [(0, 256)] + [(256 + 512 * i, 512) for i in range(8)]
                segs = [(0, CTX), (CTX, TT)]
                nabc = 0
                for p in range(2):
                    P.dma("sp", pc32, self.PT[ML_LO + p * 128:ML_LO + (p + 1) * 128, :], reads=[self.t_PT], writes=[t_pc])
                    P.op("dve", lambda e, p=p: e.tensor_scalar(out=u32, in0=pc32, scalar1=cw[:, p, 1:2], scalar2=cb[:, p:p + 1], op0=ALU.mult, op1=ALU.add), reads=[t_pc, t_cw], writes=[t_u])
                    for (a, b) in segs:
                        P.op("dve", lambda e, p=p, a=a, b=b: e.scalar_tensor_tensor(out=u32[:, a + 1:b], in0=pc32[:, a:b - 1], scalar=cw[:, p, 0:1], in1=u32[:, a + 1:b], op0=ALU.mult, op1=ALU.add),
                             reads=[t_pc, t_cw], writes=[t_u])
                        P.op("dve", lambda e, p=p, a=a, b=b: e.scalar_tensor_tensor(out=u32[:, a:b - 1], in0=pc32[:, a + 1:b], scalar=cw[:, p, 2:3], in1=u32[:, a:b - 1], op0=ALU.mult, op1=ALU.add),
                             reads=[t_pc, t_cw], writes=[t_u])
                    P.op("act", lambda e, p=p: e.activation(out=ub[p], in_=u32, func=AF.Silu), reads=[t_u], writes=[t_ub[p]])
                    for (b0, n) in blocks:
                        P.op("pe", lambda e, p=p, b0=b0, n=n: e.matmul(self.ps[0][:, 0:n], lhsT=wqb[:, p, :], rhs=ub[p][:, b0:b0 + n], start=True, stop=True),
                             reads=[t_w, t_ub[p]], writes=[self.pst[0]], skip_self=True)
                        for d in range(2):
                            ai = nabc % 2
                            nabc += 1
                            P.op("pe", lambda e, p=p, d=d, b0=b0, n=n: e.matmul(self.ps[6][:, 0:n], lhsT=selA[0:96, p * 2 + d, :], rhs=X[0:96, b0:b0 + n], start=True, stop=True),
                                 reads=[t_sel, t_X], writes=[self.pst[6]], skip_self=True)
                            P.op("act", lambda e, ai=ai, n=n: e.activation(out=abc[ai][:, 0:n], in_=self.ps[6][:, 0:n], func=AF.Exp), reads=[self.pst[6]], writes=[t_abc[ai]])
                            P.op("dve", lambda e, p=p, d=d, ai=ai, b0=b0, n=n: e.tensor_tensor(out=qd[p][d][:, b0:b0 + n], in0=self.ps[0][:, 0:n], in1=abc[ai][:, 0:n], op=ALU.mult),
                                 reads=[self.pst[0], t_abc[ai]], writes=[t_qd[p][d]], partial=True)
                            c0 = 127 if d == 0 else 0
                            P.op("pool", lambda e, p=p, d=d, ai=ai, b0=b0, n=n, c0=c0: e.tensor_copy(out=ecol[:, p * 2 + d, b0 // 128:(b0 + n) // 128], in_=abc[ai][:, c0:n:128]),
                                 reads=[t_abc[ai]], writes=[t_ecol], partial=True)
                        P.op("pe", lambda e, p=p, b0=b0, n=n: e.matmul(self.ps[1][:, 0:n], lhsT=wkb[:, p, :], rhs=ub[p][:, b0:b0 + n], start=True, stop=True),
                             reads=[t_w, t_ub[p]], writes=[self.pst[1]], skip_self=True)
                        P.op("act", lambda e, p=p, b0=b0, n=n: e.copy(out=kT[p][:, b0:b0 + n], in_=self.ps[1][:, 0:n]), reads=[self.pst[1]], writes=[t_kT[p]], partial=True)
                    for j in range(NT):
                        pi = 2 + j % 2
                        P.op("pe", lambda e, p=p, j=j, pi=pi: e.matmul(self.ps[pi][:, 0:128], lhsT=ub[p][:, j * 128:(j + 1) * 128], rhs=wkb[:, p, :], start=True, stop=True),
                             reads=[t_w, t_ub[p]], writes=[self.pst[pi]], skip_self=True)
                        if j % 2 == 0:
                            P.op("act", lambda e, p=p, j=j, pi=pi: e.copy(out=ktok[:, j, p * 128:(p + 1) * 128], in_=self.ps[pi][:, 0:128]), reads=[self.pst[pi]], writes=[t_ktok], partial=True)
                        else:
                            P.op("dve", lambda e, p=p, j=j, pi=pi: e.tensor_copy(out=ktok[:, j, p * 128:(p + 1) * 128], in_=self.ps[pi][:, 0:128]), reads=[self.pst[pi]], writes=[t_ktok], partial=True)
                P.barrier()
            NB = 4
            hB = sb("hB", [128, NT, 256]); t_hB = Tok()
            vo = [sb(f"mlvo{i}", [128, 512]) for i in range(NB)]; t_vo = [Tok() for _ in range(NB)]
            vw = [sb(f"mlvw{i}", [128, 4, 65], BF16) for i in range(NB)]; t_vw = [Tok() for _ in range(NB)]
            Ssb = [sb(f"mlS{i}", [128, 4, 128], BF16) for i in range(NB)]; t_S = [Tok() for _ in range(NB)]
            tmpC = [sb(f"mltmpC{d}", [128, 65]) for d in range(2)]; t_tmpC = [Tok(), Tok()]
            dd = [sb(f"mldd{d}", [128, 4]) for d in range(2)]; t_dd = [Tok(), Tok()]
            orders = [list(range(NT)), [1, 0] + list(range(NT - 1, 1, -1))]
            step = 0
            for si in range(NT):
                for d in range(2):
                    j = orders[d][si]
                    bi = step % NB
                    step += 1
                    c0, c1 = j * 128, (j + 1) * 128
                    P.dma("sp", vo[bi][:, 0:256], self.PVO[c0:c1, 0:256], reads=[self.t_PVO], writes=[t_vo[bi]])
                    P.op("dve", lambda e, bi=bi, j=j, d=d: e.tensor_tensor(out=vw[bi][:, :, 0:64], in0=vo[bi][:, 0:256].rearrange("p (h c) -> p h c", c=64),
                                                                   in1=wtok[:, j, d * 4:(d + 1) * 4].unsqueeze(2).to_broadcast([128, 4, 64]), op=ALU.mult),
                         reads=[t_vo[bi], t_wtok], writes=[t_vw[bi]])
                    P.op("act", lambda e, bi=bi, j=j, d=d: e.copy(out=vw[bi][:, :, 64], in_=wtok[:, j, d * 4:(d + 1) * 4]), reads=[t_wtok], writes=[t_vw[bi]], partial=True)
                    for h in range(4):
                        p, r0 = h // 2, (h % 2) * 64
                        pS = 2 + h % 2
                        P.op("pe", lambda e, h=h, p=p, r0=r0, d=d, c0=c0, c1=c1, pS=pS: e.matmul(self.ps[pS][:, (h // 2) * 128:(h // 2 + 1) * 128], lhsT=kT[p][r0:r0 + 64, c0:c1], rhs=qd[p][d][r0:r0 + 64, c0:c1], start=True, stop=True),
                             reads=[t_kT[p], t_qd[p][d]], writes=[self.pst[pS]], skip_self=True)
                    for eo in range(2):
                        P.op("dve", lambda e, bi=bi, eo=eo, d=d: e.tensor_tensor(out=Ssb[bi][:, eo::2, :], in0=self.ps[2 + eo][:, 0:256].rearrange("p (h t) -> p h t", t=128),
                                                                         in1=masks[:, d, :].unsqueeze(1).to_broadcast([128, 2, 128]), op=ALU.mult),
                             reads=[self.pst[2 + eo], t_masks], writes=[t_S[bi]], partial=(eo > 0))
                    for h in range(4):
                        p, r0 = h // 2, (h % 2) * 64
                        pN = 4 + h % 2
                        cN = (h // 2) * 65
                        P.op("pe", lambda e, h=h, bi=bi, pN=pN, cN=cN: e.matmul(self.ps[pN][:, cN:cN + 65], lhsT=Ssb[bi][:, h, :], rhs=vw[bi][:, h, :], start=True, stop=False),
                             reads=[t_S[bi], t_vw[bi]], writes=[self.pst[pN]], skip_self=True)
                        P.op("pe", lambda e, h=h, p=p, r0=r0, d=d, si=si, c0=c0, c1=c1, pN=pN, cN=cN: e.matmul(self.ps[pN][:, cN:cN + 65], lhsT=qd[p][d][r0:r0 + 64, c0:c1], rhs=CbS[p][d][r0:r0 + 64, si, :], start=False, stop=True),
                             reads=[t_qd[p][d], t_CbS[p][d]], writes=[self.pst[pN]], skip_self=True)
                    for p in range(2):
                        pU = p + 6 * d
                        P.op("pe", lambda e, p=p, bi=bi, j=j, pU=pU: e.matmul(self.ps[pU][:, 0:130], lhsT=ktok[:, j, p * 128:(p + 1) * 128], rhs=vw[bi][:, 2 * p:2 * p + 2, :].rearrange("p a c -> p (a c)"), start=True, stop=True),
                             reads=[t_ktok, t_vw[bi]], writes=[self.pst[pU]], skip_self=True)
                        P.op("dve", lambda e, p=p, d=d, pU=pU: e.tensor_tensor(out=tmpC[d][0:64, :], in0=self.ps[pU][0:64, 0:65], in1=Cst[p][d][0:64, :], op=ALU.add), reads=[self.pst[pU], t_Cst[p][d]], writes=[t_tmpC[d]], partial=True)
                        P.op("dve", lambda e, p=p, d=d, pU=pU: e.tensor_tensor(out=tmpC[d][64:128, :], in0=self.ps[pU][64:128, 65:130], in1=Cst[p][d][64:128, :], op=ALU.add), reads=[self.pst[pU], t_Cst[p][d]], writes=[t_tmpC[d]], partial=True)
                        P.op("dve", lambda e, p=p, d=d, j=j: e.tensor_scalar(out=Cst[p][d], in0=tmpC[d], scalar1=ecol[:, p * 2 + d, j:j + 1], scalar2=None, op0=ALU.mult), reads=[t_tmpC[d], t_ecol], writes=[t_Cst[p][d]])
                        P.op("act", lambda e, p=p, d=d, si=si: e.copy(out=CbS[p][d][:, si + 1, :], in_=Cst[p][d]), reads=[t_Cst[p][d]], writes=[t_CbS[p][d]], partial=True)
                    for eo in range(2):
                        num = self.ps[4 + eo][:, 0:130].rearrange("p (h c) -> p h c", c=65)
                        dde = dd[d][:, 2 * eo:2 * eo + 2]
                        P.op("dve", lambda e, num=num, dde=dde: e.tensor_scalar(out=dde, in0=num[:, :, 64], scalar1=-1.0, scalar2=None, op0=ALU.mult), reads=[self.pst[4 + eo]], writes=[t_dd[d]], partial=(eo > 0))
                        P.op("dve", lambda e, num=num, dde=dde: e.scalar_tensor_tensor(out=dde, in0=num[:, :, 64], scalar=1.0, in1=dde, op0=ALU.max, op1=ALU.max), reads=[self.pst[4 + eo]], writes=[t_dd[d]], partial=True)
                        P.op("dve", lambda e, dde=dde: e.reciprocal(out=dde, in_=dde), reads=[t_dd[d]], writes=[t_dd[d]], partial=True)
                        dst = (hF if d == 0 else hB)[:, j, :].rearrange("p (h c) -> p h c", c=64)[:, eo::2, :]
                        P.op("dve", lambda e, num=num, dde=dde, dst=dst: e.tensor_tensor(out=dst, in0=num[:, :, 0:64], in1=dde.unsqueeze(2).to_broadcast([128, 2, 64]), op=ALU.mult),
                             reads=[self.pst[4 + eo], t_dd[d]], writes=[t_hF if d == 0 else t_hB], partial=True)
            hbs = [sb(f"mlhb{i}", [128, 256]) for i in range(2)]; t_hbs = [Tok(), Tok()]
            sgs = [sb(f"mlsg{i}", [128, 256]) for i in range(2)]; t_sgs = [Tok(), Tok()]
            hsq = sb("mlhsq", [128, 256]); t_hsq = Tok()
            ssn = sb("mlssn", [128, 8]); t_ssn = Tok()
            hn = [sb(f"mlhn{i}", [128, 256], BF16) for i in range(2)]; t_hn = [Tok(), Tok()]
            oT = [sb(f"mloT{i}", [128, 2, 128], BF16) for i in range(2)]; t_oT = [Tok(), Tok()]
            for j in range(NT):
                bi = j % 2
                c0, c1 = j * 128, (j + 1) * 128
                hb, t_hb, sg, t_sg = hbs[bi], t_hbs[bi], sgs[bi], t_sgs[bi]
                P.dma("sp", vo[bi][:, 256:512], self.PVO[c0:c1, 256:512], reads=[self.t_PVO], writes=[t_vo[bi]])
                P.op("pool", lambda e, j=j, hb=hb: e.tensor_tensor(out=hb, in0=hF[:, j, :], in1=hB[:, j, :], op=ALU.add), reads=[t_hF, t_hB], writes=[t_hb])
                P.op("act", lambda e, bi=bi, sg=sg: e.activation(out=sg, in_=vo[bi][:, 256:512], func=AF.Sigmoid), reads=[t_vo[bi]], writes=[t_sg])
                P.op("pool", lambda e, hb=hb, sg=sg: e.tensor_tensor(out=hb, in0=hb, in1=sg, op=ALU.mult), reads=[t_sg], writes=[t_hb])
                P.op("pool", lambda e, hb=hb: e.tensor_tensor(out=hsq, in0=hb, in1=hb, op=ALU.mult), reads=[t_hb], writes=[t_hsq])
                P.op("dve", lambda e: e.tensor_reduce(out=ssn[:, 0:4], in_=hsq.rearrange("p (h c) -> p h c", c=64), axis=AX.X, op=ALU.add), reads=[t_hsq], writes=[t_ssn])
                P.op("dve", lambda e: e.tensor_scalar(out=ssn[:, 0:4], in0=ssn[:, 0:4], scalar1=1.0 / 64, scalar2=EPS, op0=ALU.mult, op1=ALU.add), reads=[t_ssn], writes=[t_ssn])
                P.op("act", lambda e: e.activation(out=ssn[:, 0:4], in_=ssn[:, 0:4], func=AF.Sqrt), reads=[t_ssn], writes=[t_ssn])
                P.op("dve", lambda e: e.reciprocal(out=ssn[:, 4:8], in_=ssn[:, 0:4]), reads=[t_ssn], writes=[t_ssn])
                P.op("dve", lambda e, bi=bi, hb=hb: e.tensor_tensor(out=hn[bi].rearrange("p (h c) -> p h c", c=64), in0=hb.rearrange("p (h c) -> p h c", c=64), in1=ssn[:, 4:8].unsqueeze(2).to_broadcast([128, 4, 64]), op=ALU.mult),
                     reads=[t_hb, t_ssn], writes=[t_hn[bi]])
                pi = 6 + bi
                pT = self.ps[pi].bitcast(BF16)
                for p in range(2):
                    P.op("pe", lambda e, p=p, bi=bi, pT=pT: e.transpose(pT[:, p * 128:(p + 1) * 128], hn[bi][:, p * 128:(p + 1) * 128], self.identb), reads=[t_hn[bi], self.t_ident], writes=[self.pst[pi]], skip_self=True)
                for p in range(2):
                    P.op("act", lambda e, p=p, bi=bi, pT=pT: e.activation(out=oT[bi][:, p, :], in_=pT[:, p * 128:(p + 1) * 128], func=AF.Copy, scale=gml[:, p:p + 1]), reads=[self.pst[pi], t_gml], writes=[t_oT[bi]], partial=(p > 0))
                P.dma("pool", self.MIXT[512:768, c0:c1].rearrange("(c p) t -> p c t", p=128), oT[bi], reads=[t_oT[bi]], writes=[self.t_MIXT], partial=True)
            P.barrier()

    def phase_hyena(self, li):
        if li < DEPTH - 1:
            self.hyena_seq(li, 0, CTX, self.c_hy_z256, self.c_hy_win256, self.c_hy_C256, self.c_hy_S256, self.c_hy_wf256)
        self.hyena_seq(li, CTX, SEQ, self.c_hy_z4096, self.c_hy_win4096, self.c_hy_C4096, self.c_hy_S4096, self.c_hy_wf4096)

    def hyena_seq(self, li, tok0, L, c_z, c_win, c_C, c_S, c_wf):
        nc, P = self.nc, self.P
        NTL = L // 128
        NF = NTL + 1
        NB = (L + 511) // 512
        BW = min(L, 512)
        with contextlib.ExitStack() as st:
            sb = lambda name, shape, dt=F32: self.sb(st, name, shape, dt)
            x0T = sb("hyx0T", [128, 2, L], BF16); t_x0 = Tok()
            zT = sb("hyzT", [128, 2, L], BF16); t_zT = Tok()
            Z = sb("hyZ", [128, NTL, 256], BF16); t_Z = Tok()
            HS = sb("hyHS", [128, NTL, 256], BF16); t_HS = Tok()
            HD = sb("hyHD", [128, NTL, 256], BF16); t_HD = Tok()
            Asp = sb("hyA", [128, NF, 256], BF16); t_A = Tok()
            Bsp = sb("hyB", [128, NF, 256], BF16); t_B = Tok()
            rl1 = sb("hyrl1", [128, 256]); t_rl1 = Tok()
            wf = sb("hywf", [128, NF]); t_wf = Tok()
            P.dma("sp", wf, c_wf, writes=[t_wf])
            bd, t_bd = self.vec_pc(st, "hybd", self.hy_bias_d[li], 2)
            cw = sb("hycw", [128, 6, 3]); cb = sb("hycb", [128, 6]); t_cw = Tok()
            for jj in range(3):
                P.dma("sp", cw[:, :, jj], self.hy_conv_w[li, jj].rearrange("(c p) -> p c", p=128), writes=[t_cw], partial=True, allow_slow_non_contiguous=True)
            P.dma("sp", cb, self.hy_conv_b[li].rearrange("(c p) -> p c", p=128), writes=[t_cw], partial=True, allow_slow_non_contiguous=True)
            with contextlib.ExitStack() as st2:
                sb2 = lambda name, shape, dt=F32: self.sb(st2, name, shape, dt)
                zemb = sb2("hyzemb", [33, L]); t_zemb = Tok()
                P.dma("sp", zemb, c_z, writes=[t_zemb])
                w1 = sb2("hyw1", [33, 64]); w2 = sb2("hyw2", [64, 64]); w3 = sb2("hyw3", [64, 512]); t_wm = Tok()
                P.dma("act", w1, self.hy_w1[li], writes=[t_wm], partial=True)
                P.dma("act", w2, self.hy_w2[li], writes=[t_wm], partial=True)
                P.dma("act", w3, self.hy_w3[li], writes=[t_wm], partial=True)
                fr = sb2("hyfr", [64, 4]); t_fr = Tok()
                P.dma("sp", fr[:, 0:1], self.hy_sin_freq[li].rearrange("(p o) -> p o", o=1), writes=[t_fr], partial=True, allow_slow_non_contiguous=True)
                P.dma("sp", fr[:, 1:2], self.hy_b1[li].rearrange("(p o) -> p o", o=1), writes=[t_fr], partial=True, allow_slow_non_contiguous=True)
                P.dma("sp", fr[:, 2:3], self.hy_b2[li].rearrange("(p o) -> p o", o=1), writes=[t_fr], partial=True, allow_slow_non_contiguous=True)
                P.op("dve", lambda e: e.tensor_scalar(out=fr[:, 1:3], in0=fr[:, 1:3], scalar1=fr[:, 0:1], scalar2=None, op0=ALU.mult), reads=[t_fr], writes=[t_fr])
                h1 = sb2("hyh1", [64, L]); t_h1 = Tok()
                h2 = sb2("hyh2", [64, L]); t_h2 = Tok()
                tt = sb2("hytt", [64, 512]); t_tt = Tok()
                ti = sb2("hyti", [64, 512], I32); t_ti = Tok()
                tf = sb2("hytf", [64, 512]); t_tf = Tok()
                ones32 = sb2("hyones", [128, 128]); t_ones = Tok()
                P.op("pool", lambda e: e.memset(ones32, 1.0), writes=[t_ones])

                def sin_layer(wm, kdim, src, t_src, bcol, dst, t_dst):
                    for b in range(NB):
                        c0 = b * 512
                        P.op("pe", lambda e, c0=c0: e.matmul(self.ps[0][0:64, 0:BW], lhsT=wm[0:kdim, :], rhs=src[0:kdim, c0:c0 + BW], start=True, stop=True),
                             reads=[t_wm, t_src], writes=[self.pst[0]], skip_self=True)
                        P.op("dve", lambda e: e.tensor_scalar(out=tt[:, 0:BW], in0=self.ps[0][0:64, 0:BW], scalar1=fr[:, 0:1], scalar2=fr[:, bcol:bcol + 1], op0=ALU.mult, op1=ALU.add),
                             reads=[self.pst[0], t_fr], writes=[t_tt])
                        P.op("dve", lambda e: e.tensor_scalar(out=ti[:, 0:BW], in0=tt[:, 0:BW], scalar1=1.0 / TWO_PI, scalar2=None, op0=ALU.mult), reads=[t_tt], writes=[t_ti])
                        P.op("dve", lambda e: e.tensor_copy(out=tf[:, 0:BW], in_=ti[:, 0:BW]), reads=[t_ti], writes=[t_tf])
                        P.op("dve", lambda e: e.scalar_tensor_tensor(out=tt[:, 0:BW], in0=tf[:, 0:BW], scalar=-TWO_PI, in1=tt[:, 0:BW], op0=ALU.mult, op1=ALU.add), reads=[t_tf], writes=[t_tt])
                        P.op("act", lambda e, c0=c0: e.activation(out=dst[:, c0:c0 + BW], in_=tt[:, 0:BW], func=AF.Sin), reads=[t_tt], writes=[t_dst], partial=True)
                sin_layer(w1, 33, zemb, t_zemb, 1, h1, t_h1)
                sin_layer(w2, 64, h1, t_h1, 2, h2, t_h2)
                win = [sb2(f"hywin{i}", [128, 2, 256]) for i in range(2)]; t_win = [Tok(), Tok()]
                hfb = [sb2(f"hyhfb{i}", [128, 2, 256]) for i in range(2)]; t_hfb = [Tok(), Tok()]
                hab = [sb2(f"hyhab{i}", [128, 2, 256]) for i in range(2)]; t_hab = [Tok(), Tok()]
                for j in range(NTL):
                    bi = j % 2
                    P.dma("sp", win[bi], c_win[j * 128:(j + 1) * 128], writes=[t_win[bi]])
                    P.op("pe", lambda e, j=j: e.matmul(self.ps[1], lhsT=h2[:, j * 128:(j + 1) * 128], rhs=w3, start=True, stop=True),
                         reads=[t_h2, t_wm], writes=[self.pst[1]], skip_self=True)
                    P.op("dve", lambda e, bi=bi: e.tensor_tensor(out=hfb[bi], in0=self.ps[1].rearrange("p (a c) -> p a c", a=2), in1=win[bi], op=ALU.mult),
                         reads=[self.pst[1], t_win[bi]], writes=[t_hfb[bi]])
                    P.op("pool", lambda e, bi=bi, j=j: e.tensor_tensor(out=HS[:, j, :], in0=hfb[bi][:, 0, :], in1=hfb[bi][:, 1, :], op=ALU.add), reads=[t_hfb[bi]], writes=[t_HS], partial=True)
                    P.op("pool", lambda e, bi=bi, j=j: e.tensor_tensor(out=HD[:, j, :], in0=hfb[bi][:, 0, :], in1=hfb[bi][:, 1, :], op=ALU.subtract), reads=[t_hfb[bi]], writes=[t_HD], partial=True)
                    P.op("act", lambda e, bi=bi: e.activation(out=hab[bi], in_=hfb[bi], func=AF.Abs), reads=[t_hfb[bi]], writes=[t_hab[bi]])
                    for a in range(2):
                        P.op("pe", lambda e, bi=bi, a=a, j=j: e.matmul(self.ps[2][:, 0:256], lhsT=ones32, rhs=hab[bi][:, a, :], start=(j == 0 and a == 0), stop=(j == NTL - 1 and a == 1)),
                             reads=[t_ones, t_hab[bi]], writes=[self.pst[2]], skip_self=True)
                P.op("dve", lambda e: e.reciprocal(out=rl1, in_=self.ps[2][:, 0:256]), reads=[self.pst[2]], writes=[t_rl1])
                P.barrier()
            with contextlib.ExitStack() as st2:
                sb2 = lambda name, shape, dt=F32: self.sb(st2, name, shape, dt)
                pin = [sb2(f"hypin{i}", [128, L]) for i in range(2)]; t_pin = [Tok(), Tok()]
                uu = [sb2(f"hyu{i}", [128, L]) for i in range(2)]; t_uu = [Tok(), Tok()]

                def conv(ch, bi):
                    P.dma("sp" if bi == 0 else "act", pin[bi], self.PT[HY_LO + ch * 128:HY_LO + (ch + 1) * 128, tok0:tok0 + L], reads=[self.t_PT], writes=[t_pin[bi]])
                    eng = "dve"
                    P.op(eng, lambda e: e.tensor_scalar(out=uu[bi], in0=pin[bi], scalar1=cw[:, ch, 1:2], scalar2=cb[:, ch:ch + 1], op0=ALU.mult, op1=ALU.add), reads=[t_pin[bi], t_cw], writes=[t_uu[bi]])
                    P.op(eng, lambda e: e.scalar_tensor_tensor(out=uu[bi][:, 1:L], in0=pin[bi][:, 0:L - 1], scalar=cw[:, ch, 0:1], in1=uu[bi][:, 1:L], op0=ALU.mult, op1=ALU.add), reads=[t_pin[bi], t_cw], writes=[t_uu[bi]])
                    P.op(eng, lambda e: e.scalar_tensor_tensor(out=uu[bi][:, 0:L - 1], in0=pin[bi][:, 1:L], scalar=cw[:, ch, 2:3], in1=uu[bi][:, 0:L - 1], op0=ALU.mult, op1=ALU.add), reads=[t_pin[bi], t_cw], writes=[t_uu[bi]])
                for cc in range(2):
                    conv(cc, 0)
                    P.op("act", lambda e, cc=cc: e.copy(out=x0T[:, cc, :], in_=uu[0]), reads=[t_uu[0]], writes=[t_x0], partial=True)
                    conv(2 + cc, 0)
                    conv(4 + cc, 1)
                    P.op("pool", lambda e, cc=cc: e.tensor_tensor(out=zT[:, cc, :], in0=uu[0], in1=uu[1], op=ALU.mult), reads=[t_uu[0], t_uu[1]], writes=[t_zT], partial=True)
                g = 0
                for j0 in range(0, NTL, 2):
                    pi = 3 + g % 2
                    g += 1
                    pT = self.ps[pi].bitcast(BF16)
                    nj = min(2, NTL - j0)
                    for jj in range(nj):
                        for cc in range(2):
                            P.op("pe", lambda e, jj=jj, cc=cc, j0=j0, pT=pT: e.transpose(pT[:, (jj * 2 + cc) * 128:(jj * 2 + cc + 1) * 128], zT[:, cc, (j0 + jj) * 128:(j0 + jj + 1) * 128], self.identb),
                                 reads=[t_zT, self.t_ident], writes=[self.pst[pi]], skip_self=True)
                    if g % 2 == 0:
                        P.op("act", lambda e, j0=j0, nj=nj, pT=pT: e.copy(out=Z[:, j0:j0 + nj, :].rearrange("p j c -> p (j c)"), in_=pT[:, 0:nj * 256]), reads=[self.pst[pi]], writes=[t_Z], partial=True)
                    else:
                        P.op("dve", lambda e, j0=j0, nj=nj, pT=pT: e.tensor_copy(out=Z[:, j0:j0 + nj, :].rearrange("p j c -> p (j c)"), in_=pT[:, 0:nj * 256]), reads=[self.pst[pi]], writes=[t_Z], partial=True)
                P.barrier()
            with contextlib.ExitStack() as st2:
                sb2 = lambda name, shape, dt=F32: self.sb(st2, name, shape, dt)
                CT = [sb2(f"hyCT{i}", [128, NTL, 128], BF16) for i in range(2)]; t_CT = [Tok(), Tok()]
                ST = [sb2(f"hyST{i}", [128, NTL, 128], BF16) for i in range(2)]; t_ST = [Tok(), Tok()]
                hcs = [sb2(f"hyhcs{i}", [128, 2, 256]) for i in range(2)]; t_hcs = [Tok(), Tok()]
                t1 = sb2("hyt1", [128, 256]); t_t1 = Tok()
                t2 = sb2("hyt2", [128, 256]); t_t2 = Tok()
                t3 = sb2("hyt3", [128, 256]); t_t3 = Tok()
                t4 = sb2("hyt4", [128, 256]); t_t4 = Tok()
                for fc in range(NF):
                    bi = fc % 2
                    P.dma("sp", CT[bi], c_C[0:L, fc * 128:(fc + 1) * 128].rearrange("(j p) f -> p j f", p=128), writes=[t_CT[bi]])
                    P.dma("act", ST[bi], c_S[0:L, fc * 128:(fc + 1) * 128].rearrange("(j p) f -> p j f", p=128), writes=[t_ST[bi]])
                    pc, psn = 2 * bi, 2 * bi + 1
                    for (mat, t_mat, pi, rhs2, t_rhs2) in ((CT[bi], t_CT[bi], pc, HS, t_HS), (ST[bi], t_ST[bi], psn, HD, t_HD)):
                        for half, (rt, t_rt) in enumerate(((Z, t_Z), (rhs2, t_rhs2))):
                            for j in range(NTL):
                                P.op("pe", lambda e, mat=mat, pi=pi, rt=rt, j=j, half=half: e.matmul(self.ps[pi][:, half * 256:(half + 1) * 256], lhsT=mat[:, j, :], rhs=rt[:, j, :], start=(j == 0), stop=(j == NTL - 1)),
                                     reads=[t_mat, t_rt], writes=[self.pst[pi]], skip_self=True)
                    P.op("act", lambda e, bi=bi, pc=pc: e.copy(out=hcs[bi][:, 0, :], in_=self.ps[pc][:, 256:512]), reads=[self.pst[pc]], writes=[t_hcs[bi]], partial=True)
                    P.op("act", lambda e, bi=bi, psn=psn: e.copy(out=hcs[bi][:, 1, :], in_=self.ps[psn][:, 256:512]), reads=[self.pst[psn]], writes=[t_hcs[bi]], partial=True)
                    P.op("dve", lambda e, bi=bi, pc=pc: e.tensor_tensor(out=t1, in0=self.ps[pc][:, 0:256], in1=hcs[bi][:, 0, :], op=ALU.mult), reads=[self.pst[pc], t_hcs[bi]], writes=[t_t1])
                    P.op("dve", lambda e, bi=bi, pc=pc: e.tensor_tensor(out=t3, in0=self.ps[pc][:, 0:256], in1=hcs[bi][:, 1, :], op=ALU.mult), reads=[self.pst[pc], t_hcs[bi]], writes=[t_t3])
                    P.op("dve", lambda e, bi=bi, psn=psn: e.tensor_tensor(out=t2, in0=self.ps[psn][:, 0:256], in1=hcs[bi][:, 1, :], op=ALU.mult), reads=[self.pst[psn], t_hcs[bi]], writes=[t_t2])
                    P.op("dve", lambda e, bi=bi, psn=psn: e.tensor_tensor(out=t4, in0=self.ps[psn][:, 0:256], in1=hcs[bi][:, 0, :], op=ALU.mult), reads=[self.pst[psn], t_hcs[bi]], writes=[t_t4])
                    P.op("pool", lambda e: e.tensor_tensor(out=t1, in0=t1, in1=t2, op=ALU.subtract), reads=[t_t2], writes=[t_t1])
                    P.op("pool", lambda e: e.tensor_tensor(out=t3, in0=t3, in1=t4, op=ALU.add), reads=[t_t4], writes=[t_t3])
                    P.op("dve", lambda e, fc=fc: e.scalar_tensor_tensor(out=Asp[:, fc, :], in0=t1, scalar=wf[:, fc:fc + 1], in1=rl1, op0=ALU.mult, op1=ALU.mult), reads=[t_t1, t_wf, t_rl1], writes=[t_A], partial=True)
                    P.op("dve", lambda e, fc=fc: e.scalar_tensor_tensor(out=Bsp[:, fc, :], in0=t3, scalar=wf[:, fc:fc + 1], in1=rl1, op0=ALU.mult, op1=ALU.mult), reads=[t_t3, t_wf, t_rl1], writes=[t_B], partial=True)
                P.barrier()
            with contextlib.ExitStack() as st2:
                sb2 = lambda name, shape, dt=F32: self.sb(st2, name, shape, dt)
                TW = min(L, 1024)
                GC = [sb2(f"hyGC{i}", [128, TW], BF16) for i in range(3)]; t_GC = [Tok() for _ in range(3)]
                GS = [sb2(f"hyGS{i}", [128, TW], BF16) for i in range(3)]; t_GS = [Tok() for _ in range(3)]
                yt = sb2("hyyt", [128, 512]); t_yt = Tok()
                yo = [sb2(f"hyyo{i}", [128, 512], BF16) for i in range(2)]; t_yo = [Tok(), Tok()]
                nld = 0
                for tb0 in range(0, L, TW):
                    nsub = TW // BW
                    for fc in range(NF):
                        gi = nld % 3
                        nld += 1
                        P.dma("sp", GC[gi], c_C[fc * 128:(fc + 1) * 128, tb0:tb0 + TW], writes=[t_GC[gi]])
                        P.dma("act", GS[gi], c_S[fc * 128:(fc + 1) * 128, tb0:tb0 + TW], writes=[t_GS[gi]])
                        for sub in range(nsub):
                            for cc in range(2):
                                pi = sub * 2 + cc
                                P.op("pe", lambda e, gi=gi, fc=fc, sub=sub, cc=cc, pi=pi: e.matmul(self.ps[pi][:, 0:BW], lhsT=Asp[:, fc, cc * 128:(cc + 1) * 128], rhs=GC[gi][:, sub * BW:(sub + 1) * BW], start=(fc == 0), stop=False),
                                     reads=[t_A, t_GC[gi]], writes=[self.pst[pi]], skip_self=True)
                                P.op("pe", lambda e, gi=gi, fc=fc, sub=sub, cc=cc, pi=pi: e.matmul(self.ps[pi][:, 0:BW], lhsT=Bsp[:, fc, cc * 128:(cc + 1) * 128], rhs=GS[gi][:, sub * BW:(sub + 1) * BW], start=False, stop=(fc == NF - 1)),
                                     reads=[t_B, t_GS[gi]], writes=[self.pst[pi]], skip_self=True)
                    for sub in range(nsub):
                        for cc in range(2):
                            pi = sub * 2 + cc
                            c0 = tb0 + sub * BW
                            oi = pi % 2
                            P.op("dve", lambda e, cc=cc, c0=c0, pi=pi: e.scalar_tensor_tensor(out=yt[:, 0:BW], in0=zT[:, cc, c0:c0 + BW], scalar=bd[:, cc:cc + 1], in1=self.ps[pi][:, 0:BW], op0=ALU.mult, op1=ALU.add),
                                 reads=[t_zT, t_bd, self.pst[pi]], writes=[t_yt])
                            P.op("pool", lambda e, cc=cc, c0=c0, oi=oi: e.tensor_tensor(out=yo[oi][:, 0:BW], in0=yt[:, 0:BW], in1=x0T[:, cc, c0:c0 + BW], op=ALU.mult), reads=[t_yt, t_x0], writes=[t_yo[oi]])
                            P.dma("pool", self.MIXT[768 + cc * 128:768 + (cc + 1) * 128, tok0 + c0:tok0 + c0 + BW], yo[oi][:, 0:BW], reads=[t_yo[oi]], writes=[self.t_MIXT], partial=True)
                P.barrier()

    def phase_wout(self, li):
        nc, P = self.nc, self.P
        last = li == DEPTH - 1
        with contextlib.ExitStack() as st:
            sb = lambda name, shape, dt=F32: self.sb(st, name, shape, dt)
            wo = sb("woutb", [128, 8, D], BF16); t_wo = Tok()
            with contextlib.ExitStack() as st2:
                self.load_cast_weight(st2, wo, t_wo, self.w_out[li], D)
                P.barrier()
            G1 = sb("G1rep", [128, 2, D]); t_G1 = Tok()
            for v in range(2):
                P.dma("sp", G1[:, v, :], self.MOD[v, 2 * D:3 * D].partition_broadcast(128), reads=[self.t_MOD], writes=[t_G1], partial=True)
            mx = [sb(f"mixT{i}", [128, 8, 128], BF16) for i in range(2)]; t_mx = [Tok(), Tok()]
            xt = [sb(f"wxt{i}", [128, D]) for i in range(2)]; t_xt = [Tok(), Tok()]
            tm = [sb(f"wtm{i}", [128, 512]) for i in range(2)]; t_tm = [Tok(), Tok()]
            tiles = list(range(2 if last else 0, TT // 128))
            for n, j in enumerate(tiles):
                bi = n % 2
                v = 1 if j < 2 else 0
                c0, c1 = j * 128, (j + 1) * 128
                P.dma("sp", mx[bi], self.MIXT[:, c0:c1].rearrange("(c p) t -> p c t", p=128), reads=[self.t_MIXT], writes=[t_mx[bi]])
                P.dma("act", xt[bi], self.XS[c0:c1, :], reads=[self.t_XS], writes=[t_xt[bi]])
                for half in range(2):
                    pi = (n * 2 + half) % 4
                    for k in range(8):
                        P.op("pe", lambda e, bi=bi, k=k, half=half, pi=pi: e.matmul(self.ps[pi], lhsT=mx[bi][:, k, :], rhs=wo[:, k, half * 512:(half + 1) * 512], start=(k == 0), stop=(k == 7)),
                             reads=[t_mx[bi], t_wo], writes=[self.pst[pi]], skip_self=True)
                    P.op("dve", lambda e, half=half, pi=pi, v=v: e.tensor_tensor(out=tm[half], in0=self.ps[pi], in1=G1[:, v, half * 512:(half + 1) * 512], op=ALU.mult),
                         reads=[self.pst[pi], t_G1], writes=[t_tm[half]])
                    P.op("dve", lambda e, bi=bi, half=half: e.tensor_tensor(out=xt[bi][:, half * 512:(half + 1) * 512], in0=xt[bi][:, half * 512:(half + 1) * 512], in1=tm[half], op=ALU.add),
                         reads=[t_tm[half]], writes=[t_xt[bi]])
                P.dma("pool", self.XS[c0:c1, :], xt[bi], reads=[t_xt[bi]], writes=[self.t_XS], partial=True)
            P.barrier()

    def phase_ffn(self, li):
        nc, P = self.nc, self.P
        last = li == DEPTH - 1
        with contextlib.ExitStack() as st:
            sb = lambda name, shape, dt=F32: self.sb(st, name, shape, dt)
            wup = sb("wupb", [128, 8, 2 * DFF], BF16); t_wup = Tok()
            wdn = sb("wdnb", [128, NFC, D], BF16); t_wdn = Tok()
            with contextlib.ExitStack() as st2:
                self.load_cast_weight(st2, wup, t_wup, self.ffn_w_up[li], 2 * DFF)
                P.barrier()
            with contextlib.ExitStack() as st2:
                self.load_cast_weight(st2, wdn, t_wdn, self.ffn_w_down[li], D, blk=256, k_chunks=NFC)
                P.barrier()
            G2 = sb("G2rep", [128, 2, D]); t_G2 = Tok()
            for v in range(2):
                P.dma("sp", G2[:, v, :], self.MOD[v, 5 * D:6 * D].partition_broadcast(128), reads=[self.t_MOD], writes=[t_G2], partial=True)
            if last:
                FG = sb("FGrep", [128, D]); t_FG = Tok()
                P.dma("sp", FG, self.final_norm_g.partition_broadcast(128), writes=[t_FG])
            cw = sb("ffcw", [128, 2 * NFC, 3]); cbv = sb("ffcb", [128, 2 * NFC]); t_cw = Tok()
            for jj in range(3):
                P.dma("sp", cw[:, :, jj], self.ffn_conv_w[li, jj].rearrange("(c p) -> p c", p=128), writes=[t_cw], partial=True, allow_slow_non_contiguous=True)
            P.dma("sp", cbv, self.ffn_conv_b[li].rearrange("(c p) -> p c", p=128), writes=[t_cw], partial=True, allow_slow_non_contiguous=True)
            hx = [sb(f"hTx{i}", [128, 8, 258], BF16) for i in range(3)]; t_hx = [Tok() for _ in range(3)]
            gT = sb("ffgT", [128, NFC, 256], BF16); t_gT = Tok()
            xts = [sb(f"fxt{i}", [128, D]) for i in range(4)]; t_xts = [Tok() for _ in range(4)]
            sqj = sb("fsq", [128, D], BF16); t_sqj = Tok()
            sss = [sb(f"fss{i}", [128, 4]) for i in range(4)]; t_sss = [Tok() for _ in range(4)]
            xns = [sb(f"fxn{i}", [128, D], BF16) for i in range(2)]; t_xns = [Tok(), Tok()]
            ga = [sb(f"ffga{i}", [128, 256]) for i in range(2)]; t_ga = [Tok(), Tok()]
            gb = [sb(f"ffgb{i}", [128, 256]) for i in range(2)]; t_gb = [Tok(), Tok()]
            tmo = [sb(f"fftm{i}", [128, 512]) for i in range(2)]; t_tmo = [Tok(), Tok()]
            fss = sb("ffss", [128, 4]); t_fss = Tok()
            tiles = list(range(1 if last else 0, TT // 256))
            seq_first = {0, 1}
            seq_last = {0, TT // 256 - 1}
            nsub = 0
            sets_of = {}

            def stage_a(ti):
                nonlocal nsub
                t0 = ti * 256
                v = 1 if ti == 0 else 0
                hb = ti % 3
                sets_of[ti] = []
                for sub in range(2):
                    si = nsub % 4
                    nsub += 1
                    sets_of[ti].append(si)
                    bufs = (xts[si], t_xts[si], sqj, t_sqj, sss[si], t_sss[si], xns[si % 2], t_xns[si % 2])
                    self.norm_transpose(bufs, self.XS[t0 + sub * 128:t0 + (sub + 1) * 128, :], self.t_XS, self.s2, self.modT[:, 24:32, :], v,
                                        hx[hb], t_hx[hb], 1 + sub * 128, ps_i=6 + sub)
                if ti in seq_first:
                    P.op("pool", lambda e, hb=hb: e.memset(hx[hb][:, :, 0:1], 0.0), writes=[t_hx[hb]], partial=True)
                if ti in seq_last:
                    P.op("pool", lambda e, hb=hb: e.memset(hx[hb][:, :, 257:258], 0.0), writes=[t_hx[hb]], partial=True)

            def halo(ta, tb):
                a, b = ta % 3, tb % 3
                P.op("pool", lambda e: e.tensor_copy(out=hx[a][:, :, 257:258], in_=hx[b][:, :, 1:2]), reads=[t_hx[b]], writes=[t_hx[a]], partial=True)
                P.op("pool", lambda e: e.tensor_copy(out=hx[b][:, :, 0:1], in_=hx[a][:, :, 256:257]), reads=[t_hx[a]], writes=[t_hx[b]], partial=True)

            def stage_b(ti):
                t0 = ti * 256
                v = 1 if ti == 0 else 0
                hb = ti % 3
                for jc in range(NFC):
                    gi = jc % 2
                    for (which, col0, pi, acc, t_acc) in ((0, jc * 128, 0 + 2 * gi, ga[gi], t_ga[gi]), (1, DFF + jc * 128, 1 + 2 * gi, gb[gi], t_gb[gi])):
                        ch = (col0 // 128)
                        for k in range(8):
                            P.op("pe", lambda e, k=k, col0=col0, pi=pi: e.matmul(self.ps[pi][:, 0:258], lhsT=wup[:, k, col0:col0 + 128], rhs=hx[hb][:, k, :], start=(k == 0), stop=(k == 7)),
                                 reads=[t_wup, t_hx[hb]], writes=[self.pst[pi]], skip_self=True)
                        P.op("act", lambda e, pi=pi, acc=acc, ch=ch: e.activation(out=acc, in_=self.ps[pi][:, 1:257], func=AF.Identity, scale=cw[:, ch, 1:2], bias=cbv[:, ch:ch + 1]),
                             reads=[self.pst[pi], t_cw], writes=[t_acc])
                        P.op("dve", lambda e, pi=pi, acc=acc, ch=ch: e.scalar_tensor_tensor(out=acc, in0=self.ps[pi][:, 0:256], scalar=cw[:, ch, 0:1], in1=acc, op0=ALU.mult, op1=ALU.add),
                             reads=[self.pst[pi], t_cw], writes=[t_acc])
                        P.op("dve", lambda e, pi=pi, acc=acc, ch=ch: e.scalar_tensor_tensor(out=acc, in0=self.ps[pi][:, 2:258], scalar=cw[:, ch, 2:3], in1=acc, op0=ALU.mult, op1=ALU.add),
                             reads=[self.pst[pi], t_cw], writes=[t_acc])
                    P.op("act", lambda e, gi=gi: e.activation(out=ga[gi], in_=ga[gi], func=AF.Silu), reads=[t_ga[gi]], writes=[t_ga[gi]])
                    P.op("pool", lambda e, gi=gi, jc=jc: e.tensor_tensor(out=gT[:, jc, :], in0=ga[gi], in1=gb[gi], op=ALU.mult), reads=[t_ga[gi], t_gb[gi]], writes=[t_gT], partial=True)
                for sub in range(2):
                    si = sets_of[ti][sub]
                    xt, t_xt = xts[si], t_xts[si]
                    for half in range(2):
                        pi = 4 + half
                        for jc in range(NFC):
                            P.op("pe", lambda e, jc=jc, sub=sub, half=half, pi=pi: e.matmul(self.ps[pi], lhsT=gT[:, jc, sub * 128:(sub + 1) * 128], rhs=wdn[:, jc, half * 512:(half + 1) * 512], start=(jc == 0), stop=(jc == NFC - 1)),
                                 reads=[t_gT, t_wdn], writes=[self.pst[pi]], skip_self=True)
                        P.op("dve", lambda e, half=half, pi=pi: e.tensor_tensor(out=tmo[half], in0=self.ps[pi], in1=G2[:, v, half * 512:(half + 1) * 512], op=ALU.mult),
                             reads=[self.pst[pi], t_G2], writes=[t_tmo[half]])
                        P.op("pool", lambda e, half=half, xt=xt: e.tensor_tensor(out=xt[:, half * 512:(half + 1) * 512], in0=xt[:, half * 512:(half + 1) * 512], in1=tmo[half], op=ALU.add),
                             reads=[t_tmo[half]], writes=[t_xt])
                    r0 = t0 + sub * 128
                    if not last:
                        P.dma("pool", self.XS[r0:r0 + 128, :], xt, reads=[t_xt], writes=[self.t_XS], partial=True)
                    else:
                        P.op("act", lambda e, xt=xt: e.activation(out=sqj, in_=xt, func=AF.Square, accum_out=fss[:, 0:1]), reads=[t_xt], writes=[t_sqj, t_fss])
                        P.op("dve", lambda e: e.tensor_scalar(out=fss[:, 1:2], in0=fss[:, 0:1], scalar1=1.0 / D, scalar2=EPS, op0=ALU.mult, op1=ALU.add), reads=[t_fss], writes=[t_fss])
                        P.op("act", lambda e: e.activation(out=fss[:, 2:3], in_=fss[:, 1:2], func=AF.Sqrt), reads=[t_fss], writes=[t_fss])
                        P.op("dve", lambda e: e.reciprocal(out=fss[:, 3:4], in_=fss[:, 2:3]), reads=[t_fss], writes=[t_fss])
                        P.op("dve", lambda e, xt=xt: e.scalar_tensor_tensor(out=xt, in0=xt, scalar=fss[:, 3:4], in1=FG, op0=ALU.mult, op1=ALU.mult), reads=[t_fss, t_FG], writes=[t_xt])
                        P.dma("pool", self.out[r0 - CTX:r0 - CTX + 128, :], xt, reads=[t_xt], writes=[self.t_out], partial=True)

            prev = None
            for ti in tiles:
                stage_a(ti)
                if prev is not None:
                    if prev not in seq_last:
                        halo(prev, ti)
                    stage_b(prev)
                prev = ti
            stage_b(prev)
            P.barrier()

    def build(self, stop_after=None):
        nc, P = self.nc, self.P
        self.declare()
        P.clear_sems()
        gst = self.stack
        self.identb = self.sb(gst, "identb", [128, 128], BF16)
        self.t_ident = Tok()
        P.dma("sp", self.identb, self.c_ident, writes=[self.t_ident])
        self.t_XS = Tok()
        self.t_MOD, self.t_PT, self.t_PVO, self.t_MIXT, self.t_out = Tok(), Tok(), Tok(), Tok(), Tok()
        P.dma("sp", self.XS[0:CTX, :], self.ctx, writes=[self.t_XS], partial=True)
        for i in range(16):
            P.dma("act" if i % 2 else "sp", self.XS[CTX + i * 256:CTX + (i + 1) * 256, :], self.x[i * 256:(i + 1) * 256, :], writes=[self.t_XS], partial=True)
        done = False
        for li in range(DEPTH):
            with contextlib.ExitStack() as lst:
                self.lst = lst
                for name, fn in (("mod", self.phase_mod), ("inproj", self.phase_inproj), ("mla", self.phase_mla), ("mlstm", self.phase_mlstm), ("hyena", self.phase_hyena), ("wout", self.phase_wout), ("ffn", self.phase_ffn)):
                    fn(li)
                    if stop_after == (name, li):
                        done = True
                        break
                P.barrier()
            if done:
                break
        P.finish()
        return nc


def _prep_inputs(inputs, b):
    m = {}
    for k, v in inputs.items():
        v = np.asarray(v)
        if k in ("x", "ctx", "c"):
            m[k] = np.ascontiguousarray(v[b])
        elif k == "ml_gate_b":
            m[k] = np.ascontiguousarray(v.reshape(DEPTH, 16))
        else:
            m[k] = np.ascontiguousarray(v)
    return m


def kernel(**inputs):
    bld = Builder()
    nc = bld.build()
    consts = _consts()
    in_maps = []
    for b in range(8):
        m = _prep_inputs(inputs, b)
        m.update(consts)
        in_maps.append({k: m[k] for k in bld.inp})
    res = run_bass_kernel_spmd(nc, in_maps, core_ids=list(range(8)))
    return np.stack([np.asarray(r["out"]) for r in res.results], axis=0).astype(np.float32)
```

```python
import contextlib
import numpy as np
import ml_dtypes
import concourse.bass as bass
import concourse.mybir as mybir
from concourse.bass_utils import run_bass_kernel_spmd

F32 = mybir.dt.float32
BF16 = mybir.dt.bfloat16
I32 = mybir.dt.int32
AF = mybir.ActivationFunctionType
ALU = mybir.AluOpType
AX = mybir.AxisListType

D = 1024
SEQ = 4096
CTX = 256
TT = SEQ + CTX
DEPTH = 2
EPS = 1e-6
NH = 8
QR, KVR, ROPE = 384, 256, 32
N_MLA_IN = QR + KVR + ROPE
MLW = 256
N_ML_IN = 3 * MLW + 16
HYW = 256
N_IN = 2224
ML_LO = N_MLA_IN
HY_LO = N_MLA_IN + N_ML_IN
DFF = 2816
NFC = DFF // 128
TWO_PI = float(2 * np.pi)


class Tok:
    __slots__ = ("w", "r", "wf")

    def __init__(self):
        self.w = []
        self.wf = []
        self.r = []


class PTok(Tok):
    __slots__ = ()


class Queue:
    def __init__(self, prog, name, eng, n_dma_sems=0):
        self.p = prog
        self.name = name
        self.eng = eng
        nc = prog.nc
        self.sem = nc.alloc_semaphore(name=f"s_{name}")
        self.count = 0
        self.dma_sems = [nc.alloc_semaphore(name=f"d_{name}{i}") for i in range(n_dma_sems)]
        self.dma_counts = [0] * n_dma_sems
        self.dma_rr = 0
        self.waited = {}

    def _wait(self, tick):
        sem, val = tick
        key = id(sem)
        if self.waited.get(key, 0) >= val:
            return
        self.eng.wait_ge(sem, val)
        self.waited[key] = val
        self.p.n_waits += 1


class Prog:
    def __init__(self, nc, dma_sems=8):
        self.nc = nc
        self.n_waits = 0
        self.n_ins = 0
        self.q = {
            "pe": Queue(self, "pe", nc.tensor),
            "dve": Queue(self, "dve", nc.vector),
            "act": Queue(self, "act", nc.scalar, dma_sems),
            "pool": Queue(self, "pool", nc.gpsimd, dma_sems),
            "sp": Queue(self, "sp", nc.sync, dma_sems),
        }

    def clear_sems(self):
        for q in self.q.values():
            q.eng.sem_clear(q.sem)
            for s in q.dma_sems:
                q.eng.sem_clear(s)
        self.nc.all_engine_barrier()

    def _deps(self, q, reads, writes, skip_self=False, partial=False):
        for t in reads:
            for tk in t.w:
                if not (skip_self and tk[0] is q.sem):
                    q._wait(tk)
            if isinstance(t, PTok):
                for tk in t.r:
                    if not (skip_self and tk[0] is q.sem):
                        q._wait(tk)
        for t in writes:
            if partial and not isinstance(t, PTok):
                for tk in t.wf:
                    if not (skip_self and tk[0] is q.sem):
                        q._wait(tk)
            if not partial or isinstance(t, PTok):
                for tk in t.w:
                    if not (skip_self and tk[0] is q.sem):
                        q._wait(tk)
            for tk in t.r:
                if not (skip_self and tk[0] is q.sem):
                    q._wait(tk)

    @staticmethod
    def _compact(lst):
        best = {}
        for s, v in lst:
            k = id(s)
            if k not in best or best[k][1] < v:
                best[k] = (s, v)
        return list(best.values())

    def _record(self, tick, reads, writes, partial=False):
        for t in reads:
            if isinstance(t, PTok):
                t.w = [tick]
                t.wf = [tick]
                t.r = []
                continue
            t.r.append(tick)
            if len(t.r) > 48:
                t.r = self._compact(t.r)
        for t in writes:
            if partial and not isinstance(t, PTok):
                t.w.append(tick)
                if len(t.w) > 48:
                    t.w = self._compact(t.w)
            else:
                t.w = [tick]
                t.wf = [tick]
                t.r = []

    def op(self, qname, fn, reads=(), writes=(), skip_self=False, partial=False):
        q = self.q[qname]
        self._deps(q, reads, writes, skip_self=skip_self, partial=partial)
        ins = fn(q.eng)
        q.count += 1
        ins.then_inc(q.sem, 1)
        self._record((q.sem, q.count), reads, writes, partial=partial)
        self.n_ins += 1
        return ins

    def dma(self, qname, out, in_, reads=(), writes=(), partial=False, **kw):
        q = self.q[qname]
        j = q.dma_rr
        q.dma_rr = (j + 1) % len(q.dma_sems)
        sem = q.dma_sems[j]
        if q.dma_counts[j] > 0:
            q._wait((sem, q.dma_counts[j]))
        self._deps(q, reads, writes, partial=partial)
        ins = q.eng.dma_start(out=out, in_=in_, **kw)
        q.dma_counts[j] += 16
        ins.then_inc(sem, 16)
        self._record((sem, q.dma_counts[j]), reads, writes, partial=partial)
        self.n_ins += 1
        return ins

    def barrier(self):
        ticks = []
        for q in self.q.values():
            if q.count:
                ticks.append((q.sem, q.count))
            for j, s in enumerate(q.dma_sems):
                if q.dma_counts[j]:
                    ticks.append((s, q.dma_counts[j]))
        for q in self.q.values():
            for tk in ticks:
                q._wait(tk)

    def finish(self):
        ticks = []
        for q in self.q.values():
            if q.count:
                ticks.append((q.sem, q.count))
            for j, s in enumerate(q.dma_sems):
                if q.dma_counts[j]:
                    ticks.append((s, q.dma_counts[j]))
        for tk in ticks:
            self.q["sp"]._wait(tk)


def _consts():
    c = {}
    c["ident"] = np.eye(128, dtype=np.float32).astype(ml_dtypes.bfloat16)
    n_freq = ROPE // 4
    inv = (10000.0 ** (-np.arange(n_freq, dtype=np.float32) / n_freq)).astype(np.float32)
    row = np.repeat(np.arange(SEQ // 64, dtype=np.float32), 64)
    col = np.tile(np.arange(64, dtype=np.float32), SEQ // 64)
    ang = np.concatenate([row[:, None] * inv, col[:, None] * inv], axis=-1).astype(np.float32)
    cos = np.cos(ang).astype(np.float32).T
    sin = np.sin(ang).astype(np.float32).T
    c["rope_cos"] = np.ascontiguousarray(np.concatenate([cos, cos], 0))
    c["rope_sin"] = np.ascontiguousarray(np.concatenate([sin, sin], 0))
    ss_, tt_ = np.meshgrid(np.arange(128), np.arange(128), indexing="ij")
    c["ml_mask"] = np.stack([(ss_ <= tt_), (ss_ >= tt_)], 1).astype(np.float32).astype(ml_dtypes.bfloat16)
    selA = np.zeros((96, 4, 128), np.float32)
    selW = np.zeros((96, 8), np.float32)
    for d in range(2):
        base = 32 + 4 if d == 0 else 64 + 12
        for h in range(4):
            selA[base + h, (h // 2) * 2 + d, (h % 2) * 64:(h % 2) * 64 + 64] = 1.0
            selW[8 * d + h, d * 4 + h] = 1.0
            selW[base + h, d * 4 + h] = -1.0
    c["ml_selA"] = selA
    c["ml_selW"] = selW
    for L in (256, 4096):
        f32 = np.float32
        t = np.linspace(0.0, 1.0, L, dtype=f32)[:, None]
        omega = (f32(2.0 * np.pi) * np.arange(L, dtype=f32) / f32(L)).astype(f32)
        bands = np.linspace(1e-4, 15, 16, dtype=f32)
        ang = (omega[:, None] * bands[None, :]).astype(f32)
        z = np.concatenate([t, np.cos(ang).astype(f32), -np.sin(ang).astype(f32)], axis=-1).astype(f32)
        c[f"hy_z{L}"] = np.ascontiguousarray(z.T)
        deltas = np.abs(np.linspace(np.log(1e-2) / 1.5, np.log(1e-2) / 0.3, 256, dtype=f32)).astype(f32)
        window = (np.exp(-t * deltas).astype(f32) + f32(0.05)).astype(f32)
        wb = window.copy()
        wb[0] = 0.0
        c[f"hy_win{L}"] = np.ascontiguousarray(np.stack([window, wb], axis=1))
        NFp = L + 128
        n = 2 * L
        idx = np.arange(L + 1, dtype=np.int64)
        prod = (idx[:, None] * idx[None, :]) % n
        angm = prod.astype(np.float64) * (2.0 * np.pi / n)
        Cm = np.zeros((NFp, NFp), np.float32)
        Sm = np.zeros((NFp, NFp), np.float32)
        Cm[:L + 1, :L + 1] = np.cos(angm)
        Sm[:L + 1, :L + 1] = np.sin(angm)
        c[f"hy_C{L}"] = Cm.astype(ml_dtypes.bfloat16)
        c[f"hy_S{L}"] = Sm.astype(ml_dtypes.bfloat16)
        wfv = np.zeros(NFp, np.float32)
        wfv[:L + 1] = 2.0 / n
        wfv[0] = 1.0 / n
        wfv[L] = 1.0 / n
        c[f"hy_wf{L}"] = np.ascontiguousarray(wfv.reshape(-1, 128).T)
    return c


class Builder:
    def __init__(self, debug=None):
        self.debug = debug or set()
        self.nc = nc = bass.Bass("TRN2", target_bir_lowering=False)
        self.P = Prog(nc)
        self.inp = {}
        self.stack = contextlib.ExitStack()

    def din(self, name, shape, dt=F32):
        ap = self.nc.dram_tensor(name, list(shape), dt, kind="ExternalInput").ap()
        self.inp[name] = ap
        return ap

    def dscr(self, name, shape, dt=F32):
        kind = "ExternalOutput" if name in self.debug else "Internal"
        return self.nc.dram_tensor(name, list(shape), dt, kind=kind).ap()

    def sb(self, st, name, shape, dt=F32):
        self.uid = getattr(self, "uid", 0) + 1
        return st.enter_context(self.nc.sbuf_tensor(f"{name}_{self.uid}", list(shape), dt)).ap()

    def declare(self):
        din = self.din
        self.x = din("x", [SEQ, D])
        self.c = din("c", [D])
        self.ctx = din("ctx", [CTX, D])
        self.c_ctx = din("c_ctx", [D])
        self.ada_w = din("ada_w", [DEPTH, D, 6 * D])
        self.ada_b = din("ada_b", [DEPTH, 6 * D])
        self.norm1_g = din("norm1_g", [DEPTH, D])
        self.norm2_g = din("norm2_g", [DEPTH, D])
        self.w_in = din("w_in", [DEPTH, D, N_IN])
        self.mla_q_norm_g = din("mla_q_norm_g", [DEPTH, QR])
        self.mla_kv_norm_g = din("mla_kv_norm_g", [DEPTH, KVR])
        self.mla_w_uq = din("mla_w_uq", [DEPTH, QR, NH * 96])
        self.mla_w_ukv = din("mla_w_ukv", [DEPTH, KVR, NH * 128])
        self.ml_conv_w = din("ml_conv_w", [DEPTH, 3, MLW])
        self.ml_conv_b = din("ml_conv_b", [DEPTH, MLW])
        self.ml_wq = din("ml_wq", [DEPTH, 4, 64, 64])
        self.ml_wk = din("ml_wk", [DEPTH, 4, 64, 64])
        self.ml_gate_b = din("ml_gate_b", [DEPTH, 16])
        self.ml_norm_g = din("ml_norm_g", [DEPTH, MLW])
        self.hy_conv_w = din("hy_conv_w", [DEPTH, 3, 3 * HYW])
        self.hy_conv_b = din("hy_conv_b", [DEPTH, 3 * HYW])
        self.hy_w1 = din("hy_w1", [DEPTH, 33, 64])
        self.hy_b1 = din("hy_b1", [DEPTH, 64])
        self.hy_w2 = din("hy_w2", [DEPTH, 64, 64])
        self.hy_b2 = din("hy_b2", [DEPTH, 64])
        self.hy_w3 = din("hy_w3", [DEPTH, 64, 2 * HYW])
        self.hy_sin_freq = din("hy_sin_freq", [DEPTH, 64])
        self.hy_bias_d = din("hy_bias_d", [DEPTH, HYW])
        self.w_out = din("w_out", [DEPTH, D, D])
        self.ffn_w_up = din("ffn_w_up", [DEPTH, D, 2 * DFF])
        self.ffn_conv_w = din("ffn_conv_w", [DEPTH, 3, 2 * DFF])
        self.ffn_conv_b = din("ffn_conv_b", [DEPTH, 2 * DFF])
        self.ffn_w_down = din("ffn_w_down", [DEPTH, DFF, D])
        self.final_norm_g = din("final_norm_g", [D])
        self.c_ident = din("ident", [128, 128], BF16)
        self.c_rope_cos = din("rope_cos", [32, SEQ])
        self.c_rope_sin = din("rope_sin", [32, SEQ])
        self.c_ml_mask = din("ml_mask", [128, 2, 128], BF16)
        self.c_ml_selA = din("ml_selA", [96, 4, 128])
        self.c_ml_selW = din("ml_selW", [96, 8])
        for L in (256, 4096):
            NFp = L + 128
            setattr(self, f"c_hy_z{L}", din(f"hy_z{L}", [33, L]))
            setattr(self, f"c_hy_win{L}", din(f"hy_win{L}", [L, 2, 256]))
            setattr(self, f"c_hy_C{L}", din(f"hy_C{L}", [NFp, NFp], BF16))
            setattr(self, f"c_hy_S{L}", din(f"hy_S{L}", [NFp, NFp], BF16))
            setattr(self, f"c_hy_wf{L}", din(f"hy_wf{L}", [128, L // 128 + 1]))
        self.out = self.nc.dram_tensor("out", [SEQ, D], F32, kind="ExternalOutput").ap()
        self.XS = self.dscr("XS", [TT, D])
        self.MOD = self.dscr("MOD", [2, 6 * D])
        self.PT = self.dscr("PT", [N_IN + 32, TT])
        self.PVO = self.dscr("PVO", [TT, 512])
        self.MIXT = self.dscr("MIXT", [D, TT], BF16)
        self.psall = self.nc.alloc_psum_tensor("psall", [128, 8 * 512], F32).ap()
        self.ps = [self.psall[:, i * 512:(i + 1) * 512] for i in range(8)]
        self.pst = [PTok() for _ in range(8)]

    def vec_pc(self, st, name, src, n, q="sp"):
        t = self.sb(st, name, [128, n])
        tok = Tok()
        self.P.dma(q, t, src.rearrange("(c p) -> p c", p=128), writes=[tok], allow_slow_non_contiguous=True)
        return t, tok

    def phase_mod(self, li):
        nc, P = self.nc, self.P
        lst = self.lst
        self.modT = self.sb(lst, "modT", [128, 48, 2])
        self.t_mod = Tok()
        self.s1 = self.sb(lst, "s1", [128, 8, 2])
        self.s2 = self.sb(lst, "s2", [128, 8, 2])
        self.t_s12 = Tok()
        with contextlib.ExitStack() as st:
            cc = self.sb(st, "cc", [128, 8, 2])
            t_cc = Tok()
            P.dma("sp", cc[:, :, 0], self.c.rearrange("(c p) -> p c", p=128), writes=[t_cc], partial=True, allow_slow_non_contiguous=True)
            P.dma("sp", cc[:, :, 1], self.c_ctx.rearrange("(c p) -> p c", p=128), writes=[t_cc], partial=True, allow_slow_non_contiguous=True)
            sc = self.sb(st, "sc", [128, 8, 2])
            t_sc = Tok()
            P.op("act", lambda e: e.activation(out=sc, in_=cc, func=AF.Silu), reads=[t_cc], writes=[t_sc])
            ab = self.sb(st, "ab", [128, 48])
            t_ab = Tok()
            P.dma("sp", ab, self.ada_b[li].rearrange("(c p) -> p c", p=128), writes=[t_ab], allow_slow_non_contiguous=True)
            g12 = self.sb(st, "g12", [128, 8, 2])
            t_g12 = Tok()
            P.dma("sp", g12[:, :, 0], self.norm1_g[li].rearrange("(c p) -> p c", p=128), writes=[t_g12], partial=True, allow_slow_non_contiguous=True)
            P.dma("sp", g12[:, :, 1], self.norm2_g[li].rearrange("(c p) -> p c", p=128), writes=[t_g12], partial=True, allow_slow_non_contiguous=True)
            wt = [self.sb(st, f"adaw{i}", [128, 8, 512]) for i in range(2)]
            t_wt = [Tok(), Tok()]
            acc = self.ps[0]
            t_acc = self.pst[0]
            for nb in range(12):
                s = nb % 2
                P.dma("sp" if nb % 2 == 0 else "act", wt[s],
                      self.ada_w[li, :, nb * 512:(nb + 1) * 512].rearrange("(c p) n -> p c n", p=128),
                      writes=[t_wt[s]])
                for j in range(4):
                    n = nb * 4 + j
                    for k in range(8):
                        P.op("pe", lambda e, s=s, j=j, k=k, n=n: e.matmul(
                            acc[:, 2 * n:2 * n + 2], lhsT=wt[s][:, k, j * 128:(j + 1) * 128], rhs=sc[:, k, :],
                            start=(k == 0), stop=(k == 7)),
                            reads=[t_wt[s], t_sc], writes=[t_acc], skip_self=True)
            mod = self.modT
            P.op("dve", lambda e: e.tensor_tensor(out=mod, in0=acc[:, 0:96].rearrange("p (n v) -> p n v", v=2),
                                                  in1=ab.unsqueeze(2).to_broadcast([128, 48, 2]), op=ALU.add),
                 reads=[t_acc, t_ab], writes=[self.t_mod])
            for (dst, gi, c0) in ((self.s1, 0, 8), (self.s2, 1, 32)):
                P.op("dve", lambda e, dst=dst, gi=gi, c0=c0: e.scalar_tensor_tensor(
                    out=dst, in0=mod[:, c0:c0 + 8, :], scalar=1.0, in1=g12[:, :, gi:gi + 1].to_broadcast([128, 8, 2]),
                    op0=ALU.add, op1=ALU.mult), reads=[self.t_mod, t_g12], writes=[self.t_s12], partial=True)
            for v in range(2):
                P.dma("sp", self.MOD[v].rearrange("(c p) -> p c", p=128), mod[:, :, v], reads=[self.t_mod], writes=[self.t_MOD],
                      partial=True, allow_slow_non_contiguous=True)
            P.barrier()

    def norm_transpose(self, st_bufs, rows_ap, t_rows, svec, bvec, v, dst, t_dst, col0, ps_i):
        P = self.P
        xt, t_xt, sq, t_sq, ss, t_ss, xn, t_xn = st_bufs
        P.dma("sp", xt, rows_ap, reads=[t_rows], writes=[t_xt])
        P.op("act", lambda e: e.activation(out=sq, in_=xt, func=AF.Square, accum_out=ss[:, 0:1]), reads=[t_xt], writes=[t_sq, t_ss])
        P.op("dve", lambda e: e.tensor_scalar(out=ss[:, 1:2], in0=ss[:, 0:1], scalar1=1.0 / D, scalar2=EPS, op0=ALU.mult, op1=ALU.add),
             reads=[t_ss], writes=[t_ss])
        P.op("act", lambda e: e.activation(out=ss[:, 2:3], in_=ss[:, 1:2], func=AF.Sqrt), reads=[t_ss], writes=[t_ss])
        P.op("dve", lambda e: e.reciprocal(out=ss[:, 3:4], in_=ss[:, 2:3]), reads=[t_ss], writes=[t_ss])
        P.op("dve", lambda e: e.tensor_scalar(out=xn, in0=xt, scalar1=ss[:, 3:4], scalar2=None, op0=ALU.mult), reads=[t_xt, t_ss], writes=[t_xn])
        pb = self.ps[ps_i].bitcast(BF16)
        t_pb = self.pst[ps_i]
        for c in range(8):
            P.op("pe", lambda e, c=c: e.transpose(pb[:, c * 128:(c + 1) * 128], xn[:, c * 128:(c + 1) * 128], self.identb),
                 reads=[t_xn, self.t_ident], writes=[t_pb], skip_self=True, partial=(c > 0))
        for c in range(8):
            if c % 2 == 0:
                P.op("act", lambda e, c=c: e.activation(out=dst[:, c, col0:col0 + 128], in_=pb[:, c * 128:(c + 1) * 128], func=AF.Identity,
                                                         scale=svec[:, c, v:v + 1], bias=bvec[:, c, v:v + 1]),
                     reads=[t_pb, self.t_s12, self.t_mod], writes=[t_dst], partial=True)
            else:
                P.op("dve", lambda e, c=c: e.tensor_scalar(out=dst[:, c, col0:col0 + 128], in0=pb[:, c * 128:(c + 1) * 128],
                                                           scalar1=svec[:, c, v:v + 1], scalar2=bvec[:, c, v:v + 1], op0=ALU.mult, op1=ALU.add),
                     reads=[t_pb, self.t_s12, self.t_mod], writes=[t_dst], partial=True)

    def load_cast_weight(self, st, dst, t_dst, src_ap, ncols, blk=512, k_chunks=8, engs=None):
        P = self.P
        engs = engs or ("dve", "act")
        stg = [self.sb(st, f"wstg{id(dst) % 9973}_{i}", [128, k_chunks, blk]) for i in range(2)]
        t_stg = [Tok(), Tok()]
        i = 0
        for c0 in range(0, ncols, blk):
            w = min(blk, ncols - c0)
            s = i % 2
            P.dma("sp" if i % 2 == 0 else "act", stg[s][:, :, 0:w], src_ap[:, c0:c0 + w].rearrange("(c p) n -> p c n", p=128), writes=[t_stg[s]])
            eng = engs[i % len(engs)]
            if eng == "act":
                P.op("act", lambda e, s=s, c0=c0, w=w: e.copy(out=dst[:, :, c0:c0 + w], in_=stg[s][:, :, 0:w]), reads=[t_stg[s]], writes=[t_dst], partial=True)
            else:
                P.op(eng, lambda e, s=s, c0=c0, w=w: e.tensor_copy(out=dst[:, :, c0:c0 + w], in_=stg[s][:, :, 0:w]), reads=[t_stg[s]], writes=[t_dst], partial=True)
            i += 1

    def phase_inproj(self, li):
        nc, P = self.nc, self.P
        NW = N_IN + 32
        with contextlib.ExitStack() as st:
            wb = self.sb(st, "winb", [128, 8, NW], BF16)
            t_wb = Tok()
            with contextlib.ExitStack() as st2:
                self.load_cast_weight(st2, wb, t_wb, self.w_in[li], N_IN)
                P.op("dve", lambda e: e.tensor_scalar(out=wb[:, :, N_IN:N_IN + 16], in0=wb[:, :, 656:672], scalar1=-1.0, scalar2=None, op0=ALU.mult),
                     reads=[t_wb], writes=[t_wb], partial=True)
                P.op("dve", lambda e: e.tensor_copy(out=wb[:, :, N_IN + 16:N_IN + 32], in_=wb[:, :, 640:656]), reads=[t_wb], writes=[t_wb], partial=True)
                P.barrier()
            NB = 2
            bufs = []
            for i in range(NB):
                bufs.append((self.sb(st, f"xt{i}", [128, D]), Tok(), self.sb(st, f"sq{i}", [128, D], BF16), Tok(),
                             self.sb(st, f"ss{i}", [128, 4]), Tok(), self.sb(st, f"xn{i}", [128, D], BF16), Tok()))
            hT = [self.sb(st, f"hT{i}", [128, 8, 256], BF16) for i in range(2)]
            t_hT = [Tok(), Tok()]
            stage = [self.sb(st, f"stg{i}", [128, 16, 256]) for i in range(2)]
            t_stage = [Tok(), Tok()]
            svo = [self.sb(st, f"svo{i}", [128, 512]) for i in range(2)]
            t_svo = [Tok(), Tok()]
            chunks = [(0, 128), (128, 128), (256, 128), (384, 128), (512, 128), (640, 32), (672, 128), (800, 128), (1440, 16)]
            chunks += [(HY_LO + 128 * i, 128) for i in range(6)] + [(N_IN, 32)]
            groups = [(0, 3), (3, 2), (5, 1), (6, 2), (8, 1), (9, 6), (15, 1)]
            nsub = 0
            for ti in range(TT // 256):
                t0 = ti * 256
                v = 1 if ti == 0 else 0
                hs = ti % 2
                for sub in range(2):
                    self.norm_transpose(bufs[nsub % NB], self.XS[t0 + sub * 128:t0 + (sub + 1) * 128, :], self.t_XS, self.s1,
                                        self.modT[:, 0:8, :], v, hT[hs], t_hT[hs], sub * 128, ps_i=nsub % 2)
                    nsub += 1
                sg = stage[hs]
                for ci, (c0, M) in enumerate(chunks):
                    pi = 2 + ci % 4
                    for k in range(8):
                        P.op("pe", lambda e, pi=pi, k=k, c0=c0, M=M, hs=hs: e.matmul(self.ps[pi][0:M, 0:256], lhsT=wb[:, k, c0:c0 + M], rhs=hT[hs][:, k, :],
                                                                               start=(k == 0), stop=(k == 7)),
                             reads=[t_wb, t_hT[hs]], writes=[self.pst[pi]], skip_self=True)
                    if ci % 2 == 0:
                        P.op("act", lambda e, pi=pi, M=M, ci=ci: e.copy(out=sg[0:M, ci, :], in_=self.ps[pi][0:M, 0:256]), reads=[self.pst[pi]], writes=[t_stage[hs]], partial=True)
                    else:
                        P.op("dve", lambda e, pi=pi, M=M, ci=ci: e.tensor_copy(out=sg[0:M, ci, :], in_=self.ps[pi][0:M, 0:256]), reads=[self.pst[pi]], writes=[t_stage[hs]], partial=True)
                for (g0, gn) in groups:
                    c0, M = chunks[g0]
                    dst = self.PT[c0:c0 + M * gn, t0:t0 + 256]
                    if gn > 1:
                        dst = dst.rearrange("(c p) t -> p c t", p=128)
                        P.dma("pool", dst, sg[:, g0:g0 + gn, :], reads=[t_stage[hs]], writes=[self.t_PT], partial=True)
                    else:
                        P.dma("pool", dst, sg[0:M, g0, :], reads=[t_stage[hs]], writes=[self.t_PT], partial=True)
                for sub in range(2):
                    pi = 6 + sub
                    for k in range(8):
                        P.op("pe", lambda e, pi=pi, k=k, sub=sub, hs=hs: e.matmul(self.ps[pi], lhsT=hT[hs][:, k, sub * 128:(sub + 1) * 128], rhs=wb[:, k, 928:1440],
                                                                               start=(k == 0), stop=(k == 7)),
                             reads=[t_wb, t_hT[hs]], writes=[self.pst[pi]], skip_self=True)
                    P.op("act" if sub == 0 else "dve", (lambda e, pi=pi, sub=sub: e.copy(out=svo[sub], in_=self.ps[pi])) if sub == 0 else
                         (lambda e, pi=pi, sub=sub: e.tensor_copy(out=svo[sub], in_=self.ps[pi])), reads=[self.pst[pi]], writes=[t_svo[sub]])
                    P.dma("pool", self.PVO[t0 + sub * 128:t0 + (sub + 1) * 128, :], svo[sub], reads=[t_svo[sub]], writes=[self.t_PVO], partial=True)
            P.barrier()

    def rms_bcast(self, src, nk, n, ones, t_ones, nfeat, ps_i, R, t_R, sq, t_sq, t_src):
        P = self.P
        P.op("act", lambda e: e.activation(out=sq[:, 0:nk, 0:n], in_=src[:, 0:nk, 0:n], func=AF.Square), reads=[t_src], writes=[t_sq])
        ps, t_ps = self.ps[ps_i], self.pst[ps_i]
        for k in range(nk):
            P.op("pe", lambda e, k=k: e.matmul(ps[:, 0:n], lhsT=ones, rhs=sq[:, k, 0:n], start=(k == 0), stop=(k == nk - 1)),
                 reads=[t_ones, t_sq], writes=[t_ps], skip_self=True)
        P.op("dve", lambda e: e.tensor_scalar(out=R[:, 0:n], in0=ps[:, 0:n], scalar1=1.0 / nfeat, scalar2=EPS, op0=ALU.mult, op1=ALU.add),
             reads=[t_ps], writes=[t_R])
        P.op("act", lambda e: e.activation(out=R[:, 0:n], in_=R[:, 0:n], func=AF.Sqrt), reads=[t_R], writes=[t_R])
        P.op("dve", lambda e: e.reciprocal(out=R[:, 0:n], in_=R[:, 0:n]), reads=[t_R], writes=[t_R])

    def phase_mla(self, li):
        nc, P = self.nc, self.P
        last = li == DEPTH - 1
        scale = float(96 ** -0.5)
        with contextlib.ExitStack() as st:
            sb = lambda name, shape, dt=F32: self.sb(st, name, shape, dt)
            ones = sb("onesb", [128, 128], BF16); t_ones = Tok()
            P.op("pool", lambda e: e.memset(ones, 1.0), writes=[t_ones])
            KT = sb("KT", [128, NH, TT], BF16); t_KT = Tok()
            VP = sb("VP", [128, TT // 128, NH, 65], BF16); t_VP = Tok()
            P.op("pool", lambda e: e.memset(VP, 1.0), writes=[t_VP])
            sel65 = sb("sel65", [128, 64]); t_sel = Tok()
            P.op("pool", lambda e: e.memset(sel65, 0.0), writes=[t_sel])
            P.op("pool", lambda e: e.memset(sel65[64:65, :], 1.0), reads=[t_sel], writes=[t_sel])
            wqb = sb("wqb", [128, 3, NH, 192], BF16); t_wq = Tok()
            wkb = sb("wkb", [128, 2, NH, 64], BF16); t_wk = Tok()
            wvb = sb("wvb", [128, 2, NH, 64], BF16); t_wv = Tok()
            mk = sb("mk", [128, NH]); t_mk = Tok()
            P.op("pool", lambda e: e.memset(mk, 0.0), writes=[t_mk])
            with contextlib.ExitStack() as st2:
                wq = self.sb(st2, "wq32", [128, 3, NH * 96]); t_wq32 = Tok()
                wkv = self.sb(st2, "wkv32", [128, 2, NH * 128]); t_wkv32 = Tok()
                gq, t_gq = self.vec_pc(st2, "gq", self.mla_q_norm_g[li], 3)
                gkv, t_gkv = self.vec_pc(st2, "gkv", self.mla_kv_norm_g[li], 2)
                P.dma("sp", wq, self.mla_w_uq[li].rearrange("(c p) n -> p c n", p=128), writes=[t_wq32])
                P.dma("act", wkv, self.mla_w_ukv[li].rearrange("(c p) n -> p c n", p=128), writes=[t_wkv32])
                for k in range(3):
                    P.op("dve", lambda e, k=k: e.tensor_scalar(out=wq[:, k, :], in0=wq[:, k, :], scalar1=gq[:, k:k + 1], scalar2=None, op0=ALU.mult),
                         reads=[t_gq], writes=[t_wq32])
                for k in range(2):
                    P.op("dve", lambda e, k=k: e.tensor_scalar(out=wkv[:, k, :], in0=wkv[:, k, :], scalar1=gkv[:, k:k + 1], scalar2=None, op0=ALU.mult),
                         reads=[t_gkv], writes=[t_wkv32])
                wq4 = wq.rearrange("p k (h d) -> p k h d", d=96)
                wkv4 = wkv.rearrange("p k (h d) -> p k h d", d=128)
                P.op("pool", lambda e: e.memset(wqb, 0.0), writes=[t_wq])
                for k in range(3):
                    P.op("dve", lambda e, k=k: e.tensor_copy(out=wqb[:, k, :, 0:96], in_=wq4[:, k, :, 0:96]), reads=[t_wq32], writes=[t_wq], partial=True)
                    P.op("dve", lambda e, k=k: e.tensor_scalar(out=wqb[:, k, :, 160:176], in0=wq4[:, k, :, 80:96], scalar1=-1.0, scalar2=None, op0=ALU.mult),
                         reads=[t_wq32], writes=[t_wq], partial=True)
                    P.op("dve", lambda e, k=k: e.tensor_copy(out=wqb[:, k, :, 176:192], in_=wq4[:, k, :, 64:80]), reads=[t_wq32], writes=[t_wq], partial=True)
                for k in range(2):
                    P.op("dve", lambda e, k=k: e.tensor_copy(out=wkb[:, k, :, :], in_=wkv4[:, k, :, 0:64]), reads=[t_wkv32], writes=[t_wk], partial=True)
                    P.op("dve", lambda e, k=k: e.tensor_copy(out=wvb[:, k, :, :], in_=wkv4[:, k, :, 64:128]), reads=[t_wkv32], writes=[t_wv], partial=True)
                P.barrier()
            NBUF = 2
            pin = [sb(f"pin{i}", [128, 3, 512]) for i in range(NBUF)]; t_pin = [Tok() for _ in range(NBUF)]
            cs = [sb(f"cs{i}", [128, 2, 512]) for i in range(NBUF)]; t_cs = [Tok() for _ in range(NBUF)]
            sq = sb("sqm", [128, 3, 512], BF16); t_sq = Tok()
            R = sb("Rm", [128, 512]); t_R = Tok()
            pn = sb("pn", [128, 3, 512], BF16); t_pn = Tok()
            tmpa = sb("tmpa", [128, 512]); t_tmpa = Tok()
            tmpb = sb("tmpb", [128, 512]); t_tmpb = Tok()
            sqk = sb("sqk", [128, NH, 512], BF16); t_sqk = Tok()
            B_singles = (sq, t_sq, R, t_R, pn, t_pn, tmpa, t_tmpa, tmpb, t_tmpb, sqk, t_sqk)
            stA = contextlib.ExitStack()
            krin = [self.sb(stA, f"krin{i}", [128, 2, 512]) for i in range(NBUF)]; t_krin = [Tok() for _ in range(NBUF)]
            tchunks = [(0, CTX)] + [(CTX + 512 * j, 512) for j in range(SEQ // 512)]

            def load_rope_tables(bi, t0, n):
                p0 = t0 - CTX
                P.dma("act", cs[bi][64:96, 0, 0:n], self.c_rope_cos[:, p0:p0 + n], writes=[t_cs[bi]], partial=True)
                P.dma("act", cs[bi][64:96, 1, 0:n], self.c_rope_sin[:, p0:p0 + n], writes=[t_cs[bi]], partial=True)

            A_sq = [self.sb(stA, f"Asq{i}", [128, 3, 512], BF16) for i in range(2)]; tA_sq = [Tok(), Tok()]
            A_R = [self.sb(stA, f"AR{i}", [128, 512]) for i in range(2)]; tA_R = [Tok(), Tok()]
            A_pn = [self.sb(stA, f"Apn{i}", [128, 2, 512], BF16) for i in range(2)]; tA_pn = [Tok(), Tok()]
            A_ta = [self.sb(stA, f"Ata{i}", [128, 512]) for i in range(2)]; tA_ta = [Tok(), Tok()]
            A_tb = [self.sb(stA, f"Atb{i}", [128, 512]) for i in range(2)]; tA_tb = [Tok(), Tok()]
            A_krb = [self.sb(stA, f"Akrb{i}", [128, 512], BF16) for i in range(2)]; tA_krb = [Tok(), Tok()]
            A_sqk = [self.sb(stA, f"Asqk{i}", [128, NH, 512], BF16) for i in range(2)]; tA_sqk = [Tok(), Tok()]
            A_mt = [self.sb(stA, f"Amt{i}", [128, NH]) for i in range(2)]; tA_mt = [Tok(), Tok()]
            for ci, (t0, n) in enumerate(tchunks):
                bi = ci % NBUF
                is_ctx = ci == 0
                sq, t_sq, R, t_R, pn, t_pn = A_sq[bi], tA_sq[bi], A_R[bi], tA_R[bi], A_pn[bi], tA_pn[bi]
                tmpa, t_tmpa, tmpb, t_tmpb, krb, t_krb = A_ta[bi], tA_ta[bi], A_tb[bi], tA_tb[bi], A_krb[bi], tA_krb[bi]
                sqk, t_sqk, mtmp, t_mtmp = A_sqk[bi], tA_sqk[bi], A_mt[bi], tA_mt[bi]
                P.dma("sp", pin[bi][:, 0:2, 0:n], self.PT[QR:QR + KVR, t0:t0 + n].rearrange("(c p) t -> p c t", p=128), reads=[self.t_PT], writes=[t_pin[bi]])
                P.dma("sp", krin[bi][64:96, 0, 0:n], self.PT[640:672, t0:t0 + n], reads=[self.t_PT], writes=[t_krin[bi]], partial=True)
                if not is_ctx:
                    P.dma("sp", krin[bi][64:96, 1, 0:n], self.PT[N_IN:N_IN + 32, t0:t0 + n], reads=[self.t_PT], writes=[t_krin[bi]], partial=True)
                    load_rope_tables(bi, t0, n)
                self.rms_bcast(pin[bi], 2, n, ones, t_ones, KVR, 6, R, t_R, sq, t_sq, t_pin[bi])
                P.op("dve", lambda e, bi=bi, n=n: e.tensor_tensor(out=pn[:, 0:2, 0:n], in0=pin[bi][:, 0:2, 0:n], in1=R[:, 0:n].unsqueeze(1).to_broadcast([128, 2, n]), op=ALU.mult),
                     reads=[t_pin[bi], t_R], writes=[t_pn])
                if is_ctx:
                    P.op("dve", lambda e, bi=bi, n=n: e.tensor_copy(out=krb[64:96, 0:n], in_=krin[bi][64:96, 0, 0:n]), reads=[t_krin[bi]], writes=[t_krb])
                else:
                    P.op("dve", lambda e, bi=bi, n=n: e.tensor_tensor(out=tmpa[64:96, 0:n], in0=krin[bi][64:96, 0, 0:n], in1=cs[bi][64:96, 0, 0:n], op=ALU.mult), reads=[t_krin[bi], t_cs[bi]], writes=[t_tmpa])
                    P.op("dve", lambda e, bi=bi, n=n: e.tensor_tensor(out=tmpb[64:96, 0:n], in0=krin[bi][64:96, 1, 0:n], in1=cs[bi][64:96, 1, 0:n], op=ALU.mult), reads=[t_krin[bi], t_cs[bi]], writes=[t_tmpb])
                    P.op("dve", lambda e, n=n: e.tensor_tensor(out=krb[64:96, 0:n], in0=tmpa[64:96, 0:n], in1=tmpb[64:96, 0:n], op=ALU.add), reads=[t_tmpa, t_tmpb], writes=[t_krb])
                P.op("dve", lambda e, t0=t0, n=n: e.tensor_copy(out=KT[64:96, :, t0:t0 + n], in_=krb[64:96, 0:n].unsqueeze(1).to_broadcast([32, NH, n])), reads=[t_krb], writes=[t_KT], partial=True)
                for h in range(NH):
                    pi = 4 + h % 2
                    for k in range(2):
                        P.op("pe", lambda e, h=h, k=k, pi=pi, n=n: e.matmul(self.ps[pi][0:64, 0:n], lhsT=wkb[:, k, h, :], rhs=pn[:, k, 0:n], start=(k == 0), stop=(k == 1)),
                             reads=[t_wk, t_pn], writes=[self.pst[pi]], skip_self=True)
                    if h % 2 == 0:
                        P.op("act", lambda e, h=h, pi=pi, t0=t0, n=n: e.copy(out=KT[0:64, h, t0:t0 + n], in_=self.ps[pi][0:64, 0:n]), reads=[self.pst[pi]], writes=[t_KT], partial=True)
                    else:
                        P.op("dve", lambda e, h=h, pi=pi, t0=t0, n=n: e.tensor_copy(out=KT[0:64, h, t0:t0 + n], in_=self.ps[pi][0:64, 0:n]), reads=[self.pst[pi]], writes=[t_KT], partial=True)
                for sub in range(n // 128):
                    j = (t0 + sub * 128) // 128
                    pi = 2 + sub % 2
                    for k in range(2):
                        P.op("pe", lambda e, k=k, pi=pi, sub=sub: e.matmul(self.ps[pi], lhsT=pn[:, k, sub * 128:(sub + 1) * 128], rhs=wvb[:, k, :, :].rearrange("p h d -> p (h d)"), start=(k == 0), stop=(k == 1)),
                             reads=[t_wv, t_pn], writes=[self.pst[pi]], skip_self=True)
                    src = self.ps[pi].rearrange("p (h d) -> p h d", d=64)
                    if sub % 2 == 0:
                        P.op("act", lambda e, j=j, src=src: e.copy(out=VP[:, j, :, 0:64], in_=src), reads=[self.pst[pi]], writes=[t_VP], partial=True)
                    else:
                        P.op("dve", lambda e, j=j, src=src: e.tensor_copy(out=VP[:, j, :, 0:64], in_=src), reads=[self.pst[pi]], writes=[t_VP], partial=True)
                P.op("act", lambda e, t0=t0, n=n: e.activation(out=sqk[0:96, :, 0:n], in_=KT[0:96, :, t0:t0 + n], func=AF.Square), reads=[t_KT], writes=[t_sqk])
                for h in range(NH):
                    pi = (7, 0, 1)[h % 3]
                    P.op("pe", lambda e, h=h, n=n, pi=pi: e.matmul(self.ps[pi][:, 0:n], lhsT=ones[0:96, :], rhs=sqk[0:96, h, 0:n], start=True, stop=True),
                         reads=[t_ones, t_sqk], writes=[self.pst[pi]], skip_self=True)
                    P.op("dve", lambda e, h=h, n=n, pi=pi: e.tensor_reduce(out=mtmp[:, h:h + 1], in_=self.ps[pi][:, 0:n], axis=AX.X, op=ALU.max), reads=[self.pst[pi]], writes=[t_mtmp], partial=True)
                P.op("dve", lambda e: e.tensor_tensor(out=mk, in0=mk, in1=mtmp, op=ALU.max), reads=[t_mtmp], writes=[t_mk])
            P.barrier()
            stA.close()
            sq, t_sq, R, t_R, pn, t_pn, tmpa, t_tmpa, tmpb, t_tmpb, sqk, t_sqk = B_singles
            QT = [sb(f"QT{i}", [128, NH, 512], BF16) for i in range(2)]; t_QT = [Tok(), Tok()]
            pts = [sb(f"pts{i}", [128, 2, 512], BF16) for i in range(3)]; t_pts = [Tok() for _ in range(3)]
            den = sb("den", [128, 512]); t_den = Tok()
            rden = sb("rden", [128, 512]); t_rden = Tok()
            ot = [sb(f"ot{i}", [128, 512], BF16) for i in range(2)]; t_ot = [Tok(), Tok()]
            mq = [sb(f"mq{i}", [128, NH]) for i in range(2)]; t_mq = [Tok(), Tok()]
            negm = [sb(f"negm{i}", [128, NH]) for i in range(2)]; t_negm = [Tok(), Tok()]
            qchunks = ([] if last else [(0, CTX)]) + tchunks[1:]
            nq = len(qchunks)

            def prologue_parts(qi):
                t0, n = qchunks[qi]
                bi = qi % NBUF
                is_ctx = t0 == 0
                qt, t_qt = QT[qi % 2], t_QT[qi % 2]
                parts = {}

                def part_load():
                    P.dma("sp", pin[bi][:, 0:3, 0:n], self.PT[0:QR, t0:t0 + n].rearrange("(c p) t -> p c t", p=128), reads=[self.t_PT], writes=[t_pin[bi]])
                    if not is_ctx:
                        load_rope_tables(bi, t0, n)
                    self.rms_bcast(pin[bi], 3, n, ones, t_ones, QR, 5, R, t_R, sq, t_sq, t_pin[bi])
                    P.op("dve", lambda e: e.tensor_tensor(out=pn[:, 0:3, 0:n], in0=pin[bi][:, 0:3, 0:n], in1=R[:, 0:n].unsqueeze(1).to_broadcast([128, 3, n]), op=ALU.mult),
                         reads=[t_pin[bi], t_R], writes=[t_pn])
                parts["load"] = part_load

                def part_A(h):
                    for k in range(3):
                        P.op("pe", lambda e, k=k: e.matmul(self.ps[7][0:96, 0:n], lhsT=wqb[:, k, h, 0:96], rhs=pn[:, k, 0:n], start=(k == 0), stop=(k == 2)),
                             reads=[t_wq, t_pn], writes=[self.pst[7]], skip_self=True)
                    if is_ctx:
                        P.op("dve", lambda e: e.tensor_copy(out=qt[0:96, h, 0:n], in_=self.ps[7][0:96, 0:n]), reads=[self.pst[7]], writes=[t_qt], partial=True)
                        return
                    P.op("dve", lambda e: e.tensor_tensor(out=tmpa[64:96, 0:n], in0=self.ps[7][64:96, 0:n], in1=cs[bi][64:96, 0, 0:n], op=ALU.mult), reads=[self.pst[7], t_cs[bi]], writes=[t_tmpa])
                    P.op("dve", lambda e: e.tensor_copy(out=qt[0:64, h, 0:n], in_=self.ps[7][0:64, 0:n]), reads=[self.pst[7]], writes=[t_qt], partial=True)

                def part_B(h):
                    if is_ctx:
                        return
                    for k in range(3):
                        P.op("pe", lambda e, k=k: e.matmul(self.ps[6][0:96, 0:n], lhsT=wqb[:, k, h, 96:192], rhs=pn[:, k, 0:n], start=(k == 0), stop=(k == 2)),
                             reads=[t_wq, t_pn], writes=[self.pst[6]], skip_self=True)
                    P.op("dve", lambda e: e.tensor_tensor(out=tmpb[64:96, 0:n], in0=self.ps[6][64:96, 0:n], in1=cs[bi][64:96, 1, 0:n], op=ALU.mult), reads=[self.pst[6], t_cs[bi]], writes=[t_tmpb])
                    P.op("dve", lambda e: e.tensor_tensor(out=qt[64:96, h, 0:n], in0=tmpa[64:96, 0:n], in1=tmpb[64:96, 0:n], op=ALU.add), reads=[t_tmpa, t_tmpb], writes=[t_qt], partial=True)

                def part_N(h):
                    P.op("dve", lambda e: e.tensor_tensor(out=sqk[0:96, h, 0:n], in0=qt[0:96, h, 0:n], in1=qt[0:96, h, 0:n], op=ALU.mult), reads=[t_qt], writes=[t_sqk], partial=True)
                    P.op("pe", lambda e: e.matmul(self.ps[7][:, 0:n], lhsT=ones[0:96, :], rhs=sqk[0:96, h, 0:n], start=True, stop=True),
                         reads=[t_ones, t_sqk], writes=[self.pst[7]], skip_self=True)
                    P.op("dve", lambda e: e.tensor_reduce(out=mq[qi % 2][:, h:h + 1], in_=self.ps[7][:, 0:n], axis=AX.X, op=ALU.max), reads=[self.pst[7]], writes=[t_mq[qi % 2]], partial=True)

                def part_fin():
                    nm, t_nm = negm[qi % 2], t_negm[qi % 2]
                    P.op("dve", lambda e: e.tensor_tensor(out=nm, in0=mq[qi % 2], in1=mk, op=ALU.mult), reads=[t_mq[qi % 2], t_mk], writes=[t_nm])
                    P.op("act", lambda e: e.activation(out=nm, in_=nm, func=AF.Sqrt), reads=[t_nm], writes=[t_nm])
                    P.op("dve", lambda e: e.tensor_scalar(out=nm, in0=nm, scalar1=-scale, scalar2=None, op0=ALU.mult), reads=[t_nm], writes=[t_nm])
                for h in range(NH):
                    parts[("A", h)] = (lambda h=h: part_A(h))
                    parts[("B", h)] = (lambda h=h: part_B(h))
                    parts[("N", h)] = (lambda h=h: part_N(h))
                parts["fin"] = part_fin
                return parts

            def emit_all(parts):
                parts["load"]()
                for h in range(NH):
                    parts[("A", h)](); parts[("B", h)](); parts[("N", h)]()
                parts["fin"]()

            def groups_of(qi):
                t0, n = qchunks[qi]
                ktiles = list(range(CTX // 128)) if t0 == 0 else list(range(TT // 128))
                return [ktiles[i:i + 2] for i in range(0, len(ktiles), 2)]

            def s_mm(qi, h, g):
                t0, n = qchunks[qi]
                qt, t_qt = QT[qi % 2], t_QT[qi % 2]
                b0 = 2 * (g % 2)
                for i, j in enumerate(groups_of(qi)[g]):
                    P.op("pe", lambda e, j=j, i=i: e.matmul(self.ps[b0 + i][:, 0:n], lhsT=KT[0:96, h, j * 128:(j + 1) * 128], rhs=qt[0:96, h, 0:n], start=True, stop=True),
                         reads=[t_KT, t_qt], writes=[self.pst[b0 + i]], skip_self=True)

            emit_all(prologue_parts(0))
            jobs = [(qi, h) for qi in range(nq) for h in range(NH)]
            pcount = 0
            pre_issued = set()
            nxt_parts = None
            for ji, (qi, h) in enumerate(jobs):
                t0, n = qchunks[qi]
                groups = groups_of(qi)
                ng = len(groups)
                po = 4
                nm, t_nm = negm[qi % 2], t_negm[qi % 2]
                if h == 0:
                    nxt_parts = prologue_parts(qi + 1) if qi + 1 < nq else None
                sched = {}
                if nxt_parts is not None and ng >= 16:
                    if h == 0:
                        sched[2] = ["load"]
                    else:
                        sched[3] = [("A", h - 1)]
                        sched[7] = [("B", h - 1)]
                        sched[11] = [("N", h - 1)]
                    if h == NH - 1:
                        sched[12] = [("A", h)]
                        sched[14] = [("B", h)]
                        sched[16] = [("N", h), "fin"]
                if (qi, h) not in pre_issued:
                    s_mm(qi, h, 0)
                    if ng > 1:
                        s_mm(qi, h, 1)
                for g in range(ng):
                    b0 = 2 * (g % 2)
                    nj = len(groups[g])
                    pb = pcount % 3
                    pcount += 1
                    src = self.psall[:, b0 * 512:(b0 + nj) * 512].rearrange("p (a c) -> p a c", c=512)[:, :, 0:n]
                    P.op("act", lambda e, pb=pb, nj=nj, src=src: e.activation(out=pts[pb][:, 0:nj, 0:n], in_=src, func=AF.Exp, bias=nm[:, h:h + 1], scale=scale),
                         reads=[self.pst[b0 + i] for i in range(nj)] + [t_nm], writes=[t_pts[pb]])
                    for i, j in enumerate(groups[g]):
                        first = (g == 0 and i == 0)
                        lastk = (g == ng - 1 and i == nj - 1)
                        P.op("pe", lambda e, pb=pb, j=j, i=i, first=first, lastk=lastk: e.matmul(self.ps[po][0:65, 0:n], lhsT=VP[:, j, h, :], rhs=pts[pb][:, i, 0:n], start=first, stop=lastk),
                             reads=[t_VP, t_pts[pb]], writes=[self.pst[po]], skip_self=True)
                    if g + 2 < ng:
                        s_mm(qi, h, g + 2)
                    for key in sched.get(g, []):
                        nxt_parts[key]()
                if ji + 1 < len(jobs):
                    qn, hn = jobs[ji + 1]
                    if qn == qi:
                        s_mm(qn, hn, 0)
                        if len(groups_of(qn)) > 1:
                            s_mm(qn, hn, 1)
                        pre_issued.add((qn, hn))
                o = ot[h % 2]
                t_o = t_ot[h % 2]
                P.op("act", lambda e: e.copy(out=den[0:65, 0:n], in_=self.ps[po][0:65, 0:n]), reads=[self.pst[po]], writes=[t_den])
                P.op("pe", lambda e: e.matmul(self.ps[5][0:64, 0:n], lhsT=sel65[0:65, :], rhs=den[0:65, 0:n], start=True, stop=True),
                     reads=[t_sel, t_den], writes=[self.pst[5]], skip_self=True)
                P.op("dve", lambda e: e.reciprocal(out=rden[0:64, 0:n], in_=self.ps[5][0:64, 0:n]), reads=[self.pst[5]], writes=[t_rden])
                P.op("dve", lambda e, o=o: e.tensor_tensor(out=o[0:64, 0:n], in0=den[0:64, 0:n], in1=rden[0:64, 0:n], op=ALU.mult),
                     reads=[t_den, t_rden], writes=[t_o])
                P.dma("pool", self.MIXT[h * 64:(h + 1) * 64, t0:t0 + n], o[0:64, 0:n], reads=[t_o], writes=[self.t_MIXT], partial=True)
                if nxt_parts is not None and ng < 16 and h == NH - 1:
                    emit_all(nxt_parts)
            P.barrier()

    def phase_mlstm(self, li):
        nc, P = self.nc, self.P
        NT = TT // 128
        with contextlib.ExitStack() as st:
            sb = lambda name, shape, dt=F32: self.sb(st, name, shape, dt)
            ub = [sb(f"ub{p}", [128, TT], BF16) for p in range(2)]; t_ub = [Tok(), Tok()]
            kT = [sb(f"kT{p}", [128, TT], BF16) for p in range(2)]; t_kT = [Tok(), Tok()]
            qd = [[sb(f"qd{p}{d}", [128, TT], BF16) for d in range(2)] for p in range(2)]
            t_qd = [[Tok(), Tok()], [Tok(), Tok()]]
            ktok = sb("ktok", [128, NT, 256], BF16); t_ktok = Tok()
            wtok = sb("wtok", [128, NT, 8]); t_wtok = Tok()
            ecol = sb("ecol", [128, 4, NT]); t_ecol = Tok()
            hF = sb("hF", [128, NT, 256]); t_hF = Tok()
            CbS = [[sb(f"CbS{p}{d}", [128, NT + 1, 65], BF16) for d in range(2)] for p in range(2)]
            t_CbS = [[Tok(), Tok()], [Tok(), Tok()]]
            Cst = [[sb(f"Cst{p}{d}", [128, 65]) for d in range(2)] for p in range(2)]
            t_Cst = [[Tok(), Tok()], [Tok(), Tok()]]
            masks = sb("mlmask", [128, 2, 128], BF16); t_masks = Tok()
            P.dma("sp", masks, self.c_ml_mask, writes=[t_masks])
            wqb = sb("mlwq", [128, 2, 128], BF16); wkb = sb("mlwk", [128, 2, 128], BF16); t_w = Tok()
            gml, t_gml = self.vec_pc(st, "gml", self.ml_norm_g[li], 2)
            cw = sb("mlcw", [128, 2, 3]); cb = sb("mlcb", [128, 2]); t_cw = Tok()
            for jj in range(3):
                P.dma("sp", cw[:, :, jj], self.ml_conv_w[li, jj].rearrange("(c p) -> p c", p=128), writes=[t_cw], partial=True, allow_slow_non_contiguous=True)
            P.dma("sp", cb, self.ml_conv_b[li].rearrange("(c p) -> p c", p=128), writes=[t_cw], partial=True, allow_slow_non_contiguous=True)
            for p in range(2):
                for d in range(2):
                    P.op("pool", lambda e, p=p, d=d: e.memset(Cst[p][d], 0.0), writes=[t_Cst[p][d]])
                    P.op("pool", lambda e, p=p, d=d: e.memset(CbS[p][d][:, 0, :], 0.0), writes=[t_CbS[p][d]])
            with contextlib.ExitStack() as st2:
                sb2 = lambda name, shape, dt=F32: self.sb(st2, name, shape, dt)
                w32 = sb2("mlw32", [128, 4, 128]); t_w32 = Tok()
                P.op("pool", lambda e: e.memset(w32, 0.0), writes=[t_w32])
                for p in range(2):
                    for hh in range(2):
                        P.dma("sp", w32[hh * 64:(hh + 1) * 64, p, hh * 64:(hh + 1) * 64], self.ml_wq[li, 2 * p + hh], reads=[], writes=[t_w32], partial=True)
                        P.dma("sp", w32[hh * 64:(hh + 1) * 64, 2 + p, hh * 64:(hh + 1) * 64], self.ml_wk[li, 2 * p + hh], reads=[], writes=[t_w32], partial=True)
                P.op("dve", lambda e: e.tensor_copy(out=wqb, in_=w32[:, 0:2, :]), reads=[t_w32], writes=[t_w], partial=True)
                P.op("dve", lambda e: e.tensor_scalar(out=wkb, in0=w32[:, 2:4, :], scalar1=0.125, scalar2=None, op0=ALU.mult), reads=[t_w32], writes=[t_w], partial=True)
                selA = sb2("selA", [128, 4, 128]); selW = sb2("selW", [128, 8]); t_sel = Tok()
                P.dma("act", selA[0:96], self.c_ml_selA, writes=[t_sel], partial=True)
                P.dma("act", selW[0:96], self.c_ml_selW, writes=[t_sel], partial=True)
                gb = sb2("mlgb", [16, 1]); t_gb = Tok()
                P.dma("sp", gb, self.ml_gate_b[li].rearrange("(p o) -> p o", o=1), writes=[t_gb], allow_slow_non_contiguous=True)
                ones16 = sb2("ones16", [16, 128]); t_o16 = Tok()
                P.op("pool", lambda e: e.memset(ones16, 1.0), writes=[t_o16])
                X = sb2("mlX", [128, TT]); t_X = Tok()
                P.op("pool", lambda e: e.memset(X, 0.0), writes=[t_X])
                P.dma("sp", X[0:16, :], self.PT[1440:1456, :], reads=[self.t_PT], writes=[t_X], partial=True)
                P.op("dve", lambda e: e.tensor_scalar(out=X[0:16, :], in0=X[0:16, :], scalar1=gb[:, 0:1], scalar2=None, op0=ALU.add), reads=[t_gb, t_X], writes=[t_X])
                with contextlib.ExitStack() as st3:
                    LF = self.sb(st3, "mlLF", [16, TT]); t_LF = Tok()
                    CF = self.sb(st3, "mlCF", [16, TT]); t_CF = Tok()
                    P.op("act", lambda e: e.activation(out=LF, in_=X[0:16, :], func=AF.Exp, scale=-1.0), reads=[t_X], writes=[t_LF])
                    P.op("act", lambda e: e.activation(out=LF, in_=LF, func=AF.Ln, bias=1.0), reads=[t_LF], writes=[t_LF])
                    P.op("dve", lambda e: e.tensor_scalar(out=LF, in0=LF, scalar1=-1.0, scalar2=None, op0=ALU.mult), reads=[t_LF], writes=[t_LF])
                    for j in range(NT):
                        P.op("dve", lambda e, j=j: e.tensor_tensor_scan(out=CF[:, j * 128:(j + 1) * 128], data0=ones16, data1=LF[:, j * 128:(j + 1) * 128],
                                                                        initial=0.0, op0=ALU.mult, op1=ALU.add), reads=[t_LF, t_o16], writes=[t_CF], partial=True)
                    P.op("dve", lambda e: e.tensor_tensor(out=LF, in0=LF, in1=CF, op=ALU.subtract), reads=[t_CF], writes=[t_LF])
                    LF3 = LF.rearrange("p (j t) -> p j t", t=128)
                    CF3 = CF.rearrange("p (j t) -> p j t", t=128)
                    P.op("dve", lambda e: e.tensor_tensor(out=LF3, in0=LF3, in1=CF3[:, :, 127:128].to_broadcast([16, NT, 128]), op=ALU.add), reads=[t_CF], writes=[t_LF])
                    P.op("act", lambda e: e.copy(out=X[32:48, :], in_=CF), reads=[t_CF], writes=[t_X], partial=True)
                    P.op("act", lambda e: e.copy(out=X[64:80, :], in_=LF), reads=[t_LF], writes=[t_X], partial=True)
                    P.barrier()
                for j in range(NT):
                    P.op("pe", lambda e, j=j: e.matmul(self.ps[7][:, j * 8:(j + 1) * 8], lhsT=X[0:96, j * 128:(j + 1) * 128], rhs=selW[0:96, :], start=True, stop=True),
                         reads=[t_X, t_sel], writes=[self.pst[7]], skip_self=True)
                P.op("act", lambda e: e.activation(out=wtok.rearrange("p j g -> p (j g)"), in_=self.ps[7][:, 0:NT * 8], func=AF.Exp), reads=[self.pst[7]], writes=[t_wtok])
                pc32 = sb2("mlpc", [128, TT]); t_pc = Tok()
                u32 = sb2("mlu", [128, TT]); t_u = Tok()
                abc = [sb2(f"abc{i}", [128, 512]) for i in range(2)]; t_abc = [Tok(), Tok()]
                blocks = [(0, 256)] + [(256 + 512 * i, 512) for i in range(8)]
                segs = [(0, CTX), (CTX, TT)]
                nabc = 0
                for p in range(2):
                    P.dma("sp", pc32, self.PT[ML_LO + p * 128:ML_LO + (p + 1) * 128, :], reads=[self.t_PT], writes=[t_pc])
                    P.op("dve", lambda e, p=p: e.tensor_scalar(out=u32, in0=pc32, scalar1=cw[:, p, 1:2], scalar2=cb[:, p:p + 1], op0=ALU.mult, op1=ALU.add), reads=[t_pc, t_cw], writes=[t_u])
                    for (a, b) in segs:
                        P.op("dve", lambda e, p=p, a=a, b=b: e.scalar_tensor_tensor(out=u32[:, a + 1:b], in0=pc32[:, a:b - 1], scalar=cw[:, p, 0:1], in1=u32[:, a + 1:b], op0=ALU.mult, op1=ALU.add),
                             reads=[t_pc, t_cw], writes=[t_u])
                        P.op("dve", lambda e, p=p, a=a, b=b: e.scalar_tensor_tensor(out=u32[:, a:b - 1], in0=pc32[:, a + 1:b], scalar=cw[:, p, 2:3], in1=u32[:, a:b - 1], op0=ALU.mult, op1=ALU.add),
                             reads=[t_pc, t_cw], writes=[t_u])
                    P.op("act", lambda e, p=p: e.activation(out=ub[p], in_=u32, func=AF.Silu), reads=[t_u], writes=[t_ub[p]])
                    for (b0, n) in blocks:
                        P.op("pe", lambda e, p=p, b0=b0, n=n: e.matmul(self.ps[0][:, 0:n], lhsT=wqb[:, p, :], rhs=ub[p][:, b0:b0 + n], start=True, stop=True),
                             reads=[t_w, t_ub[p]], writes=[self.pst[0]], skip_self=True)
                        for d in range(2):
                            ai = nabc % 2
                            nabc += 1
                            P.op("pe", lambda e, p=p, d=d, b0=b0, n=n: e.matmul(self.ps[6][:, 0:n], lhsT=selA[0:96, p * 2 + d, :], rhs=X[0:96, b0:b0 + n], start=True, stop=True),
                                 reads=[t_sel, t_X], writes=[self.pst[6]], skip_self=True)
                            P.op("act", lambda e, ai=ai, n=n: e.activation(out=abc[ai][:, 0:n], in_=self.ps[6][:, 0:n], func=AF.Exp), reads=[self.pst[6]], writes=[t_abc[ai]])
                            P.op("dve", lambda e, p=p, d=d, ai=ai, b0=b0, n=n: e.tensor_tensor(out=qd[p][d][:, b0:b0 + n], in0=self.ps[0][:, 0:n], in1=abc[ai][:, 0:n], op=ALU.mult),
                                 reads=[self.pst[0], t_abc[ai]], writes=[t_qd[p][d]], partial=True)
                            c0 = 127 if d == 0 else 0
                            P.op("dve", lambda e, p=p, d=d, ai=ai, b0=b0, n=n, c0=c0: e.tensor_copy(out=ecol[:, p * 2 + d, b0 // 128:(b0 + n) // 128], in_=abc[ai][:, c0:n:128]),
                                 reads=[t_abc[ai]], writes=[t_ecol], partial=True)
                        P.op("pe", lambda e, p=p, b0=b0, n=n: e.matmul(self.ps[1][:, 0:n], lhsT=wkb[:, p, :], rhs=ub[p][:, b0:b0 + n], start=True, stop=True),
                             reads=[t_w, t_ub[p]], writes=[self.pst[1]], skip_self=True)
                        P.op("act", lambda e, p=p, b0=b0, n=n: e.copy(out=kT[p][:, b0:b0 + n], in_=self.ps[1][:, 0:n]), reads=[self.pst[1]], writes=[t_kT[p]], partial=True)
                    for j in range(NT):
                        pi = 2 + j % 2
                        P.op("pe", lambda e, p=p, j=j, pi=pi: e.matmul(self.ps[pi][:, 0:128], lhsT=ub[p][:, j * 128:(j + 1) * 128], rhs=wkb[:, p, :], start=True, stop=True),
                             reads=[t_w, t_ub[p]], writes=[self.pst[pi]], skip_self=True)
                        if j % 2 == 0:
                            P.op("act", lambda e, p=p, j=j, pi=pi: e.copy(out=ktok[:, j, p * 128:(p + 1) * 128], in_=self.ps[pi][:, 0:128]), reads=[self.pst[pi]], writes=[t_ktok], partial=True)
                        else:
                            P.op("dve", lambda e, p=p, j=j, pi=pi: e.tensor_copy(out=ktok[:, j, p * 128:(p + 1) * 128], in_=self.ps[pi][:, 0:128]), reads=[self.pst[pi]], writes=[t_ktok], partial=True)
                P.barrier()
            NB = 4
            hB = sb("hB", [128, NT, 256]); t_hB = Tok()
            vo = [sb(f"mlvo{i}", [128, 512]) for i in range(NB)]; t_vo = [Tok() for _ in range(NB)]
            vw = [sb(f"mlvw{i}", [128, 4, 65], BF16) for i in range(NB)]; t_vw = [Tok() for _ in range(NB)]
            Ssb = [sb(f"mlS{i}", [128, 4, 128], BF16) for i in range(NB)]; t_S = [Tok() for _ in range(NB)]
            tmpC = [sb(f"mltmpC{d}", [128, 65]) for d in range(2)]; t_tmpC = [Tok(), Tok()]
            dd = [sb(f"mldd{d}", [128, 4]) for d in range(2)]; t_dd = [Tok(), Tok()]
            orders = [list(range(NT)), [1, 0] + list(range(NT - 1, 1, -1))]
            step = 0
            for si in range(NT):
                for d in range(2):
                    j = orders[d][si]
                    bi = step % NB
                    step += 1
                    c0, c1 = j * 128, (j + 1) * 128
                    P.dma("sp", vo[bi][:, 0:256], self.PVO[c0:c1, 0:256], reads=[self.t_PVO], writes=[t_vo[bi]])
                    P.op("dve", lambda e, bi=bi, j=j, d=d: e.tensor_tensor(out=vw[bi][:, :, 0:64], in0=vo[bi][:, 0:256].rearrange("p (h c) -> p h c", c=64),
                                                                   in1=wtok[:, j, d * 4:(d + 1) * 4].unsqueeze(2).to_broadcast([128, 4, 64]), op=ALU.mult),
                         reads=[t_vo[bi], t_wtok], writes=[t_vw[bi]])
                    P.op("act", lambda e, bi=bi, j=j, d=d: e.copy(out=vw[bi][:, :, 64], in_=wtok[:, j, d * 4:(d + 1) * 4]), reads=[t_wtok], writes=[t_vw[bi]], partial=True)
                    for h in range(4):
                        p, r0 = h // 2, (h % 2) * 64
                        pS = 2 + h % 2
                        P.op("pe", lambda e, h=h, p=p, r0=r0, d=d, c0=c0, c1=c1, pS=pS: e.matmul(self.ps[pS][:, (h // 2) * 128:(h // 2 + 1) * 128], lhsT=kT[p][r0:r0 + 64, c0:c1], rhs=qd[p][d][r0:r0 + 64, c0:c1], start=True, stop=True),
                             reads=[t_kT[p], t_qd[p][d]], writes=[self.pst[pS]], skip_self=True)
                    for eo in range(2):
                        P.op("dve", lambda e, bi=bi, eo=eo, d=d: e.tensor_tensor(out=Ssb[bi][:, eo::2, :], in0=self.ps[2 + eo][:, 0:256].rearrange("p (h t) -> p h t", t=128),
                                                                         in1=masks[:, d, :].unsqueeze(1).to_broadcast([128, 2, 128]), op=ALU.mult),
                             reads=[self.pst[2 + eo], t_masks], writes=[t_S[bi]], partial=(eo > 0))
                    for h in range(4):
                        p, r0 = h // 2, (h % 2) * 64
                        pN = 4 + h % 2
                        cN = (h // 2) * 65
                        P.op("pe", lambda e, h=h, bi=bi, pN=pN, cN=cN: e.matmul(self.ps[pN][:, cN:cN + 65], lhsT=Ssb[bi][:, h, :], rhs=vw[bi][:, h, :], start=True, stop=False),
                             reads=[t_S[bi], t_vw[bi]], writes=[self.pst[pN]], skip_self=True)
                        P.op("pe", lambda e, h=h, p=p, r0=r0, d=d, si=si, c0=c0, c1=c1, pN=pN, cN=cN: e.matmul(self.ps[pN][:, cN:cN + 65], lhsT=qd[p][d][r0:r0 + 64, c0:c1], rhs=CbS[p][d][r0:r0 + 64, si, :], start=False, stop=True),
                             reads=[t_qd[p][d], t_CbS[p][d]], writes=[self.pst[pN]], skip_self=True)
                    for p in range(2):
                        pU = p + 6 * d
                        P.op("pe", lambda e, p=p, bi=bi, j=j, pU=pU: e.matmul(self.ps[pU][:, 0:130], lhsT=ktok[:, j, p * 128:(p + 1) * 128], rhs=vw[bi][:, 2 * p:2 * p + 2, :].rearrange("p a c -> p (a c)"), start=True, stop=True),
                             reads=[t_ktok, t_vw[bi]], writes=[self.pst[pU]], skip_self=True)
                        P.op("dve", lambda e, p=p, d=d, pU=pU: e.tensor_tensor(out=tmpC[d][0:64, :], in0=self.ps[pU][0:64, 0:65], in1=Cst[p][d][0:64, :], op=ALU.add), reads=[self.pst[pU], t_Cst[p][d]], writes=[t_tmpC[d]], partial=True)
                        P.op("dve", lambda e, p=p, d=d, pU=pU: e.tensor_tensor(out=tmpC[d][64:128, :], in0=self.ps[pU][64:128, 65:130], in1=Cst[p][d][64:128, :], op=ALU.add), reads=[self.pst[pU], t_Cst[p][d]], writes=[t_tmpC[d]], partial=True)
                        P.op("dve", lambda e, p=p, d=d, j=j: e.tensor_scalar(out=Cst[p][d], in0=tmpC[d], scalar1=ecol[:, p * 2 + d, j:j + 1], scalar2=None, op0=ALU.mult), reads=[t_tmpC[d], t_ecol], writes=[t_Cst[p][d]])
                        P.op("act", lambda e, p=p, d=d, si=si: e.copy(out=CbS[p][d][:, si + 1, :], in_=Cst[p][d]), reads=[t_Cst[p][d]], writes=[t_CbS[p][d]], partial=True)
                    for eo in range(2):
                        num = self.ps[4 + eo][:, 0:130].rearrange("p (h c) -> p h c", c=65)
                        dde = dd[d][:, 2 * eo:2 * eo + 2]
                        P.op("dve", lambda e, num=num, dde=dde: e.tensor_scalar(out=dde, in0=num[:, :, 64], scalar1=-1.0, scalar2=None, op0=ALU.mult), reads=[self.pst[4 + eo]], writes=[t_dd[d]], partial=(eo > 0))
                        P.op("dve", lambda e, num=num, dde=dde: e.scalar_tensor_tensor(out=dde, in0=num[:, :, 64], scalar=1.0, in1=dde, op0=ALU.max, op1=ALU.max), reads=[self.pst[4 + eo]], writes=[t_dd[d]], partial=True)
                        P.op("dve", lambda e, dde=dde: e.reciprocal(out=dde, in_=dde), reads=[t_dd[d]], writes=[t_dd[d]], partial=True)
                        dst = (hF if d == 0 else hB)[:, j, :].rearrange("p (h c) -> p h c", c=64)[:, eo::2, :]
                        P.op("dve", lambda e, num=num, dde=dde, dst=dst: e.tensor_tensor(out=dst, in0=num[:, :, 0:64], in1=dde.unsqueeze(2).to_broadcast([128, 2, 64]), op=ALU.mult),
                             reads=[self.pst[4 + eo], t_dd[d]], writes=[t_hF if d == 0 else t_hB], partial=True)
            hbs = [sb(f"mlhb{i}", [128, 256]) for i in range(2)]; t_hbs = [Tok(), Tok()]
            sgs = [sb(f"mlsg{i}", [128, 256]) for i in range(2)]; t_sgs = [Tok(), Tok()]
            hsq = sb("mlhsq", [128, 256]); t_hsq = Tok()
            ssn = sb("mlssn", [128, 8]); t_ssn = Tok()
            hn = [sb(f"mlhn{i}", [128, 256], BF16) for i in range(2)]; t_hn = [Tok(), Tok()]
            oT = [sb(f"mloT{i}", [128, 2, 128], BF16) for i in range(2)]; t_oT = [Tok(), Tok()]
            for j in range(NT):
                bi = j % 2
                c0, c1 = j * 128, (j + 1) * 128
                hb, t_hb, sg, t_sg = hbs[bi], t_hbs[bi], sgs[bi], t_sgs[bi]
                P.dma("sp", vo[bi][:, 256:512], self.PVO[c0:c1, 256:512], reads=[self.t_PVO], writes=[t_vo[bi]])
                P.op("dve", lambda e, j=j, hb=hb: e.tensor_tensor(out=hb, in0=hF[:, j, :], in1=hB[:, j, :], op=ALU.add), reads=[t_hF, t_hB], writes=[t_hb])
                P.op("act", lambda e, bi=bi, sg=sg: e.activation(out=sg, in_=vo[bi][:, 256:512], func=AF.Exp, scale=-1.0), reads=[t_vo[bi]], writes=[t_sg])
                P.op("dve", lambda e, sg=sg: e.tensor_scalar(out=sg, in0=sg, scalar1=1.0, scalar2=None, op0=ALU.add), reads=[t_sg], writes=[t_sg])
                P.op("dve", lambda e, sg=sg: e.reciprocal(out=sg, in_=sg), reads=[t_sg], writes=[t_sg])
                P.op("dve", lambda e, hb=hb, sg=sg: e.tensor_tensor(out=hb, in0=hb, in1=sg, op=ALU.mult), reads=[t_sg], writes=[t_hb])
                P.op("dve", lambda e, hb=hb: e.tensor_tensor(out=hsq, in0=hb, in1=hb, op=ALU.mult), reads=[t_hb], writes=[t_hsq])
                P.op("dve", lambda e: e.tensor_reduce(out=ssn[:, 0:4], in_=hsq.rearrange("p (h c) -> p h c", c=64), axis=AX.X, op=ALU.add), reads=[t_hsq], writes=[t_ssn])
                P.op("dve", lambda e: e.tensor_scalar(out=ssn[:, 0:4], in0=ssn[:, 0:4], scalar1=1.0 / 64, scalar2=EPS, op0=ALU.mult, op1=ALU.add), reads=[t_ssn], writes=[t_ssn])
                P.op("act", lambda e: e.activation(out=ssn[:, 0:4], in_=ssn[:, 0:4], func=AF.Ln), reads=[t_ssn], writes=[t_ssn])
                P.op("act", lambda e: e.activation(out=ssn[:, 4:8], in_=ssn[:, 0:4], func=AF.Exp, scale=-0.5), reads=[t_ssn], writes=[t_ssn])
                P.op("dve", lambda e, bi=bi, hb=hb: e.tensor_tensor(out=hn[bi].rearrange("p (h c) -> p h c", c=64), in0=hb.rearrange("p (h c) -> p h c", c=64), in1=ssn[:, 4:8].unsqueeze(2).to_broadcast([128, 4, 64]), op=ALU.mult),
                     reads=[t_hb, t_ssn], writes=[t_hn[bi]])
                pi = 6 + bi
                pT = self.ps[pi].bitcast(BF16)
                for p in range(2):
                    P.op("pe", lambda e, p=p, bi=bi, pT=pT: e.transpose(pT[:, p * 128:(p + 1) * 128], hn[bi][:, p * 128:(p + 1) * 128], self.identb), reads=[t_hn[bi], self.t_ident], writes=[self.pst[pi]], skip_self=True)
                for p in range(2):
                    P.op("act", lambda e, p=p, bi=bi, pT=pT: e.activation(out=oT[bi][:, p, :], in_=pT[:, p * 128:(p + 1) * 128], func=AF.Copy, scale=gml[:, p:p + 1]), reads=[self.pst[pi], t_gml], writes=[t_oT[bi]], partial=(p > 0))
                P.dma("pool", self.MIXT[512:768, c0:c1].rearrange("(c p) t -> p c t", p=128), oT[bi], reads=[t_oT[bi]], writes=[self.t_MIXT], partial=True)
            P.barrier()

    def phase_hyena(self, li):
        if li < DEPTH - 1:
            self.hyena_seq(li, 0, CTX, self.c_hy_z256, self.c_hy_win256, self.c_hy_C256, self.c_hy_S256, self.c_hy_wf256)
        self.hyena_seq(li, CTX, SEQ, self.c_hy_z4096, self.c_hy_win4096, self.c_hy_C4096, self.c_hy_S4096, self.c_hy_wf4096)

    def hyena_seq(self, li, tok0, L, c_z, c_win, c_C, c_S, c_wf):
        nc, P = self.nc, self.P
        NTL = L // 128
        NF = NTL + 1
        NB = (L + 511) // 512
        BW = min(L, 512)
        with contextlib.ExitStack() as st:
            sb = lambda name, shape, dt=F32: self.sb(st, name, shape, dt)
            x0T = sb("hyx0T", [128, 2, L], BF16); t_x0 = Tok()
            zT = sb("hyzT", [128, 2, L], BF16); t_zT = Tok()
            Z = sb("hyZ", [128, NTL, 256], BF16); t_Z = Tok()
            HS = sb("hyHS", [128, NTL, 256], BF16); t_HS = Tok()
            HD = sb("hyHD", [128, NTL, 256], BF16); t_HD = Tok()
            Asp = sb("hyA", [128, NF, 256], BF16); t_A = Tok()
            Bsp = sb("hyB", [128, NF, 256], BF16); t_B = Tok()
            rl1 = sb("hyrl1", [128, 256]); t_rl1 = Tok()
            wf = sb("hywf", [128, NF]); t_wf = Tok()
            P.dma("sp", wf, c_wf, writes=[t_wf])
            bd, t_bd = self.vec_pc(st, "hybd", self.hy_bias_d[li], 2)
            cw = sb("hycw", [128, 6, 3]); cb = sb("hycb", [128, 6]); t_cw = Tok()
            for jj in range(3):
                P.dma("sp", cw[:, :, jj], self.hy_conv_w[li, jj].rearrange("(c p) -> p c", p=128), writes=[t_cw], partial=True, allow_slow_non_contiguous=True)
            P.dma("sp", cb, self.hy_conv_b[li].rearrange("(c p) -> p c", p=128), writes=[t_cw], partial=True, allow_slow_non_contiguous=True)
            with contextlib.ExitStack() as st2:
                sb2 = lambda name, shape, dt=F32: self.sb(st2, name, shape, dt)
                zemb = sb2("hyzemb", [33, L]); t_zemb = Tok()
                P.dma("sp", zemb, c_z, writes=[t_zemb])
                w1 = sb2("hyw1", [33, 64]); w2 = sb2("hyw2", [64, 64]); w3 = sb2("hyw3", [64, 512]); t_wm = Tok()
                P.dma("act", w1, self.hy_w1[li], writes=[t_wm], partial=True)
                P.dma("act", w2, self.hy_w2[li], writes=[t_wm], partial=True)
                P.dma("act", w3, self.hy_w3[li], writes=[t_wm], partial=True)
                fr = sb2("hyfr", [64, 4]); t_fr = Tok()
                P.dma("sp", fr[:, 0:1], self.hy_sin_freq[li].rearrange("(p o) -> p o", o=1), writes=[t_fr], partial=True, allow_slow_non_contiguous=True)
                P.dma("sp", fr[:, 1:2], self.hy_b1[li].rearrange("(p o) -> p o", o=1), writes=[t_fr], partial=True, allow_slow_non_contiguous=True)
                P.dma("sp", fr[:, 2:3], self.hy_b2[li].rearrange("(p o) -> p o", o=1), writes=[t_fr], partial=True, allow_slow_non_contiguous=True)
                P.op("dve", lambda e: e.tensor_scalar(out=fr[:, 1:3], in0=fr[:, 1:3], scalar1=fr[:, 0:1], scalar2=None, op0=ALU.mult), reads=[t_fr], writes=[t_fr])
                h1 = sb2("hyh1", [64, L]); t_h1 = Tok()
                h2 = sb2("hyh2", [64, L]); t_h2 = Tok()
                tt = sb2("hytt", [64, 512]); t_tt = Tok()
                ti = sb2("hyti", [64, 512], I32); t_ti = Tok()
                tf = sb2("hytf", [64, 512]); t_tf = Tok()
                ones32 = sb2("hyones", [128, 128]); t_ones = Tok()
                P.op("pool", lambda e: e.memset(ones32, 1.0), writes=[t_ones])

                def sin_layer(wm, kdim, src, t_src, bcol, dst, t_dst):
                    for b in range(NB):
                        c0 = b * 512
                        P.op("pe", lambda e, c0=c0: e.matmul(self.ps[0][0:64, 0:BW], lhsT=wm[0:kdim, :], rhs=src[0:kdim, c0:c0 + BW], start=True, stop=True),
                             reads=[t_wm, t_src], writes=[self.pst[0]], skip_self=True)
                        P.op("dve", lambda e: e.tensor_scalar(out=tt[:, 0:BW], in0=self.ps[0][0:64, 0:BW], scalar1=fr[:, 0:1], scalar2=fr[:, bcol:bcol + 1], op0=ALU.mult, op1=ALU.add),
                             reads=[self.pst[0], t_fr], writes=[t_tt])
                        P.op("dve", lambda e: e.tensor_scalar(out=ti[:, 0:BW], in0=tt[:, 0:BW], scalar1=1.0 / TWO_PI, scalar2=None, op0=ALU.mult), reads=[t_tt], writes=[t_ti])
                        P.op("dve", lambda e: e.tensor_copy(out=tf[:, 0:BW], in_=ti[:, 0:BW]), reads=[t_ti], writes=[t_tf])
                        P.op("dve", lambda e: e.scalar_tensor_tensor(out=tt[:, 0:BW], in0=tf[:, 0:BW], scalar=-TWO_PI, in1=tt[:, 0:BW], op0=ALU.mult, op1=ALU.add), reads=[t_tf], writes=[t_tt])
                        P.op("act", lambda e, c0=c0: e.activation(out=dst[:, c0:c0 + BW], in_=tt[:, 0:BW], func=AF.Sin), reads=[t_tt], writes=[t_dst], partial=True)
                sin_layer(w1, 33, zemb, t_zemb, 1, h1, t_h1)
                sin_layer(w2, 64, h1, t_h1, 2, h2, t_h2)
                win = [sb2(f"hywin{i}", [128, 2, 256]) for i in range(2)]; t_win = [Tok(), Tok()]
                hfb = [sb2(f"hyhfb{i}", [128, 2, 256]) for i in range(2)]; t_hfb = [Tok(), Tok()]
                hab = [sb2(f"hyhab{i}", [128, 2, 256]) for i in range(2)]; t_hab = [Tok(), Tok()]
                for j in range(NTL):
                    bi = j % 2
                    P.dma("sp", win[bi], c_win[j * 128:(j + 1) * 128], writes=[t_win[bi]])
                    P.op("pe", lambda e, j=j: e.matmul(self.ps[1], lhsT=h2[:, j * 128:(j + 1) * 128], rhs=w3, start=True, stop=True),
                         reads=[t_h2, t_wm], writes=[self.pst[1]], skip_self=True)
                    P.op("dve", lambda e, bi=bi: e.tensor_tensor(out=hfb[bi], in0=self.ps[1].rearrange("p (a c) -> p a c", a=2), in1=win[bi], op=ALU.mult),
                         reads=[self.pst[1], t_win[bi]], writes=[t_hfb[bi]])
                    P.op("pool", lambda e, bi=bi, j=j: e.tensor_tensor(out=HS[:, j, :], in0=hfb[bi][:, 0, :], in1=hfb[bi][:, 1, :], op=ALU.add), reads=[t_hfb[bi]], writes=[t_HS], partial=True)
                    P.op("pool", lambda e, bi=bi, j=j: e.tensor_tensor(out=HD[:, j, :], in0=hfb[bi][:, 0, :], in1=hfb[bi][:, 1, :], op=ALU.subtract), reads=[t_hfb[bi]], writes=[t_HD], partial=True)
                    P.op("act", lambda e, bi=bi: e.activation(out=hab[bi], in_=hfb[bi], func=AF.Abs), reads=[t_hfb[bi]], writes=[t_hab[bi]])
                    for a in range(2):
                        P.op("pe", lambda e, bi=bi, a=a, j=j: e.matmul(self.ps[2][:, 0:256], lhsT=ones32, rhs=hab[bi][:, a, :], start=(j == 0 and a == 0), stop=(j == NTL - 1 and a == 1)),
                             reads=[t_ones, t_hab[bi]], writes=[self.pst[2]], skip_self=True)
                P.op("dve", lambda e: e.reciprocal(out=rl1, in_=self.ps[2][:, 0:256]), reads=[self.pst[2]], writes=[t_rl1])
                P.barrier()
            with contextlib.ExitStack() as st2:
                sb2 = lambda name, shape, dt=F32: self.sb(st2, name, shape, dt)
                pin = [sb2(f"hypin{i}", [128, L]) for i in range(2)]; t_pin = [Tok(), Tok()]
                uu = [sb2(f"hyu{i}", [128, L]) for i in range(2)]; t_uu = [Tok(), Tok()]

                def conv(ch, bi):
                    P.dma("sp" if bi == 0 else "act", pin[bi], self.PT[HY_LO + ch * 128:HY_LO + (ch + 1) * 128, tok0:tok0 + L], reads=[self.t_PT], writes=[t_pin[bi]])
                    eng = "dve"
                    P.op(eng, lambda e: e.tensor_scalar(out=uu[bi], in0=pin[bi], scalar1=cw[:, ch, 1:2], scalar2=cb[:, ch:ch + 1], op0=ALU.mult, op1=ALU.add), reads=[t_pin[bi], t_cw], writes=[t_uu[bi]])
                    P.op(eng, lambda e: e.scalar_tensor_tensor(out=uu[bi][:, 1:L], in0=pin[bi][:, 0:L - 1], scalar=cw[:, ch, 0:1], in1=uu[bi][:, 1:L], op0=ALU.mult, op1=ALU.add), reads=[t_pin[bi], t_cw], writes=[t_uu[bi]])
                    P.op(eng, lambda e: e.scalar_tensor_tensor(out=uu[bi][:, 0:L - 1], in0=pin[bi][:, 1:L], scalar=cw[:, ch, 2:3], in1=uu[bi][:, 0:L - 1], op0=ALU.mult, op1=ALU.add), reads=[t_pin[bi], t_cw], writes=[t_uu[bi]])
                for cc in range(2):
                    conv(cc, 0)
                    P.op("act", lambda e, cc=cc: e.copy(out=x0T[:, cc, :], in_=uu[0]), reads=[t_uu[0]], writes=[t_x0], partial=True)
                    conv(2 + cc, 0)
                    conv(4 + cc, 1)
                    P.op("pool", lambda e, cc=cc: e.tensor_tensor(out=zT[:, cc, :], in0=uu[0], in1=uu[1], op=ALU.mult), reads=[t_uu[0], t_uu[1]], writes=[t_zT], partial=True)
                g = 0
                for j0 in range(0, NTL, 2):
                    pi = 3 + g % 2
                    g += 1
                    pT = self.ps[pi].bitcast(BF16)
                    nj = min(2, NTL - j0)
                    for jj in range(nj):
                        for cc in range(2):
                            P.op("pe", lambda e, jj=jj, cc=cc, j0=j0, pT=pT: e.transpose(pT[:, (jj * 2 + cc) * 128:(jj * 2 + cc + 1) * 128], zT[:, cc, (j0 + jj) * 128:(j0 + jj + 1) * 128], self.identb),
                                 reads=[t_zT, self.t_ident], writes=[self.pst[pi]], skip_self=True)
                    if g % 2 == 0:
                        P.op("act", lambda e, j0=j0, nj=nj, pT=pT: e.copy(out=Z[:, j0:j0 + nj, :].rearrange("p j c -> p (j c)"), in_=pT[:, 0:nj * 256]), reads=[self.pst[pi]], writes=[t_Z], partial=True)
                    else:
                        P.op("dve", lambda e, j0=j0, nj=nj, pT=pT: e.tensor_copy(out=Z[:, j0:j0 + nj, :].rearrange("p j c -> p (j c)"), in_=pT[:, 0:nj * 256]), reads=[self.pst[pi]], writes=[t_Z], partial=True)
                P.barrier()
            with contextlib.ExitStack() as st2:
                sb2 = lambda name, shape, dt=F32: self.sb(st2, name, shape, dt)
                CT = [sb2(f"hyCT{i}", [128, NTL, 128], BF16) for i in range(2)]; t_CT = [Tok(), Tok()]
                ST = [sb2(f"hyST{i}", [128, NTL, 128], BF16) for i in range(2)]; t_ST = [Tok(), Tok()]
                hcs = [sb2(f"hyhcs{i}", [128, 2, 256]) for i in range(2)]; t_hcs = [Tok(), Tok()]
                t1 = sb2("hyt1", [128, 256]); t_t1 = Tok()
                t2 = sb2("hyt2", [128, 256]); t_t2 = Tok()
                t3 = sb2("hyt3", [128, 256]); t_t3 = Tok()
                t4 = sb2("hyt4", [128, 256]); t_t4 = Tok()
                for fc in range(NF):
                    bi = fc % 2
                    P.dma("sp", CT[bi], c_C[0:L, fc * 128:(fc + 1) * 128].rearrange("(j p) f -> p j f", p=128), writes=[t_CT[bi]])
                    P.dma("act", ST[bi], c_S[0:L, fc * 128:(fc + 1) * 128].rearrange("(j p) f -> p j f", p=128), writes=[t_ST[bi]])
                    pc, psn = 2 * bi, 2 * bi + 1
                    for (mat, t_mat, pi, rhs2, t_rhs2) in ((CT[bi], t_CT[bi], pc, HS, t_HS), (ST[bi], t_ST[bi], psn, HD, t_HD)):
                        for half, (rt, t_rt) in enumerate(((Z, t_Z), (rhs2, t_rhs2))):
                            for j in range(NTL):
                                P.op("pe", lambda e, mat=mat, pi=pi, rt=rt, j=j, half=half: e.matmul(self.ps[pi][:, half * 256:(half + 1) * 256], lhsT=mat[:, j, :], rhs=rt[:, j, :], start=(j == 0), stop=(j == NTL - 1)),
                                     reads=[t_mat, t_rt], writes=[self.pst[pi]], skip_self=True)
                    P.op("act", lambda e, bi=bi, pc=pc: e.copy(out=hcs[bi][:, 0, :], in_=self.ps[pc][:, 256:512]), reads=[self.pst[pc]], writes=[t_hcs[bi]], partial=True)
                    P.op("act", lambda e, bi=bi, psn=psn: e.copy(out=hcs[bi][:, 1, :], in_=self.ps[psn][:, 256:512]), reads=[self.pst[psn]], writes=[t_hcs[bi]], partial=True)
                    P.op("dve", lambda e, bi=bi, pc=pc: e.tensor_tensor(out=t1, in0=self.ps[pc][:, 0:256], in1=hcs[bi][:, 0, :], op=ALU.mult), reads=[self.pst[pc], t_hcs[bi]], writes=[t_t1])
                    P.op("dve", lambda e, bi=bi, pc=pc: e.tensor_tensor(out=t3, in0=self.ps[pc][:, 0:256], in1=hcs[bi][:, 1, :], op=ALU.mult), reads=[self.pst[pc], t_hcs[bi]], writes=[t_t3])
                    P.op("dve", lambda e, bi=bi, psn=psn: e.tensor_tensor(out=t2, in0=self.ps[psn][:, 0:256], in1=hcs[bi][:, 1, :], op=ALU.mult), reads=[self.pst[psn], t_hcs[bi]], writes=[t_t2])
                    P.op("dve", lambda e, bi=bi, psn=psn: e.tensor_tensor(out=t4, in0=self.ps[psn][:, 0:256], in1=hcs[bi][:, 0, :], op=ALU.mult), reads=[self.pst[psn], t_hcs[bi]], writes=[t_t4])
                    P.op("pool", lambda e: e.tensor_tensor(out=t1, in0=t1, in1=t2, op=ALU.subtract), reads=[t_t2], writes=[t_t1])
                    P.op("pool", lambda e: e.tensor_tensor(out=t3, in0=t3, in1=t4, op=ALU.add), reads=[t_t4], writes=[t_t3])
                    P.op("dve", lambda e, fc=fc: e.scalar_tensor_tensor(out=Asp[:, fc, :], in0=t1, scalar=wf[:, fc:fc + 1], in1=rl1, op0=ALU.mult, op1=ALU.mult), reads=[t_t1, t_wf, t_rl1], writes=[t_A], partial=True)
                    P.op("dve", lambda e, fc=fc: e.scalar_tensor_tensor(out=Bsp[:, fc, :], in0=t3, scalar=wf[:, fc:fc + 1], in1=rl1, op0=ALU.mult, op1=ALU.mult), reads=[t_t3, t_wf, t_rl1], writes=[t_B], partial=True)
                P.barrier()
            with contextlib.ExitStack() as st2:
                sb2 = lambda name, shape, dt=F32: self.sb(st2, name, shape, dt)
                TW = min(L, 1024)
                GC = [sb2(f"hyGC{i}", [128, TW], BF16) for i in range(3)]; t_GC = [Tok() for _ in range(3)]
                GS = [sb2(f"hyGS{i}", [128, TW], BF16) for i in range(3)]; t_GS = [Tok() for _ in range(3)]
                yt = sb2("hyyt", [128, 512]); t_yt = Tok()
                yo = [sb2(f"hyyo{i}", [128, 512], BF16) for i in range(2)]; t_yo = [Tok(), Tok()]
                nld = 0
                for tb0 in range(0, L, TW):
                    nsub = TW // BW
                    for fc in range(NF):
                        gi = nld % 3
                        nld += 1
                        P.dma("sp", GC[gi], c_C[fc * 128:(fc + 1) * 128, tb0:tb0 + TW], writes=[t_GC[gi]])
                        P.dma("act", GS[gi], c_S[fc * 128:(fc + 1) * 128, tb0:tb0 + TW], writes=[t_GS[gi]])
                        for sub in range(nsub):
                            for cc in range(2):
                                pi = sub * 2 + cc
                                P.op("pe", lambda e, gi=gi, fc=fc, sub=sub, cc=cc, pi=pi: e.matmul(self.ps[pi][:, 0:BW], lhsT=Asp[:, fc, cc * 128:(cc + 1) * 128], rhs=GC[gi][:, sub * BW:(sub + 1) * BW], start=(fc == 0), stop=False),
                                     reads=[t_A, t_GC[gi]], writes=[self.pst[pi]], skip_self=True)
                                P.op("pe", lambda e, gi=gi, fc=fc, sub=sub, cc=cc, pi=pi: e.matmul(self.ps[pi][:, 0:BW], lhsT=Bsp[:, fc, cc * 128:(cc + 1) * 128], rhs=GS[gi][:, sub * BW:(sub + 1) * BW], start=False, stop=(fc == NF - 1)),
                                     reads=[t_B, t_GS[gi]], writes=[self.pst[pi]], skip_self=True)
                    for sub in range(nsub):
                        for cc in range(2):
                            pi = sub * 2 + cc
                            c0 = tb0 + sub * BW
                            oi = pi % 2
                            P.op("dve", lambda e, cc=cc, c0=c0, pi=pi: e.scalar_tensor_tensor(out=yt[:, 0:BW], in0=zT[:, cc, c0:c0 + BW], scalar=bd[:, cc:cc + 1], in1=self.ps[pi][:, 0:BW], op0=ALU.mult, op1=ALU.add),
                                 reads=[t_zT, t_bd, self.pst[pi]], writes=[t_yt])
                            P.op("pool", lambda e, cc=cc, c0=c0, oi=oi: e.tensor_tensor(out=yo[oi][:, 0:BW], in0=yt[:, 0:BW], in1=x0T[:, cc, c0:c0 + BW], op=ALU.mult), reads=[t_yt, t_x0], writes=[t_yo[oi]])
                            P.dma("pool", self.MIXT[768 + cc * 128:768 + (cc + 1) * 128, tok0 + c0:tok0 + c0 + BW], yo[oi][:, 0:BW], reads=[t_yo[oi]], writes=[self.t_MIXT], partial=True)
                P.barrier()

    def phase_wout(self, li):
        nc, P = self.nc, self.P
        last = li == DEPTH - 1
        with contextlib.ExitStack() as st:
            sb = lambda name, shape, dt=F32: self.sb(st, name, shape, dt)
            wo = sb("woutb", [128, 8, D], BF16); t_wo = Tok()
            with contextlib.ExitStack() as st2:
                self.load_cast_weight(st2, wo, t_wo, self.w_out[li], D)
                P.barrier()
            G1 = sb("G1rep", [128, 2, D]); t_G1 = Tok()
            for v in range(2):
                P.dma("sp", G1[:, v, :], self.MOD[v, 2 * D:3 * D].partition_broadcast(128), reads=[self.t_MOD], writes=[t_G1], partial=True)
            mx = [sb(f"mixT{i}", [128, 8, 128], BF16) for i in range(2)]; t_mx = [Tok(), Tok()]
            xt = [sb(f"wxt{i}", [128, D]) for i in range(2)]; t_xt = [Tok(), Tok()]
            tm = [sb(f"wtm{i}", [128, 512]) for i in range(2)]; t_tm = [Tok(), Tok()]
            tiles = list(range(2 if last else 0, TT // 128))
            for n, j in enumerate(tiles):
                bi = n % 2
                v = 1 if j < 2 else 0
                c0, c1 = j * 128, (j + 1) * 128
                P.dma("sp", mx[bi], self.MIXT[:, c0:c1].rearrange("(c p) t -> p c t", p=128), reads=[self.t_MIXT], writes=[t_mx[bi]])
                P.dma("act", xt[bi], self.XS[c0:c1, :], reads=[self.t_XS], writes=[t_xt[bi]])
                for half in range(2):
                    pi = (n * 2 + half) % 4
                    for k in range(8):
                        P.op("pe", lambda e, bi=bi, k=k, half=half, pi=pi: e.matmul(self.ps[pi], lhsT=mx[bi][:, k, :], rhs=wo[:, k, half * 512:(half + 1) * 512], start=(k == 0), stop=(k == 7)),
                             reads=[t_mx[bi], t_wo], writes=[self.pst[pi]], skip_self=True)
                    P.op("dve", lambda e, half=half, pi=pi, v=v: e.tensor_tensor(out=tm[half], in0=self.ps[pi], in1=G1[:, v, half * 512:(half + 1) * 512], op=ALU.mult),
                         reads=[self.pst[pi], t_G1], writes=[t_tm[half]])
                    P.op("dve", lambda e, bi=bi, half=half: e.tensor_tensor(out=xt[bi][:, half * 512:(half + 1) * 512], in0=xt[bi][:, half * 512:(half + 1) * 512], in1=tm[half], op=ALU.add),
                         reads=[t_tm[half]], writes=[t_xt[bi]])
                P.dma("pool", self.XS[c0:c1, :], xt[bi], reads=[t_xt[bi]], writes=[self.t_XS], partial=True)
            P.barrier()

    def phase_ffn(self, li):
        nc, P = self.nc, self.P
        last = li == DEPTH - 1
        with contextlib.ExitStack() as st:
            sb = lambda name, shape, dt=F32: self.sb(st, name, shape, dt)
            wup = sb("wupb", [128, 8, 2 * DFF], BF16); t_wup = Tok()
            wdn = sb("wdnb", [128, NFC, D], BF16); t_wdn = Tok()
            with contextlib.ExitStack() as st2:
                self.load_cast_weight(st2, wup, t_wup, self.ffn_w_up[li], 2 * DFF)
                P.barrier()
            with contextlib.ExitStack() as st2:
                self.load_cast_weight(st2, wdn, t_wdn, self.ffn_w_down[li], D, blk=256, k_chunks=NFC)
                P.barrier()
            G2 = sb("G2rep", [128, 2, D]); t_G2 = Tok()
            for v in range(2):
                P.dma("sp", G2[:, v, :], self.MOD[v, 5 * D:6 * D].partition_broadcast(128), reads=[self.t_MOD], writes=[t_G2], partial=True)
            if last:
                FG = sb("FGrep", [128, D]); t_FG = Tok()
                P.dma("sp", FG, self.final_norm_g.partition_broadcast(128), writes=[t_FG])
            cw = sb("ffcw", [128, 2 * NFC, 3]); cbv = sb("ffcb", [128, 2 * NFC]); t_cw = Tok()
            for jj in range(3):
                P.dma("sp", cw[:, :, jj], self.ffn_conv_w[li, jj].rearrange("(c p) -> p c", p=128), writes=[t_cw], partial=True, allow_slow_non_contiguous=True)
            P.dma("sp", cbv, self.ffn_conv_b[li].rearrange("(c p) -> p c", p=128), writes=[t_cw], partial=True, allow_slow_non_contiguous=True)
            hx = [sb(f"hTx{i}", [128, 8, 258], BF16) for i in range(3)]; t_hx = [Tok() for _ in range(3)]
            gT = sb("ffgT", [128, NFC, 256], BF16); t_gT = Tok()
            xts = [sb(f"fxt{i}", [128, D]) for i in range(4)]; t_xts = [Tok() for _ in range(4)]
            sqj = sb("fsq", [128, D], BF16); t_sqj = Tok()
            sss = [sb(f"fss{i}", [128, 4]) for i in range(4)]; t_sss = [Tok() for _ in range(4)]
            xns = [sb(f"fxn{i}", [128, D], BF16) for i in range(2)]; t_xns = [Tok(), Tok()]
            ga = [sb(f"ffga{i}", [128, 256]) for i in range(2)]; t_ga = [Tok(), Tok()]
            gb = [sb(f"ffgb{i}", [128, 256]) for i in range(2)]; t_gb = [Tok(), Tok()]
            tmo = [sb(f"fftm{i}", [128, 512]) for i in range(2)]; t_tmo = [Tok(), Tok()]
            fss = sb("ffss", [128, 4]); t_fss = Tok()
            tiles = list(range(1 if last else 0, TT // 256))
            seq_first = {0, 1}
            seq_last = {0, TT // 256 - 1}
            nsub = 0
            sets_of = {}

            def stage_a(ti):
                nonlocal nsub
                t0 = ti * 256
                v = 1 if ti == 0 else 0
                hb = ti % 3
                sets_of[ti] = []
                for sub in range(2):
                    si = nsub % 4
                    nsub += 1
                    sets_of[ti].append(si)
                    bufs = (xts[si], t_xts[si], sqj, t_sqj, sss[si], t_sss[si], xns[si % 2], t_xns[si % 2])
                    self.norm_transpose(bufs, self.XS[t0 + sub * 128:t0 + (sub + 1) * 128, :], self.t_XS, self.s2, self.modT[:, 24:32, :], v,
                                        hx[hb], t_hx[hb], 1 + sub * 128, ps_i=6 + sub)
                if ti in seq_first:
                    P.op("pool", lambda e, hb=hb: e.memset(hx[hb][:, :, 0:1], 0.0), writes=[t_hx[hb]], partial=True)
                if ti in seq_last:
                    P.op("pool", lambda e, hb=hb: e.memset(hx[hb][:, :, 257:258], 0.0), writes=[t_hx[hb]], partial=True)

            def halo(ta, tb):
                a, b = ta % 3, tb % 3
                P.op("pool", lambda e: e.tensor_copy(out=hx[a][:, :, 257:258], in_=hx[b][:, :, 1:2]), reads=[t_hx[b]], writes=[t_hx[a]], partial=True)
                P.op("pool", lambda e: e.tensor_copy(out=hx[b][:, :, 0:1], in_=hx[a][:, :, 256:257]), reads=[t_hx[a]], writes=[t_hx[b]], partial=True)

            def stage_b(ti):
                t0 = ti * 256
                v = 1 if ti == 0 else 0
                hb = ti % 3
                for jc in range(NFC):
                    gi = jc % 2
                    for (which, col0, pi, acc, t_acc) in ((0, jc * 128, 0 + 2 * gi, ga[gi], t_ga[gi]), (1, DFF + jc * 128, 1 + 2 * gi, gb[gi], t_gb[gi])):
                        ch = (col0 // 128)
                        for k in range(8):
                            P.op("pe", lambda e, k=k, col0=col0, pi=pi: e.matmul(self.ps[pi][:, 0:258], lhsT=wup[:, k, col0:col0 + 128], rhs=hx[hb][:, k, :], start=(k == 0), stop=(k == 7)),
                                 reads=[t_wup, t_hx[hb]], writes=[self.pst[pi]], skip_self=True)
                        P.op("act", lambda e, pi=pi, acc=acc, ch=ch: e.activation(out=acc, in_=self.ps[pi][:, 1:257], func=AF.Identity, scale=cw[:, ch, 1:2], bias=cbv[:, ch:ch + 1]),
                             reads=[self.pst[pi], t_cw], writes=[t_acc])
                        P.op("dve", lambda e, pi=pi, acc=acc, ch=ch: e.scalar_tensor_tensor(out=acc, in0=self.ps[pi][:, 0:256], scalar=cw[:, ch, 0:1], in1=acc, op0=ALU.mult, op1=ALU.add),
                             reads=[self.pst[pi], t_cw], writes=[t_acc])
                        P.op("dve", lambda e, pi=pi, acc=acc, ch=ch: e.scalar_tensor_tensor(out=acc, in0=self.ps[pi][:, 2:258], scalar=cw[:, ch, 2:3], in1=acc, op0=ALU.mult, op1=ALU.add),
                             reads=[self.pst[pi], t_cw], writes=[t_acc])
                    P.op("act", lambda e, gi=gi: e.activation(out=ga[gi], in_=ga[gi], func=AF.Silu), reads=[t_ga[gi]], writes=[t_ga[gi]])
                    P.op("pool", lambda e, gi=gi, jc=jc: e.tensor_tensor(out=gT[:, jc, :], in0=ga[gi], in1=gb[gi], op=ALU.mult), reads=[t_ga[gi], t_gb[gi]], writes=[t_gT], partial=True)
                for sub in range(2):
                    si = sets_of[ti][sub]
                    xt, t_xt = xts[si], t_xts[si]
                    for half in range(2):
                        pi = 4 + half
                        for jc in range(NFC):
                            P.op("pe", lambda e, jc=jc, sub=sub, half=half, pi=pi: e.matmul(self.ps[pi], lhsT=gT[:, jc, sub * 128:(sub + 1) * 128], rhs=wdn[:, jc, half * 512:(half + 1) * 512], start=(jc == 0), stop=(jc == NFC - 1)),
                                 reads=[t_gT, t_wdn], writes=[self.pst[pi]], skip_self=True)
                        P.op("dve", lambda e, half=half, pi=pi: e.tensor_tensor(out=tmo[half], in0=self.ps[pi], in1=G2[:, v, half * 512:(half + 1) * 512], op=ALU.mult),
                             reads=[self.pst[pi], t_G2], writes=[t_tmo[half]])
                        P.op("pool", lambda e, half=half, xt=xt: e.tensor_tensor(out=xt[:, half * 512:(half + 1) * 512], in0=xt[:, half * 512:(half + 1) * 512], in1=tmo[half], op=ALU.add),
                             reads=[t_tmo[half]], writes=[t_xt])
                    r0 = t0 + sub * 128
                    if not last:
                        P.dma("pool", self.XS[r0:r0 + 128, :], xt, reads=[t_xt], writes=[self.t_XS], partial=True)
                    else:
                        P.op("act", lambda e, xt=xt: e.activation(out=sqj, in_=xt, func=AF.Square, accum_out=fss[:, 0:1]), reads=[t_xt], writes=[t_sqj, t_fss])
                        P.op("dve", lambda e: e.tensor_scalar(out=fss[:, 1:2], in0=fss[:, 0:1], scalar1=1.0 / D, scalar2=EPS, op0=ALU.mult, op1=ALU.add), reads=[t_fss], writes=[t_fss])
                        P.op("act", lambda e: e.activation(out=fss[:, 2:3], in_=fss[:, 1:2], func=AF.Sqrt), reads=[t_fss], writes=[t_fss])
                        P.op("dve", lambda e: e.reciprocal(out=fss[:, 3:4], in_=fss[:, 2:3]), reads=[t_fss], writes=[t_fss])
                        P.op("dve", lambda e, xt=xt: e.scalar_tensor_tensor(out=xt, in0=xt, scalar=fss[:, 3:4], in1=FG, op0=ALU.mult, op1=ALU.mult), reads=[t_fss, t_FG], writes=[t_xt])
                        P.dma("pool", self.out[r0 - CTX:r0 - CTX + 128, :], xt, reads=[t_xt], writes=[self.t_out], partial=True)

            prev = None
            for ti in tiles:
                stage_a(ti)
                if prev is not None:
                    if prev not in seq_last:
                        halo(prev, ti)
                    stage_b(prev)
                prev = ti
            stage_b(prev)
            P.barrier()

    def build(self, stop_after=None):
        nc, P = self.nc, self.P
        self.declare()
        P.clear_sems()
        gst = self.stack
        self.identb = self.sb(gst, "identb", [128, 128], BF16)
        self.t_ident = Tok()
        P.dma("sp", self.identb, self.c_ident, writes=[self.t_ident])
        self.t_XS = Tok()
        self.t_MOD, self.t_PT, self.t_PVO, self.t_MIXT, self.t_out = Tok(), Tok(), Tok(), Tok(), Tok()
        P.dma("sp", self.XS[0:CTX, :], self.ctx, writes=[self.t_XS], partial=True)
        for i in range(16):
            P.dma("act" if i % 2 else "sp", self.XS[CTX + i * 256:CTX + (i + 1) * 256, :], self.x[i * 256:(i + 1) * 256, :], writes=[self.t_XS], partial=True)
        done = False
        for li in range(DEPTH):
            with contextlib.ExitStack() as lst:
                self.lst = lst
                for name, fn in (("mod", self.phase_mod), ("inproj", self.phase_inproj), ("mla", self.phase_mla), ("mlstm", self.phase_mlstm), ("hyena", self.phase_hyena), ("wout", self.phase_wout), ("ffn", self.phase_ffn)):
                    fn(li)
                    if stop_after == (name, li):
                        done = True
                        break
                P.barrier()
            if done:
                break
        P.finish()
        return nc


def _prep_inputs(inputs, b):
    m = {}
    for k, v in inputs.items():
        v = np.asarray(v)
        if k in ("x", "ctx", "c"):
            m[k] = np.ascontiguousarray(v[b])
        elif k == "ml_gate_b":
            m[k] = np.ascontiguousarray(v.reshape(DEPTH, 16))
        else:
            m[k] = np.ascontiguousarray(v)
    return m


def kernel(**inputs):
    bld = Builder()
    nc = bld.build()
    consts = _consts()
    in_maps = []
    for b in range(8):
        m = _prep_inputs(inputs, b)
        m.update(consts)
        in_maps.append({k: m[k] for k in bld.inp})
    res = run_bass_kernel_spmd(nc, in_maps, core_ids=list(range(8)))
    return np.stack([np.asarray(r["out"]) for r in res.results], axis=0).astype(np.float32)
```

```python
import contextlib
import numpy as np
import ml_dtypes
import concourse.bass as bass
import concourse.mybir as mybir
from concourse.bass_utils import run_bass_kernel_spmd

F32 = mybir.dt.float32
BF16 = mybir.dt.bfloat16
I32 = mybir.dt.int32
AF = mybir.ActivationFunctionType
ALU = mybir.AluOpType
AX = mybir.AxisListType

D = 1024
SEQ = 4096
CTX = 256
TT = SEQ + CTX
DEPTH = 2
EPS = 1e-6
NH = 8
QR, KVR, ROPE = 384, 256, 32
N_MLA_IN = QR + KVR + ROPE
MLW = 256
N_ML_IN = 3 * MLW + 16
HYW = 256
N_IN = 2224
ML_LO = N_MLA_IN
HY_LO = N_MLA_IN + N_ML_IN
DFF = 2816
NFC = DFF // 128
TWO_PI = float(2 * np.pi)


class Tok:
    __slots__ = ("w", "r", "wf")

    def __init__(self):
        self.w = []
        self.wf = []
        self.r = []


class PTok(Tok):
    __slots__ = ()


class Queue:
    def __init__(self, prog, name, eng, n_dma_sems=0):
        self.p = prog
        self.name = name
        self.eng = eng
        nc = prog.nc
        self.sem = nc.alloc_semaphore(name=f"s_{name}")
        self.count = 0
        self.dma_sems = [nc.alloc_semaphore(name=f"d_{name}{i}") for i in range(n_dma_sems)]
        self.dma_counts = [0] * n_dma_sems
        self.dma_rr = 0
        self.waited = {}

    def _wait(self, tick):
        sem, val = tick
        key = id(sem)
        if self.waited.get(key, 0) >= val:
            return
        self.eng.wait_ge(sem, val)
        self.waited[key] = val
        self.p.n_waits += 1


class Prog:
    def __init__(self, nc, dma_sems=8):
        self.nc = nc
        self.n_waits = 0
        self.n_ins = 0
        self.q = {
            "pe": Queue(self, "pe", nc.tensor),
            "dve": Queue(self, "dve", nc.vector),
            "act": Queue(self, "act", nc.scalar, dma_sems),
            "pool": Queue(self, "pool", nc.gpsimd, dma_sems),
            "sp": Queue(self, "sp", nc.sync, dma_sems),
        }

    def clear_sems(self):
        for q in self.q.values():
            q.eng.sem_clear(q.sem)
            for s in q.dma_sems:
                q.eng.sem_clear(s)
        self.nc.all_engine_barrier()

    def _deps(self, q, reads, writes, skip_self=False, partial=False):
        for t in reads:
            for tk in t.w:
                if not (skip_self and tk[0] is q.sem):
                    q._wait(tk)
            if isinstance(t, PTok):
                for tk in t.r:
                    if not (skip_self and tk[0] is q.sem):
                        q._wait(tk)
        for t in writes:
            if partial and not isinstance(t, PTok):
                for tk in t.wf:
                    if not (skip_self and tk[0] is q.sem):
                        q._wait(tk)
            if not partial or isinstance(t, PTok):
                for tk in t.w:
                    if not (skip_self and tk[0] is q.sem):
                        q._wait(tk)
            for tk in t.r:
                if not (skip_self and tk[0] is q.sem):
                    q._wait(tk)

    @staticmethod
    def _compact(lst):
        best = {}
        for s, v in lst:
            k = id(s)
            if k not in best or best[k][1] < v:
                best[k] = (s, v)
        return list(best.values())

    def _record(self, tick, reads, writes, partial=False):
        for t in reads:
            if isinstance(t, PTok):
                t.w = [tick]
                t.wf = [tick]
                t.r = []
                continue
            t.r.append(tick)
            if len(t.r) > 48:
                t.r = self._compact(t.r)
        for t in writes:
            if partial and not isinstance(t, PTok):
                t.w.append(tick)
                if len(t.w) > 48:
                    t.w = self._compact(t.w)
            else:
                t.w = [tick]
                t.wf = [tick]
                t.r = []

    def op(self, qname, fn, reads=(), writes=(), skip_self=False, partial=False):
        q = self.q[qname]
        self._deps(q, reads, writes, skip_self=skip_self, partial=partial)
        ins = fn(q.eng)
        q.count += 1
        ins.then_inc(q.sem, 1)
        self._record((q.sem, q.count), reads, writes, partial=partial)
        self.n_ins += 1
        return ins

    def dma(self, qname, out, in_, reads=(), writes=(), partial=False, **kw):
        q = self.q[qname]
        j = q.dma_rr
        q.dma_rr = (j + 1) % len(q.dma_sems)
        sem = q.dma_sems[j]
        if q.dma_counts[j] > 0:
            q._wait((sem, q.dma_counts[j]))
        self._deps(q, reads, writes, partial=partial)
        ins = q.eng.dma_start(out=out, in_=in_, **kw)
        q.dma_counts[j] += 16
        ins.then_inc(sem, 16)
        self._record((sem, q.dma_counts[j]), reads, writes, partial=partial)
        self.n_ins += 1
        return ins

    def barrier(self):
        ticks = []
        for q in self.q.values():
            if q.count:
                ticks.append((q.sem, q.count))
            for j, s in enumerate(q.dma_sems):
                if q.dma_counts[j]:
                    ticks.append((s, q.dma_counts[j]))
        for q in self.q.values():
            for tk in ticks:
                q._wait(tk)

    def finish(self):
        ticks = []
        for q in self.q.values():
            if q.count:
                ticks.append((q.sem, q.count))
            for j, s in enumerate(q.dma_sems):
                if q.dma_counts[j]:
                    ticks.append((s, q.dma_counts[j]))
        for tk in ticks:
            self.q["sp"]._wait(tk)


def _consts():
    c = {}
    c["ident"] = np.eye(128, dtype=np.float32).astype(ml_dtypes.bfloat16)
    n_freq = ROPE // 4
    inv = (10000.0 ** (-np.arange(n_freq, dtype=np.float32) / n_freq)).astype(np.float32)
    row = np.repeat(np.arange(SEQ // 64, dtype=np.float32), 64)
    col = np.tile(np.arange(64, dtype=np.float32), SEQ // 64)
    ang = np.concatenate([row[:, None] * inv, col[:, None] * inv], axis=-1).astype(np.float32)
    cos = np.cos(ang).astype(np.float32).T
    sin = np.sin(ang).astype(np.float32).T
    c["rope_cos"] = np.ascontiguousarray(np.concatenate([cos, cos], 0))
    c["rope_sin"] = np.ascontiguousarray(np.concatenate([sin, sin], 0))
    ss_, tt_ = np.meshgrid(np.arange(128), np.arange(128), indexing="ij")
    c["ml_mask"] = np.stack([(ss_ <= tt_), (ss_ >= tt_)], 1).astype(np.float32).astype(ml_dtypes.bfloat16)
    selA = np.zeros((96, 4, 128), np.float32)
    selW = np.zeros((96, 8), np.float32)
    for d in range(2):
        base = 32 + 4 if d == 0 else 64 + 12
        for h in range(4):
            selA[base + h, (h // 2) * 2 + d, (h % 2) * 64:(h % 2) * 64 + 64] = 1.0
            selW[8 * d + h, d * 4 + h] = 1.0
            selW[base + h, d * 4 + h] = -1.0
    c["ml_selA"] = selA
    c["ml_selW"] = selW
    for L in (256, 4096):
        f32 = np.float32
        t = np.linspace(0.0, 1.0, L, dtype=f32)[:, None]
        omega = (f32(2.0 * np.pi) * np.arange(L, dtype=f32) / f32(L)).astype(f32)
        bands = np.linspace(1e-4, 15, 16, dtype=f32)
        ang = (omega[:, None] * bands[None, :]).astype(f32)
        z = np.concatenate([t, np.cos(ang).astype(f32), -np.sin(ang).astype(f32)], axis=-1).astype(f32)
        c[f"hy_z{L}"] = np.ascontiguousarray(z.T)
        deltas = np.abs(np.linspace(np.log(1e-2) / 1.5, np.log(1e-2) / 0.3, 256, dtype=f32)).astype(f32)
        window = (np.exp(-t * deltas).astype(f32) + f32(0.05)).astype(f32)
        wb = window.copy()
        wb[0] = 0.0
        c[f"hy_win{L}"] = np.ascontiguousarray(np.stack([window, wb], axis=1))
        NFp = L + 128
        n = 2 * L
        idx = np.arange(L + 1, dtype=np.int64)
        prod = (idx[:, None] * idx[None, :]) % n
        angm = prod.astype(np.float64) * (2.0 * np.pi / n)
        Cm = np.zeros((NFp, NFp), np.float32)
        Sm = np.zeros((NFp, NFp), np.float32)
        Cm[:L + 1, :L + 1] = np.cos(angm)
        Sm[:L + 1, :L + 1] = np.sin(angm)
        c[f"hy_C{L}"] = Cm.astype(ml_dtypes.bfloat16)
        c[f"hy_S{L}"] = Sm.astype(ml_dtypes.bfloat16)
        wfv = np.zeros(NFp, np.float32)
        wfv[:L + 1] = 2.0 / n
        wfv[0] = 1.0 / n
        wfv[L] = 1.0 / n
        c[f"hy_wf{L}"] = np.ascontiguousarray(wfv.reshape(-1, 128).T)
    return c


class Builder:
    def __init__(self, debug=None):
        self.debug = debug or set()
        self.nc = nc = bass.Bass("TRN2", target_bir_lowering=False)
        self.P = Prog(nc)
        self.inp = {}
        self.stack = contextlib.ExitStack()

    def din(self, name, shape, dt=F32):
        ap = self.nc.dram_tensor(name, list(shape), dt, kind="ExternalInput").ap()
        self.inp[name] = ap
        return ap

    def dscr(self, name, shape, dt=F32):
        kind = "ExternalOutput" if name in self.debug else "Internal"
        return self.nc.dram_tensor(name, list(shape), dt, kind=kind).ap()

    def sb(self, st, name, shape, dt=F32):
        self.uid = getattr(self, "uid", 0) + 1
        return st.enter_context(self.nc.sbuf_tensor(f"{name}_{self.uid}", list(shape), dt)).ap()

    def declare(self):
        din = self.din
        self.x = din("x", [SEQ, D])
        self.c = din("c", [D])
        self.ctx = din("ctx", [CTX, D])
        self.c_ctx = din("c_ctx", [D])
        self.ada_w = din("ada_w", [DEPTH, D, 6 * D])
        self.ada_b = din("ada_b", [DEPTH, 6 * D])
        self.norm1_g = din("norm1_g", [DEPTH, D])
        self.norm2_g = din("norm2_g", [DEPTH, D])
        self.w_in = din("w_in", [DEPTH, D, N_IN])
        self.mla_q_norm_g = din("mla_q_norm_g", [DEPTH, QR])
        self.mla_kv_norm_g = din("mla_kv_norm_g", [DEPTH, KVR])
        self.mla_w_uq = din("mla_w_uq", [DEPTH, QR, NH * 96])
        self.mla_w_ukv = din("mla_w_ukv", [DEPTH, KVR, NH * 128])
        self.ml_conv_w = din("ml_conv_w", [DEPTH, 3, MLW])
        self.ml_conv_b = din("ml_conv_b", [DEPTH, MLW])
        self.ml_wq = din("ml_wq", [DEPTH, 4, 64, 64])
        self.ml_wk = din("ml_wk", [DEPTH, 4, 64, 64])
        self.ml_gate_b = din("ml_gate_b", [DEPTH, 16])
        self.ml_norm_g = din("ml_norm_g", [DEPTH, MLW])
        self.hy_conv_w = din("hy_conv_w", [DEPTH, 3, 3 * HYW])
        self.hy_conv_b = din("hy_conv_b", [DEPTH, 3 * HYW])
        self.hy_w1 = din("hy_w1", [DEPTH, 33, 64])
        self.hy_b1 = din("hy_b1", [DEPTH, 64])
        self.hy_w2 = din("hy_w2", [DEPTH, 64, 64])
        self.hy_b2 = din("hy_b2", [DEPTH, 64])
        self.hy_w3 = din("hy_w3", [DEPTH, 64, 2 * HYW])
        self.hy_sin_freq = din("hy_sin_freq", [DEPTH, 64])
        self.hy_bias_d = din("hy_bias_d", [DEPTH, HYW])
        self.w_out = din("w_out", [DEPTH, D, D])
        self.ffn_w_up = din("ffn_w_up", [DEPTH, D, 2 * DFF])
        self.ffn_conv_w = din("ffn_conv_w", [DEPTH, 3, 2 * DFF])
        self.ffn_conv_b = din("ffn_conv_b", [DEPTH, 2 * DFF])
        self.ffn_w_down = din("ffn_w_down", [DEPTH, DFF, D])
        self.final_norm_g = din("final_norm_g", [D])
        self.c_ident = din("ident", [128, 128], BF16)
        self.c_rope_cos = din("rope_cos", [32, SEQ])
        self.c_rope_sin = din("rope_sin", [32, SEQ])
        self.c_ml_mask = din("ml_mask", [128, 2, 128], BF16)
        self.c_ml_selA = din("ml_selA", [96, 4, 128])
        self.c_ml_selW = din("ml_selW", [96, 8])
        for L in (256, 4096):
            NFp = L + 128
            setattr(self, f"c_hy_z{L}", din(f"hy_z{L}", [33, L]))
            setattr(self, f"c_hy_win{L}", din(f"hy_win{L}", [L, 2, 256]))
            setattr(self, f"c_hy_C{L}", din(f"hy_C{L}", [NFp, NFp], BF16))
            setattr(self, f"c_hy_S{L}", din(f"hy_S{L}", [NFp, NFp], BF16))
            setattr(self, f"c_hy_wf{L}", din(f"hy_wf{L}", [128, L // 128 + 1]))
        self.out = self.nc.dram_tensor("out", [SEQ, D], F32, kind="ExternalOutput").ap()
        self.XS = self.dscr("XS", [TT, D])
        self.MOD = self.dscr("MOD", [2, 6 * D])
        self.PT = self.dscr("PT", [N_IN + 32, TT])
        self.PVO = self.dscr("PVO", [TT, 512])
        self.MIXT = self.dscr("MIXT", [D, TT], BF16)
        self.psall = self.nc.alloc_psum_tensor("psall", [128, 8 * 512], F32).ap()
        self.ps = [self.psall[:, i * 512:(i + 1) * 512] for i in range(8)]
        self.pst = [PTok() for _ in range(8)]

    def vec_pc(self, st, name, src, n, q="sp"):
        t = self.sb(st, name, [128, n])
        tok = Tok()
        self.P.dma(q, t, src.rearrange("(c p) -> p c", p=128), writes=[tok], allow_slow_non_contiguous=True)
        return t, tok

    def phase_mod(self, li):
        nc, P = self.nc, self.P
        lst = self.lst
        self.modT = self.sb(lst, "modT", [128, 48, 2])
        self.t_mod = Tok()
        self.s1 = self.sb(lst, "s1", [128, 8, 2])
        self.s2 = self.sb(lst, "s2", [128, 8, 2])
        self.t_s12 = Tok()
        with contextlib.ExitStack() as st:
            cc = self.sb(st, "cc", [128, 8, 2])
            t_cc = Tok()
            P.dma("sp", cc[:, :, 0], self.c.rearrange("(c p) -> p c", p=128), writes=[t_cc], partial=True, allow_slow_non_contiguous=True)
            P.dma("sp", cc[:, :, 1], self.c_ctx.rearrange("(c p) -> p c", p=128), writes=[t_cc], partial=True, allow_slow_non_contiguous=True)
            sc = self.sb(st, "sc", [128, 8, 2])
            t_sc = Tok()
            P.op("act", lambda e: e.activation(out=sc, in_=cc, func=AF.Silu), reads=[t_cc], writes=[t_sc])
            ab = self.sb(st, "ab", [128, 48])
            t_ab = Tok()
            P.dma("sp", ab, self.ada_b[li].rearrange("(c p) -> p c", p=128), writes=[t_ab], allow_slow_non_contiguous=True)
            g12 = self.sb(st, "g12", [128, 8, 2])
            t_g12 = Tok()
            P.dma("sp", g12[:, :, 0], self.norm1_g[li].rearrange("(c p) -> p c", p=128), writes=[t_g12], partial=True, allow_slow_non_contiguous=True)
            P.dma("sp", g12[:, :, 1], self.norm2_g[li].rearrange("(c p) -> p c", p=128), writes=[t_g12], partial=True, allow_slow_non_contiguous=True)
            wt = [self.sb(st, f"adaw{i}", [128, 8, 512]) for i in range(2)]
            t_wt = [Tok(), Tok()]
            acc = self.ps[0]
            t_acc = self.pst[0]
            for nb in range(12):
                s = nb % 2
                P.dma("sp" if nb % 2 == 0 else "act", wt[s],
                      self.ada_w[li, :, nb * 512:(nb + 1) * 512].rearrange("(c p) n -> p c n", p=128),
                      writes=[t_wt[s]])
                for j in range(4):
                    n = nb * 4 + j
                    for k in range(8):
                        P.op("pe", lambda e, s=s, j=j, k=k, n=n: e.matmul(
                            acc[:, 2 * n:2 * n + 2], lhsT=wt[s][:, k, j * 128:(j + 1) * 128], rhs=sc[:, k, :],
                            start=(k == 0), stop=(k == 7)),
                            reads=[t_wt[s], t_sc], writes=[t_acc], skip_self=True)
            mod = self.modT
            P.op("dve", lambda e: e.tensor_tensor(out=mod, in0=acc[:, 0:96].rearrange("p (n v) -> p n v", v=2),
                                                  in1=ab.unsqueeze(2).to_broadcast([128, 48, 2]), op=ALU.add),
                 reads=[t_acc, t_ab], writes=[self.t_mod])
            for (dst, gi, c0) in ((self.s1, 0, 8), (self.s2, 1, 32)):
                P.op("dve", lambda e, dst=dst, gi=gi, c0=c0: e.scalar_tensor_tensor(
                    out=dst, in0=mod[:, c0:c0 + 8, :], scalar=1.0, in1=g12[:, :, gi:gi + 1].to_broadcast([128, 8, 2]),
                    op0=ALU.add, op1=ALU.mult), reads=[self.t_mod, t_g12], writes=[self.t_s12], partial=True)
            for v in range(2):
                P.dma("sp", self.MOD[v].rearrange("(c p) -> p c", p=128), mod[:, :, v], reads=[self.t_mod], writes=[self.t_MOD],
                      partial=True, allow_slow_non_contiguous=True)
            P.barrier()

    def norm_transpose(self, st_bufs, rows_ap, t_rows, svec, bvec, v, dst, t_dst, col0, ps_i):
        P = self.P
        xt, t_xt, sq, t_sq, ss, t_ss, xn, t_xn = st_bufs
        P.dma("sp", xt, rows_ap, reads=[t_rows], writes=[t_xt])
        P.op("act", lambda e: e.activation(out=sq, in_=xt, func=AF.Square, accum_out=ss[:, 0:1]), reads=[t_xt], writes=[t_sq, t_ss])
        P.op("dve", lambda e: e.tensor_scalar(out=ss[:, 1:2], in0=ss[:, 0:1], scalar1=1.0 / D, scalar2=EPS, op0=ALU.mult, op1=ALU.add),
             reads=[t_ss], writes=[t_ss])
        P.op("act", lambda e: e.activation(out=ss[:, 2:3], in_=ss[:, 1:2], func=AF.Sqrt), reads=[t_ss], writes=[t_ss])
        P.op("dve", lambda e: e.reciprocal(out=ss[:, 3:4], in_=ss[:, 2:3]), reads=[t_ss], writes=[t_ss])
        P.op("dve", lambda e: e.tensor_scalar(out=xn, in0=xt, scalar1=ss[:, 3:4], scalar2=None, op0=ALU.mult), reads=[t_xt, t_ss], writes=[t_xn])
        pb = self.ps[ps_i].bitcast(BF16)
        t_pb = self.pst[ps_i]
        for c in range(8):
            P.op("pe", lambda e, c=c: e.transpose(pb[:, c * 128:(c + 1) * 128], xn[:, c * 128:(c + 1) * 128], self.identb),
                 reads=[t_xn, self.t_ident], writes=[t_pb], skip_self=True, partial=(c > 0))
        for c in range(8):
            if c % 2 == 0:
                P.op("act", lambda e, c=c: e.activation(out=dst[:, c, col0:col0 + 128], in_=pb[:, c * 128:(c + 1) * 128], func=AF.Identity,
                                                         scale=svec[:, c, v:v + 1], bias=bvec[:, c, v:v + 1]),
                     reads=[t_pb, self.t_s12, self.t_mod], writes=[t_dst], partial=True)
            else:
                P.op("dve", lambda e, c=c: e.tensor_scalar(out=dst[:, c, col0:col0 + 128], in0=pb[:, c * 128:(c + 1) * 128],
                                                           scalar1=svec[:, c, v:v + 1], scalar2=bvec[:, c, v:v + 1], op0=ALU.mult, op1=ALU.add),
                     reads=[t_pb, self.t_s12, self.t_mod], writes=[t_dst], partial=True)

    def load_cast_weight(self, st, dst, t_dst, src_ap, ncols, blk=512, k_chunks=8, engs=None):
        P = self.P
        engs = engs or ("dve", "act")
        stg = [self.sb(st, f"wstg{id(dst) % 9973}_{i}", [128, k_chunks, blk]) for i in range(2)]
        t_stg = [Tok(), Tok()]
        i = 0
        for c0 in range(0, ncols, blk):
            w = min(blk, ncols - c0)
            s = i % 2
            P.dma("sp" if i % 2 == 0 else "act", stg[s][:, :, 0:w], src_ap[:, c0:c0 + w].rearrange("(c p) n -> p c n", p=128), writes=[t_stg[s]])
            eng = engs[i % len(engs)]
            if eng == "act":
                P.op("act", lambda e, s=s, c0=c0, w=w: e.copy(out=dst[:, :, c0:c0 + w], in_=stg[s][:, :, 0:w]), reads=[t_stg[s]], writes=[t_dst], partial=True)
            else:
                P.op(eng, lambda e, s=s, c0=c0, w=w: e.tensor_copy(out=dst[:, :, c0:c0 + w], in_=stg[s][:, :, 0:w]), reads=[t_stg[s]], writes=[t_dst], partial=True)
            i += 1

    def phase_inproj(self, li):
        nc, P = self.nc, self.P
        NW = N_IN + 32
        with contextlib.ExitStack() as st:
            wb = self.sb(st, "winb", [128, 8, NW], BF16)
            t_wb = Tok()
            with contextlib.ExitStack() as st2:
                self.load_cast_weight(st2, wb, t_wb, self.w_in[li], N_IN)
                P.op("dve", lambda e: e.tensor_scalar(out=wb[:, :, N_IN:N_IN + 16], in0=wb[:, :, 656:672], scalar1=-1.0, scalar2=None, op0=ALU.mult),
                     reads=[t_wb], writes=[t_wb], partial=True)
                P.op("dve", lambda e: e.tensor_copy(out=wb[:, :, N_IN + 16:N_IN + 32], in_=wb[:, :, 640:656]), reads=[t_wb], writes=[t_wb], partial=True)
                P.barrier()
            NB = 2
            bufs = []
            for i in range(NB):
                bufs.append((self.sb(st, f"xt{i}", [128, D]), Tok(), self.sb(st, f"sq{i}", [128, D], BF16), Tok(),
                             self.sb(st, f"ss{i}", [128, 4]), Tok(), self.sb(st, f"xn{i}", [128, D], BF16), Tok()))
            hT = [self.sb(st, f"hT{i}", [128, 8, 256], BF16) for i in range(2)]
            t_hT = [Tok(), Tok()]
            stage = [self.sb(st, f"stg{i}", [128, 16, 256]) for i in range(2)]
            t_stage = [Tok(), Tok()]
            svo = [self.sb(st, f"svo{i}", [128, 512]) for i in range(2)]
            t_svo = [Tok(), Tok()]
            chunks = [(0, 128), (128, 128), (256, 128), (384, 128), (512, 128), (640, 32), (672, 128), (800, 128), (1440, 16)]
            chunks += [(HY_LO + 128 * i, 128) for i in range(6)] + [(N_IN, 32)]
            groups = [(0, 3), (3, 2), (5, 1), (6, 2), (8, 1), (9, 6), (15, 1)]
            nsub = 0
            for ti in range(TT // 256):
                t0 = ti * 256
                v = 1 if ti == 0 else 0
                hs = ti % 2
                for sub in range(2):
                    self.norm_transpose(bufs[nsub % NB], self.XS[t0 + sub * 128:t0 + (sub + 1) * 128, :], self.t_XS, self.s1,
                                        self.modT[:, 0:8, :], v, hT[hs], t_hT[hs], sub * 128, ps_i=nsub % 2)
                    nsub += 1
                sg = stage[hs]
                for ci, (c0, M) in enumerate(chunks):
                    pi = 2 + ci % 4
                    for k in range(8):
                        P.op("pe", lambda e, pi=pi, k=k, c0=c0, M=M, hs=hs: e.matmul(self.ps[pi][0:M, 0:256], lhsT=wb[:, k, c0:c0 + M], rhs=hT[hs][:, k, :],
                                                                               start=(k == 0), stop=(k == 7)),
                             reads=[t_wb, t_hT[hs]], writes=[self.pst[pi]], skip_self=True)
                    if ci % 2 == 0:
                        P.op("act", lambda e, pi=pi, M=M, ci=ci: e.copy(out=sg[0:M, ci, :], in_=self.ps[pi][0:M, 0:256]), reads=[self.pst[pi]], writes=[t_stage[hs]], partial=True)
                    else:
                        P.op("dve", lambda e, pi=pi, M=M, ci=ci: e.tensor_copy(out=sg[0:M, ci, :], in_=self.ps[pi][0:M, 0:256]), reads=[self.pst[pi]], writes=[t_stage[hs]], partial=True)
                for (g0, gn) in groups:
                    c0, M = chunks[g0]
                    dst = self.PT[c0:c0 + M * gn, t0:t0 + 256]
                    if gn > 1:
                        dst = dst.rearrange("(c p) t -> p c t", p=128)
                        P.dma("pool", dst, sg[:, g0:g0 + gn, :], reads=[t_stage[hs]], writes=[self.t_PT], partial=True)
                    else:
                        P.dma("pool", dst, sg[0:M, g0, :], reads=[t_stage[hs]], writes=[self.t_PT], partial=True)
                for sub in range(2):
                    pi = 6 + sub
                    for k in range(8):
                        P.op("pe", lambda e, pi=pi, k=k, sub=sub, hs=hs: e.matmul(self.ps[pi], lhsT=hT[hs][:, k, sub * 128:(sub + 1) * 128], rhs=wb[:, k, 928:1440],
                                                                               start=(k == 0), stop=(k == 7)),
                             reads=[t_wb, t_hT[hs]], writes=[self.pst[pi]], skip_self=True)
                    P.op("act" if sub == 0 else "dve", (lambda e, pi=pi, sub=sub: e.copy(out=svo[sub], in_=self.ps[pi])) if sub == 0 else
                         (lambda e, pi=pi, sub=sub: e.tensor_copy(out=svo[sub], in_=self.ps[pi])), reads=[self.pst[pi]], writes=[t_svo[sub]])
                    P.dma("pool", self.PVO[t0 + sub * 128:t0 + (sub + 1) * 128, :], svo[sub], reads=[t_svo[sub]], writes=[self.t_PVO], partial=True)
            P.barrier()

    def rms_bcast(self, src, nk, n, ones, t_ones, nfeat, ps_i, R, t_R, sq, t_sq, t_src):
        P = self.P
        P.op("dve", lambda e: e.tensor_tensor(out=sq[:, 0:nk, 0:n], in0=src[:, 0:nk, 0:n], in1=src[:, 0:nk, 0:n], op=ALU.mult), reads=[t_src], writes=[t_sq])
        ps, t_ps = self.ps[ps_i], self.pst[ps_i]
        for k in range(nk):
            P.op("pe", lambda e, k=k: e.matmul(ps[:, 0:n], lhsT=ones, rhs=sq[:, k, 0:n], start=(k == 0), stop=(k == nk - 1)),
                 reads=[t_ones, t_sq], writes=[t_ps], skip_self=True)
        P.op("dve", lambda e: e.tensor_scalar(out=R[:, 0:n], in0=ps[:, 0:n], scalar1=1.0 / nfeat, scalar2=EPS, op0=ALU.mult, op1=ALU.add),
             reads=[t_ps], writes=[t_R])
        P.op("act", lambda e: e.activation(out=R[:, 0:n], in_=R[:, 0:n], func=AF.Ln), reads=[t_R], writes=[t_R])
        P.op("act", lambda e: e.activation(out=R[:, 0:n], in_=R[:, 0:n], func=AF.Exp, scale=-0.5), reads=[t_R], writes=[t_R])

    def phase_mla(self, li):
        nc, P = self.nc, self.P
        last = li == DEPTH - 1
        scale = float(96 ** -0.5)
        with contextlib.ExitStack() as st:
            sb = lambda name, shape, dt=F32: self.sb(st, name, shape, dt)
            ones = sb("onesb", [128, 128], BF16); t_ones = Tok()
            P.op("pool", lambda e: e.memset(ones, 1.0), writes=[t_ones])
            KT = sb("KT", [128, NH, TT], BF16); t_KT = Tok()
            VP = sb("VP", [128, TT // 128, NH, 65], BF16); t_VP = Tok()
            P.op("pool", lambda e: e.memset(VP, 1.0), writes=[t_VP])
            sel65 = sb("sel65", [128, 64]); t_sel = Tok()
            P.op("pool", lambda e: e.memset(sel65, 0.0), writes=[t_sel])
            P.op("pool", lambda e: e.memset(sel65[64:65, :], 1.0), reads=[t_sel], writes=[t_sel])
            wqb = sb("wqb", [128, 3, NH, 192], BF16); t_wq = Tok()
            wkb = sb("wkb", [128, 2, NH, 64], BF16); t_wk = Tok()
            wvb = sb("wvb", [128, 2, NH, 64], BF16); t_wv = Tok()
            mk = sb("mk", [128, NH]); t_mk = Tok()
            P.op("pool", lambda e: e.memset(mk, 0.0), writes=[t_mk])
            with contextlib.ExitStack() as st2:
                wq = self.sb(st2, "wq32", [128, 3, NH * 96]); t_wq32 = Tok()
                wkv = self.sb(st2, "wkv32", [128, 2, NH * 128]); t_wkv32 = Tok()
                gq, t_gq = self.vec_pc(st2, "gq", self.mla_q_norm_g[li], 3)
                gkv, t_gkv = self.vec_pc(st2, "gkv", self.mla_kv_norm_g[li], 2)
                P.dma("sp", wq, self.mla_w_uq[li].rearrange("(c p) n -> p c n", p=128), writes=[t_wq32])
                P.dma("act", wkv, self.mla_w_ukv[li].rearrange("(c p) n -> p c n", p=128), writes=[t_wkv32])
                for k in range(3):
                    P.op("dve", lambda e, k=k: e.tensor_scalar(out=wq[:, k, :], in0=wq[:, k, :], scalar1=gq[:, k:k + 1], scalar2=None, op0=ALU.mult),
                         reads=[t_gq], writes=[t_wq32])
                for k in range(2):
                    P.op("dve", lambda e, k=k: e.tensor_scalar(out=wkv[:, k, :], in0=wkv[:, k, :], scalar1=gkv[:, k:k + 1], scalar2=None, op0=ALU.mult),
                         reads=[t_gkv], writes=[t_wkv32])
                wq4 = wq.rearrange("p k (h d) -> p k h d", d=96)
                wkv4 = wkv.rearrange("p k (h d) -> p k h d", d=128)
                P.op("pool", lambda e: e.memset(wqb, 0.0), writes=[t_wq])
                for k in range(3):
                    P.op("dve", lambda e, k=k: e.tensor_copy(out=wqb[:, k, :, 0:96], in_=wq4[:, k, :, 0:96]), reads=[t_wq32], writes=[t_wq], partial=True)
                    P.op("dve", lambda e, k=k: e.tensor_scalar(out=wqb[:, k, :, 160:176], in0=wq4[:, k, :, 80:96], scalar1=-1.0, scalar2=None, op0=ALU.mult),
                         reads=[t_wq32], writes=[t_wq], partial=True)
                    P.op("dve", lambda e, k=k: e.tensor_copy(out=wqb[:, k, :, 176:192], in_=wq4[:, k, :, 64:80]), reads=[t_wq32], writes=[t_wq], partial=True)
                for k in range(2):
                    P.op("dve", lambda e, k=k: e.tensor_copy(out=wkb[:, k, :, :], in_=wkv4[:, k, :, 0:64]), reads=[t_wkv32], writes=[t_wk], partial=True)
                    P.op("dve", lambda e, k=k: e.tensor_copy(out=wvb[:, k, :, :], in_=wkv4[:, k, :, 64:128]), reads=[t_wkv32], writes=[t_wv], partial=True)
                P.barrier()
            NBUF = 2
            pin = [sb(f"pin{i}", [128, 3, 512]) for i in range(NBUF)]; t_pin = [Tok() for _ in range(NBUF)]
            cs = [sb(f"cs{i}", [128, 2, 512]) for i in range(NBUF)]; t_cs = [Tok() for _ in range(NBUF)]
            sq = sb("sqm", [128, 3, 512], BF16); t_sq = Tok()
            R = sb("Rm", [128, 512]); t_R = Tok()
            pn = sb("pn", [128, 3, 512], BF16); t_pn = Tok()
            tmpa = sb("tmpa", [128, 512]); t_tmpa = Tok()
            tmpb = sb("tmpb", [128, 512]); t_tmpb = Tok()
            sqk = sb("sqk", [128, NH, 512], BF16); t_sqk = Tok()
            B_singles = (sq, t_sq, R, t_R, pn, t_pn, tmpa, t_tmpa, tmpb, t_tmpb, sqk, t_sqk)
            stA = contextlib.ExitStack()
            krin = [self.sb(stA, f"krin{i}", [128, 2, 512]) for i in range(NBUF)]; t_krin = [Tok() for _ in range(NBUF)]
            tchunks = [(0, CTX)] + [(CTX + 512 * j, 512) for j in range(SEQ // 512)]

            def load_rope_tables(bi, t0, n):
                p0 = t0 - CTX
                P.dma("act", cs[bi][64:96, 0, 0:n], self.c_rope_cos[:, p0:p0 + n], writes=[t_cs[bi]], partial=True)
                P.dma("act", cs[bi][64:96, 1, 0:n], self.c_rope_sin[:, p0:p0 + n], writes=[t_cs[bi]], partial=True)

            A_sq = [self.sb(stA, f"Asq{i}", [128, 3, 512], BF16) for i in range(2)]; tA_sq = [Tok(), Tok()]
            A_R = [self.sb(stA, f"AR{i}", [128, 512]) for i in range(2)]; tA_R = [Tok(), Tok()]
            A_pn = [self.sb(stA, f"Apn{i}", [128, 2, 512], BF16) for i in range(2)]; tA_pn = [Tok(), Tok()]
            A_ta = [self.sb(stA, f"Ata{i}", [128, 512]) for i in range(2)]; tA_ta = [Tok(), Tok()]
            A_tb = [self.sb(stA, f"Atb{i}", [128, 512]) for i in range(2)]; tA_tb = [Tok(), Tok()]
            A_krb = [self.sb(stA, f"Akrb{i}", [128, 512], BF16) for i in range(2)]; tA_krb = [Tok(), Tok()]
            A_sqk = [self.sb(stA, f"Asqk{i}", [128, NH, 512], BF16) for i in range(2)]; tA_sqk = [Tok(), Tok()]
            A_mt = [self.sb(stA, f"Amt{i}", [128, NH]) for i in range(2)]; tA_mt = [Tok(), Tok()]
            for ci, (t0, n) in enumerate(tchunks):
                bi = ci % NBUF
                is_ctx = ci == 0
                sq, t_sq, R, t_R, pn, t_pn = A_sq[bi], tA_sq[bi], A_R[bi], tA_R[bi], A_pn[bi], tA_pn[bi]
                tmpa, t_tmpa, tmpb, t_tmpb, krb, t_krb = A_ta[bi], tA_ta[bi], A_tb[bi], tA_tb[bi], A_krb[bi], tA_krb[bi]
                sqk, t_sqk, mtmp, t_mtmp = A_sqk[bi], tA_sqk[bi], A_mt[bi], tA_mt[bi]
                P.dma("sp", pin[bi][:, 0:2, 0:n], self.PT[QR:QR + KVR, t0:t0 + n].rearrange("(c p) t -> p c t", p=128), reads=[self.t_PT], writes=[t_pin[bi]])
                P.dma("sp", krin[bi][64:96, 0, 0:n], self.PT[640:672, t0:t0 + n], reads=[self.t_PT], writes=[t_krin[bi]], partial=True)
                if not is_ctx:
                    P.dma("sp", krin[bi][64:96, 1, 0:n], self.PT[N_IN:N_IN + 32, t0:t0 + n], reads=[self.t_PT], writes=[t_krin[bi]], partial=True)
                    load_rope_tables(bi, t0, n)
                self.rms_bcast(pin[bi], 2, n, ones, t_ones, KVR, 6, R, t_R, sq, t_sq, t_pin[bi])
                P.op("dve", lambda e, bi=bi, n=n: e.tensor_tensor(out=pn[:, 0:2, 0:n], in0=pin[bi][:, 0:2, 0:n], in1=R[:, 0:n].unsqueeze(1).to_broadcast([128, 2, n]), op=ALU.mult),
                     reads=[t_pin[bi], t_R], writes=[t_pn])
                if is_ctx:
                    P.op("dve", lambda e, bi=bi, n=n: e.tensor_copy(out=krb[64:96, 0:n], in_=krin[bi][64:96, 0, 0:n]), reads=[t_krin[bi]], writes=[t_krb])
                else:
                    P.op("dve", lambda e, bi=bi, n=n: e.tensor_tensor(out=tmpa[64:96, 0:n], in0=krin[bi][64:96, 0, 0:n], in1=cs[bi][64:96, 0, 0:n], op=ALU.mult), reads=[t_krin[bi], t_cs[bi]], writes=[t_tmpa])
                    P.op("dve", lambda e, bi=bi, n=n: e.tensor_tensor(out=tmpb[64:96, 0:n], in0=krin[bi][64:96, 1, 0:n], in1=cs[bi][64:96, 1, 0:n], op=ALU.mult), reads=[t_krin[bi], t_cs[bi]], writes=[t_tmpb])
                    P.op("dve", lambda e, n=n: e.tensor_tensor(out=krb[64:96, 0:n], in0=tmpa[64:96, 0:n], in1=tmpb[64:96, 0:n], op=ALU.add), reads=[t_tmpa, t_tmpb], writes=[t_krb])
                P.op("dve", lambda e, t0=t0, n=n: e.tensor_copy(out=KT[64:96, :, t0:t0 + n], in_=krb[64:96, 0:n].unsqueeze(1).to_broadcast([32, NH, n])), reads=[t_krb], writes=[t_KT], partial=True)
                for h in range(NH):
                    pi = 4 + h % 2
                    for k in range(2):
                        P.op("pe", lambda e, h=h, k=k, pi=pi, n=n: e.matmul(self.ps[pi][0:64, 0:n], lhsT=wkb[:, k, h, :], rhs=pn[:, k, 0:n], start=(k == 0), stop=(k == 1)),
                             reads=[t_wk, t_pn], writes=[self.pst[pi]], skip_self=True)
                    if h % 2 == 0:
                        P.op("act", lambda e, h=h, pi=pi, t0=t0, n=n: e.copy(out=KT[0:64, h, t0:t0 + n], in_=self.ps[pi][0:64, 0:n]), reads=[self.pst[pi]], writes=[t_KT], partial=True)
                    else:
                        P.op("dve", lambda e, h=h, pi=pi, t0=t0, n=n: e.tensor_copy(out=KT[0:64, h, t0:t0 + n], in_=self.ps[pi][0:64, 0:n]), reads=[self.pst[pi]], writes=[t_KT], partial=True)
                for sub in range(n // 128):
                    j = (t0 + sub * 128) // 128
                    pi = 2 + sub % 2
                    for k in range(2):
                        P.op("pe", lambda e, k=k, pi=pi, sub=sub: e.matmul(self.ps[pi], lhsT=pn[:, k, sub * 128:(sub + 1) * 128], rhs=wvb[:, k, :, :].rearrange("p h d -> p (h d)"), start=(k == 0), stop=(k == 1)),
                             reads=[t_wv, t_pn], writes=[self.pst[pi]], skip_self=True)
                    src = self.ps[pi].rearrange("p (h d) -> p h d", d=64)
                    if sub % 2 == 0:
                        P.op("act", lambda e, j=j, src=src: e.copy(out=VP[:, j, :, 0:64], in_=src), reads=[self.pst[pi]], writes=[t_VP], partial=True)
                    else:
                        P.op("dve", lambda e, j=j, src=src: e.tensor_copy(out=VP[:, j, :, 0:64], in_=src), reads=[self.pst[pi]], writes=[t_VP], partial=True)
                P.op("act", lambda e, t0=t0, n=n: e.activation(out=sqk[0:96, :, 0:n], in_=KT[0:96, :, t0:t0 + n], func=AF.Square), reads=[t_KT], writes=[t_sqk])
                for h in range(NH):
                    pi = (7, 0, 1)[h % 3]
                    P.op("pe", lambda e, h=h, n=n, pi=pi: e.matmul(self.ps[pi][:, 0:n], lhsT=ones[0:96, :], rhs=sqk[0:96, h, 0:n], start=True, stop=True),
                         reads=[t_ones, t_sqk], writes=[self.pst[pi]], skip_self=True)
                    P.op("dve", lambda e, h=h, n=n, pi=pi: e.tensor_reduce(out=mtmp[:, h:h + 1], in_=self.ps[pi][:, 0:n], axis=AX.X, op=ALU.max), reads=[self.pst[pi]], writes=[t_mtmp], partial=True)
                P.op("dve", lambda e: e.tensor_tensor(out=mk, in0=mk, in1=mtmp, op=ALU.max), reads=[t_mtmp], writes=[t_mk])
            P.barrier()
            stA.close()
            sq, t_sq, R, t_R, pn, t_pn, tmpa, t_tmpa, tmpb, t_tmpb, sqk, t_sqk = B_singles
            QT = [sb(f"QT{i}", [128, NH, 512], BF16) for i in range(2)]; t_QT = [Tok(), Tok()]
            pts = [sb(f"pts{i}", [128, 2, 512], BF16) for i in range(3)]; t_pts = [Tok() for _ in range(3)]
            den = sb("den", [128, 512]); t_den = Tok()
            rden = sb("rden", [128, 512]); t_rden = Tok()
            ot = [sb(f"ot{i}", [128, 512], BF16) for i in range(2)]; t_ot = [Tok(), Tok()]
            mq = [sb(f"mq{i}", [128, NH]) for i in range(2)]; t_mq = [Tok(), Tok()]
            negm = [sb(f"negm{i}", [128, NH]) for i in range(2)]; t_negm = [Tok(), Tok()]
            qchunks = ([] if last else [(0, CTX)]) + tchunks[1:]
            nq = len(qchunks)

            def prologue_parts(qi):
                t0, n = qchunks[qi]
                bi = qi % NBUF
                is_ctx = t0 == 0
                qt, t_qt = QT[qi % 2], t_QT[qi % 2]
                parts = {}

                def part_load():
                    P.dma("sp", pin[bi][:, 0:3, 0:n], self.PT[0:QR, t0:t0 + n].rearrange("(c p) t -> p c t", p=128), reads=[self.t_PT], writes=[t_pin[bi]])
                    if not is_ctx:
                        load_rope_tables(bi, t0, n)
                    self.rms_bcast(pin[bi], 3, n, ones, t_ones, QR, 5, R, t_R, sq, t_sq, t_pin[bi])
                    P.op("dve", lambda e: e.tensor_tensor(out=pn[:, 0:3, 0:n], in0=pin[bi][:, 0:3, 0:n], in1=R[:, 0:n].unsqueeze(1).to_broadcast([128, 3, n]), op=ALU.mult),
                         reads=[t_pin[bi], t_R], writes=[t_pn])
                parts["load"] = part_load

                def part_A(h):
                    for k in range(3):
                        P.op("pe", lambda e, k=k: e.matmul(self.ps[7][0:96, 0:n], lhsT=wqb[:, k, h, 0:96], rhs=pn[:, k, 0:n], start=(k == 0), stop=(k == 2)),
                             reads=[t_wq, t_pn], writes=[self.pst[7]], skip_self=True)
                    if is_ctx:
                        P.op("dve", lambda e: e.tensor_copy(out=qt[0:96, h, 0:n], in_=self.ps[7][0:96, 0:n]), reads=[self.pst[7]], writes=[t_qt], partial=True)
                        return
                    P.op("dve", lambda e: e.tensor_tensor(out=tmpa[64:96, 0:n], in0=self.ps[7][64:96, 0:n], in1=cs[bi][64:96, 0, 0:n], op=ALU.mult), reads=[self.pst[7], t_cs[bi]], writes=[t_tmpa])
                    P.op("dve", lambda e: e.tensor_copy(out=qt[0:64, h, 0:n], in_=self.ps[7][0:64, 0:n]), reads=[self.pst[7]], writes=[t_qt], partial=True)

                def part_B(h):
                    if is_ctx:
                        return
                    for k in range(3):
                        P.op("pe", lambda e, k=k: e.matmul(self.ps[6][0:96, 0:n], lhsT=wqb[:, k, h, 96:192], rhs=pn[:, k, 0:n], start=(k == 0), stop=(k == 2)),
                             reads=[t_wq, t_pn], writes=[self.pst[6]], skip_self=True)
                    P.op("dve", lambda e: e.tensor_tensor(out=tmpb[64:96, 0:n], in0=self.ps[6][64:96, 0:n], in1=cs[bi][64:96, 1, 0:n], op=ALU.mult), reads=[self.pst[6], t_cs[bi]], writes=[t_tmpb])
                    P.op("dve", lambda e: e.tensor_tensor(out=qt[64:96, h, 0:n], in0=tmpa[64:96, 0:n], in1=tmpb[64:96, 0:n], op=ALU.add), reads=[t_tmpa, t_tmpb], writes=[t_qt], partial=True)

                def part_N(h):
                    P.op("dve", lambda e: e.tensor_tensor(out=sqk[0:96, h, 0:n], in0=qt[0:96, h, 0:n], in1=qt[0:96, h, 0:n], op=ALU.mult), reads=[t_qt], writes=[t_sqk], partial=True)
                    P.op("pe", lambda e: e.matmul(self.ps[7][:, 0:n], lhsT=ones[0:96, :], rhs=sqk[0:96, h, 0:n], start=True, stop=True),
                         reads=[t_ones, t_sqk], writes=[self.pst[7]], skip_self=True)
                    P.op("dve", lambda e: e.tensor_reduce(out=mq[qi % 2][:, h:h + 1], in_=self.ps[7][:, 0:n], axis=AX.X, op=ALU.max), reads=[self.pst[7]], writes=[t_mq[qi % 2]], partial=True)

                def part_fin():
                    nm, t_nm = negm[qi % 2], t_negm[qi % 2]
                    P.op("dve", lambda e: e.tensor_tensor(out=nm, in0=mq[qi % 2], in1=mk, op=ALU.mult), reads=[t_mq[qi % 2], t_mk], writes=[t_nm])
                    P.op("act", lambda e: e.activation(out=nm, in_=nm, func=AF.Ln), reads=[t_nm], writes=[t_nm])
                    P.op("act", lambda e: e.activation(out=nm, in_=nm, func=AF.Exp, scale=0.5), reads=[t_nm], writes=[t_nm])
                    P.op("dve", lambda e: e.tensor_scalar(out=nm, in0=nm, scalar1=-scale, scalar2=None, op0=ALU.mult), reads=[t_nm], writes=[t_nm])
                for h in range(NH):
                    parts[("A", h)] = (lambda h=h: part_A(h))
                    parts[("B", h)] = (lambda h=h: part_B(h))
                    parts[("N", h)] = (lambda h=h: part_N(h))
                parts["fin"] = part_fin
                return parts

            def emit_all(parts):
                parts["load"]()
                for h in range(NH):
                    parts[("A", h)](); parts[("B", h)](); parts[("N", h)]()
                parts["fin"]()

            def groups_of(qi):
                t0, n = qchunks[qi]
                ktiles = list(range(CTX // 128)) if t0 == 0 else list(range(TT // 128))
                return [ktiles[i:i + 2] for i in range(0, len(ktiles), 2)]

            def s_mm(qi, h, g):
                t0, n = qchunks[qi]
                qt, t_qt = QT[qi % 2], t_QT[qi % 2]
                b0 = 2 * (g % 2)
                for i, j in enumerate(groups_of(qi)[g]):
                    P.op("pe", lambda e, j=j, i=i: e.matmul(self.ps[b0 + i][:, 0:n], lhsT=KT[0:96, h, j * 128:(j + 1) * 128], rhs=qt[0:96, h, 0:n], start=True, stop=True),
                         reads=[t_KT, t_qt], writes=[self.pst[b0 + i]], skip_self=True)

            emit_all(prologue_parts(0))
            jobs = [(qi, h) for qi in range(nq) for h in range(NH)]
            pcount = 0
            pre_issued = set()
            nxt_parts = None
            for ji, (qi, h) in enumerate(jobs):
                t0, n = qchunks[qi]
                groups = groups_of(qi)
                ng = len(groups)
                po = 4
                nm, t_nm = negm[qi % 2], t_negm[qi % 2]
                if h == 0:
                    nxt_parts = prologue_parts(qi + 1) if qi + 1 < nq else None
                sched = {}
                if nxt_parts is not None and ng >= 16:
                    if h == 0:
                        sched[2] = ["load"]
                    else:
                        sched[3] = [("A", h - 1)]
                        sched[7] = [("B", h - 1)]
                        sched[11] = [("N", h - 1)]
                    if h == NH - 1:
                        sched[12] = [("A", h)]
                        sched[14] = [("B", h)]
                        sched[16] = [("N", h), "fin"]
                if (qi, h) not in pre_issued:
                    s_mm(qi, h, 0)
                    if ng > 1:
                        s_mm(qi, h, 1)
                for g in range(ng):
                    b0 = 2 * (g % 2)
                    nj = len(groups[g])
                    pb = pcount % 3
                    pcount += 1
                    src = self.psall[:, b0 * 512:(b0 + nj) * 512].rearrange("p (a c) -> p a c", c=512)[:, :, 0:n]
                    P.op("act", lambda e, pb=pb, nj=nj, src=src: e.activation(out=pts[pb][:, 0:nj, 0:n], in_=src, func=AF.Exp, bias=nm[:, h:h + 1], scale=scale),
                         reads=[self.pst[b0 + i] for i in range(nj)] + [t_nm], writes=[t_pts[pb]])
                    for i, j in enumerate(groups[g]):
                        first = (g == 0 and i == 0)
                        lastk = (g == ng - 1 and i == nj - 1)
                        P.op("pe", lambda e, pb=pb, j=j, i=i, first=first, lastk=lastk: e.matmul(self.ps[po][0:65, 0:n], lhsT=VP[:, j, h, :], rhs=pts[pb][:, i, 0:n], start=first, stop=lastk),
                             reads=[t_VP, t_pts[pb]], writes=[self.pst[po]], skip_self=True)
                    if g + 2 < ng:
                        s_mm(qi, h, g + 2)
                    for key in sched.get(g, []):
                        nxt_parts[key]()
                if ji + 1 < len(jobs):
                    qn, hn = jobs[ji + 1]
                    if qn == qi:
                        s_mm(qn, hn, 0)
                        if len(groups_of(qn)) > 1:
                            s_mm(qn, hn, 1)
                        pre_issued.add((qn, hn))
                o = ot[h % 2]
                t_o = t_ot[h % 2]
                P.op("act", lambda e: e.copy(out=den[0:65, 0:n], in_=self.ps[po][0:65, 0:n]), reads=[self.pst[po]], writes=[t_den])
                P.op("pe", lambda e: e.matmul(self.ps[5][0:64, 0:n], lhsT=sel65[0:65, :], rhs=den[0:65, 0:n], start=True, stop=True),
                     reads=[t_sel, t_den], writes=[self.pst[5]], skip_self=True)
                P.op("dve", lambda e: e.reciprocal(out=rden[0:64, 0:n], in_=self.ps[5][0:64, 0:n]), reads=[self.pst[5]], writes=[t_rden])
                P.op("dve", lambda e, o=o: e.tensor_tensor(out=o[0:64, 0:n], in0=den[0:64, 0:n], in1=rden[0:64, 0:n], op=ALU.mult),
                     reads=[t_den, t_rden], writes=[t_o])
                P.dma("pool", self.MIXT[h * 64:(h + 1) * 64, t0:t0 + n], o[0:64, 0:n], reads=[t_o], writes=[self.t_MIXT], partial=True)
                if nxt_parts is not None and ng < 16 and h == NH - 1:
                    emit_all(nxt_parts)
            P.barrier()

    def phase_mlstm(self, li):
        nc, P = self.nc, self.P
        NT = TT // 128
        with contextlib.ExitStack() as st:
            sb = lambda name, shape, dt=F32: self.sb(st, name, shape, dt)
            ub = [sb(f"ub{p}", [128, TT], BF16) for p in range(2)]; t_ub = [Tok(), Tok()]
            kT = [sb(f"kT{p}", [128, TT], BF16) for p in range(2)]; t_kT = [Tok(), Tok()]
            qd = [[sb(f"qd{p}{d}", [128, TT], BF16) for d in range(2)] for p in range(2)]
            t_qd = [[Tok(), Tok()], [Tok(), Tok()]]
            ktok = sb("ktok", [128, NT, 256], BF16); t_ktok = Tok()
            wtok = sb("wtok", [128, NT, 8]); t_wtok = Tok()
            ecol = sb("ecol", [128, 4, NT]); t_ecol = Tok()
            hF = sb("hF", [128, NT, 256]); t_hF = Tok()
            CbS = [[sb(f"CbS{p}{d}", [128, NT + 1, 65], BF16) for d in range(2)] for p in range(2)]
            t_CbS = [[Tok(), Tok()], [Tok(), Tok()]]
            Cst = [[sb(f"Cst{p}{d}", [128, 65]) for d in range(2)] for p in range(2)]
            t_Cst = [[Tok(), Tok()], [Tok(), Tok()]]
            masks = sb("mlmask", [128, 2, 128], BF16); t_masks = Tok()
            P.dma("sp", masks, self.c_ml_mask, writes=[t_masks])
            wqb = sb("mlwq", [128, 2, 128], BF16); wkb = sb("mlwk", [128, 2, 128], BF16); t_w = Tok()
            gml, t_gml = self.vec_pc(st, "gml", self.ml_norm_g[li], 2)
            cw = sb("mlcw", [128, 2, 3]); cb = sb("mlcb", [128, 2]); t_cw = Tok()
            for jj in range(3):
                P.dma("sp", cw[:, :, jj], self.ml_conv_w[li, jj].rearrange("(c p) -> p c", p=128), writes=[t_cw], partial=True, allow_slow_non_contiguous=True)
            P.dma("sp", cb, self.ml_conv_b[li].rearrange("(c p) -> p c", p=128), writes=[t_cw], partial=True, allow_slow_non_contiguous=True)
            for p in range(2):
                for d in range(2):
                    P.op("pool", lambda e, p=p, d=d: e.memset(Cst[p][d], 0.0), writes=[t_Cst[p][d]])
                    P.op("pool", lambda e, p=p, d=d: e.memset(CbS[p][d][:, 0, :], 0.0), writes=[t_CbS[p][d]])
            with contextlib.ExitStack() as st2:
                sb2 = lambda name, shape, dt=F32: self.sb(st2, name, shape, dt)
                w32 = sb2("mlw32", [128, 4, 128]); t_w32 = Tok()
                P.op("pool", lambda e: e.memset(w32, 0.0), writes=[t_w32])
                for p in range(2):
                    for hh in range(2):
                        P.dma("sp", w32[hh * 64:(hh + 1) * 64, p, hh * 64:(hh + 1) * 64], self.ml_wq[li, 2 * p + hh], reads=[], writes=[t_w32], partial=True)
                        P.dma("sp", w32[hh * 64:(hh + 1) * 64, 2 + p, hh * 64:(hh + 1) * 64], self.ml_wk[li, 2 * p + hh], reads=[], writes=[t_w32], partial=True)
                P.op("dve", lambda e: e.tensor_copy(out=wqb, in_=w32[:, 0:2, :]), reads=[t_w32], writes=[t_w], partial=True)
                P.op("dve", lambda e: e.tensor_scalar(out=wkb, in0=w32[:, 2:4, :], scalar1=0.125, scalar2=None, op0=ALU.mult), reads=[t_w32], writes=[t_w], partial=True)
                selA = sb2("selA", [128, 4, 128]); selW = sb2("selW", [128, 8]); t_sel = Tok()
                P.dma("act", selA[0:96], self.c_ml_selA, writes=[t_sel], partial=True)
                P.dma("act", selW[0:96], self.c_ml_selW, writes=[t_sel], partial=True)
                gb = sb2("mlgb", [16, 1]); t_gb = Tok()
                P.dma("sp", gb, self.ml_gate_b[li].rearrange("(p o) -> p o", o=1), writes=[t_gb], allow_slow_non_contiguous=True)
                ones16 = sb2("ones16", [16, 128]); t_o16 = Tok()
                P.op("pool", lambda e: e.memset(ones16, 1.0), writes=[t_o16])
                X = sb2("mlX", [128, TT]); t_X = Tok()
                P.op("pool", lambda e: e.memset(X, 0.0), writes=[t_X])
                P.dma("sp", X[0:16, :], self.PT[1440:1456, :], reads=[self.t_PT], writes=[t_X], partial=True)
                P.op("dve", lambda e: e.tensor_scalar(out=X[0:16, :], in0=X[0:16, :], scalar1=gb[:, 0:1], scalar2=None, op0=ALU.add), reads=[t_gb, t_X], writes=[t_X])
                with contextlib.ExitStack() as st3:
                    LF = self.sb(st3, "mlLF", [16, TT]); t_LF = Tok()
                    CF = self.sb(st3, "mlCF", [16, TT]); t_CF = Tok()
                    P.op("act", lambda e: e.activation(out=LF, in_=X[0:16, :], func=AF.Exp, scale=-1.0), reads=[t_X], writes=[t_LF])
                    P.op("act", lambda e: e.activation(out=LF, in_=LF, func=AF.Ln, bias=1.0), reads=[t_LF], writes=[t_LF])
                    P.op("dve", lambda e: e.tensor_scalar(out=LF, in0=LF, scalar1=-1.0, scalar2=None, op0=ALU.mult), reads=[t_LF], writes=[t_LF])
                    for j in range(NT):
                        P.op("dve", lambda e, j=j: e.tensor_tensor_scan(out=CF[:, j * 128:(j + 1) * 128], data0=ones16, data1=LF[:, j * 128:(j + 1) * 128],
                                                                        initial=0.0, op0=ALU.mult, op1=ALU.add), reads=[t_LF, t_o16], writes=[t_CF], partial=True)
                    P.op("dve", lambda e: e.tensor_tensor(out=LF, in0=LF, in1=CF, op=ALU.subtract), reads=[t_CF], writes=[t_LF])
                    LF3 = LF.rearrange("p (j t) -> p j t", t=128)
                    CF3 = CF.rearrange("p (j t) -> p j t", t=128)
                    P.op("dve", lambda e: e.tensor_tensor(out=LF3, in0=LF3, in1=CF3[:, :, 127:128].to_broadcast([16, NT, 128]), op=ALU.add), reads=[t_CF], writes=[t_LF])
                    P.op("act", lambda e: e.copy(out=X[32:48, :], in_=CF), reads=[t_CF], writes=[t_X], partial=True)
                    P.op("act", lambda e: e.copy(out=X[64:80, :], in_=LF), reads=[t_LF], writes=[t_X], partial=True)
                    P.barrier()
                for j in range(NT):
                    P.op("pe", lambda e, j=j: e.matmul(self.ps[7][:, j * 8:(j + 1) * 8], lhsT=X[0:96, j * 128:(j + 1) * 128], rhs=selW[0:96, :], start=True, stop=True),
                         reads=[t_X, t_sel], writes=[self.pst[7]], skip_self=True)
                P.op("act", lambda e: e.activation(out=wtok.rearrange("p j g -> p (j g)"), in_=self.ps[7][:, 0:NT * 8], func=AF.Exp), reads=[self.pst[7]], writes=[t_wtok])
                pc32 = sb2("mlpc", [128, TT]); t_pc = Tok()
                u32 = sb2("mlu", [128, TT]); t_u = Tok()
                abc = [sb2(f"abc{i}", [128, 512]) for i in range(2)]; t_abc = [Tok(), Tok()]
                blocks = [(0, 256)] + [(256 + 512 * i, 512) for i in range(8)]
                segs = [(0, CTX), (CTX, TT)]
                nabc = 0
                for p in range(2):
                    P.dma("sp", pc32, self.PT[ML_LO + p * 128:ML_LO + (p + 1) * 128, :], reads=[self.t_PT], writes=[t_pc])
                    P.op("dve", lambda e, p=p: e.tensor_scalar(out=u32, in0=pc32, scalar1=cw[:, p, 1:2], scalar2=cb[:, p:p + 1], op0=ALU.mult, op1=ALU.add), reads=[t_pc, t_cw], writes=[t_u])
                    for (a, b) in segs:
                        P.op("dve", lambda e, p=p, a=a, b=b: e.scalar_tensor_tensor(out=u32[:, a + 1:b], in0=pc32[:, a:b - 1], scalar=cw[:, p, 0:1], in1=u32[:, a + 1:b], op0=ALU.mult, op1=ALU.add),
                             reads=[t_pc, t_cw], writes=[t_u])
                        P.op("dve", lambda e, p=p, a=a, b=b: e.scalar_tensor_tensor(out=u32[:, a:b - 1], in0=pc32[:, a + 1:b], scalar=cw[:, p, 2:3], in1=u32[:, a:b - 1], op0=ALU.mult, op1=ALU.add),
                             reads=[t_pc, t_cw], writes=[t_u])
                    P.op("act", lambda e, p=p: e.activation(out=ub[p], in_=u32, func=AF.Silu), reads=[t_u], writes=[t_ub[p]])
                    for (b0, n) in blocks:
                        P.op("pe", lambda e, p=p, b0=b0, n=n: e.matmul(self.ps[0][:, 0:n], lhsT=wqb[:, p, :], rhs=ub[p][:, b0:b0 + n], start=True, stop=True),
                             reads=[t_w, t_ub[p]], writes=[self.pst[0]], skip_self=True)
                        for d in range(2):
                            ai = nabc % 2
                            nabc += 1
                            P.op("pe", lambda e, p=p, d=d, b0=b0, n=n: e.matmul(self.ps[6][:, 0:n], lhsT=selA[0:96, p * 2 + d, :], rhs=X[0:96, b0:b0 + n], start=True, stop=True),
                                 reads=[t_sel, t_X], writes=[self.pst[6]], skip_self=True)
                            P.op("act", lambda e, ai=ai, n=n: e.activation(out=abc[ai][:, 0:n], in_=self.ps[6][:, 0:n], func=AF.Exp), reads=[self.pst[6]], writes=[t_abc[ai]])
                            P.op("dve", lambda e, p=p, d=d, ai=ai, b0=b0, n=n: e.tensor_tensor(out=qd[p][d][:, b0:b0 + n], in0=self.ps[0][:, 0:n], in1=abc[ai][:, 0:n], op=ALU.mult),
                                 reads=[self.pst[0], t_abc[ai]], writes=[t_qd[p][d]], partial=True)
                            c0 = 127 if d == 0 else 0
                            P.op("dve", lambda e, p=p, d=d, ai=ai, b0=b0, n=n, c0=c0: e.tensor_copy(out=ecol[:, p * 2 + d, b0 // 128:(b0 + n) // 128], in_=abc[ai][:, c0:n:128]),
                                 reads=[t_abc[ai]], writes=[t_ecol], partial=True)
                        P.op("pe", lambda e, p=p, b0=b0, n=n: e.matmul(self.ps[1][:, 0:n], lhsT=wkb[:, p, :], rhs=ub[p][:, b0:b0 + n], start=True, stop=True),
                             reads=[t_w, t_ub[p]], writes=[self.pst[1]], skip_self=True)
                        P.op("act", lambda e, p=p, b0=b0, n=n: e.copy(out=kT[p][:, b0:b0 + n], in_=self.ps[1][:, 0:n]), reads=[self.pst[1]], writes=[t_kT[p]], partial=True)
                    for j in range(NT):
                        pi = 2 + j % 2
                        P.op("pe", lambda e, p=p, j=j, pi=pi: e.matmul(self.ps[pi][:, 0:128], lhsT=ub[p][:, j * 128:(j + 1) * 128], rhs=wkb[:, p, :], start=True, stop=True),
                             reads=[t_w, t_ub[p]], writes=[self.pst[pi]], skip_self=True)
                        if j % 2 == 0:
                            P.op("act", lambda e, p=p, j=j, pi=pi: e.copy(out=ktok[:, j, p * 128:(p + 1) * 128], in_=self.ps[pi][:, 0:128]), reads=[self.pst[pi]], writes=[t_ktok], partial=True)
                        else:
                            P.op("dve", lambda e, p=p, j=j, pi=pi: e.tensor_copy(out=ktok[:, j, p * 128:(p + 1) * 128], in_=self.ps[pi][:, 0:128]), reads=[self.pst[pi]], writes=[t_ktok], partial=True)
                P.barrier()
            NB = 4
            hB = sb("hB", [128, NT, 256]); t_hB = Tok()
            vo = [sb(f"mlvo{i}", [128, 512]) for i in range(NB)]; t_vo = [Tok() for _ in range(NB)]
            vw = [sb(f"mlvw{i}", [128, 4, 65], BF16) for i in range(NB)]; t_vw = [Tok() for _ in range(NB)]
            Ssb = [sb(f"mlS{i}", [128, 4, 128], BF16) for i in range(NB)]; t_S = [Tok() for _ in range(NB)]
            tmpC = [sb(f"mltmpC{d}", [128, 65]) for d in range(2)]; t_tmpC = [Tok(), Tok()]
            dd = [sb(f"mldd{d}", [128, 4]) for d in range(2)]; t_dd = [Tok(), Tok()]
            orders = [list(range(NT)), [1, 0] + list(range(NT - 1, 1, -1))]
            step = 0
            for si in range(NT):
                for d in range(2):
                    j = orders[d][si]
                    bi = step % NB
                    step += 1
                    c0, c1 = j * 128, (j + 1) * 128
                    P.dma("sp", vo[bi][:, 0:256], self.PVO[c0:c1, 0:256], reads=[self.t_PVO], writes=[t_vo[bi]])
                    P.op("dve", lambda e, bi=bi, j=j, d=d: e.tensor_tensor(out=vw[bi][:, :, 0:64], in0=vo[bi][:, 0:256].rearrange("p (h c) -> p h c", c=64),
                                                                   in1=wtok[:, j, d * 4:(d + 1) * 4].unsqueeze(2).to_broadcast([128, 4, 64]), op=ALU.mult),
                         reads=[t_vo[bi], t_wtok], writes=[t_vw[bi]])
                    P.op("act", lambda e, bi=bi, j=j, d=d: e.copy(out=vw[bi][:, :, 64], in_=wtok[:, j, d * 4:(d + 1) * 4]), reads=[t_wtok], writes=[t_vw[bi]], partial=True)
                    for h in range(4):
                        p, r0 = h // 2, (h % 2) * 64
                        pS = 2 + h % 2
                        P.op("pe", lambda e, h=h, p=p, r0=r0, d=d, c0=c0, c1=c1, pS=pS: e.matmul(self.ps[pS][:, (h // 2) * 128:(h // 2 + 1) * 128], lhsT=kT[p][r0:r0 + 64, c0:c1], rhs=qd[p][d][r0:r0 + 64, c0:c1], start=True, stop=True),
                             reads=[t_kT[p], t_qd[p][d]], writes=[self.pst[pS]], skip_self=True)
                    for eo in range(2):
                        P.op("dve", lambda e, bi=bi, eo=eo, d=d: e.tensor_tensor(out=Ssb[bi][:, eo::2, :], in0=self.ps[2 + eo][:, 0:256].rearrange("p (h t) -> p h t", t=128),
                                                                         in1=masks[:, d, :].unsqueeze(1).to_broadcast([128, 2, 128]), op=ALU.mult),
                             reads=[self.pst[2 + eo], t_masks], writes=[t_S[bi]], partial=(eo > 0))
                    for h in range(4):
                        p, r0 = h // 2, (h % 2) * 64
                        pN = 4 + h % 2
                        cN = (h // 2) * 65
                        P.op("pe", lambda e, h=h, bi=bi, pN=pN, cN=cN: e.matmul(self.ps[pN][:, cN:cN + 65], lhsT=Ssb[bi][:, h, :], rhs=vw[bi][:, h, :], start=True, stop=False),
                             reads=[t_S[bi], t_vw[bi]], writes=[self.pst[pN]], skip_self=True)
                        P.op("pe", lambda e, h=h, p=p, r0=r0, d=d, si=si, c0=c0, c1=c1, pN=pN, cN=cN: e.matmul(self.ps[pN][:, cN:cN + 65], lhsT=qd[p][d][r0:r0 + 64, c0:c1], rhs=CbS[p][d][r0:r0 + 64, si, :], start=False, stop=True),
                             reads=[t_qd[p][d], t_CbS[p][d]], writes=[self.pst[pN]], skip_self=True)
                    for p in range(2):
                        pU = p + 6 * d
                        P.op("pe", lambda e, p=p, bi=bi, j=j, pU=pU: e.matmul(self.ps[pU][:, 0:130], lhsT=ktok[:, j, p * 128:(p + 1) * 128], rhs=vw[bi][:, 2 * p:2 * p + 2, :].rearrange("p a c -> p (a c)"), start=True, stop=True),
                             reads=[t_ktok, t_vw[bi]], writes=[self.pst[pU]], skip_self=True)
                        P.op("dve", lambda e, p=p, d=d, pU=pU: e.tensor_tensor(out=tmpC[d][0:64, :], in0=self.ps[pU][0:64, 0:65], in1=Cst[p][d][0:64, :], op=ALU.add), reads=[self.pst[pU], t_Cst[p][d]], writes=[t_tmpC[d]], partial=True)
                        P.op("dve", lambda e, p=p, d=d, pU=pU: e.tensor_tensor(out=tmpC[d][64:128, :], in0=self.ps[pU][64:128, 65:130], in1=Cst[p][d][64:128, :], op=ALU.add), reads=[self.pst[pU], t_Cst[p][d]], writes=[t_tmpC[d]], partial=True)
                        P.op("dve", lambda e, p=p, d=d, j=j: e.tensor_scalar(out=Cst[p][d], in0=tmpC[d], scalar1=ecol[:, p * 2 + d, j:j + 1], scalar2=None, op0=ALU.mult), reads=[t_tmpC[d], t_ecol], writes=[t_Cst[p][d]])
                        P.op("act", lambda e, p=p, d=d, si=si: e.copy(out=CbS[p][d][:, si + 1, :], in_=Cst[p][d]), reads=[t_Cst[p][d]], writes=[t_CbS[p][d]], partial=True)
                    for eo in range(2):
                        num = self.ps[4 + eo][:, 0:130].rearrange("p (h c) -> p h c", c=65)
                        dde = dd[d][:, 2 * eo:2 * eo + 2]
                        P.op("dve", lambda e, num=num, dde=dde: e.tensor_scalar(out=dde, in0=num[:, :, 64], scalar1=-1.0, scalar2=None, op0=ALU.mult), reads=[self.pst[4 + eo]], writes=[t_dd[d]], partial=(eo > 0))
                        P.op("dve", lambda e, num=num, dde=dde: e.scalar_tensor_tensor(out=dde, in0=num[:, :, 64], scalar=1.0, in1=dde, op0=ALU.max, op1=ALU.max), reads=[self.pst[4 + eo]], writes=[t_dd[d]], partial=True)
                        P.op("dve", lambda e, dde=dde: e.reciprocal(out=dde, in_=dde), reads=[t_dd[d]], writes=[t_dd[d]], partial=True)
                        dst = (hF if d == 0 else hB)[:, j, :].rearrange("p (h c) -> p h c", c=64)[:, eo::2, :]
                        P.op("dve", lambda e, num=num, dde=dde, dst=dst: e.tensor_tensor(out=dst, in0=num[:, :, 0:64], in1=dde.unsqueeze(2).to_broadcast([128, 2, 64]), op=ALU.mult),
                             reads=[self.pst[4 + eo], t_dd[d]], writes=[t_hF if d == 0 else t_hB], partial=True)
            hbs = [sb(f"mlhb{i}", [128, 256]) for i in range(2)]; t_hbs = [Tok(), Tok()]
            sgs = [sb(f"mlsg{i}", [128, 256]) for i in range(2)]; t_sgs = [Tok(), Tok()]
            hsqs = [sb(f"mlhsq{i}", [128, 256]) for i in range(2)]; t_hsqs = [Tok(), Tok()]
            ssns = [sb(f"mlssn{i}", [128, 8]) for i in range(2)]; t_ssns = [Tok(), Tok()]
            hn = [sb(f"mlhn{i}", [128, 256], BF16) for i in range(2)]; t_hn = [Tok(), Tok()]
            oT = [sb(f"mloT{i}", [128, 2, 128], BF16) for i in range(2)]; t_oT = [Tok(), Tok()]
            for j in range(NT):
                bi = j % 2
                c0, c1 = j * 128, (j + 1) * 128
                hb, t_hb, sg, t_sg = hbs[bi], t_hbs[bi], sgs[bi], t_sgs[bi]
                hsq, t_hsq, ssn, t_ssn = hsqs[bi], t_hsqs[bi], ssns[bi], t_ssns[bi]
                P.dma("sp", vo[bi][:, 256:512], self.PVO[c0:c1, 256:512], reads=[self.t_PVO], writes=[t_vo[bi]])
                P.op("dve", lambda e, j=j, hb=hb: e.tensor_tensor(out=hb, in0=hF[:, j, :], in1=hB[:, j, :], op=ALU.add), reads=[t_hF, t_hB], writes=[t_hb])
                P.op("act", lambda e, bi=bi, sg=sg: e.activation(out=sg, in_=vo[bi][:, 256:512], func=AF.Exp, scale=-1.0), reads=[t_vo[bi]], writes=[t_sg])
                P.op("act", lambda e, sg=sg: e.activation(out=sg, in_=sg, func=AF.Ln, bias=1.0), reads=[t_sg], writes=[t_sg])
                P.op("act", lambda e, sg=sg: e.activation(out=sg, in_=sg, func=AF.Exp, scale=-1.0), reads=[t_sg], writes=[t_sg])
                P.op("dve", lambda e, hb=hb, sg=sg: e.tensor_tensor(out=hb, in0=hb, in1=sg, op=ALU.mult), reads=[t_sg], writes=[t_hb])
                P.op("dve", lambda e, hb=hb: e.tensor_tensor(out=hsq, in0=hb, in1=hb, op=ALU.mult), reads=[t_hb], writes=[t_hsq])
                P.op("dve", lambda e: e.tensor_reduce(out=ssn[:, 0:4], in_=hsq.rearrange("p (h c) -> p h c", c=64), axis=AX.X, op=ALU.add), reads=[t_hsq], writes=[t_ssn])
                P.op("dve", lambda e: e.tensor_scalar(out=ssn[:, 0:4], in0=ssn[:, 0:4], scalar1=1.0 / 64, scalar2=EPS, op0=ALU.mult, op1=ALU.add), reads=[t_ssn], writes=[t_ssn])
                P.op("act", lambda e: e.activation(out=ssn[:, 0:4], in_=ssn[:, 0:4], func=AF.Ln), reads=[t_ssn], writes=[t_ssn])
                P.op("act", lambda e: e.activation(out=ssn[:, 4:8], in_=ssn[:, 0:4], func=AF.Exp, scale=-0.5), reads=[t_ssn], writes=[t_ssn])
                P.op("dve", lambda e, bi=bi, hb=hb: e.tensor_tensor(out=hn[bi].rearrange("p (h c) -> p h c", c=64), in0=hb.rearrange("p (h c) -> p h c", c=64), in1=ssn[:, 4:8].unsqueeze(2).to_broadcast([128, 4, 64]), op=ALU.mult),
                     reads=[t_hb, t_ssn], writes=[t_hn[bi]])
                pi = 6 + bi
                pT = self.ps[pi].bitcast(BF16)
                for p in range(2):
                    P.op("pe", lambda e, p=p, bi=bi, pT=pT: e.transpose(pT[:, p * 128:(p + 1) * 128], hn[bi][:, p * 128:(p + 1) * 128], self.identb), reads=[t_hn[bi], self.t_ident], writes=[self.pst[pi]], skip_self=True)
                for p in range(2):
                    P.op("act", lambda e, p=p, bi=bi, pT=pT: e.activation(out=oT[bi][:, p, :], in_=pT[:, p * 128:(p + 1) * 128], func=AF.Copy, scale=gml[:, p:p + 1]), reads=[self.pst[pi], t_gml], writes=[t_oT[bi]], partial=(p > 0))
                P.dma("pool", self.MIXT[512:768, c0:c1].rearrange("(c p) t -> p c t", p=128), oT[bi], reads=[t_oT[bi]], writes=[self.t_MIXT], partial=True)
            P.barrier()

    def phase_hyena(self, li):
        if li < DEPTH - 1:
            self.hyena_seq(li, 0, CTX, self.c_hy_z256, self.c_hy_win256, self.c_hy_C256, self.c_hy_S256, self.c_hy_wf256)
        self.hyena_seq(li, CTX, SEQ, self.c_hy_z4096, self.c_hy_win4096, self.c_hy_C4096, self.c_hy_S4096, self.c_hy_wf4096)

    def hyena_seq(self, li, tok0, L, c_z, c_win, c_C, c_S, c_wf):
        nc, P = self.nc, self.P
        NTL = L // 128
        NF = NTL + 1
        NB = (L + 511) // 512
        BW = min(L, 512)
        with contextlib.ExitStack() as st:
            sb = lambda name, shape, dt=F32: self.sb(st, name, shape, dt)
            x0T = sb("hyx0T", [128, 2, L], BF16); t_x0 = Tok()
            zT = sb("hyzT", [128, 2, L], BF16); t_zT = Tok()
            Z = sb("hyZ", [128, NTL, 256], BF16); t_Z = Tok()
            HS = sb("hyHS", [128, NTL, 256], BF16); t_HS = Tok()
            HD = sb("hyHD", [128, NTL, 256], BF16); t_HD = Tok()
            Asp = sb("hyA", [128, NF, 256], BF16); t_A = Tok()
            Bsp = sb("hyB", [128, NF, 256], BF16); t_B = Tok()
            rl1 = sb("hyrl1", [128, 256]); t_rl1 = Tok()
            wf = sb("hywf", [128, NF]); t_wf = Tok()
            P.dma("sp", wf, c_wf, writes=[t_wf])
            bd, t_bd = self.vec_pc(st, "hybd", self.hy_bias_d[li], 2)
            cw = sb("hycw", [128, 6, 3]); cb = sb("hycb", [128, 6]); t_cw = Tok()
            for jj in range(3):
                P.dma("sp", cw[:, :, jj], self.hy_conv_w[li, jj].rearrange("(c p) -> p c", p=128), writes=[t_cw], partial=True, allow_slow_non_contiguous=True)
            P.dma("sp", cb, self.hy_conv_b[li].rearrange("(c p) -> p c", p=128), writes=[t_cw], partial=True, allow_slow_non_contiguous=True)
            with contextlib.ExitStack() as st2:
                sb2 = lambda name, shape, dt=F32: self.sb(st2, name, shape, dt)
                zemb = sb2("hyzemb", [33, L]); t_zemb = Tok()
                P.dma("sp", zemb, c_z, writes=[t_zemb])
                w1 = sb2("hyw1", [33, 64]); w2 = sb2("hyw2", [64, 64]); w3 = sb2("hyw3", [64, 512]); t_wm = Tok()
                P.dma("act", w1, self.hy_w1[li], writes=[t_wm], partial=True)
                P.dma("act", w2, self.hy_w2[li], writes=[t_wm], partial=True)
                P.dma("act", w3, self.hy_w3[li], writes=[t_wm], partial=True)
                fr = sb2("hyfr", [64, 4]); t_fr = Tok()
                P.dma("sp", fr[:, 0:1], self.hy_sin_freq[li].rearrange("(p o) -> p o", o=1), writes=[t_fr], partial=True, allow_slow_non_contiguous=True)
                P.dma("sp", fr[:, 1:2], self.hy_b1[li].rearrange("(p o) -> p o", o=1), writes=[t_fr], partial=True, allow_slow_non_contiguous=True)
                P.dma("sp", fr[:, 2:3], self.hy_b2[li].rearrange("(p o) -> p o", o=1), writes=[t_fr], partial=True, allow_slow_non_contiguous=True)
                P.op("dve", lambda e: e.tensor_scalar(out=fr[:, 1:3], in0=fr[:, 1:3], scalar1=fr[:, 0:1], scalar2=None, op0=ALU.mult), reads=[t_fr], writes=[t_fr])
                h1 = sb2("hyh1", [64, L]); t_h1 = Tok()
                h2 = sb2("hyh2", [64, L]); t_h2 = Tok()
                tt = sb2("hytt", [64, 512]); t_tt = Tok()
                ti = sb2("hyti", [64, 512], I32); t_ti = Tok()
                tf = sb2("hytf", [64, 512]); t_tf = Tok()
                ones32 = sb2("hyones", [128, 128]); t_ones = Tok()
                P.op("pool", lambda e: e.memset(ones32, 1.0), writes=[t_ones])

                def sin_layer(wm, kdim, src, t_src, bcol, dst, t_dst):
                    for b in range(NB):
                        c0 = b * 512
                        P.op("pe", lambda e, c0=c0: e.matmul(self.ps[0][0:64, 0:BW], lhsT=wm[0:kdim, :], rhs=src[0:kdim, c0:c0 + BW], start=True, stop=True),
                             reads=[t_wm, t_src], writes=[self.pst[0]], skip_self=True)
                        P.op("dve", lambda e: e.tensor_scalar(out=tt[:, 0:BW], in0=self.ps[0][0:64, 0:BW], scalar1=fr[:, 0:1], scalar2=fr[:, bcol:bcol + 1], op0=ALU.mult, op1=ALU.add),
                             reads=[self.pst[0], t_fr], writes=[t_tt])
                        P.op("dve", lambda e: e.tensor_scalar(out=ti[:, 0:BW], in0=tt[:, 0:BW], scalar1=1.0 / TWO_PI, scalar2=None, op0=ALU.mult), reads=[t_tt], writes=[t_ti])
                        P.op("dve", lambda e: e.tensor_copy(out=tf[:, 0:BW], in_=ti[:, 0:BW]), reads=[t_ti], writes=[t_tf])
                        P.op("dve", lambda e: e.scalar_tensor_tensor(out=tt[:, 0:BW], in0=tf[:, 0:BW], scalar=-TWO_PI, in1=tt[:, 0:BW], op0=ALU.mult, op1=ALU.add), reads=[t_tf], writes=[t_tt])
                        P.op("act", lambda e, c0=c0: e.activation(out=dst[:, c0:c0 + BW], in_=tt[:, 0:BW], func=AF.Sin), reads=[t_tt], writes=[t_dst], partial=True)
                sin_layer(w1, 33, zemb, t_zemb, 1, h1, t_h1)
                sin_layer(w2, 64, h1, t_h1, 2, h2, t_h2)
                win = [sb2(f"hywin{i}", [128, 2, 256]) for i in range(2)]; t_win = [Tok(), Tok()]
                hfb = [sb2(f"hyhfb{i}", [128, 2, 256]) for i in range(2)]; t_hfb = [Tok(), Tok()]
                hab = [sb2(f"hyhab{i}", [128, 2, 256]) for i in range(2)]; t_hab = [Tok(), Tok()]
                for j in range(NTL):
                    bi = j % 2
                    P.dma("sp", win[bi], c_win[j * 128:(j + 1) * 128], writes=[t_win[bi]])
                    P.op("pe", lambda e, j=j: e.matmul(self.ps[1], lhsT=h2[:, j * 128:(j + 1) * 128], rhs=w3, start=True, stop=True),
                         reads=[t_h2, t_wm], writes=[self.pst[1]], skip_self=True)
                    P.op("dve", lambda e, bi=bi: e.tensor_tensor(out=hfb[bi], in0=self.ps[1].rearrange("p (a c) -> p a c", a=2), in1=win[bi], op=ALU.mult),
                         reads=[self.pst[1], t_win[bi]], writes=[t_hfb[bi]])
                    P.op("pool", lambda e, bi=bi, j=j: e.tensor_tensor(out=HS[:, j, :], in0=hfb[bi][:, 0, :], in1=hfb[bi][:, 1, :], op=ALU.add), reads=[t_hfb[bi]], writes=[t_HS], partial=True)
                    P.op("pool", lambda e, bi=bi, j=j: e.tensor_tensor(out=HD[:, j, :], in0=hfb[bi][:, 0, :], in1=hfb[bi][:, 1, :], op=ALU.subtract), reads=[t_hfb[bi]], writes=[t_HD], partial=True)
                    P.op("act", lambda e, bi=bi: e.activation(out=hab[bi], in_=hfb[bi], func=AF.Abs), reads=[t_hfb[bi]], writes=[t_hab[bi]])
                    for a in range(2):
                        P.op("pe", lambda e, bi=bi, a=a, j=j: e.matmul(self.ps[2][:, 0:256], lhsT=ones32, rhs=hab[bi][:, a, :], start=(j == 0 and a == 0), stop=(j == NTL - 1 and a == 1)),
                             reads=[t_ones, t_hab[bi]], writes=[self.pst[2]], skip_self=True)
                P.op("dve", lambda e: e.reciprocal(out=rl1, in_=self.ps[2][:, 0:256]), reads=[self.pst[2]], writes=[t_rl1])
                P.barrier()
            with contextlib.ExitStack() as st2:
                sb2 = lambda name, shape, dt=F32: self.sb(st2, name, shape, dt)
                pin = [sb2(f"hypin{i}", [128, L]) for i in range(2)]; t_pin = [Tok(), Tok()]
                uu = [sb2(f"hyu{i}", [128, L]) for i in range(2)]; t_uu = [Tok(), Tok()]

                def conv(ch, bi):
                    P.dma("sp" if bi == 0 else "act", pin[bi], self.PT[HY_LO + ch * 128:HY_LO + (ch + 1) * 128, tok0:tok0 + L], reads=[self.t_PT], writes=[t_pin[bi]])
                    eng = "dve"
                    P.op(eng, lambda e: e.tensor_scalar(out=uu[bi], in0=pin[bi], scalar1=cw[:, ch, 1:2], scalar2=cb[:, ch:ch + 1], op0=ALU.mult, op1=ALU.add), reads=[t_pin[bi], t_cw], writes=[t_uu[bi]])
                    P.op(eng, lambda e: e.scalar_tensor_tensor(out=uu[bi][:, 1:L], in0=pin[bi][:, 0:L - 1], scalar=cw[:, ch, 0:1], in1=uu[bi][:, 1:L], op0=ALU.mult, op1=ALU.add), reads=[t_pin[bi], t_cw], writes=[t_uu[bi]])
                    P.op(eng, lambda e: e.scalar_tensor_tensor(out=uu[bi][:, 0:L - 1], in0=pin[bi][:, 1:L], scalar=cw[:, ch, 2:3], in1=uu[bi][:, 0:L - 1], op0=ALU.mult, op1=ALU.add), reads=[t_pin[bi], t_cw], writes=[t_uu[bi]])
                for cc in range(2):
                    conv(cc, 0)
                    P.op("act", lambda e, cc=cc: e.copy(out=x0T[:, cc, :], in_=uu[0]), reads=[t_uu[0]], writes=[t_x0], partial=True)
                    conv(2 + cc, 0)
                    conv(4 + cc, 1)
                    P.op("pool", lambda e, cc=cc: e.tensor_tensor(out=zT[:, cc, :], in0=uu[0], in1=uu[1], op=ALU.mult), reads=[t_uu[0], t_uu[1]], writes=[t_zT], partial=True)
                g = 0
                for j0 in range(0, NTL, 2):
                    pi = 3 + g % 2
                    g += 1
                    pT = self.ps[pi].bitcast(BF16)
                    nj = min(2, NTL - j0)
                    for jj in range(nj):
                        for cc in range(2):
                            P.op("pe", lambda e, jj=jj, cc=cc, j0=j0, pT=pT: e.transpose(pT[:, (jj * 2 + cc) * 128:(jj * 2 + cc + 1) * 128], zT[:, cc, (j0 + jj) * 128:(j0 + jj + 1) * 128], self.identb),
                                 reads=[t_zT, self.t_ident], writes=[self.pst[pi]], skip_self=True)
                    if g % 2 == 0:
                        P.op("act", lambda e, j0=j0, nj=nj, pT=pT: e.copy(out=Z[:, j0:j0 + nj, :].rearrange("p j c -> p (j c)"), in_=pT[:, 0:nj * 256]), reads=[self.pst[pi]], writes=[t_Z], partial=True)
                    else:
                        P.op("dve", lambda e, j0=j0, nj=nj, pT=pT: e.tensor_copy(out=Z[:, j0:j0 + nj, :].rearrange("p j c -> p (j c)"), in_=pT[:, 0:nj * 256]), reads=[self.pst[pi]], writes=[t_Z], partial=True)
                P.barrier()
            with contextlib.ExitStack() as st2:
                sb2 = lambda name, shape, dt=F32: self.sb(st2, name, shape, dt)
                CT = [sb2(f"hyCT{i}", [128, NTL, 128], BF16) for i in range(2)]; t_CT = [Tok(), Tok()]
                ST = [sb2(f"hyST{i}", [128, NTL, 128], BF16) for i in range(2)]; t_ST = [Tok(), Tok()]
                hcs = [sb2(f"hyhcs{i}", [128, 2, 256]) for i in range(2)]; t_hcs = [Tok(), Tok()]
                t1 = sb2("hyt1", [128, 256]); t_t1 = Tok()
                t2 = sb2("hyt2", [128, 256]); t_t2 = Tok()
                t3 = sb2("hyt3", [128, 256]); t_t3 = Tok()
                t4 = sb2("hyt4", [128, 256]); t_t4 = Tok()
                for fc in range(NF):
                    bi = fc % 2
                    P.dma("sp", CT[bi], c_C[0:L, fc * 128:(fc + 1) * 128].rearrange("(j p) f -> p j f", p=128), writes=[t_CT[bi]])
                    P.dma("act", ST[bi], c_S[0:L, fc * 128:(fc + 1) * 128].rearrange("(j p) f -> p j f", p=128), writes=[t_ST[bi]])
                    pc, psn = 2 * bi, 2 * bi + 1
                    for (mat, t_mat, pi, rhs2, t_rhs2) in ((CT[bi], t_CT[bi], pc, HS, t_HS), (ST[bi], t_ST[bi], psn, HD, t_HD)):
                        for half, (rt, t_rt) in enumerate(((Z, t_Z), (rhs2, t_rhs2))):
                            for j in range(NTL):
                                P.op("pe", lambda e, mat=mat, pi=pi, rt=rt, j=j, half=half: e.matmul(self.ps[pi][:, half * 256:(half + 1) * 256], lhsT=mat[:, j, :], rhs=rt[:, j, :], start=(j == 0), stop=(j == NTL - 1)),
                                     reads=[t_mat, t_rt], writes=[self.pst[pi]], skip_self=True)
                    P.op("act", lambda e, bi=bi, pc=pc: e.copy(out=hcs[bi][:, 0, :], in_=self.ps[pc][:, 256:512]), reads=[self.pst[pc]], writes=[t_hcs[bi]], partial=True)
                    P.op("act", lambda e, bi=bi, psn=psn: e.copy(out=hcs[bi][:, 1, :], in_=self.ps[psn][:, 256:512]), reads=[self.pst[psn]], writes=[t_hcs[bi]], partial=True)
                    P.op("dve", lambda e, bi=bi, pc=pc: e.tensor_tensor(out=t1, in0=self.ps[pc][:, 0:256], in1=hcs[bi][:, 0, :], op=ALU.mult), reads=[self.pst[pc], t_hcs[bi]], writes=[t_t1])
                    P.op("dve", lambda e, bi=bi, pc=pc: e.tensor_tensor(out=t3, in0=self.ps[pc][:, 0:256], in1=hcs[bi][:, 1, :], op=ALU.mult), reads=[self.pst[pc], t_hcs[bi]], writes=[t_t3])
                    P.op("dve", lambda e, bi=bi, psn=psn: e.tensor_tensor(out=t2, in0=self.ps[psn][:, 0:256], in1=hcs[bi][:, 1, :], op=ALU.mult), reads=[self.pst[psn], t_hcs[bi]], writes=[t_t2])
                    P.op("dve", lambda e, bi=bi, psn=psn: e.tensor_tensor(out=t4, in0=self.ps[psn][:, 0:256], in1=hcs[bi][:, 0, :], op=ALU.mult), reads=[self.pst[psn], t_hcs[bi]], writes=[t_t4])
                    P.op("pool", lambda e: e.tensor_tensor(out=t1, in0=t1, in1=t2, op=ALU.subtract), reads=[t_t2], writes=[t_t1])
                    P.op("pool", lambda e: e.tensor_tensor(out=t3, in0=t3, in1=t4, op=ALU.add), reads=[t_t4], writes=[t_t3])
                    P.op("dve", lambda e, fc=fc: e.scalar_tensor_tensor(out=Asp[:, fc, :], in0=t1, scalar=wf[:, fc:fc + 1], in1=rl1, op0=ALU.mult, op1=ALU.mult), reads=[t_t1, t_wf, t_rl1], writes=[t_A], partial=True)
                    P.op("dve", lambda e, fc=fc: e.scalar_tensor_tensor(out=Bsp[:, fc, :], in0=t3, scalar=wf[:, fc:fc + 1], in1=rl1, op0=ALU.mult, op1=ALU.mult), reads=[t_t3, t_wf, t_rl1], writes=[t_B], partial=True)
                P.barrier()
            with contextlib.ExitStack() as st2:
                sb2 = lambda name, shape, dt=F32: self.sb(st2, name, shape, dt)
                TW = min(L, 1024)
                GC = [sb2(f"hyGC{i}", [128, TW], BF16) for i in range(3)]; t_GC = [Tok() for _ in range(3)]
                GS = [sb2(f"hyGS{i}", [128, TW], BF16) for i in range(3)]; t_GS = [Tok() for _ in range(3)]
                yt = sb2("hyyt", [128, 512]); t_yt = Tok()
                yo = [sb2(f"hyyo{i}", [128, 512], BF16) for i in range(2)]; t_yo = [Tok(), Tok()]
                nld = 0
                for tb0 in range(0, L, TW):
                    nsub = TW // BW
                    for fc in range(NF):
                        gi = nld % 3
                        nld += 1
                        P.dma("sp", GC[gi], c_C[fc * 128:(fc + 1) * 128, tb0:tb0 + TW], writes=[t_GC[gi]])
                        P.dma("act", GS[gi], c_S[fc * 128:(fc + 1) * 128, tb0:tb0 + TW], writes=[t_GS[gi]])
                        for sub in range(nsub):
                            for cc in range(2):
                                pi = sub * 2 + cc
                                P.op("pe", lambda e, gi=gi, fc=fc, sub=sub, cc=cc, pi=pi: e.matmul(self.ps[pi][:, 0:BW], lhsT=Asp[:, fc, cc * 128:(cc + 1) * 128], rhs=GC[gi][:, sub * BW:(sub + 1) * BW], start=(fc == 0), stop=False),
                                     reads=[t_A, t_GC[gi]], writes=[self.pst[pi]], skip_self=True)
                                P.op("pe", lambda e, gi=gi, fc=fc, sub=sub, cc=cc, pi=pi: e.matmul(self.ps[pi][:, 0:BW], lhsT=Bsp[:, fc, cc * 128:(cc + 1) * 128], rhs=GS[gi][:, sub * BW:(sub + 1) * BW], start=False, stop=(fc == NF - 1)),
                                     reads=[t_B, t_GS[gi]], writes=[self.pst[pi]], skip_self=True)
                    for sub in range(nsub):
                        for cc in range(2):
                            pi = sub * 2 + cc
                            c0 = tb0 + sub * BW
                            oi = pi % 2
                            P.op("dve", lambda e, cc=cc, c0=c0, pi=pi: e.scalar_tensor_tensor(out=yt[:, 0:BW], in0=zT[:, cc, c0:c0 + BW], scalar=bd[:, cc:cc + 1], in1=self.ps[pi][:, 0:BW], op0=ALU.mult, op1=ALU.add),
                                 reads=[t_zT, t_bd, self.pst[pi]], writes=[t_yt])
                            P.op("pool", lambda e, cc=cc, c0=c0, oi=oi: e.tensor_tensor(out=yo[oi][:, 0:BW], in0=yt[:, 0:BW], in1=x0T[:, cc, c0:c0 + BW], op=ALU.mult), reads=[t_yt, t_x0], writes=[t_yo[oi]])
                            P.dma("pool", self.MIXT[768 + cc * 128:768 + (cc + 1) * 128, tok0 + c0:tok0 + c0 + BW], yo[oi][:, 0:BW], reads=[t_yo[oi]], writes=[self.t_MIXT], partial=True)
                P.barrier()

    def phase_wout(self, li):
        nc, P = self.nc, self.P
        last = li == DEPTH - 1
        with contextlib.ExitStack() as st:
            sb = lambda name, shape, dt=F32: self.sb(st, name, shape, dt)
            wo = sb("woutb", [128, 8, D], BF16); t_wo = Tok()
            with contextlib.ExitStack() as st2:
                self.load_cast_weight(st2, wo, t_wo, self.w_out[li], D)
                P.barrier()
            G1 = sb("G1rep", [128, 2, D]); t_G1 = Tok()
            for v in range(2):
                P.dma("sp", G1[:, v, :], self.MOD[v, 2 * D:3 * D].partition_broadcast(128), reads=[self.t_MOD], writes=[t_G1], partial=True)
            mx = [sb(f"mixT{i}", [128, 8, 128], BF16) for i in range(2)]; t_mx = [Tok(), Tok()]
            xt = [sb(f"wxt{i}", [128, D]) for i in range(2)]; t_xt = [Tok(), Tok()]
            tm = [sb(f"wtm{i}", [128, 512]) for i in range(2)]; t_tm = [Tok(), Tok()]
            tiles = list(range(2 if last else 0, TT // 128))
            for n, j in enumerate(tiles):
                bi = n % 2
                v = 1 if j < 2 else 0
                c0, c1 = j * 128, (j + 1) * 128
                P.dma("sp", mx[bi], self.MIXT[:, c0:c1].rearrange("(c p) t -> p c t", p=128), reads=[self.t_MIXT], writes=[t_mx[bi]])
                P.dma("act", xt[bi], self.XS[c0:c1, :], reads=[self.t_XS], writes=[t_xt[bi]])
                for half in range(2):
                    pi = (n * 2 + half) % 4
                    for k in range(8):
                        P.op("pe", lambda e, bi=bi, k=k, half=half, pi=pi: e.matmul(self.ps[pi], lhsT=mx[bi][:, k, :], rhs=wo[:, k, half * 512:(half + 1) * 512], start=(k == 0), stop=(k == 7)),
                             reads=[t_mx[bi], t_wo], writes=[self.pst[pi]], skip_self=True)
                    P.op("dve", lambda e, half=half, pi=pi, v=v: e.tensor_tensor(out=tm[half], in0=self.ps[pi], in1=G1[:, v, half * 512:(half + 1) * 512], op=ALU.mult),
                         reads=[self.pst[pi], t_G1], writes=[t_tm[half]])
                    P.op("dve", lambda e, bi=bi, half=half: e.tensor_tensor(out=xt[bi][:, half * 512:(half + 1) * 512], in0=xt[bi][:, half * 512:(half + 1) * 512], in1=tm[half], op=ALU.add),
                         reads=[t_tm[half]], writes=[t_xt[bi]])
                P.dma("pool", self.XS[c0:c1, :], xt[bi], reads=[t_xt[bi]], writes=[self.t_XS], partial=True)
            P.barrier()

    def phase_ffn(self, li):
        nc, P = self.nc, self.P
        last = li == DEPTH - 1
        with contextlib.ExitStack() as st:
            sb = lambda name, shape, dt=F32: self.sb(st, name, shape, dt)
            wup = sb("wupb", [128, 8, 2 * DFF], BF16); t_wup = Tok()
            wdn = sb("wdnb", [128, NFC, D], BF16); t_wdn = Tok()
            with contextlib.ExitStack() as st2:
                self.load_cast_weight(st2, wup, t_wup, self.ffn_w_up[li], 2 * DFF)
                P.barrier()
            with contextlib.ExitStack() as st2:
                self.load_cast_weight(st2, wdn, t_wdn, self.ffn_w_down[li], D, blk=256, k_chunks=NFC)
                P.barrier()
            G2 = sb("G2rep", [128, 2, D]); t_G2 = Tok()
            for v in range(2):
                P.dma("sp", G2[:, v, :], self.MOD[v, 5 * D:6 * D].partition_broadcast(128), reads=[self.t_MOD], writes=[t_G2], partial=True)
            if last:
                FG = sb("FGrep", [128, D]); t_FG = Tok()
                P.dma("sp", FG, self.final_norm_g.partition_broadcast(128), writes=[t_FG])
            cw = sb("ffcw", [128, 2 * NFC, 3]); cbv = sb("ffcb", [128, 2 * NFC]); t_cw = Tok()
            for jj in range(3):
                P.dma("sp", cw[:, :, jj], self.ffn_conv_w[li, jj].rearrange("(c p) -> p c", p=128), writes=[t_cw], partial=True, allow_slow_non_contiguous=True)
            P.dma("sp", cbv, self.ffn_conv_b[li].rearrange("(c p) -> p c", p=128), writes=[t_cw], partial=True, allow_slow_non_contiguous=True)
            hx = [sb(f"hTx{i}", [128, 8, 258], BF16) for i in range(3)]; t_hx = [Tok() for _ in range(3)]
            gT = sb("ffgT", [128, NFC, 256], BF16); t_gT = Tok()
            xts = [sb(f"fxt{i}", [128, D]) for i in range(4)]; t_xts = [Tok() for _ in range(4)]
            sqj = sb("fsq", [128, D], BF16); t_sqj = Tok()
            sss = [sb(f"fss{i}", [128, 4]) for i in range(4)]; t_sss = [Tok() for _ in range(4)]
            xns = [sb(f"fxn{i}", [128, D], BF16) for i in range(2)]; t_xns = [Tok(), Tok()]
            ga = [sb(f"ffga{i}", [128, 256]) for i in range(2)]; t_ga = [Tok(), Tok()]
            gb = [sb(f"ffgb{i}", [128, 256]) for i in range(2)]; t_gb = [Tok(), Tok()]
            tmo = [sb(f"fftm{i}", [128, 512]) for i in range(2)]; t_tmo = [Tok(), Tok()]
            fss = sb("ffss", [128, 4]); t_fss = Tok()
            tiles = list(range(1 if last else 0, TT // 256))
            seq_first = {0, 1}
            seq_last = {0, TT // 256 - 1}
            nsub = 0
            sets_of = {}

            def stage_a(ti):
                nonlocal nsub
                t0 = ti * 256
                v = 1 if ti == 0 else 0
                hb = ti % 3
                sets_of[ti] = []
                for sub in range(2):
                    si = nsub % 4
                    nsub += 1
                    sets_of[ti].append(si)
                    bufs = (xts[si], t_xts[si], sqj, t_sqj, sss[si], t_sss[si], xns[si % 2], t_xns[si % 2])
                    self.norm_transpose(bufs, self.XS[t0 + sub * 128:t0 + (sub + 1) * 128, :], self.t_XS, self.s2, self.modT[:, 24:32, :], v,
                                        hx[hb], t_hx[hb], 1 + sub * 128, ps_i=6 + sub)
                if ti in seq_first:
                    P.op("pool", lambda e, hb=hb: e.memset(hx[hb][:, :, 0:1], 0.0), writes=[t_hx[hb]], partial=True)
                if ti in seq_last:
                    P.op("pool", lambda e, hb=hb: e.memset(hx[hb][:, :, 257:258], 0.0), writes=[t_hx[hb]], partial=True)

            def halo(ta, tb):
                a, b = ta % 3, tb % 3
                P.op("pool", lambda e: e.tensor_copy(out=hx[a][:, :, 257:258], in_=hx[b][:, :, 1:2]), reads=[t_hx[b]], writes=[t_hx[a]], partial=True)
                P.op("pool", lambda e: e.tensor_copy(out=hx[b][:, :, 0:1], in_=hx[a][:, :, 256:257]), reads=[t_hx[a]], writes=[t_hx[b]], partial=True)

            def stage_b(ti):
                t0 = ti * 256
                v = 1 if ti == 0 else 0
                hb = ti % 3
                for jc in range(NFC):
                    gi = jc % 2
                    for (which, col0, pi, acc, t_acc) in ((0, jc * 128, 0 + 2 * gi, ga[gi], t_ga[gi]), (1, DFF + jc * 128, 1 + 2 * gi, gb[gi], t_gb[gi])):
                        ch = (col0 // 128)
                        for k in range(8):
                            P.op("pe", lambda e, k=k, col0=col0, pi=pi: e.matmul(self.ps[pi][:, 0:258], lhsT=wup[:, k, col0:col0 + 128], rhs=hx[hb][:, k, :], start=(k == 0), stop=(k == 7)),
                                 reads=[t_wup, t_hx[hb]], writes=[self.pst[pi]], skip_self=True)
                        P.op("act", lambda e, pi=pi, acc=acc, ch=ch: e.activation(out=acc, in_=self.ps[pi][:, 1:257], func=AF.Identity, scale=cw[:, ch, 1:2], bias=cbv[:, ch:ch + 1]),
                             reads=[self.pst[pi], t_cw], writes=[t_acc])
                        P.op("dve", lambda e, pi=pi, acc=acc, ch=ch: e.scalar_tensor_tensor(out=acc, in0=self.ps[pi][:, 0:256], scalar=cw[:, ch, 0:1], in1=acc, op0=ALU.mult, op1=ALU.add),
                             reads=[self.pst[pi], t_cw], writes=[t_acc])
                        P.op("dve", lambda e, pi=pi, acc=acc, ch=ch: e.scalar_tensor_tensor(out=acc, in0=self.ps[pi][:, 2:258], scalar=cw[:, ch, 2:3], in1=acc, op0=ALU.mult, op1=ALU.add),
                             reads=[self.pst[pi], t_cw], writes=[t_acc])
                    P.op("act", lambda e, gi=gi: e.activation(out=ga[gi], in_=ga[gi], func=AF.Silu), reads=[t_ga[gi]], writes=[t_ga[gi]])
                    P.op("pool", lambda e, gi=gi, jc=jc: e.tensor_tensor(out=gT[:, jc, :], in0=ga[gi], in1=gb[gi], op=ALU.mult), reads=[t_ga[gi], t_gb[gi]], writes=[t_gT], partial=True)
                for sub in range(2):
                    si = sets_of[ti][sub]
                    xt, t_xt = xts[si], t_xts[si]
                    for half in range(2):
                        pi = 4 + half
                        for jc in range(NFC):
                            P.op("pe", lambda e, jc=jc, sub=sub, half=half, pi=pi: e.matmul(self.ps[pi], lhsT=gT[:, jc, sub * 128:(sub + 1) * 128], rhs=wdn[:, jc, half * 512:(half + 1) * 512], start=(jc == 0), stop=(jc == NFC - 1)),
                                 reads=[t_gT, t_wdn], writes=[self.pst[pi]], skip_self=True)
                        P.op("dve", lambda e, half=half, pi=pi: e.tensor_tensor(out=tmo[half], in0=self.ps[pi], in1=G2[:, v, half * 512:(half + 1) * 512], op=ALU.mult),
                             reads=[self.pst[pi], t_G2], writes=[t_tmo[half]])
                        P.op("pool", lambda e, half=half, xt=xt: e.tensor_tensor(out=xt[:, half * 512:(half + 1) * 512], in0=xt[:, half * 512:(half + 1) * 512], in1=tmo[half], op=ALU.add),
                             reads=[t_tmo[half]], writes=[t_xt])
                    r0 = t0 + sub * 128
                    if not last:
                        P.dma("pool", self.XS[r0:r0 + 128, :], xt, reads=[t_xt], writes=[self.t_XS], partial=True)
                    else:
                        P.op("act", lambda e, xt=xt: e.activation(out=sqj, in_=xt, func=AF.Square, accum_out=fss[:, 0:1]), reads=[t_xt], writes=[t_sqj, t_fss])
                        P.op("dve", lambda e: e.tensor_scalar(out=fss[:, 1:2], in0=fss[:, 0:1], scalar1=1.0 / D, scalar2=EPS, op0=ALU.mult, op1=ALU.add), reads=[t_fss], writes=[t_fss])
                        P.op("act", lambda e: e.activation(out=fss[:, 2:3], in_=fss[:, 1:2], func=AF.Sqrt), reads=[t_fss], writes=[t_fss])
                        P.op("dve", lambda e: e.reciprocal(out=fss[:, 3:4], in_=fss[:, 2:3]), reads=[t_fss], writes=[t_fss])
                        P.op("dve", lambda e, xt=xt: e.scalar_tensor_tensor(out=xt, in0=xt, scalar=fss[:, 3:4], in1=FG, op0=ALU.mult, op1=ALU.mult), reads=[t_fss, t_FG], writes=[t_xt])
                        P.dma("pool", self.out[r0 - CTX:r0 - CTX + 128, :], xt, reads=[t_xt], writes=[self.t_out], partial=True)

            prev = None
            for ti in tiles:
                stage_a(ti)
                if prev is not None:
                    if prev not in seq_last:
                        halo(prev, ti)
                    stage_b(prev)
                prev = ti
            stage_b(prev)
            P.barrier()

    def build(self, stop_after=None):
        nc, P = self.nc, self.P
        self.declare()
        P.clear_sems()
        gst = self.stack
        self.identb = self.sb(gst, "identb", [128, 128], BF16)
        self.t_ident = Tok()
        P.dma("sp", self.identb, self.c_ident, writes=[self.t_ident])
        self.t_XS = Tok()
        self.t_MOD, self.t_PT, self.t_PVO, self.t_MIXT, self.t_out = Tok(), Tok(), Tok(), Tok(), Tok()
        P.dma("sp", self.XS[0:CTX, :], self.ctx, writes=[self.t_XS], partial=True)
        for i in range(16):
            P.dma("act" if i % 2 else "sp", self.XS[CTX + i * 256:CTX + (i + 1) * 256, :], self.x[i * 256:(i + 1) * 256, :], writes=[self.t_XS], partial=True)
        done = False
        for li in range(DEPTH):
            with contextlib.ExitStack() as lst:
                self.lst = lst
                for name, fn in (("mod", self.phase_mod), ("inproj", self.phase_inproj), ("mla", self.phase_mla), ("mlstm", self.phase_mlstm), ("hyena", self.phase_hyena), ("wout", self.phase_wout), ("ffn", self.phase_ffn)):
                    fn(li)
                    if stop_after == (name, li):
                        done = True
                        break
                P.barrier()
            if done:
                break
        P.finish()
        return nc


def _prep_inputs(inputs, b):
    m = {}
    for k, v in inputs.items():
        v = np.asarray(v)
        if k in ("x", "ctx", "c"):
            m[k] = np.ascontiguousarray(v[b])
        elif k == "ml_gate_b":
            m[k] = np.ascontiguousarray(v.reshape(DEPTH, 16))
        else:
            m[k] = np.ascontiguousarray(v)
    return m


def kernel(**inputs):
    bld = Builder()
    nc = bld.build()
    consts = _consts()
    in_maps = []
    for b in range(8):
        m = _prep_inputs(inputs, b)
        m.update(consts)
        in_maps.append({k: m[k] for k in bld.inp})
    res = run_bass_kernel_spmd(nc, in_maps, core_ids=list(range(8)))
    return np.stack([np.asarray(r["out"]) for r in res.results], axis=0).astype(np.float32)
```
